# Optimizing a Trainium2 kernel written in Bass

```python
import math
import jax
import jax.numpy as jnp
from jax import lax
import numpy as np

D_MODEL = 1024
BATCH = 4
SEQ = 8192
DEPTH = 2

A_HEADS = 4
A_DK = 64
A_DV = 128
B_HEADS = 4
B_DK = 128
B_DV = 128
B_CONV = 5
C_WIDTH = 512
C_CONV = 31
D_HEADS = 8
D_DH = 64
D_GROUPS = ((128, 1), (512, 4), (2048, 16))
REL_BUCKETS = 32
REL_MAX_DIST = 1024
CHUNK = 64
EPS = 1e-6
NEG = -1e30
MIX_EVEN = A_HEADS * A_DV + B_HEADS * B_DV
MIX_ODD = C_WIDTH + D_HEADS * D_DH
B_QKV = B_HEADS * (2 * B_DK + B_DV)
EVEN_SPLITS = (A_HEADS * A_DK, A_HEADS * A_DK, A_HEADS * A_DV, A_HEADS * A_DV, 4 * A_HEADS, B_QKV, 4 * B_HEADS, MIX_EVEN)
ODD_SPLITS = (C_WIDTH, C_WIDTH, D_HEADS * D_DH, D_HEADS * D_DH, D_HEADS * D_DH, MIX_ODD)
EVEN_IN = sum(EVEN_SPLITS)
ODD_IN = sum(ODD_SPLITS)
N_EVEN = (DEPTH + 1) // 2
N_ODD = DEPTH // 2

kernel_name = 'bidir_hybrid_mlstm_gdn_conformer_dilated'


def _split(p, sizes):
    return jnp.split(p, np.cumsum(sizes)[:-1].tolist(), axis=-1)


def _rms(x, g):
    xf = x.astype(jnp.float32)
    y = xf * lax.rsqrt(jnp.mean(xf * xf, axis=-1, keepdims=True) + EPS)
    return (y * g.astype(jnp.float32)).astype(x.dtype)


def _layernorm(x, g, b):
    xf = x.astype(jnp.float32)
    xc = xf - jnp.mean(xf, axis=-1, keepdims=True)
    y = xc * lax.rsqrt(jnp.mean(xc * xc, axis=-1, keepdims=True) + EPS)
    return (y * g.astype(jnp.float32) + b.astype(jnp.float32)).astype(x.dtype)


def _head_rms(t, g):
    bsz, s, h, d = t.shape
    y = t * lax.rsqrt(jnp.mean(t * t, axis=-1, keepdims=True) + EPS)
    return y.reshape(bsz, s, h * d) * g.astype(jnp.float32)


def _l2n(t):
    return t * lax.rsqrt(jnp.sum(t * t, axis=-1, keepdims=True) + EPS)


def _dwconv(x, w):
    return lax.conv_general_dilated(x, w[:, None, :].astype(x.dtype), window_strides=(1,), padding='SAME', dimension_numbers=('NWC', 'WIO', 'NWC'), feature_group_count=x.shape[-1])


def _flip(t):
    return jnp.flip(t, axis=1)


def _to_chunks(t):
    bsz, s, h = t.shape[:3]
    t = t.reshape((bsz, s // CHUNK, CHUNK, h) + t.shape[3:])
    return jnp.moveaxis(t, (1, 3), (0, 2))


def _from_chunks(t):
    nc, bsz, h, l = t.shape[:4]
    t = jnp.moveaxis(t, (0, 2), (1, 3))
    return t.reshape((bsz, nc * l, h) + t.shape[4:])


def _mlstm_chunkwise(q, k, v, i_pre, logf):
    q, k, v, i_pre, logf = (_to_chunks(t) for t in (q, k, v, i_pre, logf))
    nc, bsz, h = q.shape[:3]
    causal = jnp.tril(jnp.ones((CHUNK, CHUNK), dtype=bool))
    b = jnp.cumsum(logf, axis=-1)
    dmat = jnp.where(causal, b[..., :, None] - b[..., None, :] + i_pre[..., None, :], -jnp.inf)
    dmax = jnp.max(dmat, axis=-1)
    qk = jnp.einsum('nbhld,nbhsd->nbhls', q, k)
    a_end = b[..., -1:] - b + i_pre

    def step(carry, xs):
        cmat, nvec, m = carry
        qc, kc, vc, bc, dc, dmc, qkc, aec = xs
        inter = bc + m[..., None]
        mt = jnp.maximum(inter, dmc)
        w_int = jnp.exp(inter - mt)
        sc = jnp.exp(dc - mt[..., None]) * qkc
        num = w_int[..., None] * jnp.einsum('bhld,bhde->bhle', qc, cmat) + jnp.einsum('bhls,bhse->bhle', sc, vc)
        den = w_int * jnp.einsum('bhld,bhd->bhl', qc, nvec) + jnp.sum(sc, axis=-1)
        hc = num / jnp.maximum(jnp.abs(den), jnp.exp(-mt))[..., None]
        m_new = jnp.maximum(bc[..., -1] + m, jnp.max(aec, axis=-1))
        w_old = jnp.exp(bc[..., -1] + m - m_new)
        kw = kc * jnp.exp(aec - m_new[..., None])[..., None]
        cmat = w_old[..., None, None] * cmat + jnp.einsum('bhld,bhle->bhde', kw, vc)
        nvec = w_old[..., None] * nvec + jnp.sum(kw, axis=-2)
        return (cmat, nvec, m_new), hc

    init = (jnp.zeros((bsz, h, A_DK, A_DV), jnp.float32), jnp.zeros((bsz, h, A_DK), jnp.float32), jnp.zeros((bsz, h), jnp.float32))
    _, hs = lax.scan(step, init, (q, k, v, b, dmat, dmax, qk, a_end))
    return _from_chunks(hs)


def _gdn_chunked(q, k, v, beta, g):
    q, k, v, beta, g = (_to_chunks(t) for t in (q, k, v, beta, g))
    nc, bsz, h = q.shape[:3]
    tril = jnp.tril(jnp.ones((CHUNK, CHUNK), dtype=bool))
    strict = jnp.tril(jnp.ones((CHUNK, CHUNK), dtype=bool), -1)
    gc = jnp.cumsum(g, axis=-1)
    gam = jnp.exp(jnp.where(tril, gc[..., :, None] - gc[..., None, :], -jnp.inf))
    a = jnp.where(strict, beta[..., :, None] * jnp.einsum('nbhid,nbhjd->nbhij', k, k) * gam, 0.0)
    tmat = a + jnp.eye(CHUNK, dtype=a.dtype)
    u = lax.linalg.triangular_solve(tmat, beta[..., None] * v, left_side=True, lower=True, unit_diagonal=True)
    w = lax.linalg.triangular_solve(tmat, (beta * jnp.exp(gc))[..., None] * k, left_side=True, lower=True, unit_diagonal=True)
    attn = jnp.einsum('nbhid,nbhjd->nbhij', q, k) * gam

    def step(state, xs):
        qc, kc, uc, wc, gcc, ac = xs
        v_new = uc - jnp.einsum('bhld,bhde->bhle', wc, state)
        o = jnp.einsum('bhld,bhde->bhle', qc * jnp.exp(gcc)[..., None], state) + jnp.einsum('bhls,bhse->bhle', ac, v_new)
        gl = gcc[..., -1]
        state = jnp.exp(gl)[..., None, None] * state + jnp.einsum('bhld,bhle->bhde', kc * jnp.exp(gl[..., None] - gcc)[..., None], v_new)
        return state, o

    _, os_ = lax.scan(step, jnp.zeros((bsz, h, B_DK, B_DV), jnp.float32), (q, k, u, w, gc, attn))
    return _from_chunks(os_)


def _t5_bucket(rel):
    half = REL_BUCKETS // 2
    exact = half // 2
    n = jnp.abs(rel)
    large = exact + (jnp.log(jnp.maximum(n, 1).astype(jnp.float32) / exact) / math.log(REL_MAX_DIST / exact) * (half - exact)).astype(jnp.int32)
    large = jnp.minimum(large, half - 1)
    return (rel > 0).astype(jnp.int32) * half + jnp.where(n < exact, n, large)


def _dilated_group(q, k, v, dilation, radius, rel_bias):
    bsz, s, h, dh = q.shape
    ls = s // dilation
    nb = -(-ls // radius)
    lp = nb * radius

    def sub(t, lo, hi):
        t = t.reshape(bsz, ls, dilation, h, dh).transpose(0, 3, 2, 1, 4)
        return jnp.pad(t, ((0, 0), (0, 0), (0, 0), (lo, hi), (0, 0)))

    qb = sub(q, 0, lp - ls).reshape(bsz, h, dilation, nb, radius, dh)

    def band(t):
        t = sub(t, radius, lp - ls + radius).reshape(bsz, h, dilation, nb + 2, radius, dh)
        return jnp.concatenate([t[:, :, :, :-2], t[:, :, :, 1:-1], t[:, :, :, 2:]], axis=4)

    kb, vb = band(k), band(v)
    qi = jnp.arange(radius)[:, None]
    kj = jnp.arange(3 * radius)[None, :]
    rel = kj - radius - qi
    kpos = jnp.arange(nb)[:, None, None] * radius + kj - radius
    valid = (jnp.abs(rel) <= radius) & (kpos >= 0) & (kpos < ls)
    bias = jnp.transpose(rel_bias[_t5_bucket(rel * dilation)], (2, 0, 1)).astype(jnp.float32)
    sc = jnp.einsum('bhrnid,bhrnjd->bhrnij', qb, kb).astype(jnp.float32) * (dh ** -0.5) + bias[:, None, None]
    sc = jnp.where(valid, sc, NEG)
    m = jnp.max(sc, axis=-1, keepdims=True)
    p = jnp.exp(sc - m)
    den = jnp.sum(p, axis=-1)
    o = jnp.einsum('bhrnij,bhrnjd->bhrnid', p, vb.astype(jnp.float32)) / den[..., None]
    lse = m[..., 0] + jnp.log(den)
    o = o.reshape(bsz, h, dilation, lp, dh)[:, :, :, :ls].transpose(0, 3, 2, 1, 4).reshape(bsz, s, h, dh)
    lse = lse.reshape(bsz, h, dilation, lp)[:, :, :, :ls].transpose(0, 3, 2, 1).reshape(bsz, s, h)
    return o, lse


def _dilated_attention(q, k, v, rel_bias):
    outs, lses = [], []
    for window, dilation in D_GROUPS:
        o, l = _dilated_group(q, k, v, dilation, window // (2 * dilation), rel_bias)
        outs.append(o)
        lses.append(l)
    wts = jax.nn.softmax(jnp.stack(lses, axis=0), axis=0)
    return jnp.sum(wts[..., None] * jnp.stack(outs, axis=0), axis=0)


def _even_mixer(h, w_in, m_gate_b, dn_dt_bias, dn_a_log, dn_conv_w, m_norm_g, dn_norm_g, w_out):
    bsz, s, _ = h.shape
    f32 = jnp.float32
    mq, mk, mv, mo, mg, dqkv, dg, z = _split(h @ w_in, EVEN_SPLITS)
    q = mq.reshape(bsz, s, A_HEADS, A_DK).astype(f32)
    k = mk.reshape(bsz, s, A_HEADS, A_DK).astype(f32) * (A_DK ** -0.5)
    v = mv.reshape(bsz, s, A_HEADS, A_DV).astype(f32)
    gt = mg.reshape(bsz, s, 4, A_HEADS).astype(f32) + m_gate_b.astype(f32)
    logf = jax.nn.log_sigmoid(gt[:, :, 2:4])
    h_fwd = _mlstm_chunkwise(q, k, v, gt[:, :, 0], logf[:, :, 0])
    h_bwd = _flip(_mlstm_chunkwise(_flip(q), _flip(k), _flip(v), _flip(gt[:, :, 1]), _flip(logf[:, :, 1])))
    out_a = jax.nn.sigmoid(mo.astype(f32)) * _head_rms(h_fwd + h_bwd, m_norm_g)
    qkv = jax.nn.silu(_dwconv(dqkv, dn_conv_w))
    bq, bk, bv = _split(qkv, (B_HEADS * B_DK, B_HEADS * B_DK, B_HEADS * B_DV))
    q = _l2n(bq.reshape(bsz, s, B_HEADS, B_DK).astype(f32)) * (B_DK ** -0.5)
    k = _l2n(bk.reshape(bsz, s, B_HEADS, B_DK).astype(f32))
    v = bv.reshape(bsz, s, B_HEADS, B_DV).astype(f32)
    gb = dg.reshape(bsz, s, 4, B_HEADS).astype(f32)
    beta = jax.nn.sigmoid(gb[:, :, 0:2])
    decay = -jnp.exp(dn_a_log.astype(f32)) * jax.nn.softplus(gb[:, :, 2:4] + dn_dt_bias.astype(f32))
    o_fwd = _gdn_chunked(q, k, v, beta[:, :, 0], decay[:, :, 0])
    o_bwd = _flip(_gdn_chunked(_flip(q), _flip(k), _flip(v), _flip(beta[:, :, 1]), _flip(decay[:, :, 1])))
    out_b = _head_rms(o_fwd + o_bwd, dn_norm_g)
    mix = jnp.concatenate([out_a, out_b], axis=-1).astype(h.dtype) * jax.nn.silu(z)
    return mix @ w_out


def _odd_mixer(h, w_in, dw_w, dw_b, ln_g, ln_b, rel_bias, w_out):
    bsz, s, _ = h.shape
    ga, gb, aq, ak, av, z = _split(h @ w_in, ODD_SPLITS)
    u = _dwconv(ga * jax.nn.sigmoid(gb), dw_w) + dw_b
    out_c = jax.nn.silu(_layernorm(u, ln_g, ln_b))
    shp = (bsz, s, D_HEADS, D_DH)
    out_d = _dilated_attention(aq.reshape(shp), ak.reshape(shp), av.reshape(shp), rel_bias).reshape(bsz, s, D_HEADS * D_DH)
    mix = jnp.concatenate([out_c, out_d.astype(h.dtype)], axis=-1) * jax.nn.silu(z)
    return mix @ w_out


def setup_inputs(seed: int = 0) -> dict:
    key = jax.random.key(seed)
    ks = jax.random.split(key, 24)
    f32 = jnp.float32

    def nrm(k, shape, sd):
        return jax.random.normal(k, shape, f32) * sd

    forget_b = jnp.linspace(3.0, 6.0, A_HEADS, dtype=f32)
    m_gate_b = jnp.concatenate([nrm(ks[6], (N_EVEN, 2, A_HEADS), 0.1), forget_b + nrm(ks[7], (N_EVEN, 2, A_HEADS), 0.1)], axis=1)
    dt = jnp.exp(jax.random.uniform(ks[8], (N_EVEN, 2, B_HEADS), f32, math.log(1e-3), math.log(1e-1)))
    dt_bias = dt + jnp.log(-jnp.expm1(-dt))
    a_log = jnp.log(jax.random.uniform(ks[9], (N_EVEN, 2, B_HEADS), f32, 1.0, 16.0))
    return {
        'x': nrm(ks[0], (BATCH, SEQ, D_MODEL), 1.0),
        'c': nrm(ks[1], (BATCH, D_MODEL), 1.0),
        'norm_g': 1.0 + nrm(ks[2], (DEPTH, D_MODEL), 0.02),
        'ada_w': nrm(ks[3], (DEPTH, D_MODEL, 3 * D_MODEL), D_MODEL ** -0.5),
        'ada_b': nrm(ks[4], (DEPTH, 3 * D_MODEL), 0.02),
        'ev_w_in': nrm(ks[5], (N_EVEN, D_MODEL, EVEN_IN), D_MODEL ** -0.5),
        'ev_m_gate_b': m_gate_b,
        'ev_dn_dt_bias': dt_bias,
        'ev_dn_a_log': a_log,
        'ev_dn_conv_w': nrm(ks[10], (N_EVEN, B_CONV, B_QKV), B_CONV ** -0.5),
        'ev_m_norm_g': 1.0 + nrm(ks[11], (N_EVEN, A_HEADS * A_DV), 0.02),
        'ev_dn_norm_g': 1.0 + nrm(ks[12], (N_EVEN, B_HEADS * B_DV), 0.02),
        'ev_w_out': nrm(ks[13], (N_EVEN, MIX_EVEN, D_MODEL), MIX_EVEN ** -0.5),
        'od_w_in': nrm(ks[14], (N_ODD, D_MODEL, ODD_IN), D_MODEL ** -0.5),
        'od_dw_w': nrm(ks[15], (N_ODD, C_CONV, C_WIDTH), C_CONV ** -0.5),
        'od_dw_b': nrm(ks[16], (N_ODD, C_WIDTH), 0.02),
        'od_ln_g': 1.0 + nrm(ks[17], (N_ODD, C_WIDTH), 0.02),
        'od_ln_b': nrm(ks[18], (N_ODD, C_WIDTH), 0.02),
        'od_w_out': nrm(ks[19], (N_ODD, MIX_ODD, D_MODEL), MIX_ODD ** -0.5),
        'rel_bias': nrm(ks[20], (REL_BUCKETS, D_HEADS), 0.5),
        'final_g': 1.0 + nrm(ks[21], (D_MODEL,), 0.02),
    }


def reference(x, c, norm_g, ada_w, ada_b, ev_w_in, ev_m_gate_b, ev_dn_dt_bias, ev_dn_a_log, ev_dn_conv_w, ev_m_norm_g, ev_dn_norm_g, ev_w_out, od_w_in, od_dw_w, od_dw_b, od_ln_g, od_ln_b, od_w_out, rel_bias, final_g):
    cs = jax.nn.silu(c)
    for layer in range(DEPTH):
        mod = (cs @ ada_w[layer] + ada_b[layer])[:, None, :]
        shift, scale, gate = jnp.split(mod, 3, axis=-1)
        h = _rms(x, norm_g[layer]) * (1.0 + scale) + shift
        j = layer // 2
        if layer % 2 == 0:
            y = _even_mixer(h, ev_w_in[j], ev_m_gate_b[j], ev_dn_dt_bias[j], ev_dn_a_log[j], ev_dn_conv_w[j], ev_m_norm_g[j], ev_dn_norm_g[j], ev_w_out[j])
        else:
            y = _odd_mixer(h, od_w_in[j], od_dw_w[j], od_dw_b[j], od_ln_g[j], od_ln_b[j], rel_bias, od_w_out[j])
        x = x + gate * y
    return _rms(x, final_g)
```

```python
import contextlib
import numpy as np
import concourse.bass as bass
import concourse.mybir as mybir
from concourse.bass_utils import run_bass_kernel_spmd

F32 = mybir.dt.float32
BF16 = mybir.dt.bfloat16
ALU = mybir.AluOpType
AF = mybir.ActivationFunctionType
AX = mybir.AxisListType

SEM_LIMIT = 8000
EPS = 1e-6
NEG = -1e30


class Buf:
    __slots__ = ("t", "name", "writer", "readers", "dsem", "dtot", "depth")

    def __init__(self, t, name):
        self.t = t
        self.name = name
        self.writer = None
        self.readers = []
        self.dsem = None
        self.dtot = 0

    def __getitem__(self, idx):
        return self.t[idx]


class KB:
    def __init__(self, nc, same_engine_sync=True):
        self.nc = nc
        self.es = contextlib.ExitStack()
        self.root_es = self.es
        self.depth = 0
        self.engs = {"pe": nc.tensor, "dve": nc.vector, "act": nc.scalar, "pool": nc.gpsimd, "sp": nc.sync}
        self.csem = {}
        self.ccnt = {}
        self.seen = {k: {} for k in self.engs}
        self.same = same_engine_sync
        self.pool_used = False
        self.free_dsems = []
        self.nsem = 0
        self.ninst = 0
        self.bufs = []

    def sem(self, name, root=False):
        self.nsem += 1
        es = self.root_es if root else self.es
        return es.enter_context(self.nc.semaphore(f"{name}_{self.nsem}"))

    def sb(self, name, shape, dt=F32):
        t = self.es.enter_context(self.nc.sbuf_tensor("s_" + name, list(shape), dt))
        b = Buf(t, name)
        b.depth = self.depth
        self.bufs.append(b)
        return b

    def ps(self, name, shape, dt=F32):
        t = self.es.enter_context(self.nc.psum_tensor("p_" + name, list(shape), dt))
        b = Buf(t, name)
        b.depth = self.depth
        self.bufs.append(b)
        return b

    def dram(self, name, shape, dt=F32, kind="Internal"):
        t = self.nc.dram_tensor(name, list(shape), dt, kind=kind)
        b = Buf(t.ap(), name)
        b.depth = 0
        self.bufs.append(b)
        return b

    def _wait(self, ek, dep):
        sem, val, dek = dep
        if dek == ek and not self.same:
            return
        sid = id(sem)
        if self.seen[ek].get(sid, 0) >= val:
            return
        self.engs[ek].wait_ge(sem, val)
        self.seen[ek][sid] = val

    def _deps(self, ek, reads, writes, skip_same_w=False):
        for b in reads:
            if b.writer is not None:
                self._wait(ek, b.writer)
        for b in writes:
            if b.writer is not None:
                if not (skip_same_w and b.writer[2] == ek):
                    self._wait(ek, b.writer)
            for r in b.readers:
                self._wait(ek, r)

    def _tick(self, ek):
        if ek not in self.csem or self.ccnt[ek] >= SEM_LIMIT:
            self.csem[ek] = self.sem("c" + ek, root=True)
            self.ccnt[ek] = 0
        self.ccnt[ek] += 1
        return (self.csem[ek], self.ccnt[ek], ek)

    def _mark(self, tok, reads, writes):
        for b in reads:
            b.readers.append(tok)
            if len(b.readers) > 16:
                last = {}
                for r in b.readers:
                    k = id(r[0])
                    if k not in last or last[k][1] < r[1]:
                        last[k] = r
                b.readers = list(last.values())
        for b in writes:
            b.writer = tok
            b.readers = []

    def op(self, ek, fn, reads, writes, acc=False):
        if ek == "pool":
            self.pool_used = True
        self._deps(ek, reads, writes, skip_same_w=acc)
        tok = self._tick(ek)
        ins = fn(self.engs[ek])
        ins.then_inc(tok[0], 1)
        self._mark(tok, reads, writes)
        self.ninst += 1
        return ins

    def dma(self, qk, out_ap, in_ap, reads, writes, sb, grouped=False, **kw):
        if sb.dsem is None:
            sb.dsem, sb.dtot = self._alloc_dsem()
        for b in reads:
            if b.writer is not None:
                self._wait(qk, b.writer)
        for b in writes:
            if b.writer is not None:
                if not (grouped and b.writer[0] is sb.dsem):
                    self._wait(qk, b.writer)
            for r in b.readers:
                self._wait(qk, r)
        if not grouped and sb.dtot > 0:
            self._wait(qk, (sb.dsem, sb.dtot, "dma"))
        if sb.dtot + 16 > SEM_LIMIT:
            self._wait(qk, (sb.dsem, sb.dtot, "dma"))
            sb.dsem, sb.dtot = self._alloc_dsem()
        sb.dtot += 16
        tok = (sb.dsem, sb.dtot, "dma")
        ins = self.engs[qk].dma_start(out=out_ap, in_=in_ap, **kw)
        ins.then_inc(sb.dsem, 16)
        self._mark(tok, reads, writes)
        self.ninst += 1
        return ins


    def allgather(self, in_buf, out_buf, groups):
        self.pool_used = True
        self._deps("pool", [in_buf], [out_buf])
        sem = self.sem("cc", root=True)
        ins = self.nc.gpsimd.collective_compute("AllGather", ALU.bypass, replica_groups=groups,
                                                ins=[in_buf[:].opt()], outs=[out_buf[:].opt()])
        ins.then_inc(sem, 1)
        self._mark((sem, 1, "cc"), [in_buf], [out_buf])
        self.ninst += 1

    def barrier(self):
        toks = [(self.csem[e], self.ccnt[e], e) for e in self.csem]
        for b in self.bufs:
            if b.dsem is not None and b.dtot > 0:
                toks.append((b.dsem, b.dtot, "dma"))
        for b in self.bufs:
            for t in ([b.writer] if b.writer is not None else []) + list(b.readers):
                if t[2] == "cc":
                    toks.append(t)
        same = self.same
        self.same = True
        for e in list(self.engs.keys()):
            if e == "pool" and "pool" not in self.csem and not self.pool_used:
                continue
            for t in toks:
                self._wait(e, t)
        self.same = same
        for b in self.bufs:
            b.writer = None
            b.readers = []

    @contextlib.contextmanager
    def scope(self):
        old = self.es
        self.es = contextlib.ExitStack()
        self.depth += 1
        nb = len(self.bufs)
        try:
            yield
        finally:
            self.barrier()
            dead = self.bufs[nb:]
            for b in dead:
                if b.depth >= self.depth and b.dsem is not None:
                    if b.dtot + 16 * 64 < SEM_LIMIT:
                        self.free_dsems.append((b.dsem, b.dtot))
                    b.dsem = None
                    b.dtot = 0
            self.bufs = self.bufs[:nb] + [b for b in dead if b.depth < self.depth]
            self.es.close()
            self.es = old
            self.depth -= 1

    def ts(self, out, in0, s1, op0, R, W, s2=None, op1=None, eng="dve"):
        if op1 is None:
            return self.op(eng, lambda e: e.tensor_scalar(out=out, in0=in0, scalar1=s1, scalar2=None, op0=op0), R, W)
        return self.op(eng, lambda e: e.tensor_scalar(out=out, in0=in0, scalar1=s1, scalar2=s2, op0=op0, op1=op1), R, W)

    def tt(self, out, a, b, op, R, W, eng="dve"):
        return self.op(eng, lambda e: e.tensor_tensor(out=out, in0=a, in1=b, op=op), R, W)

    def stt(self, out, in0, scalar, in1, op0, op1, R, W):
        return self.op("dve", lambda e: e.scalar_tensor_tensor(out=out, in0=in0, scalar=scalar, in1=in1, op0=op0, op1=op1), R, W)

    def act(self, out, in_, func, R, W, scale=None, bias=None):
        kw = {}
        if scale is not None:
            kw["scale"] = scale
        if bias is not None:
            kw["bias"] = bias
        return self.op("act", lambda e: e.activation(out=out, in_=in_, func=func, **kw), R, W)

    def mm(self, out, lhsT, rhs, R, W, start=True, stop=True):
        return self.op("pe", lambda e: e.matmul(out, lhsT=lhsT, rhs=rhs, start=start, stop=stop), R, W, acc=True)

    def cp(self, out, in_, R, W, eng="dve"):
        if eng == "act":
            return self.op("act", lambda e: e.activation(out=out, in_=in_, func=AF.Copy), R, W)
        return self.op(eng, lambda e: e.tensor_copy(out=out, in_=in_), R, W)

    def scan(self, out, d0, d1, init, op0, op1, R, W):
        return self.op("dve", lambda e: e.tensor_tensor_scan(out=out, data0=d0, data1=d1, initial=init, op0=op0, op1=op1), R, W)

    def _alloc_dsem(self):
        if self.free_dsems:
            self.free_dsems.sort(key=lambda t: t[1])
            return self.free_dsems.pop(0)
        return (self.sem("dq", root=True), 0)

    def finish(self, ek="sp"):
        for b in self.bufs:
            if b.dsem is not None and b.dtot > 0:
                self._wait(ek, (b.dsem, b.dtot, "dma"))

    def close(self):
        self.es.close()


class Ctx:
    pass


DBG = {}


def dbg(k, name, buf, shape):
    if not DBG.get("on"):
        return
    d = k.dram("dbg_" + name, list(shape), F32, kind="ExternalOutput")
    k.dma("sp", d[:], buf[:], [buf], [d], buf)


def dbgap(k, name, buf, ap, shape):
    if not DBG.get("on"):
        return
    d = k.dram("dbg_" + name, list(shape), F32, kind="ExternalOutput")
    k.dma("sp", d[:], ap, [buf], [d], buf)


def fv(buf, p0, npart, off, dims):
    a = buf.t[:]
    pstep = a.ap[0][0]
    return bass.AP(a.tensor, a.offset + p0 * pstep + off, [[pstep, npart]] + [[st, ct] for (st, ct) in dims])


def setup_common(k, cx):
    cx.psum = [k.ps(f"psb{i}", [128, 512], F32) for i in range(8)]
    cx.consts_d = k.dram("consts", [128, CONST_COLS], F32, kind="ExternalInput")
    cx.consts = k.sb("consts_sb", [128, CONST_COLS], F32)
    k.dma("sp", cx.consts[:], cx.consts_d[:], [cx.consts_d], [cx.consts], cx.consts)
    cx.ones_bf = k.sb("ones_bf", [128, 128], BF16)
    k.op("dve", lambda e: e.memset(cx.ones_bf[:], 1.0), [], [cx.ones_bf])


C_IDENT = 0
C_ONES = 128
C_RST0 = 256
C_RSTN = 768
C_MASK = 1280
M_LE, M_GE, M_LT, M_GT = 0, 1, 2, 3
C_SEL = 3328
CONST_COLS = 3584


def make_consts():
    c = np.zeros((128, CONST_COLS), np.float32)
    c[:, C_IDENT:C_IDENT + 128] = np.eye(128, dtype=np.float32)
    c[:, C_ONES:C_ONES + 128] = 1.0
    col = np.arange(512)
    c[:, C_RST0:C_RST0 + 512] = np.where(col % 64 == 0, 0.0, 1.0)[None, :]
    c[:, C_RSTN:C_RSTN + 512] = np.where(col % 64 == 0, NEG, 0.0)[None, :]
    jj = np.arange(64)[:, None]
    ii = np.arange(64)[None, :]
    for m, ok in enumerate([jj <= ii, jj >= ii, jj < ii, jj > ii]):
        c[:64, C_MASK + m * 512:C_MASK + (m + 1) * 512] = np.tile(np.where(ok, 0.0, NEG), (1, 8))
    for hh in range(2):
        c[hh, C_SEL + hh * 128:C_SEL + (hh + 1) * 128] = 1.0
    return c


def phase_mod(k, cx, cT_d, adaw_d, adab_d, ncol_chunks, tag="m0"):
    cs = k.sb(tag + "cs", [128, 8], F32)
    k.dma("sp", cs[:], cT_d[:], [cT_d], [cs], cs)
    k.op("act", lambda e: e.activation(out=cs[:], in_=cs[:], func=AF.Silu), [cs], [cs])
    ncols = ncol_chunks * 128
    wbuf = [k.sb(tag + f"adaw{i}", [128, ncols], F32) for i in range(2)]
    pm = cx.psum[0]
    for kc in range(8):
        wb = wbuf[kc % 2]
        k.dma("sp", wb[:], adaw_d[kc * 128:(kc + 1) * 128, :], [adaw_d], [wb], wb)
        for cc in range(ncol_chunks):
            k.op("pe", lambda e, wb=wb, cc=cc, kc=kc: e.matmul(
                pm[:, cc * 8 + kc:cc * 8 + kc + 1], lhsT=wb[:, cc * 128:(cc + 1) * 128], rhs=cs[:, kc:kc + 1],
                start=True, stop=True), [wb, cs], [pm], acc=True)
    mod = k.sb(tag + "mod", [128, ncol_chunks], F32)
    k.op("dve", lambda e: e.tensor_reduce(
        out=mod[:], in_=pm[:, 0:ncol_chunks * 8].rearrange("p (c k) -> p c k", k=8), axis=AX.X, op=ALU.add),
        [pm], [mod])
    ab = k.sb(tag + "adab", [128, ncol_chunks], F32)
    k.dma("sp", ab[:], adab_d[:], [adab_d], [ab], ab)
    k.op("dve", lambda e: e.tensor_tensor(out=mod[:], in0=mod[:], in1=ab[:], op=ALU.add), [mod, ab], [mod])
    return mod


def load_w_bf16(k, Wb, W_d, ncols, tag):
    with k.scope():
        wst = [k.sb(tag + f"wst{i}", [128, 1024], F32) for i in range(2)]
        n = 0
        for kc in range(8):
            for c0 in range(0, ncols, 1024):
                w = min(1024, ncols - c0)
                ws = wst[n % 2]
                k.dma("sp", ws[:, 0:w], W_d[kc * 128:(kc + 1) * 128, c0:c0 + w], [W_d], [ws], ws)
                k.cp(Wb[:, kc, c0:c0 + w], ws[:, 0:w], [ws], [Wb], eng=("act" if n % 2 else "dve"))
                n += 1


def phase_norm_proj(k, cx, T, xT_ap_fn, xT_bufs, A, B, W_d, ncols, fm_specs, tm_specs, tag="p1", producer=None):
    Wb = k.sb(tag + "Wb", [128, 8, ncols], BF16)
    load_w_bf16(k, Wb, W_d, ncols, tag)
    X = [k.sb(tag + f"X{i}", [128, 8, 512], F32) for i in range(2)]
    sq = k.sb(tag + "sq", [128, 8, 512], BF16)
    rstd = k.sb(tag + "rstd", [128, 512], F32)
    tmp = [k.sb(tag + f"tmp{i}", [128, 512], F32) for i in range(2)]
    hT = k.sb(tag + "hT", [128, 8, 512], BF16)
    stg = [k.sb(tag + f"stg{i}", [128, 512], F32) for i in range(4)]
    nst = 0
    npb = 0
    ntiles = T // 512
    for tt in range(ntiles):
        Xt = X[tt % 2]
        if producer is None:
            k.dma("sp", Xt[:], xT_ap_fn(tt), xT_bufs, [Xt], Xt)
        else:
            producer(tt, Xt)
        k.op("act", lambda e, Xt=Xt: e.activation(out=sq[:], in_=Xt[:], func=AF.Square), [Xt], [sq])
        pss = cx.psum[7]
        for kc in range(8):
            k.op("pe", lambda e, kc=kc: e.matmul(pss[:], lhsT=cx.ones_bf[:], rhs=sq[:, kc, :],
                                                 start=(kc == 0), stop=(kc == 7)), [cx.ones_bf, sq], [pss], acc=True)
        k.op("act", lambda e: e.activation(out=rstd[:], in_=pss[:], func=AF.Sqrt, scale=1.0 / 1024.0, bias=float(EPS)),
             [pss], [rstd])
        k.op("dve", lambda e: e.reciprocal(out=rstd[:], in_=rstd[:]), [rstd], [rstd])
        for kc in range(8):
            tb = tmp[kc % 2]
            k.op("dve", lambda e, kc=kc, tb=tb, Xt=Xt: e.tensor_tensor(out=tb[:], in0=Xt[:, kc, :], in1=rstd[:], op=ALU.mult),
                 [Xt, rstd], [tb])
            k.op("act", lambda e, kc=kc, tb=tb: e.activation(out=hT[:, kc, :], in_=tb[:], func=AF.Identity,
                                                             scale=A[:, kc:kc + 1], bias=B[:, kc:kc + 1]),
                 [tb, A, B], [hT])
        for (c0, nr, dbuf, dfn, scale) in fm_specs:
            if dfn(tt) is None:
                continue
            pb = cx.psum[npb % 6]
            npb += 1
            for kc in range(8):
                k.op("pe", lambda e, kc=kc, pb=pb, c0=c0, nr=nr: e.matmul(
                    pb[0:nr, :], lhsT=Wb[:, kc, c0:c0 + nr], rhs=hT[:, kc, :], start=(kc == 0), stop=(kc == 7)),
                    [Wb, hT], [pb], acc=True)
            sg = stg[nst % 4]
            nst += 1
            if nst % 2:
                k.op("act", lambda e, pb=pb, sg=sg, nr=nr, scale=scale: e.activation(
                    out=sg[0:nr, :], in_=pb[0:nr, :], func=AF.Copy, scale=float(scale)), [pb], [sg])
            else:
                k.op("dve", lambda e, pb=pb, sg=sg, nr=nr, scale=scale: e.tensor_scalar(
                    out=sg[0:nr, :], in0=pb[0:nr, :], scalar1=float(scale), scalar2=None, op0=ALU.mult), [pb], [sg])
            k.dma("sp", dfn(tt), sg[0:nr, :], [sg], [dbuf], sg)
        for ts in range(4):
            for (c0, ncl, dbuf, dfn) in tm_specs:
                if dfn(tt, ts) is None:
                    continue
                pb = cx.psum[npb % 6]
                npb += 1
                for kc in range(8):
                    k.op("pe", lambda e, kc=kc, pb=pb, c0=c0, ncl=ncl, ts=ts: e.matmul(
                        pb[:, 0:ncl], lhsT=hT[:, kc, ts * 128:(ts + 1) * 128], rhs=Wb[:, kc, c0:c0 + ncl],
                        start=(kc == 0), stop=(kc == 7)), [Wb, hT], [pb], acc=True)
                sg = stg[nst % 4]
                nst += 1
                if nst % 2:
                    k.op("act", lambda e, pb=pb, sg=sg, ncl=ncl: e.activation(
                        out=sg[:, 0:ncl], in_=pb[:, 0:ncl], func=AF.Copy), [pb], [sg])
                else:
                    k.op("dve", lambda e, pb=pb, sg=sg, ncl=ncl: e.tensor_copy(out=sg[:, 0:ncl], in_=pb[:, 0:ncl]), [pb], [sg])
                k.dma("sp", dfn(tt, ts), sg[:, 0:ncl], [sg], [dbuf], sg)


FM_MQ, FM_MK, FM_G, FM_DQ, FM_DK, FM_DV = 0, 128, 256, 272, 528, 784
FM_ROWS = 1040
TM0 = FM_ROWS
TM_MK, TM_MV, TM_MO, TM_Z = TM0, TM0 + 128, TM0 + 384, TM0 + 640
NC1 = TM0 + 1152


def row_softplus_neg(k, out, x, tmp1, tmp2, n, R):
    k.stt(tmp1[0:n, :], x[0:n, :], -1.0, x[0:n, :], ALU.mult, ALU.max, [x], [tmp1])
    k.act(tmp1[0:n, :], tmp1[0:n, :], AF.Exp, [tmp1], [tmp1], scale=-1.0)
    k.act(tmp1[0:n, :], tmp1[0:n, :], AF.Ln, [tmp1], [tmp1], bias=1.0)
    k.stt(out[0:n, :], x[0:n, :], 0.0, tmp1[0:n, :], ALU.min, ALU.subtract, [x, tmp1], [out])


def dirview(buf, n, d, G=512):
    if d == 0:
        return fv(buf, 0, n, 0, [(1, G)])
    return fv(buf, 0, n, G - 1, [(-1, G)])


def phase_mlstm(k, cx, T, FM, TM, gb_d, HS):
    NG = T // 512
    cs = cx.consts
    ident = cs
    st = []
    for d in range(2):
        s = Ctx()
        s.gi = k.sb(f"ml_gi{d}", [2, 512], F32)
        s.gf = k.sb(f"ml_gf{d}", [2, 512], F32)
        s.t1 = k.sb(f"ml_t1{d}", [2, 512], F32)
        s.t2 = k.sb(f"ml_t2{d}", [2, 512], F32)
        s.b = k.sb(f"ml_b{d}", [2, 512], F32)
        s.g = k.sb(f"ml_g{d}", [2, 512], F32)
        s.pm = k.sb(f"ml_pm{d}", [2, 512], F32)
        s.negmu = k.sb(f"ml_negmu{d}", [2, 512], F32)
        s.wint = k.sb(f"ml_wint{d}", [2, 512], F32)
        s.emt = k.sb(f"ml_emt{d}", [2, 512], F32)
        s.kwf = k.sb(f"ml_kwf{d}", [2, 512], F32)
        s.mnext = k.sb(f"ml_mnext{d}", [2, 8], F32)
        s.mcur = k.sb(f"ml_mcur{d}", [2, 8], F32)
        s.c8 = k.sb(f"ml_c8{d}", [2, 8], F32)
        s.wold = k.sb(f"ml_wold{d}", [2, 8], F32)
        s.carry = k.sb(f"ml_carry{d}", [2, 1], F32)
        s.bi = k.sb(f"ml_bi{d}", [2, 1], F32)
        s.bf = k.sb(f"ml_bf{d}", [2, 1], F32)
        k.dma("sp", s.bi[:], gb_d[2 * d:2 * d + 2, :], [gb_d], [s.bi], s.bi)
        k.dma("sp", s.bf[:], gb_d[4 + 2 * d:4 + 2 * d + 2, :], [gb_d], [s.bf], s.bf)
        k.op("dve", lambda e, s=s: e.memset(s.carry[:], 0.0), [], [s.carry])
        s.cols = k.sb(f"ml_cols{d}", [64, 64], F32)
        s.woldc = k.sb(f"ml_woldc{d}", [64, 16], F32)
        s.QT = [k.sb(f"ml_QT{d}{h}", [64, 512], F32) for h in range(2)]
        s.KT = [k.sb(f"ml_KT{d}{h}", [64, 512], F32) for h in range(2)]
        s.Ktm = k.sb(f"ml_Ktm{d}", [64, 8, 128], F32)
        s.Va = [k.sb(f"ml_Va{d}{h}", [64, 8, 129], F32) for h in range(2)]
        for h in range(2):
            k.op("dve", lambda e, s=s, h=h: e.memset(s.Va[h][:], 1.0), [], [s.Va[h]])
        s.E = [k.sb(f"ml_E{d}{h}", [64, 512], F32) for h in range(2)]
        s.AT = [k.sb(f"ml_AT{d}{h}", [64, 512], F32) for h in range(2)]
        s.C = [k.sb(f"ml_C{d}{h}", [64, 129], F32) for h in range(2)]
        for h in range(2):
            k.op("dve", lambda e, s=s, h=h: e.memset(s.C[h][:], 0.0), [], [s.C[h]])
        s.tmp = [k.sb(f"ml_tmp{d}{h}", [64, 129], F32) for h in range(2)]
        s.R = [k.sb(f"ml_R{d}{h}", [64, 129], F32) for h in range(2)]
        s.dn = [k.sb(f"ml_dn{d}{h}", [64, 2], F32) for h in range(2)]
        s.KW = [k.sb(f"ml_KW{d}{h}", [64, 64], F32) for h in range(2)]
        s.Hg = k.sb(f"ml_Hg{d}", [64, 8, 256], F32)
        st.append(s)
    TMr = TM[:].rearrange("(c p) f -> p c f", p=64)
    for step in range(NG):
        for d in range(2):
            s = st[d]
            grp = step if d == 0 else NG - 1 - step
            t0 = grp * 512
            c0 = grp * 8
            k.dma("sp", s.gi[:], FM[FM_G + 2 * d:FM_G + 2 * d + 2, t0:t0 + 512], [FM], [s.gi], s.gi)
            k.dma("sp", s.gf[:], FM[FM_G + 4 + 2 * d:FM_G + 4 + 2 * d + 2, t0:t0 + 512], [FM], [s.gf], s.gf)
            for h in range(2):
                k.dma("sp", s.QT[h][:], FM[FM_MQ + 64 * h:FM_MQ + 64 * h + 64, t0:t0 + 512], [FM], [s.QT[h]], s.QT[h])
                k.dma("sp", s.KT[h][:], FM[FM_MK + 64 * h:FM_MK + 64 * h + 64, t0:t0 + 512], [FM], [s.KT[h]], s.KT[h])
                k.dma("sp", s.Va[h][:, :, 0:128], TMr[:, c0:c0 + 8, 128 + 128 * h:256 + 128 * h], [TM], [s.Va[h]], s.Va[h])
            k.dma("sp", s.Ktm[:], TMr[:, c0:c0 + 8, 0:128], [TM], [s.Ktm], s.Ktm)
            k.ts(s.gi[:], s.gi[:], s.bi[:, 0:1], ALU.add, [s.gi, s.bi], [s.gi])
            k.ts(s.gf[:], s.gf[:], s.bf[:, 0:1], ALU.add, [s.gf, s.bf], [s.gf])
            row_softplus_neg(k, s.t2, s.gf, s.t1, None, 2, None)
            k.scan(dirview(s.b, 2, d), cs[0:2, C_RST0:C_RST0 + 512], dirview(s.t2, 2, d), 0.0, ALU.mult, ALU.add,
                   [s.t2, cs], [s.b])
            k.tt(s.g[:], s.gi[:], s.b[:], ALU.subtract, [s.gi, s.b], [s.g])
            k.scan(dirview(s.pm, 2, d), cs[0:2, C_RSTN:C_RSTN + 512], dirview(s.g, 2, d), 0.0, ALU.add, ALU.max,
                   [s.g, cs], [s.pm])
            last = 63 if d == 0 else 0
            bL = fv(s.b, 0, 2, last, [(64, 8)])
            pmL = fv(s.pm, 0, 2, last, [(64, 8)])
            if d == 0:
                o8 = lambda buf: fv(buf, 0, 2, 0, [(1, 8)])
            else:
                o8 = lambda buf: fv(buf, 0, 2, 7, [(-1, 8)])
            bLd = fv(s.b, 0, 2, last, [(64, 8)]) if d == 0 else fv(s.b, 0, 2, last + 64 * 7, [(-64, 8)])
            pmLd = fv(s.pm, 0, 2, last, [(64, 8)]) if d == 0 else fv(s.pm, 0, 2, last + 64 * 7, [(-64, 8)])
            k.scan(o8(s.mnext), pmLd, bLd, s.carry[:, 0:1], ALU.max, ALU.add, [s.pm, s.b, s.carry], [s.mnext])
            if d == 0:
                k.cp(s.mcur[:, 0:1], s.carry[:, 0:1], [s.carry], [s.mcur])
                k.cp(s.mcur[:, 1:8], s.mnext[:, 0:7], [s.mnext], [s.mcur])
                k.cp(s.carry[:, 0:1], s.mnext[:, 7:8], [s.mnext, s.mcur], [s.carry])
            else:
                k.cp(s.mcur[:, 7:8], s.carry[:, 0:1], [s.carry], [s.mcur])
                k.cp(s.mcur[:, 0:7], s.mnext[:, 1:8], [s.mnext], [s.mcur])
                k.cp(s.carry[:, 0:1], s.mnext[:, 0:1], [s.mnext, s.mcur], [s.carry])
            mc_b = fv(s.mcur, 0, 2, 0, [(1, 8), (0, 64)])
            v3 = lambda buf: fv(buf, 0, 2, 0, [(64, 8), (1, 64)])
            k.tt(v3(s.t1), v3(s.pm), mc_b, ALU.max, [s.pm, s.mcur], [s.t1])
            k.ts(s.negmu[:], s.t1[:], -1.0, ALU.mult, [s.t1], [s.negmu])
            k.tt(v3(s.wint), mc_b, v3(s.t1), ALU.subtract, [s.mcur, s.t1], [s.wint])
            k.act(s.wint[:], s.wint[:], AF.Exp, [s.wint], [s.wint])
            k.tt(s.emt[:], s.b[:], s.t1[:], ALU.add, [s.b, s.t1], [s.emt])
            k.act(s.emt[:], s.emt[:], AF.Exp, [s.emt], [s.emt], scale=-1.0)
            k.tt(s.c8[:], bL, s.mnext[:], ALU.subtract, [s.b, s.mnext], [s.c8])
            k.tt(v3(s.kwf), v3(s.g), fv(s.c8, 0, 2, 0, [(1, 8), (0, 64)]), ALU.add, [s.g, s.c8], [s.kwf])
            k.act(s.kwf[:], s.kwf[:], AF.Exp, [s.kwf], [s.kwf])
            k.tt(s.wold[:], s.c8[:], s.mcur[:], ALU.add, [s.c8, s.mcur], [s.wold])
            k.act(s.wold[:], s.wold[:], AF.Exp, [s.wold], [s.wold])
            pc = cx.psum[6]
            for c in range(8):
                for qi, qb in enumerate([s.g, s.wint, s.emt, s.kwf]):
                    col = (c * 4 + qi) * 2
                    k.mm(pc[0:64, col:col + 2], qb[0:2, c * 64:(c + 1) * 64], cs[0:2, C_IDENT:C_IDENT + 2], [qb, cs], [pc])
            for h in range(2):
                k.mm(pc[0:64, 64 + 8 * h:64 + 8 * h + 8], cs[0:2, C_SEL + 128 * h:C_SEL + 128 * h + 64], s.wold[0:2, :], [cs, s.wold], [pc])
            k.cp(s.cols[:], pc[0:64, 0:64], [pc], [s.cols])
            k.cp(s.woldc[:], pc[0:64, 64:80], [pc], [s.woldc], eng="act")
            mk = M_LE if d == 0 else M_GE
            for h in range(2):
                pD = cx.psum[h]
                sel = cs[0:2, C_SEL + 128 * h:C_SEL + 128 * h + 64]
                k.mm(pD[0:64, :], sel, s.negmu[0:2, :], [cs, s.negmu], [pD], start=True, stop=False)
                k.mm(pD[0:64, :], cs[0:64, C_IDENT:C_IDENT + 64], cs[0:64, C_MASK + mk * 512:C_MASK + (mk + 1) * 512], [cs], [pD],
                     start=False, stop=False)
                for c in range(8):
                    k.mm(pD[0:64, c * 64:(c + 1) * 64], s.g[0:2, c * 64:(c + 1) * 64], sel, [s.g, cs], [pD],
                         start=False, stop=(c == 7))
                k.act(s.E[h][:], pD[0:64, :], AF.Exp, [pD], [s.E[h]])
                pS = cx.psum[2 + h]
                for c in range(8):
                    k.mm(pS[0:64, c * 64:(c + 1) * 64], s.KT[h][:, c * 64:(c + 1) * 64], s.QT[h][:, c * 64:(c + 1) * 64],
                         [s.KT[h], s.QT[h]], [pS])
                k.tt(s.AT[h][:], pS[0:64, :], s.E[h][:], ALU.mult, [pS, s.E[h]], [s.AT[h]])
            if step == 0:
                for nm in ["b", "g", "pm", "negmu", "wint", "emt", "kwf"]:
                    dbg(k, f"{nm}{d}", getattr(s, nm), [2, 512])
                dbg(k, f"mnext{d}", s.mnext, [2, 8]); dbg(k, f"mcur{d}", s.mcur, [2, 8]); dbg(k, f"wold{d}", s.wold, [2, 8])
                dbg(k, f"cols{d}", s.cols, [64, 64]); dbg(k, f"woldc{d}", s.woldc, [64, 16])
                dbg(k, f"E{d}", s.E[0], [64, 512]); dbg(k, f"AT{d}", s.AT[0], [64, 512])
            for ci in range(8):
                c = ci if d == 0 else 7 - ci
                for h in range(2):
                    pH = cx.psum[4 + h]
                    cl = lambda qi: s.cols[:, (c * 4 + qi) * 2 + h:(c * 4 + qi) * 2 + h + 1]
                    k.mm(pH[0:64, 0:129], s.AT[h][:, c * 64:(c + 1) * 64], s.Va[h][:, c, :], [s.AT[h], s.Va[h]], [pH])
                    k.mm(pH[0:64, 256:385], s.QT[h][:, c * 64:(c + 1) * 64], s.C[h][:], [s.QT[h], s.C[h]], [pH])
                    k.ts(s.tmp[h][:], pH[0:64, 256:385], cl(1), ALU.mult, [pH, s.cols], [s.tmp[h]])
                    k.tt(s.R[h][:], s.tmp[h][:], pH[0:64, 0:129], ALU.add, [s.tmp[h], pH], [s.R[h]])
                    k.stt(s.dn[h][:, 0:1], s.R[h][:, 128:129], -1.0, s.R[h][:, 128:129], ALU.mult, ALU.max, [s.R[h]], [s.dn[h]])
                    k.ts(s.dn[h][:, 0:1], s.dn[h][:, 0:1], cl(2), ALU.max, [s.dn[h], s.cols], [s.dn[h]])
                    k.op("dve", lambda e, h=h: e.reciprocal(out=s.dn[h][:, 1:2], in_=s.dn[h][:, 0:1]), [s.dn[h]], [s.dn[h]])
                    k.ts(s.Hg[:, c, 128 * h:128 * h + 128], s.R[h][:, 0:128], s.dn[h][:, 1:2], ALU.mult, [s.R[h], s.dn[h]], [s.Hg])
                    k.ts(s.KW[h][:], s.Ktm[:, c, 64 * h:64 * h + 64], cl(3), ALU.mult, [s.Ktm, s.cols], [s.KW[h]], s2=0.125, op1=ALU.mult)
                    pU = cx.psum[6 + h] if False else cx.psum[7]
                    k.mm(pU[0:64, 129 * h:129 * h + 129], s.KW[h][:], s.Va[h][:, c, :], [s.KW[h], s.Va[h]], [pU])
                    k.stt(s.C[h][:], s.C[h][:], s.woldc[:, 8 * h + c:8 * h + c + 1], pU[0:64, 129 * h:129 * h + 129], ALU.mult, ALU.add,
                          [s.C[h], s.woldc, pU], [s.C[h]])
            k.dma("sp", HS[d][t0:t0 + 512, :].rearrange("(c p) f -> p c f", p=64), s.Hg[:], [s.Hg], [HS[d]], s.Hg)


def phase_gdn_pre(k, cx, T, FM, dcw_d, QN, KN, KTM, VTM):
    cs = cx.consts
    NT_ = T // 512
    dcw = k.sb("g_dcw", [128, 6, 5], F32)
    k.dma("sp", dcw[:], dcw_d[:], [dcw_d], [dcw], dcw)
    dg = k.sb("g_diag", [128, 30, 128], F32)
    for i in range(6):
        for j in range(5):
            k.ts(dg[:, i * 5 + j, :], cs[:, C_IDENT:C_IDENT + 128], dcw[:, i, j:j + 1], ALU.mult, [cs, dcw], [dg],
                 eng=("dve" if (i + j) % 2 else "pool"))
    Xs = [k.sb(f"g_X{i}", [128, 516], F32) for i in range(2)]
    Y = [k.sb(f"g_Y{i}", [128, 512], F32) for i in range(2)]
    sq = k.sb("g_sq", [128, 512], F32)
    rs = k.sb("g_rs", [128, 512], F32)
    Yn = [k.sb(f"g_Yn{i}", [128, 512], F32) for i in range(2)]
    tr = [k.sb(f"g_tr{i}", [128, 512], F32) for i in range(2)]
    n = 0
    for tt in range(NT_):
        t0 = tt * 512
        for i in range(6):
            X = Xs[n % 2]
            row0 = FM_DQ + 128 * i
            lo = max(t0 - 2, 0)
            hi = min(t0 + 514, T)
            if tt == 0:
                k.op("dve", lambda e, X=X: e.memset(X[:, 0:2], 0.0), [], [X])
            if tt == NT_ - 1:
                k.op("dve", lambda e, X=X: e.memset(X[:, 514:516], 0.0), [], [X])
            k.dma("sp", X[:, lo - (t0 - 2):hi - (t0 - 2)], FM[row0:row0 + 128, lo:hi], [FM], [X], X)
            pc_ = cx.psum[n % 2]
            for j in range(5):
                k.mm(pc_[:, :], dg[:, i * 5 + j, :], X[:, j:j + 512], [dg, X], [pc_], start=(j == 0), stop=(j == 4))
            Yt = Y[n % 2]
            k.act(Yt[:], pc_[:, :], AF.Silu, [pc_], [Yt])
            h = i % 2
            kind = i // 2
            if kind < 2:
                k.tt(sq[:], Yt[:], Yt[:], ALU.mult, [Yt], [sq])
                pss = cx.psum[2]
                k.mm(pss[:, :], cs[:, C_ONES:C_ONES + 128], sq[:], [cs, sq], [pss])
                k.act(rs[:], pss[:, :], AF.Sqrt, [pss], [rs], bias=float(EPS))
                k.op("dve", lambda e: e.reciprocal(out=rs[:], in_=rs[:]), [rs], [rs])
                Ynt = Yn[n % 2]
                k.stt(Ynt[:], Yt[:], (128.0 ** -0.5) if kind == 0 else 1.0, rs[:], ALU.mult, ALU.mult, [Yt, rs], [Ynt])
                dst = QN[h] if kind == 0 else KN[h]
                k.dma("sp", dst[:, t0:t0 + 512], Ynt[:], [Ynt], [dst], Ynt)
                src = Ynt
            else:
                src = Yt
            if kind >= 1:
                pt = cx.psum[3 + n % 2]
                for ts_ in range(4):
                    k.mm(pt[:, ts_ * 128:(ts_ + 1) * 128], src[:, ts_ * 128:(ts_ + 1) * 128], cs[:, C_IDENT:C_IDENT + 128], [src, cs], [pt])
                trt = tr[n % 2]
                k.cp(trt[:], pt[:, :], [pt], [trt], eng="act")
                dst = KTM[h] if kind == 1 else VTM[h]
                k.dma("sp", dst[t0:t0 + 512, :].rearrange("(s p) f -> p s f", p=128), trt[:].rearrange("p (s f) -> p s f", f=128),
                      [trt], [dst], trt)
            n += 1


def phase_gdn(k, cx, T, FM, QN, KN, KTM, VTM, gpar_d, OS):
    NG = T // 512
    cs = cx.consts
    ID64 = cs[0:64, C_IDENT:C_IDENT + 64]
    G1 = k.sb("gd_G1", [64, 512], F32)
    Nm = k.sb("gd_N", [64, 512], F32)
    NTm = k.sb("gd_NT", [64, 512], F32)
    P2 = [k.sb(f"gd_P2{i}", [64, 512], F32) for i in range(2)]
    PT2 = [k.sb(f"gd_PT2{i}", [64, 512], F32) for i in range(2)]
    XT = k.sb("gd_XT", [64, 512], F32)
    gamT = k.sb("gd_gamT", [64, 512], F32)
    st = []
    for d in range(2):
        s = Ctx()
        for nm in ["br", "ar", "t1", "t2", "beta", "gc", "ngc", "gcb", "bg", "kd", "eg"]:
            setattr(s, nm, k.sb(f"gd_{nm}{d}", [2, 512], F32))
        s.gl8 = k.sb(f"gd_gl8{d}", [2, 8], F32)
        s.egl = k.sb(f"gd_egl{d}", [2, 8], F32)
        s.par = k.sb(f"gd_par{d}", [2, 2], F32)
        k.dma("sp", s.par[:], gpar_d[2 * d:2 * d + 2, :], [gpar_d], [s.par], s.par)
        k.act(s.par[:, 1:2], s.par[:, 1:2], AF.Exp, [s.par], [s.par])
        k.ts(s.par[:, 1:2], s.par[:, 1:2], -1.0, ALU.mult, [s.par], [s.par])
        s.cols = k.sb(f"gd_cols{d}", [64, 48], F32)
        s.eglc = k.sb(f"gd_eglc{d}", [128, 16], F32)
        s.QN = [k.sb(f"gd_QN{d}{h}", [128, 512], F32) for h in range(2)]
        s.KN = [k.sb(f"gd_KN{d}{h}", [128, 512], F32) for h in range(2)]
        s.qg = [k.sb(f"gd_qg{d}{h}", [128, 512], F32) for h in range(2)]
        s.Ktm = [k.sb(f"gd_Ktm{d}{h}", [64, 8, 128], F32) for h in range(2)]
        s.Vtm = [k.sb(f"gd_Vtm{d}{h}", [64, 8, 128], F32) for h in range(2)]
        s.XTb = [k.sb(f"gd_XTb{d}{h}", [64, 512], F32) for h in range(2)]
        s.XTbg = [k.sb(f"gd_XTbg{d}{h}", [64, 512], F32) for h in range(2)]
        s.attnT = [k.sb(f"gd_attnT{d}{h}", [64, 512], F32) for h in range(2)]
        s.nwT = [k.sb(f"gd_nwT{d}{h}", [128, 512], F32) for h in range(2)]
        s.S = [k.sb(f"gd_S{d}{h}", [128, 128], F32) for h in range(2)]
        for h in range(2):
            k.op("dve", lambda e, s=s, h=h: e.memset(s.S[h][:], 0.0), [], [s.S[h]])
        s.vn = [k.sb(f"gd_vn{d}{h}", [64, 128], F32) for h in range(2)]
        s.kdm = [k.sb(f"gd_kdm{d}{h}", [64, 128], F32) for h in range(2)]
        s.Og = k.sb(f"gd_Og{d}", [64, 8, 256], F32)
        st.append(s)
    for step in range(NG):
        for d in range(2):
            s = st[d]
            grp = step if d == 0 else NG - 1 - step
            t0 = grp * 512
            c0 = grp * 8
            k.dma("sp", s.br[:], FM[FM_G + 8 + 2 * d:FM_G + 8 + 2 * d + 2, t0:t0 + 512], [FM], [s.br], s.br)
            k.dma("sp", s.ar[:], FM[FM_G + 12 + 2 * d:FM_G + 12 + 2 * d + 2, t0:t0 + 512], [FM], [s.ar], s.ar)
            for h in range(2):
                k.dma("sp", s.QN[h][:], QN[h][:, t0:t0 + 512], [QN[h]], [s.QN[h]], s.QN[h])
                k.dma("sp", s.KN[h][:], KN[h][:, t0:t0 + 512], [KN[h]], [s.KN[h]], s.KN[h])
                k.dma("sp", s.Ktm[h][:], KTM[h][t0:t0 + 512, :].rearrange("(c p) f -> p c f", p=64), [KTM[h]], [s.Ktm[h]], s.Ktm[h])
                k.dma("sp", s.Vtm[h][:], VTM[h][t0:t0 + 512, :].rearrange("(c p) f -> p c f", p=64), [VTM[h]], [s.Vtm[h]], s.Vtm[h])
            k.act(s.beta[:], s.br[:], AF.Sigmoid, [s.br], [s.beta])
            row_softplus_neg(k, s.t2, s.br, s.t1, None, 2, None)
            k.ts(s.ar[:], s.ar[:], s.par[:, 0:1], ALU.add, [s.ar, s.par], [s.ar], s2=-1.0, op1=ALU.mult)
            row_softplus_neg(k, s.eg, s.ar, s.t1, None, 2, None)
            k.ts(s.eg[:], s.eg[:], s.par[:, 1:2], ALU.mult, [s.eg, s.par], [s.eg], s2=-1.0, op1=ALU.mult)
            k.scan(dirview(s.gc, 2, d), cs[0:2, C_RST0:C_RST0 + 512], dirview(s.eg, 2, d), 0.0, ALU.mult, ALU.add, [s.eg, cs], [s.gc])
            k.ts(s.ngc[:], s.gc[:], -1.0, ALU.mult, [s.gc], [s.ngc])
            k.tt(s.gcb[:], s.gc[:], s.t2[:], ALU.add, [s.gc, s.t2], [s.gcb])
            last = 63 if d == 0 else 0
            gl = fv(s.gc, 0, 2, last, [(64, 8)])
            k.cp(s.gl8[:], gl, [s.gc], [s.gl8])
            k.act(s.egl[:], s.gl8[:], AF.Exp, [s.gl8], [s.egl])
            k.act(s.eg[:], s.gc[:], AF.Exp, [s.gc], [s.eg])
            k.tt(s.bg[:], s.beta[:], s.eg[:], ALU.mult, [s.beta, s.eg], [s.bg])
            v3 = lambda buf: fv(buf, 0, 2, 0, [(64, 8), (1, 64)])
            k.tt(v3(s.kd), fv(s.gl8, 0, 2, 0, [(1, 8), (0, 64)]), v3(s.gc), ALU.subtract, [s.gl8, s.gc], [s.kd])
            k.act(s.kd[:], s.kd[:], AF.Exp, [s.kd], [s.kd])
            pc = cx.psum[5]
            for c in range(8):
                for qi, qb in enumerate([s.beta, s.bg, s.kd]):
                    col = (c * 3 + qi) * 2
                    k.mm(pc[0:64, col:col + 2], qb[0:2, c * 64:(c + 1) * 64], cs[0:2, C_IDENT:C_IDENT + 2], [qb, cs], [pc])
            for h in range(2):
                k.mm(pc[:, 64 + 8 * h:64 + 8 * h + 8], cs[0:2, C_SEL + 128 * h:C_SEL + 128 * h + 128], s.egl[0:2, :], [cs, s.egl], [pc])
            k.cp(s.cols[:], pc[0:64, 0:48], [pc], [s.cols])
            k.cp(s.eglc[:], pc[:, 64:80], [pc], [s.eglc], eng="act")
            mT = M_LE if d == 0 else M_GE
            mS = M_GT if d == 0 else M_LT
            for h in range(2):
                sel64 = cs[0:2, C_SEL + 128 * h:C_SEL + 128 * h + 64]
                sel128 = cs[0:2, C_SEL + 128 * h:C_SEL + 128 * h + 128]
                pq = cx.psum[6]
                k.mm(pq[:, :], sel128, s.eg[0:2, :], [cs, s.eg], [pq])
                k.tt(s.qg[h][:], pq[:, :], s.QN[h][:], ALU.mult, [pq, s.QN[h]], [s.qg[h]])
                pD = cx.psum[3]
                k.mm(pD[0:64, :], sel64, s.ngc[0:2, :], [cs, s.ngc], [pD], start=True, stop=False)
                k.mm(pD[0:64, :], ID64, cs[0:64, C_MASK + mS * 512:C_MASK + (mS + 1) * 512], [cs], [pD], start=False, stop=False)
                for c in range(8):
                    k.mm(pD[0:64, c * 64:(c + 1) * 64], s.gcb[0:2, c * 64:(c + 1) * 64], sel64, [s.gcb, cs], [pD], start=False, stop=(c == 7))
                k.act(G1[:], pD[0:64, :], AF.Exp, [pD], [G1])
                pK = cx.psum[4]
                for c in range(8):
                    k.mm(pK[0:64, c * 64:(c + 1) * 64], s.KN[h][:, c * 64:(c + 1) * 64], s.KN[h][:, c * 64:(c + 1) * 64], [s.KN[h]], [pK])
                k.stt(Nm[:], pK[0:64, :], -1.0, G1[:], ALU.mult, ALU.mult, [pK, G1], [Nm])
                k.mm(pD[0:64, :], sel64, s.gc[0:2, :], [cs, s.gc], [pD], start=True, stop=False)
                k.mm(pD[0:64, :], ID64, cs[0:64, C_MASK + mT * 512:C_MASK + (mT + 1) * 512], [cs], [pD], start=False, stop=False)
                for c in range(8):
                    k.mm(pD[0:64, c * 64:(c + 1) * 64], s.ngc[0:2, c * 64:(c + 1) * 64], sel64, [s.ngc, cs], [pD], start=False, stop=(c == 7))
                k.act(gamT[:], pD[0:64, :], AF.Exp, [pD], [gamT])
                for c in range(8):
                    k.mm(pK[0:64, c * 64:(c + 1) * 64], s.KN[h][:, c * 64:(c + 1) * 64], s.QN[h][:, c * 64:(c + 1) * 64], [s.KN[h], s.QN[h]], [pK])
                k.tt(s.attnT[h][:], pK[0:64, :], gamT[:], ALU.mult, [pK, gamT], [s.attnT[h]])
                pA, pB, pC = cx.psum[0], cx.psum[1], cx.psum[2]
                for c in range(8):
                    k.mm(pA[0:64, c * 64:(c + 1) * 64], Nm[:, c * 64:(c + 1) * 64], ID64, [Nm, cs], [pA])
                k.cp(NTm[:], pA[0:64, :], [pA], [NTm], eng="act")
                k.tt(fv(XT, 0, 64, 0, [(64, 8), (1, 64)]), fv(NTm, 0, 64, 0, [(64, 8), (1, 64)]),
                     fv(cs, 0, 64, C_IDENT, [(0, 8), (1, 64)]), ALU.add, [NTm, cs], [XT])
                Pc, PTc = Nm, NTm
                for m in range(5):
                    Pn, PTn = P2[m % 2], PT2[m % 2]
                    for c in range(8):
                        sl = slice(c * 64, (c + 1) * 64)
                        k.mm(pA[0:64, sl], PTc[:, sl], Pc[:, sl], [PTc, Pc], [pA])
                    if m < 4:
                        for c in range(8):
                            sl = slice(c * 64, (c + 1) * 64)
                            k.mm(pB[0:64, sl], Pc[:, sl], PTc[:, sl], [PTc, Pc], [pB])
                    k.cp(Pn[:], pA[0:64, :], [pA], [Pn], eng="act")
                    if m < 4:
                        k.cp(PTn[:], pB[0:64, :], [pB], [PTn])
                    for c in range(8):
                        sl = slice(c * 64, (c + 1) * 64)
                        k.mm(pC[0:64, sl], Pn[:, sl], XT[:, sl], [Pn, XT], [pC])
                    k.tt(XT[:], XT[:], pC[0:64, :], ALU.add, [XT, pC], [XT])
                    Pc, PTc = Pn, PTn
                for c in range(8):
                    sl = slice(c * 64, (c + 1) * 64)
                    k.ts(s.XTb[h][:, sl], XT[:, sl], s.cols[:, (c * 3 + 0) * 2 + h:(c * 3 + 0) * 2 + h + 1], ALU.mult, [XT, s.cols], [s.XTb[h]])
                    k.ts(s.XTbg[h][:, sl], XT[:, sl], s.cols[:, (c * 3 + 1) * 2 + h:(c * 3 + 1) * 2 + h + 1], ALU.mult, [XT, s.cols], [s.XTbg[h]], eng="pool")
                for c in range(8):
                    sl = slice(c * 64, (c + 1) * 64)
                    k.mm(pA[:, sl], s.Ktm[h][:, c, :], s.XTbg[h][:, sl], [s.Ktm[h], s.XTbg[h]], [pA])
                k.act(s.nwT[h][:], pA[:, :], AF.Copy, [pA], [s.nwT[h]], scale=-1.0)
            for ci in range(8):
                c = ci if d == 0 else 7 - ci
                sl = slice(c * 64, (c + 1) * 64)
                for h in range(2):
                    pV = cx.psum[6]
                    pU = cx.psum[7]
                    vr = pV[0:64, 256 * h:256 * h + 128]
                    orr = pV[0:64, 256 * h + 128:256 * h + 256]
                    k.mm(vr, s.XTb[h][:, sl], s.Vtm[h][:, c, :], [s.XTb[h], s.Vtm[h]], [pV], start=True, stop=False)
                    k.mm(vr, s.nwT[h][:, sl], s.S[h][:], [s.nwT[h], s.S[h]], [pV], start=False, stop=True)
                    k.cp(s.vn[h][:], vr, [pV], [s.vn[h]])
                    k.mm(orr, s.qg[h][:, sl], s.S[h][:], [s.qg[h], s.S[h]], [pV], start=True, stop=False)
                    k.mm(orr, s.attnT[h][:, sl], s.vn[h][:], [s.attnT[h], s.vn[h]], [pV], start=False, stop=True)
                    k.cp(s.Og[:, c, 128 * h:128 * h + 128], orr, [pV], [s.Og], eng="act")
                    k.ts(s.kdm[h][:], s.Ktm[h][:, c, :], s.cols[:, (c * 3 + 2) * 2 + h:(c * 3 + 2) * 2 + h + 1], ALU.mult, [s.Ktm[h], s.cols], [s.kdm[h]], eng="pool")
                    ur = pU[:, 128 * h:128 * h + 128]
                    k.mm(ur, s.kdm[h][:], s.vn[h][:], [s.kdm[h], s.vn[h]], [pU])
                    k.stt(s.S[h][:], s.S[h][:], s.eglc[:, 8 * h + c:8 * h + c + 1], ur, ALU.mult, ALU.add, [s.S[h], s.eglc, pU], [s.S[h]])
            k.dma("sp", OS[d][t0:t0 + 512, :].rearrange("(c p) f -> p c f", p=64), s.Og[:], [s.Og], [OS[d]], s.Og)


def phase_even_combine(k, cx, T, TM, HS, OS, gn_d, MIX, MIXT=None):
    gn = k.sb("cb_gn", [128, 512], F32)
    k.dma("sp", gn[:], gn_d[:].partition_broadcast(128), [gn_d], [gn], gn)
    NTl = T // 128
    bufs = []
    for i in range(2):
        b = Ctx()
        b.a = k.sb(f"cb_a{i}", [128, 512], F32)
        b.b = k.sb(f"cb_b{i}", [128, 512], F32)
        b.moz = k.sb(f"cb_moz{i}", [128, 768], F32)
        b.sq = k.sb(f"cb_sq{i}", [128, 512], F32)
        b.ss = k.sb(f"cb_ss{i}", [128, 4], F32)
        b.tr = k.sb(f"cb_tr{i}", [128, 512], F32)
        bufs.append(b)
    for tt in range(NTl):
        b = bufs[tt % 2]
        r = slice(tt * 128, (tt + 1) * 128)
        k.dma("sp", b.a[:, 0:256], HS[0][r, :], [HS[0]], [b.a], b.a, grouped=True)
        k.dma("sp", b.a[:, 256:512], OS[0][r, :], [OS[0]], [b.a], b.a, grouped=True)
        k.dma("sp", b.b[:, 0:256], HS[1][r, :], [HS[1]], [b.b], b.b, grouped=True)
        k.dma("sp", b.b[:, 256:512], OS[1][r, :], [OS[1]], [b.b], b.b, grouped=True)
        k.dma("sp", b.moz[:], TM[r, 384:1152], [TM], [b.moz], b.moz)
        k.tt(b.a[:], b.a[:], b.b[:], ALU.add, [b.a, b.b], [b.a])
        k.tt(b.sq[:], b.a[:], b.a[:], ALU.mult, [b.a], [b.sq])
        k.op("dve", lambda e, b=b: e.tensor_reduce(out=b.ss[:], in_=b.sq[:].rearrange("p (h f) -> p h f", f=128), axis=AX.X, op=ALU.add),
             [b.sq], [b.ss])
        k.act(b.ss[:], b.ss[:], AF.Sqrt, [b.ss], [b.ss], scale=1.0 / 128.0, bias=float(EPS))
        k.op("dve", lambda e, b=b: e.reciprocal(out=b.ss[:], in_=b.ss[:]), [b.ss], [b.ss])
        k.tt(b.a[:].rearrange("p (h f) -> p h f", f=128), b.a[:].rearrange("p (h f) -> p h f", f=128),
             fv(b.ss, 0, 128, 0, [(1, 4), (0, 128)]), ALU.mult, [b.a, b.ss], [b.a])
        k.tt(b.a[:], b.a[:], gn[:], ALU.mult, [b.a, gn], [b.a])
        k.act(b.moz[:, 0:256], b.moz[:, 0:256], AF.Sigmoid, [b.moz], [b.moz])
        k.act(b.moz[:, 256:768], b.moz[:, 256:768], AF.Silu, [b.moz], [b.moz])
        k.tt(b.a[:, 0:256], b.a[:, 0:256], b.moz[:, 0:256], ALU.mult, [b.a, b.moz], [b.a])
        k.tt(b.a[:], b.a[:], b.moz[:, 256:768], ALU.mult, [b.a, b.moz], [b.a])
        if MIXT is None:
            k.dma("sp", MIX[r, :], b.a[:], [b.a], [MIX], b.a)
        else:
            pt = cx.psum[tt % 2]
            for fc in range(4):
                k.mm(pt[:, fc * 128:(fc + 1) * 128], b.a[:, fc * 128:(fc + 1) * 128], cx.consts[:, C_IDENT:C_IDENT + 128], [b.a, cx.consts], [pt])
            k.cp(b.tr[:], pt[:, :], [pt], [b.tr], eng="act")
            mxt = MIXT[(tt * 128) // 1024]
            tl = (tt * 128) % 1024
            k.dma("sp", mxt[:].rearrange("(c p) t -> p c t", p=128)[:, :, tl:tl + 128],
                  b.tr[:].rearrange("p (c t) -> p c t", t=128), [b.tr], [mxt], b.tr)


def build_even(T, debug=False, phases=("ml", "gdn", "comb")):
    nc = bass.Bass("TRN2", target_bir_lowering=False)
    k = KB(nc)
    cx = Ctx()
    setup_common(k, cx)
    kd = "ExternalOutput" if debug else "Internal"
    emit_even(k, cx, T, kd, phases)
    k.finish()
    k.close()
    print("even program: instructions", k.ninst, "sems", k.nsem)
    return nc


def emit_even(k, cx, T, kd, phases=("ml", "gdn", "comb"), MIXT=None):
    xT = k.dram("xT", [1024, T], F32, kind="ExternalInput")
    cT = k.dram("cT", [128, 8], F32, kind="ExternalInput")
    adaw = k.dram("adaw", [1024, 2048], F32, kind="ExternalInput")
    adab = k.dram("adab", [128, 16], F32, kind="ExternalInput")
    ng = k.dram("ng", [128, 8], F32, kind="ExternalInput")
    W = k.dram("W", [1024, NC1], F32, kind="ExternalInput")
    mgb = k.dram("mgb", [8, 1], F32, kind="ExternalInput")
    gpar = k.dram("gpar", [4, 2], F32, kind="ExternalInput")
    dcw = k.dram("dcw", [128, 6, 5], F32, kind="ExternalInput")
    gn = k.dram("gn", [1, 512], F32, kind="ExternalInput")
    MIX = k.dram("MIX", [T, 512], F32, kind="ExternalOutput") if MIXT is None else None
    OS = [k.dram(f"OS{d}", [T, 256], F32, kind=kd) for d in range(2)]
    QN = [k.dram(f"QN{h}", [128, T], F32, kind=kd) for h in range(2)]
    KN = [k.dram(f"KN{h}", [128, T], F32, kind=kd) for h in range(2)]
    KTM = [k.dram(f"KTM{h}", [T, 128], F32, kind=kd) for h in range(2)]
    VTM = [k.dram(f"VTM{h}", [T, 128], F32, kind=kd) for h in range(2)]
    FM = k.dram("FM", [FM_ROWS, T], F32, kind=kd)
    TM = k.dram("TM", [T, 1152], F32, kind=kd)
    HS = [k.dram(f"HS{d}", [T, 256], F32, kind=kd) for d in range(2)]

    A = k.sb("evA", [128, 8], F32)
    Bm = k.sb("evBm", [128, 8], F32)
    with k.scope():
        mod = phase_mod(k, cx, cT, adaw, adab, 16, tag="me")
        ngs = k.sb("evngs", [128, 8], F32)
        k.dma("sp", ngs[:], ng[:], [ng], [ngs], ngs)
        k.stt(A[:], mod[:, 8:16], 1.0, ngs[:], ALU.add, ALU.mult, [mod, ngs], [A])
        k.cp(Bm[:], mod[:, 0:8], [mod], [Bm])
    sc1 = k.scope()
    sc1.__enter__()
    xT3 = xT[:].rearrange("(k p) t -> p k t", p=128)
    fm_specs = []
    for (c0, nr, scale) in [(FM_MQ, 128, 1.0), (FM_MK, 128, 0.125), (FM_G, 16, 1.0)] + \
            [(FM_DQ + 128 * i, 128, 1.0) for i in range(6)]:
        fm_specs.append((c0, nr, FM, (lambda tt, r0=c0, nr=nr: FM[r0:r0 + nr, tt * 512:(tt + 1) * 512]), scale))
    tm_specs = []
    for (c0, ncl) in [(TM_MK, 384), (TM_MO, 256), (TM_Z, 512)]:
        tm_specs.append((c0, ncl, TM, (lambda tt, ts, c0=c0, ncl=ncl: TM[tt * 512 + ts * 128: tt * 512 + (ts + 1) * 128, c0 - TM0:c0 - TM0 + ncl])))
    phase_norm_proj(k, cx, T, lambda tt: xT3[:, :, tt * 512:(tt + 1) * 512], [xT], A, Bm, W, NC1, fm_specs, tm_specs)
    sc1.__exit__(None, None, None)
    if "ml" in phases:
        with k.scope():
            phase_mlstm(k, cx, T, FM, TM, mgb, HS)
    if "gdn" in phases:
        with k.scope():
            phase_gdn_pre(k, cx, T, FM, dcw, QN, KN, KTM, VTM)
        with k.scope():
            phase_gdn(k, cx, T, FM, QN, KN, KTM, VTM, gpar, OS)
    if "comb" in phases:
        with k.scope():
            phase_even_combine(k, cx, T, TM, HS, OS, gn, MIX, MIXT)


SEQ = 8192


def colform(v, nchunks):
    return np.ascontiguousarray(np.asarray(v, np.float32).reshape(nchunks, 128).T)


def even_core_inputs(inp, b, e, T=SEQ):
    w_in = inp["ev_w_in"][0]
    o = np.cumsum([0, 256, 256, 512, 512, 16, 1536, 16, 1024])
    mq, mk, mv, mo, mg, dqkv, dg, z = [w_in[:, o[i]:o[i + 1]] for i in range(8)]
    hs = [2 * e, 2 * e + 1]
    gsel = [t * 4 + h for t in range(4) for h in hs]
    cols = [mq[:, 128 * e:128 * e + 128], mk[:, 128 * e:128 * e + 128], mg[:, gsel], dg[:, gsel]]
    for part in range(3):
        for h in hs:
            cols.append(dqkv[:, part * 512 + h * 128: part * 512 + (h + 1) * 128])
    cols += [mk[:, 128 * e:128 * e + 128], mv[:, 256 * e:256 * e + 256], mo[:, 256 * e:256 * e + 256],
             z[:, 256 * e:256 * e + 256], z[:, 512 + 256 * e:512 + 256 * e + 256]]
    W = np.ascontiguousarray(np.concatenate(cols, axis=1))
    assert W.shape[1] == NC1
    cw = inp["ev_dn_conv_w"][0]
    dcw = np.stack([cw[:, part * 512 + h * 128: part * 512 + (h + 1) * 128] for part in range(3) for h in hs], 0)
    return {
        "consts": make_consts(),
        "xT": np.ascontiguousarray(inp["x"][b, :T].T),
        "cT": colform(inp["c"][b], 8),
        "adaw": np.ascontiguousarray(inp["ada_w"][0][:, 0:2048]),
        "adab": colform(inp["ada_b"][0][0:2048], 16),
        "ng": colform(inp["norm_g"][0], 8),
        "W": W,
        "mgb": np.ascontiguousarray(inp["ev_m_gate_b"][0][:, hs].reshape(8, 1)),
        "gpar": np.ascontiguousarray(np.stack([inp["ev_dn_dt_bias"][0][:, hs].reshape(4), inp["ev_dn_a_log"][0][:, hs].reshape(4)], 1)),
        "dcw": np.ascontiguousarray(dcw.transpose(2, 0, 1)),
        "gn": np.ascontiguousarray(np.concatenate([inp["ev_m_norm_g"][0][256 * e:256 * e + 256],
                                                   inp["ev_dn_norm_g"][0][256 * e:256 * e + 256]])[None, :]),
    }


def run_even(inp, T=SEQ):
    nc = build_even(T)
    in_maps = [even_core_inputs(inp, c // 2, c % 2, T) for c in range(8)]
    res = run_bass_kernel_spmd(nc, in_maps, core_ids=list(range(8)))
    mix = np.zeros((4, T, 1024), np.float32)
    for c in range(8):
        b, e = c // 2, c % 2
        m = res.results[c]["MIX"]
        mix[b, :, 256 * e:256 * e + 256] = m[:, 0:256]
        mix[b, :, 512 + 256 * e:512 + 256 * e + 256] = m[:, 256:512]
    return mix


HALO = 1024
NEG_ATT = -30000.0
C2_J = 0
C2_OH = 128
C2_COLS = 128 + 3 * 384
DILS = (1, 4, 16)
VV_COLS = 33 + 4 * 9 + 16 * 3


def make_vv(TO, s_half, nhalves):
    cols = []
    S = TO * nhalves
    for d in DILS:
        npos = TO // d
        nkt = npos // 128 + 1
        for r in range(d):
            p = -64 + 128 * np.arange(nkt)[None, :] + np.arange(128)[:, None]
            tok = s_half * TO + r + d * p
            cols.append(((tok >= 0) & (tok < S)).astype(np.float32))
    return np.ascontiguousarray(np.concatenate(cols, axis=1))


def t5_bucket_np(rel):
    n = np.abs(rel)
    large = 8 + (np.log(np.maximum(n, 1).astype(np.float32) / np.float32(8)) / np.float32(np.log(128.0)) * np.float32(8)).astype(np.int32)
    large = np.minimum(large, 15)
    return (rel > 0).astype(np.int32) * 16 + np.where(n < 8, n, large)


def make_consts2():
    c = np.zeros((128, C2_COLS), np.float32)
    c[np.arange(128), C2_J + 127 - np.arange(128)] = 1.0
    for g, d in enumerate(DILS):
        for u in range(383):
            rel = u - 191
            if abs(rel) <= 64:
                c[t5_bucket_np(np.array(rel * d)), C2_OH + g * 384 + u] = 1.0
            else:
                c[32, C2_OH + g * 384 + u] = NEG_ATT
    return c


def phase_attention(k, cx, TO, QT, KT, VT, vv_d, relb_d, relbT_d, GF, DT, c2):
    E = TO + 2 * HALO
    cs = cx.consts
    rb = k.sb("at_rb", [33, 8], F32)
    k.op("dve", lambda e: e.memset(rb[:], 1.0), [], [rb])
    k.dma("sp", rb[0:32, :], relb_d[:], [relb_d], [rb], rb)
    k.ts(rb[0:32, :], rb[0:32, :], 8.0, ALU.mult, [rb], [rb])
    gfs = k.sb("at_gfs", [8, 3 * 384], F32)
    pg = cx.psum[0]
    for g in range(3):
        k.mm(pg[0:8, 0:384], rb[0:33, :], c2[0:33, C2_OH + g * 384:C2_OH + (g + 1) * 384], [rb, c2], [pg])
        k.cp(gfs[:, g * 384:(g + 1) * 384], pg[0:8, 0:384], [pg], [gfs])
    k.dma("sp", GF[:], gfs[:], [gfs], [GF], gfs)
    bmx = k.sb("at_bmx", [128, 32], F32)
    bmax = k.sb("at_bmax", [128, 1], F32)
    Qa = k.sb("at_Qa", [65, TO], F32)
    Ka = k.sb("at_Ka", [65, E], F32)
    k.op("dve", lambda e: e.memset(Ka[64:65, :], 1.0), [], [Ka])
    sqt = k.sb("at_sq", [64, 512], F32)
    kmx = k.sb("at_kmx", [128, 16], F32)
    kmax = k.sb("at_kmax", [128, 1], F32)
    accN = k.sb("at_accN", [64, TO], F32)
    accD = k.sb("at_accD", [64, TO], F32)
    H = k.sb("at_H", [128, 6, 128], F32)
    Vt = [k.sb(f"at_Vt{i}", [128, 34, 64], F32) for i in range(2)]
    Vv = [k.sb(f"at_Vv{i}", [128, 34, 64], F32) for i in range(2)]
    PT = [k.sb(f"at_PT{i}", [128, 512], F32) for i in range(2)]
    ones64 = cs[0:64, C_ONES:C_ONES + 128]
    VV = k.sb("at_VV", [128, vv_d[:].shape[1]], F32)
    k.dma("sp", VV[:], vv_d[:], [vv_d], [VV], VV)
    vvoff = 0
    VVO = {}
    for g_, d_ in enumerate(DILS):
        for r_ in range(d_):
            VVO[(g_, r_)] = vvoff
            vvoff += (TO // d_) // 128 + 1
    assert vvoff == vv_d[:].shape[1]
    nv = 0
    npt = 0
    for hd in range(8):
        r0 = hd * 64
        k.dma("sp", Qa[0:64, :], QT[r0:r0 + 64, :], [QT], [Qa], Qa)
        k.dma("sp", Ka[0:64, :], KT[r0:r0 + 64, :], [KT], [Ka], Ka)
        for g in range(3):
            for kt in range(2):
                src = bass.AP(GF[:].tensor, GF[:].offset + hd * 1152 + g * 384 + kt * 128, [[1, 128], [1, 128]])
                k.dma("sp", H[:, g * 2 + kt, :], src, [GF], [H], H, grouped=True)
        k.dma("sp", bmx[:], relbT_d[hd:hd + 1, :].partition_broadcast(128), [relbT_d], [bmx], bmx)
        k.op("dve", lambda e: e.tensor_reduce(out=bmax[:], in_=bmx[:], axis=AX.X, op=ALU.max), [bmx], [bmax])
        k.ts(bmax[:], bmax[:], 8.0, ALU.mult, [bmax], [bmax])
        pk = cx.psum[1]
        for t in range(E // 512):
            k.tt(sqt[:], Ka[0:64, t * 512:(t + 1) * 512], Ka[0:64, t * 512:(t + 1) * 512], ALU.mult, [Ka], [sqt])
            k.mm(pk[:, :], ones64, sqt[:], [cs, sqt], [pk])
            k.op("dve", lambda e, t=t: e.tensor_reduce(out=kmx[:, t:t + 1], in_=pk[:, :], axis=AX.X, op=ALU.max), [pk], [kmx])
        k.op("dve", lambda e: e.tensor_reduce(out=kmax[:], in_=kmx[:, 0:E // 512], axis=AX.X, op=ALU.max), [kmx], [kmax])
        k.act(kmax[:], kmax[:], AF.Sqrt, [kmax], [kmax])
        for t in range(TO // 512):
            k.tt(sqt[:], Qa[0:64, t * 512:(t + 1) * 512], Qa[0:64, t * 512:(t + 1) * 512], ALU.mult, [Qa], [sqt])
            k.mm(pk[:, :], ones64, sqt[:], [cs, sqt], [pk])
            k.act(Qa[64:65, t * 512:(t + 1) * 512], pk[64:65, :], AF.Sqrt, [pk], [Qa])
        k.ts(Qa[64:65, :], Qa[64:65, :], kmax[64:65, 0:1], ALU.mult, [Qa, kmax], [Qa], s2=bmax[64:65, 0:1], op1=ALU.add)
        k.ts(Qa[64:65, :], Qa[64:65, :], -1.0, ALU.mult, [Qa], [Qa])
        if hd == 0:
            dbg(k, "H", H, [128, 6, 128]); dbg(k, "Qa", Qa, [65, TO]); dbg(k, "Ka", Ka, [65, E])
        first = True
        for g, d in enumerate(DILS):
            npos = TO // d
            nkt = npos // 128 + 1
            for r in range(d):
                V1 = Vt[nv % 2]
                V2 = Vv[nv % 2]
                nv += 1
                base = HALO + r - 64 * d
                for n0 in range(0, nkt, 8):
                    nn = min(8, nkt - n0)
                    vsrc = bass.AP(VT[:].tensor, VT[:].offset + (base + n0 * 128 * d) * 512 + r0, [[d * 512, 128], [128 * d * 512, nn], [1, 64]])
                    k.dma("sp", V1[:, n0:n0 + nn, :], vsrc, [VT], [V1], V1, grouped=True)
                vb_ = fv(VV, 0, 128, VVO[(g, r)], [(1, nkt), (0, 64)])
                k.tt(V1[:, 0:nkt, :], V1[:, 0:nkt, :], vb_, ALU.mult, [V1, VV], [V1])
                k.cp(V2[:, 0:nkt, :], vb_, [VV], [V2], eng="pool")
                nqt = npos // 128
                for qb in range(0, nqt, 2):
                    nq = min(2, nqt - qb)
                    pS = cx.psum[2 + npt % 2]
                    for qi in range(nq):
                        qt = qb + qi
                        qap = fv(Qa, 0, 65, r + d * 128 * qt, [(d, 128)])
                        for kt in range(2):
                            kap = fv(Ka, 0, 65, HALO + r + d * (128 * qt - 64 + 128 * kt), [(d, 128)])
                            col = (qi * 2 + kt) * 128
                            k.mm(pS[:, col:col + 128], kap, qap, [Ka, Qa], [pS], start=True, stop=False)
                            k.mm(pS[:, col:col + 128], H[:, g * 2 + kt, :], c2[:, C2_J:C2_J + 128], [H, c2], [pS], start=False, stop=True)
                    P = PT[npt % 2]
                    k.act(P[:, 0:nq * 256], pS[:, 0:nq * 256], AF.Exp, [pS], [P], scale=0.125)
                    if hd == 0 and g == 0 and qb == 0:
                        dbg(k, "P", P, [128, 512]); dbgap(k, "V1", V1, V1[:, 0:9, :], [128, 9, 64]); dbgap(k, "V2", V2, V2[:, 0:9, :], [128, 9, 64])
                    pN = cx.psum[4 + npt % 2]
                    pDn = cx.psum[6 + npt % 2]
                    npt += 1
                    for qi in range(nq):
                        qt = qb + qi
                        for kt in range(2):
                            col = (qi * 2 + kt) * 128
                            k.mm(pN[0:64, qi * 128:(qi + 1) * 128], V1[:, qt + kt, :], P[:, col:col + 128], [V1, P], [pN],
                                 start=(kt == 0), stop=(kt == 1))
                            k.mm(pDn[0:64, qi * 128:(qi + 1) * 128], V2[:, qt + kt, :], P[:, col:col + 128], [V2, P], [pDn],
                                 start=(kt == 0), stop=(kt == 1))
                    qcols = nq * 128
                    an = fv(accN, 0, 64, r + d * 128 * qb, [(d, qcols)])
                    ad = fv(accD, 0, 64, r + d * 128 * qb, [(d, qcols)])
                    if first:
                        k.cp(an, pN[0:64, 0:qcols], [pN], [accN], eng="act")
                        k.cp(ad, pDn[0:64, 0:qcols], [pDn], [accD])
                    else:
                        k.tt(an, an, pN[0:64, 0:qcols], ALU.add, [accN, pN], [accN], eng="dve")
                        k.tt(ad, ad, pDn[0:64, 0:qcols], ALU.add, [accD, pDn], [accD], eng="dve")
            first = False
        k.op("dve", lambda e: e.reciprocal(out=accD[:], in_=accD[:]), [accD], [accD])
        k.tt(accN[:], accN[:], accD[:], ALU.mult, [accN, accD], [accN])
        k.dma("sp", DT[r0:r0 + 64, :], accN[:], [accN], [DT], accN)


def phase_conv_module(k, cx, TO, GA, GB, valid_d, cw_d, cpar_d, CT):
    cs = cx.consts
    cw = k.sb("cv_w", [128, 4, 31], F32)
    k.dma("sp", cw[:], cw_d[:], [cw_d], [cw], cw)
    cpar = k.sb("cv_par", [128, 4, 3], F32)
    k.dma("sp", cpar[:], cpar_d[:], [cpar_d], [cpar], cpar)
    dg = k.sb("cv_diag", [128, 124, 128], BF16)
    for ch in range(4):
        for j in range(31):
            k.ts(dg[:, ch * 31 + j, :], cs[:, C_IDENT:C_IDENT + 128], cw[:, ch, j:j + 1], ALU.mult, [cs, cw], [dg],
                 eng=("dve" if j % 2 else "pool"))
    ga = [k.sb(f"cv_ga{i}", [128, 4, 542], F32) for i in range(2)]
    gb = [k.sb(f"cv_gb{i}", [128, 4, 542], F32) for i in range(2)]
    vb = k.sb("cv_vb", [128, 542], F32)
    uin = k.sb("cv_uin", [128, 4, 542], BF16)
    U = k.sb("cv_U", [128, 4, 512], F32)
    XC = k.sb("cv_XC", [128, 4, 512], F32)
    SQ = k.sb("cv_SQ", [128, 4, 512], F32)
    rs = k.sb("cv_rs", [128, 512], F32)
    O = [k.sb(f"cv_O{i}", [128, 4, 512], F32) for i in range(2)]
    ones = cs[:, C_ONES:C_ONES + 128]
    for tt in range(TO // 512):
        e0 = HALO + tt * 512 - 15
        a, b = ga[tt % 2], gb[tt % 2]
        k.dma("sp", a[:], GA[:].rearrange("(c p) t -> p c t", p=128)[:, :, e0:e0 + 542], [GA], [a], a)
        k.dma("sp", b[:], GB[:].rearrange("(c p) t -> p c t", p=128)[:, :, e0:e0 + 542], [GB], [b], b)
        k.dma("sp", vb[:], valid_d[0:1, e0:e0 + 542].partition_broadcast(128), [valid_d], [vb], vb)
        k.act(b[:], b[:], AF.Sigmoid, [b], [b])
        k.tt(a[:], a[:], b[:], ALU.mult, [a, b], [a])
        k.tt(uin[:], a[:], fv(vb, 0, 128, 0, [(0, 4), (1, 542)]), ALU.mult, [a, vb], [uin])
        for ch in range(4):
            pc_ = cx.psum[ch % 2]
            for j in range(31):
                k.mm(pc_[:, :], dg[:, ch * 31 + j, :], uin[:, ch, j:j + 512], [dg, uin], [pc_], start=(j == 0), stop=(j == 30))
            k.ts(U[:, ch, :], pc_[:, :], cpar[:, ch, 0:1], ALU.add, [pc_, cpar], [U])
        pm = cx.psum[2]
        for ch in range(4):
            k.mm(pm[:, :], ones, U[:, ch, :], [cs, U], [pm], start=(ch == 0), stop=(ch == 3))
        for ch in range(4):
            k.stt(XC[:, ch, :], pm[:, :], -1.0 / 512.0, U[:, ch, :], ALU.mult, ALU.add, [pm, U], [XC])
        k.tt(SQ[:], XC[:], XC[:], ALU.mult, [XC], [SQ], eng="pool")
        pv = cx.psum[3]
        for ch in range(4):
            k.mm(pv[:, :], ones, SQ[:, ch, :], [cs, SQ], [pv], start=(ch == 0), stop=(ch == 3))
        k.act(rs[:], pv[:, :], AF.Sqrt, [pv], [rs], scale=1.0 / 512.0, bias=float(EPS))
        k.op("dve", lambda e: e.reciprocal(out=rs[:], in_=rs[:]), [rs], [rs])
        Ot = O[tt % 2]
        for ch in range(4):
            k.tt(XC[:, ch, :], XC[:, ch, :], rs[:], ALU.mult, [XC, rs], [XC])
            k.act(Ot[:, ch, :], XC[:, ch, :], AF.Silu, [XC, cpar], [Ot], scale=cpar[:, ch, 1:2], bias=cpar[:, ch, 2:3])
        k.dma("sp", CT[:].rearrange("(c p) t -> p c t", p=128)[:, :, tt * 512:(tt + 1) * 512], Ot[:], [Ot], [CT], Ot)


def phase_final(k, cx, TO, CT, DT, ZT, X1, Wo_d, gate1, fg_d, outT):
    cs = cx.consts
    Wb = k.sb("fn_Wb", [128, 8, 1024], BF16)
    load_w_bf16(k, Wb, Wo_d, 1024, "fn")
    fg = k.sb("fn_fg", [128, 8], F32)
    k.dma("sp", fg[:], fg_d[:], [fg_d], [fg], fg)
    M = [k.sb(f"fn_M{i}", [128, 8, 512], F32) for i in range(2)]
    Z = [k.sb(f"fn_Z{i}", [128, 8, 512], F32) for i in range(2)]
    Mb = k.sb("fn_Mb", [128, 8, 512], BF16)
    X = [k.sb(f"fn_X{i}", [128, 8, 512], F32) for i in range(2)]
    sq = k.sb("fn_sq", [128, 8, 512], BF16)
    rs = k.sb("fn_rs", [128, 512], F32)
    O = [k.sb(f"fn_O{i}", [128, 8, 512], F32) for i in range(2)]
    r3 = lambda D: D[:].rearrange("(c p) t -> p c t", p=128)
    for tt in range(TO // 512):
        sl = slice(tt * 512, (tt + 1) * 512)
        Mt, Zt, Xt, Ot = M[tt % 2], Z[tt % 2], X[tt % 2], O[tt % 2]
        k.dma("sp", Mt[:, 0:4, :], r3(CT)[:, :, sl], [CT], [Mt], Mt, grouped=True)
        k.dma("sp", Mt[:, 4:8, :], r3(DT)[:, :, sl], [DT], [Mt], Mt, grouped=True)
        k.dma("sp", Zt[:], r3(ZT)[:, :, sl], [ZT], [Zt], Zt)
        k.dma("sp", Xt[:], r3(X1)[:, :, sl], [X1], [Xt], Xt)
        k.act(Zt[:], Zt[:], AF.Silu, [Zt], [Zt])
        k.tt(Mb[:], Mt[:], Zt[:], ALU.mult, [Mt, Zt], [Mb])
        for fc in range(8):
            pb = cx.psum[fc % 4]
            for kc in range(8):
                k.mm(pb[:, :], Wb[:, kc, fc * 128:(fc + 1) * 128], Mb[:, kc, :], [Wb, Mb], [pb], start=(kc == 0), stop=(kc == 7))
            k.stt(Xt[:, fc, :], pb[:, :], gate1[:, fc:fc + 1], Xt[:, fc, :], ALU.mult, ALU.add, [pb, gate1, Xt], [Xt])
        k.act(sq[:], Xt[:], AF.Square, [Xt], [sq])
        pss = cx.psum[7]
        for kc in range(8):
            k.mm(pss[:, :], cx.ones_bf[:], sq[:, kc, :], [cx.ones_bf, sq], [pss], start=(kc == 0), stop=(kc == 7))
        k.act(rs[:], pss[:, :], AF.Sqrt, [pss], [rs], scale=1.0 / 1024.0, bias=float(EPS))
        k.op("dve", lambda e: e.reciprocal(out=rs[:], in_=rs[:]), [rs], [rs])
        for kc in range(8):
            k.stt(Ot[:, kc, :], Xt[:, kc, :], fg[:, kc:kc + 1], rs[:], ALU.mult, ALU.mult, [Xt, fg, rs], [Ot])
        k.dma("sp", r3(outT)[:, :, sl], Ot[:], [Ot], [outT], Ot)


def build_odd(TO, debug=False, phases=("conv", "att", "fin")):
    nc = bass.Bass("TRN2", target_bir_lowering=False)
    k = KB(nc)
    cx = Ctx()
    setup_common(k, cx)
    kd = "ExternalOutput" if debug else "Internal"
    emit_odd(k, cx, TO, kd, phases)
    k.finish()
    k.close()
    print("odd program: instructions", k.ninst, "sems", k.nsem)
    return nc


def emit_odd(k, cx, TO, kd, phases=("conv", "att", "fin"), GM=None):
    E = TO + 2 * HALO
    c2d = k.dram("consts2", [128, C2_COLS], F32, kind="ExternalInput")
    c2 = k.sb("c2", [128, C2_COLS], F32)
    k.dma("sp", c2[:], c2d[:], [c2d], [c2], c2)
    x0T = k.dram("x0T", [1024, E], F32, kind="ExternalInput")
    mixT = k.dram("mixT", [1024, E], F32, kind="ExternalInput") if GM is None else None
    selw_d = k.dram("selw", [128, 2], F32, kind="ExternalInput") if GM is not None else None
    valid = k.dram("valid", [1, E], F32, kind="ExternalInput")
    cT = k.dram("cT1", [128, 8], F32, kind="ExternalInput")
    adaw0 = k.dram("adaw0", [1024, 1024], F32, kind="ExternalInput")
    adab0 = k.dram("adab0", [128, 8], F32, kind="ExternalInput")
    adaw1 = k.dram("adaw1", [1024, 3072], F32, kind="ExternalInput")
    adab1 = k.dram("adab1", [128, 24], F32, kind="ExternalInput")
    ng = k.dram("ng1", [128, 8], F32, kind="ExternalInput")
    Wo0 = k.dram("Wo0", [1024, 1024], F32, kind="ExternalInput")
    W1 = k.dram("W1", [1024, 3584], F32, kind="ExternalInput")
    Wo1 = k.dram("Wo1", [1024, 1024], F32, kind="ExternalInput")
    cw = k.dram("cw", [128, 4, 31], F32, kind="ExternalInput")
    cpar = k.dram("cpar", [128, 4, 3], F32, kind="ExternalInput")
    relb = k.dram("relb", [32, 8], F32, kind="ExternalInput")
    relbT = k.dram("relbT", [8, 32], F32, kind="ExternalInput")
    vv = k.dram("vv", [128, VV_COLS if TO == 4096 else sum(d * ((TO // d) // 128 + 1) for d in DILS)], F32, kind="ExternalInput")
    fgd = k.dram("fg", [128, 8], F32, kind="ExternalInput")
    outT = k.dram("outT", [1024, TO], F32, kind="ExternalOutput")
    X1 = k.dram("X1", [1024, TO], F32, kind=kd)
    GA = k.dram("GA", [512, E], F32, kind=kd)
    GB = k.dram("GB", [512, E], F32, kind=kd)
    QT = k.dram("QT", [512, TO], F32, kind=kd)
    KT = k.dram("KT", [512, E], F32, kind=kd)
    ZT = k.dram("ZT", [1024, TO], F32, kind=kd)
    VT = k.dram("VT", [E, 512], F32, kind=kd)
    CT = k.dram("CT", [512, TO], F32, kind=kd)
    DT = k.dram("DT", [512, TO], F32, kind=kd)
    GF = k.dram("GF", [8, 3 * 384], F32, kind=kd)

    A = k.sb("odA", [128, 8], F32)
    Bm = k.sb("odBm", [128, 8], F32)
    gate0 = k.sb("gate0", [128, 8], F32)
    gate1 = k.sb("gate1", [128, 8], F32)
    with k.scope():
        g0 = phase_mod(k, cx, cT, adaw0, adab0, 8, tag="m0")
        k.cp(gate0[:], g0[:], [g0], [gate0])
    with k.scope():
        mod1 = phase_mod(k, cx, cT, adaw1, adab1, 24, tag="m1")
        ngs = k.sb("odngs", [128, 8], F32)
        k.dma("sp", ngs[:], ng[:], [ng], [ngs], ngs)
        k.stt(A[:], mod1[:, 8:16], 1.0, ngs[:], ALU.add, ALU.mult, [mod1, ngs], [A])
        k.cp(Bm[:], mod1[:, 0:8], [mod1], [Bm])
        k.cp(gate1[:], mod1[:, 16:24], [mod1], [gate1])
    with k.scope():
        Wo0b = k.sb("pa_Wo0b", [128, 8, 1024], BF16)
        load_w_bf16(k, Wo0b, Wo0, 1024, "pa0")
        Mt = k.sb("pa_Mt", [128, 8, 512], F32)
        Mb = k.sb("pa_Mb", [128, 8, 512], BF16)
        x03 = x0T[:].rearrange("(c p) t -> p c t", p=128)
        if GM is None:
            m3 = mixT[:].rearrange("(c p) t -> p c t", p=128)
        else:
            g3 = [g[:].rearrange("(c p) t -> p c t", p=128) for g in GM]
            selw = k.sb("pa_selw", [128, 2], F32)
            k.dma("sp", selw[:], selw_d[:], [selw_d], [selw], selw)
            Mt2 = k.sb("pa_Mt2", [128, 8, 512], F32)
        x13 = X1[:].rearrange("(c p) t -> p c t", p=128)
        own_lo, own_hi = HALO // 512, (HALO + TO) // 512

        def producer(te, Xt):
            sl = slice(te * 512, (te + 1) * 512)
            k.dma("sp", Xt[:], x03[:, :, sl], [x0T], [Xt], Xt)
            if GM is None:
                k.dma("sp", Mt[:], m3[:, :, sl], [mixT], [Mt], Mt)
                k.cp(Mb[:, 0:4, :], Mt[:, 0:4, :], [Mt], [Mb], eng="act")
                k.cp(Mb[:, 4:8, :], Mt[:, 4:8, :], [Mt], [Mb], eng="dve")
            else:
                g0 = te * 512 - HALO
                g1 = te * 512 - HALO + TO
                ok0 = g0 >= 0
                ok1 = g1 + 512 <= 2 * TO
                if ok0:
                    k.dma("sp", Mt[:], g3[g0 // 1024][:, :, g0 % 1024:g0 % 1024 + 512], [GM[g0 // 1024]], [Mt], Mt)
                if ok1:
                    k.dma("sp", Mt2[:], g3[g1 // 1024][:, :, g1 % 1024:g1 % 1024 + 512], [GM[g1 // 1024]], [Mt2], Mt2)
                if ok0 and ok1:
                    k.ts(Mt[:], Mt[:], selw[:, 0:1], ALU.mult, [Mt, selw], [Mt])
                    k.stt(Mb[:], Mt2[:], selw[:, 1:2], Mt[:], ALU.mult, ALU.add, [Mt2, selw, Mt], [Mb])
                elif ok0:
                    k.ts(Mb[:], Mt[:], selw[:, 0:1], ALU.mult, [Mt, selw], [Mb])
                else:
                    k.ts(Mb[:], Mt2[:], selw[:, 1:2], ALU.mult, [Mt2, selw], [Mb])
            for fc in range(8):
                pb = cx.psum[6 + fc % 2] if False else cx.psum[6]
                for kc in range(8):
                    k.mm(pb[:, :], Wo0b[:, kc, fc * 128:(fc + 1) * 128], Mb[:, kc, :], [Wo0b, Mb], [pb], start=(kc == 0), stop=(kc == 7))
                k.stt(Xt[:, fc, :], pb[:, :], gate0[:, fc:fc + 1], Xt[:, fc, :], ALU.mult, ALU.add, [pb, gate0, Xt], [Xt])
            if own_lo <= te < own_hi:
                k.dma("sp", x13[:, :, (te - own_lo) * 512:(te - own_lo + 1) * 512], Xt[:], [Xt], [X1], Xt)

        def rng_fn(D, r0, nr, lo, hi, off):
            def f(te):
                if not (lo <= te < hi):
                    return None
                return D[r0:r0 + nr, (te - off) * 512:(te - off + 1) * 512]
            return f
        nE = E // 512
        fm_specs = []
        for ch in range(4):
            fm_specs.append((0 + 128 * ch, 128, GA, rng_fn(GA, 128 * ch, 128, own_lo - 1, own_hi + 1, 0), 1.0))
            fm_specs.append((512 + 128 * ch, 128, GB, rng_fn(GB, 128 * ch, 128, own_lo - 1, own_hi + 1, 0), 1.0))
            fm_specs.append((1024 + 128 * ch, 128, QT, rng_fn(QT, 128 * ch, 128, own_lo, own_hi, own_lo), 1.0))
            fm_specs.append((1536 + 128 * ch, 128, KT, rng_fn(KT, 128 * ch, 128, 0, nE, 0), 1.0))
        for ch in range(8):
            fm_specs.append((2560 + 128 * ch, 128, ZT, rng_fn(ZT, 128 * ch, 128, own_lo, own_hi, own_lo), 1.0))
        tm_specs = [(2048, 512, VT, (lambda te, ts: VT[te * 512 + ts * 128:te * 512 + (ts + 1) * 128, :]))]
        phase_norm_proj(k, cx, E, None, None, A, Bm, W1, 3584, fm_specs, tm_specs, tag="pa", producer=producer)
    if "conv" in phases:
        with k.scope():
            phase_conv_module(k, cx, TO, GA, GB, valid, cw, cpar, CT)
    if "att" in phases:
        with k.scope():
            phase_attention(k, cx, TO, QT, KT, VT, vv, relb, relbT, GF, DT, c2)
    if "fin" in phases:
        with k.scope():
            phase_final(k, cx, TO, CT, DT, ZT, X1, Wo1, gate1, fgd, outT)


def build_fused(T=SEQ):
    TO = T // 2
    nc = bass.Bass("TRN2", target_bir_lowering=False)
    k = KB(nc)
    cx = Ctx()
    setup_common(k, cx)
    bounce = [k.dram(f"mixT_own{i}", [512, 1024], F32) for i in range(T // 1024)]
    GM = [k.dram(f"mixT_all{i}", [1024, 1024], F32) for i in range(T // 1024)]
    emit_even(k, cx, T, "Internal", MIXT=bounce)
    for i in range(T // 1024):
        k.allgather(bounce[i], GM[i], [[0, 1], [2, 3], [4, 5], [6, 7]])
    emit_odd(k, cx, TO, "Internal", GM=GM)
    k.finish()
    k.close()
    print("fused program: instructions", k.ninst, "sems", k.nsem)
    return nc


def fused_core_inputs(inp, b, e, T=SEQ):
    TO = T // 2
    d = even_core_inputs(inp, b, e, T)
    o = odd_core_inputs_nomix(inp, b, e, TO)
    d.update(o)
    perm = np.concatenate([np.concatenate([np.arange(256 * ee, 256 * ee + 256), 512 + np.arange(256 * ee, 256 * ee + 256)]) for ee in range(2)])
    d["Wo0"] = np.ascontiguousarray(inp["ev_w_out"][0][perm, :])
    sel = np.zeros((128, 2), np.float32)
    sel[:, e] = 1.0
    d["selw"] = sel
    return d


def run_fused(inp, T=SEQ):
    TO = T // 2
    nc = build_fused(T)
    in_maps = [fused_core_inputs(inp, c // 2, c % 2, T) for c in range(8)]
    res = run_bass_kernel_spmd(nc, in_maps, core_ids=list(range(8)))
    out = np.zeros((4, T, 1024), np.float32)
    for c in range(8):
        b, sh = c // 2, c % 2
        out[b, sh * TO:(sh + 1) * TO, :] = res.results[c]["outT"].T
    return out


def odd_core_inputs_nomix(inp, b, sh, TO=4096, nhalves=2):
    d = odd_core_inputs(inp, None, b, sh, TO, nhalves)
    del d["mixT"]
    return d


def odd_core_inputs(inp, mix, b, sh, TO=4096, nhalves=2):
    S = TO * nhalves
    E = TO + 2 * HALO
    lo = sh * TO - HALO
    x0T = np.zeros((1024, E), np.float32)
    mT = np.zeros((1024, E), np.float32)
    valid = np.zeros((1, E), np.float32)
    a, bnd = max(lo, 0), min(lo + E, S)
    x0T[:, a - lo:bnd - lo] = inp["x"][b, a:bnd].T
    if mix is not None:
        mT[:, a - lo:bnd - lo] = mix[b, a:bnd].T
    valid[0, a - lo:bnd - lo] = 1.0
    cpar = np.stack([inp["od_dw_b"][0], inp["od_ln_g"][0], inp["od_ln_b"][0]], -1)
    return {
        "consts": make_consts(), "consts2": make_consts2(),
        "x0T": x0T, "mixT": mT, "valid": valid,
        "cT1": colform(inp["c"][b], 8),
        "adaw0": np.ascontiguousarray(inp["ada_w"][0][:, 2048:3072]),
        "adab0": colform(inp["ada_b"][0][2048:3072], 8),
        "adaw1": np.ascontiguousarray(inp["ada_w"][1]),
        "adab1": colform(inp["ada_b"][1], 24),
        "ng1": colform(inp["norm_g"][1], 8),
        "Wo0": np.ascontiguousarray(inp["ev_w_out"][0]),
        "W1": np.ascontiguousarray(inp["od_w_in"][0]),
        "Wo1": np.ascontiguousarray(inp["od_w_out"][0]),
        "cw": np.ascontiguousarray(inp["od_dw_w"][0].reshape(31, 4, 128).transpose(2, 1, 0)),
        "cpar": np.ascontiguousarray(cpar.reshape(4, 128, 3).transpose(1, 0, 2)),
        "relb": np.ascontiguousarray(inp["rel_bias"]),
        "relbT": np.ascontiguousarray(inp["rel_bias"].T),
        "vv": make_vv(TO, sh, nhalves),
        "fg": colform(inp["final_g"], 8),
    }


def run_odd(inp, mix, TO=4096):
    nc = build_odd(TO)
    in_maps = [odd_core_inputs(inp, mix, c // 2, c % 2, TO, 2) for c in range(8)]
    res = run_bass_kernel_spmd(nc, in_maps, core_ids=list(range(8)))
    out = np.zeros((4, 2 * TO, 1024), np.float32)
    for c in range(8):
        b, sh = c // 2, c % 2
        out[b, sh * TO:(sh + 1) * TO, :] = res.results[c]["outT"].T
    return out


def kernel(**inputs):
    inp = {k: np.asarray(v) for k, v in inputs.items()}
    return run_fused(inp)
```

```python
import contextlib
import numpy as np
import concourse.bass as bass
import concourse.mybir as mybir
from concourse.bass_utils import run_bass_kernel_spmd

F32 = mybir.dt.float32
BF16 = mybir.dt.bfloat16
ALU = mybir.AluOpType
AF = mybir.ActivationFunctionType
AX = mybir.AxisListType

SEM_LIMIT = 8000
FP32R = False
EPS = 1e-6
NEG = -1e30


class Buf:
    __slots__ = ("t", "name", "writer", "readers", "dsem", "dtot", "depth")

    def __init__(self, t, name):
        self.t = t
        self.name = name
        self.writer = None
        self.readers = []
        self.dsem = None
        self.dtot = 0

    def __getitem__(self, idx):
        return self.t[idx]


class KB:
    def __init__(self, nc, same_engine_sync=True):
        self.nc = nc
        self.es = contextlib.ExitStack()
        self.root_es = self.es
        self.depth = 0
        self.engs = {"pe": nc.tensor, "dve": nc.vector, "act": nc.scalar, "pool": nc.gpsimd, "sp": nc.sync}
        self.csem = {}
        self.ccnt = {}
        self.seen = {k: {} for k in self.engs}
        self.same = same_engine_sync
        self.pool_used = False
        self.free_dsems = []
        self.nsem = 0
        self.ninst = 0
        self.bufs = []

    def sem(self, name, root=False):
        self.nsem += 1
        es = self.root_es if root else self.es
        return es.enter_context(self.nc.semaphore(f"{name}_{self.nsem}"))

    def sb(self, name, shape, dt=F32):
        t = self.es.enter_context(self.nc.sbuf_tensor("s_" + name, list(shape), dt))
        b = Buf(t, name)
        b.depth = self.depth
        self.bufs.append(b)
        return b

    def ps(self, name, shape, dt=F32):
        t = self.es.enter_context(self.nc.psum_tensor("p_" + name, list(shape), dt))
        b = Buf(t, name)
        b.depth = self.depth
        self.bufs.append(b)
        return b

    def dram(self, name, shape, dt=F32, kind="Internal"):
        t = self.nc.dram_tensor(name, list(shape), dt, kind=kind)
        b = Buf(t.ap(), name)
        b.depth = 0
        self.bufs.append(b)
        return b

    def _wait(self, ek, dep):
        sem, val, dek = dep
        if dek == ek and not self.same:
            return
        sid = id(sem)
        if self.seen[ek].get(sid, 0) >= val:
            return
        self.engs[ek].wait_ge(sem, val)
        self.seen[ek][sid] = val

    def _deps(self, ek, reads, writes, skip_same_w=False):
        for b in reads:
            if b.writer is not None:
                self._wait(ek, b.writer)
        for b in writes:
            if b.writer is not None:
                if not (skip_same_w and b.writer[2] == ek):
                    self._wait(ek, b.writer)
            for r in b.readers:
                self._wait(ek, r)

    def _tick(self, ek):
        if ek not in self.csem or self.ccnt[ek] >= SEM_LIMIT:
            self.csem[ek] = self.sem("c" + ek, root=True)
            self.ccnt[ek] = 0
        self.ccnt[ek] += 1
        return (self.csem[ek], self.ccnt[ek], ek)

    def _mark(self, tok, reads, writes):
        for b in reads:
            b.readers.append(tok)
            if len(b.readers) > 16:
                last = {}
                for r in b.readers:
                    k = id(r[0])
                    if k not in last or last[k][1] < r[1]:
                        last[k] = r
                b.readers = list(last.values())
        for b in writes:
            b.writer = tok
            b.readers = []

    def op(self, ek, fn, reads, writes, acc=False):
        if ek == "pool":
            self.pool_used = True
        self._deps(ek, reads, writes, skip_same_w=acc)
        tok = self._tick(ek)
        ins = fn(self.engs[ek])
        ins.then_inc(tok[0], 1)
        self._mark(tok, reads, writes)
        self.ninst += 1
        return ins

    def dma(self, qk, out_ap, in_ap, reads, writes, sb, grouped=False, **kw):
        if sb.dsem is None:
            sb.dsem, sb.dtot = self._alloc_dsem()
        for b in reads:
            if b.writer is not None:
                self._wait(qk, b.writer)
        for b in writes:
            if b.writer is not None:
                if not (grouped and b.writer[0] is sb.dsem):
                    self._wait(qk, b.writer)
            for r in b.readers:
                self._wait(qk, r)
        if not grouped and sb.dtot > 0:
            self._wait(qk, (sb.dsem, sb.dtot, "dma"))
        if sb.dtot + 16 > SEM_LIMIT:
            self._wait(qk, (sb.dsem, sb.dtot, "dma"))
            sb.dsem, sb.dtot = self._alloc_dsem()
        sb.dtot += 16
        tok = (sb.dsem, sb.dtot, "dma")
        ins = self.engs[qk].dma_start(out=out_ap, in_=in_ap, **kw)
        ins.then_inc(sb.dsem, 16)
        self._mark(tok, reads, writes)
        self.ninst += 1
        return ins


    def allgather(self, in_buf, out_buf, groups):
        self.pool_used = True
        self._deps("pool", [in_buf], [out_buf])
        sem = self.sem("cc", root=True)
        ins = self.nc.gpsimd.collective_compute("AllGather", ALU.bypass, replica_groups=groups,
                                                ins=[in_buf[:].opt()], outs=[out_buf[:].opt()])
        ins.then_inc(sem, 1)
        self._mark((sem, 1, "cc"), [in_buf], [out_buf])
        self.ninst += 1

    def barrier(self):
        toks = [(self.csem[e], self.ccnt[e], e) for e in self.csem]
        for b in self.bufs:
            if b.dsem is not None and b.dtot > 0:
                toks.append((b.dsem, b.dtot, "dma"))
        for b in self.bufs:
            for t in ([b.writer] if b.writer is not None else []) + list(b.readers):
                if t[2] == "cc":
                    toks.append(t)
        same = self.same
        self.same = True
        for e in list(self.engs.keys()):
            if e == "pool" and "pool" not in self.csem and not self.pool_used:
                continue
            for t in toks:
                self._wait(e, t)
        self.same = same
        for b in self.bufs:
            b.writer = None
            b.readers = []

    @contextlib.contextmanager
    def scope(self):
        old = self.es
        self.es = contextlib.ExitStack()
        self.depth += 1
        nb = len(self.bufs)
        try:
            yield
        finally:
            self.barrier()
            dead = self.bufs[nb:]
            for b in dead:
                if b.depth >= self.depth and b.dsem is not None:
                    if b.dtot + 16 * 64 < SEM_LIMIT:
                        self.free_dsems.append((b.dsem, b.dtot))
                    b.dsem = None
                    b.dtot = 0
            self.bufs = self.bufs[:nb] + [b for b in dead if b.depth < self.depth]
            self.es.close()
            self.es = old
            self.depth -= 1

    def ts(self, out, in0, s1, op0, R, W, s2=None, op1=None, eng="dve"):
        if op1 is None:
            return self.op(eng, lambda e: e.tensor_scalar(out=out, in0=in0, scalar1=s1, scalar2=None, op0=op0), R, W)
        return self.op(eng, lambda e: e.tensor_scalar(out=out, in0=in0, scalar1=s1, scalar2=s2, op0=op0, op1=op1), R, W)

    def tt(self, out, a, b, op, R, W, eng="dve"):
        return self.op(eng, lambda e: e.tensor_tensor(out=out, in0=a, in1=b, op=op), R, W)

    def stt(self, out, in0, scalar, in1, op0, op1, R, W):
        return self.op("dve", lambda e: e.scalar_tensor_tensor(out=out, in0=in0, scalar=scalar, in1=in1, op0=op0, op1=op1), R, W)

    def act(self, out, in_, func, R, W, scale=None, bias=None):
        kw = {}
        if scale is not None:
            kw["scale"] = scale
        if bias is not None:
            kw["bias"] = bias
        return self.op("act", lambda e: e.activation(out=out, in_=in_, func=func, **kw), R, W)

    def mm(self, out, lhsT, rhs, R, W, start=True, stop=True):
        if FP32R and lhsT.dtype == F32 and rhs.dtype == F32:
            lhsT = lhsT.bitcast(mybir.dt.float32r)
            rhs = rhs.bitcast(mybir.dt.float32r)
        return self.op("pe", lambda e: e.matmul(out, lhsT=lhsT, rhs=rhs, start=start, stop=stop), R, W, acc=True)

    def cp(self, out, in_, R, W, eng="dve"):
        if eng == "act":
            return self.op("act", lambda e: e.activation(out=out, in_=in_, func=AF.Copy), R, W)
        return self.op(eng, lambda e: e.tensor_copy(out=out, in_=in_), R, W)

    def scan(self, out, d0, d1, init, op0, op1, R, W):
        return self.op("dve", lambda e: e.tensor_tensor_scan(out=out, data0=d0, data1=d1, initial=init, op0=op0, op1=op1), R, W)

    def _alloc_dsem(self):
        if self.free_dsems:
            self.free_dsems.sort(key=lambda t: t[1])
            return self.free_dsems.pop(0)
        return (self.sem("dq", root=True), 0)

    def finish(self, ek="sp"):
        for b in self.bufs:
            if b.dsem is not None and b.dtot > 0:
                self._wait(ek, (b.dsem, b.dtot, "dma"))

    def close(self):
        self.es.close()


class Ctx:
    pass


DBG = {}


def dbg(k, name, buf, shape):
    if not DBG.get("on"):
        return
    d = k.dram("dbg_" + name, list(shape), F32, kind="ExternalOutput")
    k.dma("sp", d[:], buf[:], [buf], [d], buf)


def dbgap(k, name, buf, ap, shape):
    if not DBG.get("on"):
        return
    d = k.dram("dbg_" + name, list(shape), F32, kind="ExternalOutput")
    k.dma("sp", d[:], ap, [buf], [d], buf)


def fv(buf, p0, npart, off, dims):
    a = buf.t[:]
    pstep = a.ap[0][0]
    return bass.AP(a.tensor, a.offset + p0 * pstep + off, [[pstep, npart]] + [[st, ct] for (st, ct) in dims])


def setup_common(k, cx):
    cx.psum = [k.ps(f"psb{i}", [128, 512], F32) for i in range(8)]
    cx.consts_d = k.dram("consts", [128, CONST_COLS], F32, kind="ExternalInput")
    cx.consts = k.sb("consts_sb", [128, CONST_COLS], F32)
    k.dma("sp", cx.consts[:], cx.consts_d[:], [cx.consts_d], [cx.consts], cx.consts)
    cx.ones_bf = k.sb("ones_bf", [128, 128], BF16)
    k.op("dve", lambda e: e.memset(cx.ones_bf[:], 1.0), [], [cx.ones_bf])


C_IDENT = 0
C_ONES = 128
C_RST0 = 256
C_RSTN = 768
C_MASK = 1280
M_LE, M_GE, M_LT, M_GT = 0, 1, 2, 3
C_SEL = 3328
CONST_COLS = 3584


def make_consts():
    c = np.zeros((128, CONST_COLS), np.float32)
    c[:, C_IDENT:C_IDENT + 128] = np.eye(128, dtype=np.float32)
    c[:, C_ONES:C_ONES + 128] = 1.0
    col = np.arange(512)
    c[:, C_RST0:C_RST0 + 512] = np.where(col % 64 == 0, 0.0, 1.0)[None, :]
    c[:, C_RSTN:C_RSTN + 512] = np.where(col % 64 == 0, NEG, 0.0)[None, :]
    jj = np.arange(64)[:, None]
    ii = np.arange(64)[None, :]
    for m, ok in enumerate([jj <= ii, jj >= ii, jj < ii, jj > ii]):
        c[:64, C_MASK + m * 512:C_MASK + (m + 1) * 512] = np.tile(np.where(ok, 0.0, NEG), (1, 8))
    for hh in range(2):
        c[hh, C_SEL + hh * 128:C_SEL + (hh + 1) * 128] = 1.0
    return c


def phase_mod(k, cx, cT_d, adaw_d, adab_d, ncol_chunks, tag="m0"):
    cs = k.sb(tag + "cs", [128, 8], F32)
    k.dma("sp", cs[:], cT_d[:], [cT_d], [cs], cs)
    k.op("act", lambda e: e.activation(out=cs[:], in_=cs[:], func=AF.Silu), [cs], [cs])
    ncols = ncol_chunks * 128
    wbuf = [k.sb(tag + f"adaw{i}", [128, ncols], F32) for i in range(2)]
    pm = cx.psum[0]
    for kc in range(8):
        wb = wbuf[kc % 2]
        k.dma("sp", wb[:], adaw_d[kc * 128:(kc + 1) * 128, :], [adaw_d], [wb], wb)
        for cc in range(ncol_chunks):
            k.op("pe", lambda e, wb=wb, cc=cc, kc=kc: e.matmul(
                pm[:, cc * 8 + kc:cc * 8 + kc + 1], lhsT=wb[:, cc * 128:(cc + 1) * 128], rhs=cs[:, kc:kc + 1],
                start=True, stop=True), [wb, cs], [pm], acc=True)
    mod = k.sb(tag + "mod", [128, ncol_chunks], F32)
    k.op("dve", lambda e: e.tensor_reduce(
        out=mod[:], in_=pm[:, 0:ncol_chunks * 8].rearrange("p (c k) -> p c k", k=8), axis=AX.X, op=ALU.add),
        [pm], [mod])
    ab = k.sb(tag + "adab", [128, ncol_chunks], F32)
    k.dma("sp", ab[:], adab_d[:], [adab_d], [ab], ab)
    k.op("dve", lambda e: e.tensor_tensor(out=mod[:], in0=mod[:], in1=ab[:], op=ALU.add), [mod, ab], [mod])
    return mod


def load_w_bf16(k, Wb, W_d, ncols, tag):
    with k.scope():
        wst = [k.sb(tag + f"wst{i}", [128, 1024], F32) for i in range(2)]
        n = 0
        for kc in range(8):
            for c0 in range(0, ncols, 1024):
                w = min(1024, ncols - c0)
                ws = wst[n % 2]
                k.dma("sp", ws[:, 0:w], W_d[kc * 128:(kc + 1) * 128, c0:c0 + w], [W_d], [ws], ws)
                k.cp(Wb[:, kc, c0:c0 + w], ws[:, 0:w], [ws], [Wb], eng=("act" if n % 2 else "dve"))
                n += 1


def phase_norm_proj(k, cx, T, xT_ap_fn, xT_bufs, A, B, W_d, ncols, fm_specs, tm_specs, tag="p1", producer=None):
    Wb = k.sb(tag + "Wb", [128, 8, ncols], BF16)
    load_w_bf16(k, Wb, W_d, ncols, tag)
    X = [k.sb(tag + f"X{i}", [128, 8, 512], F32) for i in range(2)]
    sq = k.sb(tag + "sq", [128, 8, 512], BF16)
    rstd = k.sb(tag + "rstd", [128, 512], F32)
    tmp = [k.sb(tag + f"tmp{i}", [128, 512], F32) for i in range(2)]
    hT = k.sb(tag + "hT", [128, 8, 512], BF16)
    stg = [k.sb(tag + f"stg{i}", [128, 512], F32) for i in range(4)]
    nst = 0
    npb = 0
    ntiles = T // 512
    for tt in range(ntiles):
        Xt = X[tt % 2]
        if producer is None:
            k.dma("sp", Xt[:], xT_ap_fn(tt), xT_bufs, [Xt], Xt)
        else:
            producer(tt, Xt)
        k.op("act", lambda e, Xt=Xt: e.activation(out=sq[:], in_=Xt[:], func=AF.Square), [Xt], [sq])
        pss = cx.psum[7]
        for kc in range(8):
            k.op("pe", lambda e, kc=kc: e.matmul(pss[:], lhsT=cx.ones_bf[:], rhs=sq[:, kc, :],
                                                 start=(kc == 0), stop=(kc == 7)), [cx.ones_bf, sq], [pss], acc=True)
        k.op("act", lambda e: e.activation(out=rstd[:], in_=pss[:], func=AF.Sqrt, scale=1.0 / 1024.0, bias=float(EPS)),
             [pss], [rstd])
        k.op("dve", lambda e: e.reciprocal(out=rstd[:], in_=rstd[:]), [rstd], [rstd])
        for kc in range(8):
            tb = tmp[kc % 2]
            k.op("dve", lambda e, kc=kc, tb=tb, Xt=Xt: e.tensor_tensor(out=tb[:], in0=Xt[:, kc, :], in1=rstd[:], op=ALU.mult),
                 [Xt, rstd], [tb])
            k.op("act", lambda e, kc=kc, tb=tb: e.activation(out=hT[:, kc, :], in_=tb[:], func=AF.Identity,
                                                             scale=A[:, kc:kc + 1], bias=B[:, kc:kc + 1]),
                 [tb, A, B], [hT])
        for (c0, nr, dbuf, dfn, scale) in fm_specs:
            if dfn(tt) is None:
                continue
            pb = cx.psum[npb % 6]
            npb += 1
            for kc in range(8):
                k.op("pe", lambda e, kc=kc, pb=pb, c0=c0, nr=nr: e.matmul(
                    pb[0:nr, :], lhsT=Wb[:, kc, c0:c0 + nr], rhs=hT[:, kc, :], start=(kc == 0), stop=(kc == 7)),
                    [Wb, hT], [pb], acc=True)
            sg = stg[nst % 4]
            nst += 1
            if nst % 2:
                k.op("act", lambda e, pb=pb, sg=sg, nr=nr, scale=scale: e.activation(
                    out=sg[0:nr, :], in_=pb[0:nr, :], func=AF.Copy, scale=float(scale)), [pb], [sg])
            else:
                k.op("dve", lambda e, pb=pb, sg=sg, nr=nr, scale=scale: e.tensor_scalar(
                    out=sg[0:nr, :], in0=pb[0:nr, :], scalar1=float(scale), scalar2=None, op0=ALU.mult), [pb], [sg])
            k.dma("sp", dfn(tt), sg[0:nr, :], [sg], [dbuf], sg)
        for ts in range(4):
            for (c0, ncl, dbuf, dfn) in tm_specs:
                if dfn(tt, ts) is None:
                    continue
                pb = cx.psum[npb % 6]
                npb += 1
                for kc in range(8):
                    k.op("pe", lambda e, kc=kc, pb=pb, c0=c0, ncl=ncl, ts=ts: e.matmul(
                        pb[:, 0:ncl], lhsT=hT[:, kc, ts * 128:(ts + 1) * 128], rhs=Wb[:, kc, c0:c0 + ncl],
                        start=(kc == 0), stop=(kc == 7)), [Wb, hT], [pb], acc=True)
                sg = stg[nst % 4]
                nst += 1
                if nst % 2:
                    k.op("act", lambda e, pb=pb, sg=sg, ncl=ncl: e.activation(
                        out=sg[:, 0:ncl], in_=pb[:, 0:ncl], func=AF.Copy), [pb], [sg])
                else:
                    k.op("dve", lambda e, pb=pb, sg=sg, ncl=ncl: e.tensor_copy(out=sg[:, 0:ncl], in_=pb[:, 0:ncl]), [pb], [sg])
                k.dma("sp", dfn(tt, ts), sg[:, 0:ncl], [sg], [dbuf], sg)


FM_MQ, FM_MK, FM_G, FM_DQ, FM_DK, FM_DV = 0, 128, 256, 272, 528, 784
FM_ROWS = 1040
TM0 = FM_ROWS
TM_MK, TM_MV, TM_MO, TM_Z = TM0, TM0 + 128, TM0 + 384, TM0 + 640
NC1 = TM0 + 1152


def row_softplus_neg(k, out, x, tmp1, tmp2, n, R):
    k.stt(tmp1[0:n, :], x[0:n, :], -1.0, x[0:n, :], ALU.mult, ALU.max, [x], [tmp1])
    k.act(tmp1[0:n, :], tmp1[0:n, :], AF.Exp, [tmp1], [tmp1], scale=-1.0)
    k.act(tmp1[0:n, :], tmp1[0:n, :], AF.Ln, [tmp1], [tmp1], bias=1.0)
    k.stt(out[0:n, :], x[0:n, :], 0.0, tmp1[0:n, :], ALU.min, ALU.subtract, [x, tmp1], [out])


def dirview(buf, n, d, G=512):
    if d == 0:
        return fv(buf, 0, n, 0, [(1, G)])
    return fv(buf, 0, n, G - 1, [(-1, G)])


def phase_mlstm(k, cx, T, FM, TM, gb_d, HS):
    NG = T // 512
    cs = cx.consts
    ident = cs
    st = []
    for d in range(2):
        s = Ctx()
        s.gi = k.sb(f"ml_gi{d}", [2, 512], F32)
        s.gf = k.sb(f"ml_gf{d}", [2, 512], F32)
        s.t1 = k.sb(f"ml_t1{d}", [2, 512], F32)
        s.t2 = k.sb(f"ml_t2{d}", [2, 512], F32)
        s.b = k.sb(f"ml_b{d}", [2, 512], F32)
        s.g = k.sb(f"ml_g{d}", [2, 512], F32)
        s.pm = k.sb(f"ml_pm{d}", [2, 512], F32)
        s.negmu = k.sb(f"ml_negmu{d}", [2, 512], F32)
        s.wint = k.sb(f"ml_wint{d}", [2, 512], F32)
        s.emt = k.sb(f"ml_emt{d}", [2, 512], F32)
        s.kwf = k.sb(f"ml_kwf{d}", [2, 512], F32)
        s.mnext = k.sb(f"ml_mnext{d}", [2, 8], F32)
        s.mcur = k.sb(f"ml_mcur{d}", [2, 8], F32)
        s.c8 = k.sb(f"ml_c8{d}", [2, 8], F32)
        s.wold = k.sb(f"ml_wold{d}", [2, 8], F32)
        s.carry = k.sb(f"ml_carry{d}", [2, 1], F32)
        s.bi = k.sb(f"ml_bi{d}", [2, 1], F32)
        s.bf = k.sb(f"ml_bf{d}", [2, 1], F32)
        k.dma("sp", s.bi[:], gb_d[2 * d:2 * d + 2, :], [gb_d], [s.bi], s.bi)
        k.dma("sp", s.bf[:], gb_d[4 + 2 * d:4 + 2 * d + 2, :], [gb_d], [s.bf], s.bf)
        k.op("dve", lambda e, s=s: e.memset(s.carry[:], 0.0), [], [s.carry])
        s.cols = k.sb(f"ml_cols{d}", [64, 64], F32)
        s.woldc = k.sb(f"ml_woldc{d}", [64, 16], F32)
        s.QT = [k.sb(f"ml_QT{d}{h}", [64, 512], F32) for h in range(2)]
        s.KT = [k.sb(f"ml_KT{d}{h}", [64, 512], F32) for h in range(2)]
        s.Ktm = k.sb(f"ml_Ktm{d}", [64, 8, 128], F32)
        s.Va = [k.sb(f"ml_Va{d}{h}", [64, 8, 129], F32) for h in range(2)]
        for h in range(2):
            k.op("dve", lambda e, s=s, h=h: e.memset(s.Va[h][:], 1.0), [], [s.Va[h]])
        s.E = [k.sb(f"ml_E{d}{h}", [64, 512], F32) for h in range(2)]
        s.AT = [k.sb(f"ml_AT{d}{h}", [64, 512], F32) for h in range(2)]
        s.C = [k.sb(f"ml_C{d}{h}", [64, 129], F32) for h in range(2)]
        for h in range(2):
            k.op("dve", lambda e, s=s, h=h: e.memset(s.C[h][:], 0.0), [], [s.C[h]])
        s.tmp = [k.sb(f"ml_tmp{d}{h}", [64, 129], F32) for h in range(2)]
        s.R = [k.sb(f"ml_R{d}{h}", [64, 129], F32) for h in range(2)]
        s.dn = [k.sb(f"ml_dn{d}{h}", [64, 2], F32) for h in range(2)]
        s.KW = [k.sb(f"ml_KW{d}{h}", [64, 64], F32) for h in range(2)]
        s.Hg = k.sb(f"ml_Hg{d}", [64, 8, 256], F32)
        st.append(s)
    TMr = TM[:].rearrange("(c p) f -> p c f", p=64)
    for step in range(NG):
        for d in range(2):
            s = st[d]
            grp = step if d == 0 else NG - 1 - step
            t0 = grp * 512
            c0 = grp * 8
            k.dma("sp", s.gi[:], FM[FM_G + 2 * d:FM_G + 2 * d + 2, t0:t0 + 512], [FM], [s.gi], s.gi)
            k.dma("sp", s.gf[:], FM[FM_G + 4 + 2 * d:FM_G + 4 + 2 * d + 2, t0:t0 + 512], [FM], [s.gf], s.gf)
            for h in range(2):
                k.dma("sp", s.QT[h][:], FM[FM_MQ + 64 * h:FM_MQ + 64 * h + 64, t0:t0 + 512], [FM], [s.QT[h]], s.QT[h])
                k.dma("sp", s.KT[h][:], FM[FM_MK + 64 * h:FM_MK + 64 * h + 64, t0:t0 + 512], [FM], [s.KT[h]], s.KT[h])
                k.dma("sp", s.Va[h][:, :, 0:128], TMr[:, c0:c0 + 8, 128 + 128 * h:256 + 128 * h], [TM], [s.Va[h]], s.Va[h])
            k.dma("sp", s.Ktm[:], TMr[:, c0:c0 + 8, 0:128], [TM], [s.Ktm], s.Ktm)
            k.ts(s.gi[:], s.gi[:], s.bi[:, 0:1], ALU.add, [s.gi, s.bi], [s.gi])
            k.ts(s.gf[:], s.gf[:], s.bf[:, 0:1], ALU.add, [s.gf, s.bf], [s.gf])
            row_softplus_neg(k, s.t2, s.gf, s.t1, None, 2, None)
            k.scan(dirview(s.b, 2, d), cs[0:2, C_RST0:C_RST0 + 512], dirview(s.t2, 2, d), 0.0, ALU.mult, ALU.add,
                   [s.t2, cs], [s.b])
            k.tt(s.g[:], s.gi[:], s.b[:], ALU.subtract, [s.gi, s.b], [s.g])
            k.scan(dirview(s.pm, 2, d), cs[0:2, C_RSTN:C_RSTN + 512], dirview(s.g, 2, d), 0.0, ALU.add, ALU.max,
                   [s.g, cs], [s.pm])
            last = 63 if d == 0 else 0
            bL = fv(s.b, 0, 2, last, [(64, 8)])
            pmL = fv(s.pm, 0, 2, last, [(64, 8)])
            if d == 0:
                o8 = lambda buf: fv(buf, 0, 2, 0, [(1, 8)])
            else:
                o8 = lambda buf: fv(buf, 0, 2, 7, [(-1, 8)])
            bLd = fv(s.b, 0, 2, last, [(64, 8)]) if d == 0 else fv(s.b, 0, 2, last + 64 * 7, [(-64, 8)])
            pmLd = fv(s.pm, 0, 2, last, [(64, 8)]) if d == 0 else fv(s.pm, 0, 2, last + 64 * 7, [(-64, 8)])
            k.scan(o8(s.mnext), pmLd, bLd, s.carry[:, 0:1], ALU.max, ALU.add, [s.pm, s.b, s.carry], [s.mnext])
            if d == 0:
                k.cp(s.mcur[:, 0:1], s.carry[:, 0:1], [s.carry], [s.mcur])
                k.cp(s.mcur[:, 1:8], s.mnext[:, 0:7], [s.mnext], [s.mcur])
                k.cp(s.carry[:, 0:1], s.mnext[:, 7:8], [s.mnext, s.mcur], [s.carry])
            else:
                k.cp(s.mcur[:, 7:8], s.carry[:, 0:1], [s.carry], [s.mcur])
                k.cp(s.mcur[:, 0:7], s.mnext[:, 1:8], [s.mnext], [s.mcur])
                k.cp(s.carry[:, 0:1], s.mnext[:, 0:1], [s.mnext, s.mcur], [s.carry])
            mc_b = fv(s.mcur, 0, 2, 0, [(1, 8), (0, 64)])
            v3 = lambda buf: fv(buf, 0, 2, 0, [(64, 8), (1, 64)])
            k.tt(v3(s.t1), v3(s.pm), mc_b, ALU.max, [s.pm, s.mcur], [s.t1])
            k.ts(s.negmu[:], s.t1[:], -1.0, ALU.mult, [s.t1], [s.negmu])
            k.tt(v3(s.wint), mc_b, v3(s.t1), ALU.subtract, [s.mcur, s.t1], [s.wint])
            k.act(s.wint[:], s.wint[:], AF.Exp, [s.wint], [s.wint])
            k.tt(s.emt[:], s.b[:], s.t1[:], ALU.add, [s.b, s.t1], [s.emt])
            k.act(s.emt[:], s.emt[:], AF.Exp, [s.emt], [s.emt], scale=-1.0)
            k.tt(s.c8[:], bL, s.mnext[:], ALU.subtract, [s.b, s.mnext], [s.c8])
            k.tt(v3(s.kwf), v3(s.g), fv(s.c8, 0, 2, 0, [(1, 8), (0, 64)]), ALU.add, [s.g, s.c8], [s.kwf])
            k.act(s.kwf[:], s.kwf[:], AF.Exp, [s.kwf], [s.kwf])
            k.tt(s.wold[:], s.c8[:], s.mcur[:], ALU.add, [s.c8, s.mcur], [s.wold])
            k.act(s.wold[:], s.wold[:], AF.Exp, [s.wold], [s.wold])
            pc = cx.psum[6]
            for c in range(8):
                for qi, qb in enumerate([s.g, s.wint, s.emt, s.kwf]):
                    col = (c * 4 + qi) * 2
                    k.mm(pc[0:64, col:col + 2], qb[0:2, c * 64:(c + 1) * 64], cs[0:2, C_IDENT:C_IDENT + 2], [qb, cs], [pc])
            for h in range(2):
                k.mm(pc[0:64, 64 + 8 * h:64 + 8 * h + 8], cs[0:2, C_SEL + 128 * h:C_SEL + 128 * h + 64], s.wold[0:2, :], [cs, s.wold], [pc])
            k.cp(s.cols[:], pc[0:64, 0:64], [pc], [s.cols])
            k.cp(s.woldc[:], pc[0:64, 64:80], [pc], [s.woldc], eng="act")
            mk = M_LE if d == 0 else M_GE
            for h in range(2):
                pD = cx.psum[h]
                sel = cs[0:2, C_SEL + 128 * h:C_SEL + 128 * h + 64]
                k.mm(pD[0:64, :], sel, s.negmu[0:2, :], [cs, s.negmu], [pD], start=True, stop=False)
                k.mm(pD[0:64, :], cs[0:64, C_IDENT:C_IDENT + 64], cs[0:64, C_MASK + mk * 512:C_MASK + (mk + 1) * 512], [cs], [pD],
                     start=False, stop=False)
                for c in range(8):
                    k.mm(pD[0:64, c * 64:(c + 1) * 64], s.g[0:2, c * 64:(c + 1) * 64], sel, [s.g, cs], [pD],
                         start=False, stop=(c == 7))
                k.act(s.E[h][:], pD[0:64, :], AF.Exp, [pD], [s.E[h]])
                pS = cx.psum[2 + h]
                for c in range(8):
                    k.mm(pS[0:64, c * 64:(c + 1) * 64], s.KT[h][:, c * 64:(c + 1) * 64], s.QT[h][:, c * 64:(c + 1) * 64],
                         [s.KT[h], s.QT[h]], [pS])
                k.tt(s.AT[h][:], pS[0:64, :], s.E[h][:], ALU.mult, [pS, s.E[h]], [s.AT[h]])
            if step == 0:
                for nm in ["b", "g", "pm", "negmu", "wint", "emt", "kwf"]:
                    dbg(k, f"{nm}{d}", getattr(s, nm), [2, 512])
                dbg(k, f"mnext{d}", s.mnext, [2, 8]); dbg(k, f"mcur{d}", s.mcur, [2, 8]); dbg(k, f"wold{d}", s.wold, [2, 8])
                dbg(k, f"cols{d}", s.cols, [64, 64]); dbg(k, f"woldc{d}", s.woldc, [64, 16])
                dbg(k, f"E{d}", s.E[0], [64, 512]); dbg(k, f"AT{d}", s.AT[0], [64, 512])
            for ci in range(8):
                c = ci if d == 0 else 7 - ci
                for h in range(2):
                    pH = cx.psum[4 + h]
                    cl = lambda qi: s.cols[:, (c * 4 + qi) * 2 + h:(c * 4 + qi) * 2 + h + 1]
                    k.mm(pH[0:64, 0:129], s.AT[h][:, c * 64:(c + 1) * 64], s.Va[h][:, c, :], [s.AT[h], s.Va[h]], [pH])
                    k.mm(pH[0:64, 256:385], s.QT[h][:, c * 64:(c + 1) * 64], s.C[h][:], [s.QT[h], s.C[h]], [pH])
                    k.ts(s.tmp[h][:], pH[0:64, 256:385], cl(1), ALU.mult, [pH, s.cols], [s.tmp[h]])
                    k.tt(s.R[h][:], s.tmp[h][:], pH[0:64, 0:129], ALU.add, [s.tmp[h], pH], [s.R[h]])
                    k.stt(s.dn[h][:, 0:1], s.R[h][:, 128:129], -1.0, s.R[h][:, 128:129], ALU.mult, ALU.max, [s.R[h]], [s.dn[h]])
                    k.ts(s.dn[h][:, 0:1], s.dn[h][:, 0:1], cl(2), ALU.max, [s.dn[h], s.cols], [s.dn[h]])
                    k.op("dve", lambda e, h=h: e.reciprocal(out=s.dn[h][:, 1:2], in_=s.dn[h][:, 0:1]), [s.dn[h]], [s.dn[h]])
                    k.ts(s.Hg[:, c, 128 * h:128 * h + 128], s.R[h][:, 0:128], s.dn[h][:, 1:2], ALU.mult, [s.R[h], s.dn[h]], [s.Hg])
                    k.ts(s.KW[h][:], s.Ktm[:, c, 64 * h:64 * h + 64], cl(3), ALU.mult, [s.Ktm, s.cols], [s.KW[h]], s2=0.125, op1=ALU.mult)
                    pU = cx.psum[6 + h] if False else cx.psum[7]
                    k.mm(pU[0:64, 129 * h:129 * h + 129], s.KW[h][:], s.Va[h][:, c, :], [s.KW[h], s.Va[h]], [pU])
                    k.stt(s.C[h][:], s.C[h][:], s.woldc[:, 8 * h + c:8 * h + c + 1], pU[0:64, 129 * h:129 * h + 129], ALU.mult, ALU.add,
                          [s.C[h], s.woldc, pU], [s.C[h]])
            k.dma("sp", HS[d][t0:t0 + 512, :].rearrange("(c p) f -> p c f", p=64), s.Hg[:], [s.Hg], [HS[d]], s.Hg)


def phase_gdn_pre(k, cx, T, FM, dcw_d, QN, KN, KTM, VTM):
    cs = cx.consts
    NT_ = T // 512
    dcw = k.sb("g_dcw", [128, 6, 5], F32)
    k.dma("sp", dcw[:], dcw_d[:], [dcw_d], [dcw], dcw)
    dg = k.sb("g_diag", [128, 30, 128], F32)
    for i in range(6):
        for j in range(5):
            k.ts(dg[:, i * 5 + j, :], cs[:, C_IDENT:C_IDENT + 128], dcw[:, i, j:j + 1], ALU.mult, [cs, dcw], [dg],
                 eng=("dve" if (i + j) % 2 else "pool"))
    Xs = [k.sb(f"g_X{i}", [128, 516], F32) for i in range(2)]
    Y = [k.sb(f"g_Y{i}", [128, 512], F32) for i in range(2)]
    sq = k.sb("g_sq", [128, 512], F32)
    rs = k.sb("g_rs", [128, 512], F32)
    Yn = [k.sb(f"g_Yn{i}", [128, 512], F32) for i in range(2)]
    tr = [k.sb(f"g_tr{i}", [128, 512], F32) for i in range(2)]
    n = 0
    for tt in range(NT_):
        t0 = tt * 512
        for i in range(6):
            X = Xs[n % 2]
            row0 = FM_DQ + 128 * i
            lo = max(t0 - 2, 0)
            hi = min(t0 + 514, T)
            if tt == 0:
                k.op("dve", lambda e, X=X: e.memset(X[:, 0:2], 0.0), [], [X])
            if tt == NT_ - 1:
                k.op("dve", lambda e, X=X: e.memset(X[:, 514:516], 0.0), [], [X])
            k.dma("sp", X[:, lo - (t0 - 2):hi - (t0 - 2)], FM[row0:row0 + 128, lo:hi], [FM], [X], X)
            pc_ = cx.psum[n % 2]
            for j in range(5):
                k.mm(pc_[:, :], dg[:, i * 5 + j, :], X[:, j:j + 512], [dg, X], [pc_], start=(j == 0), stop=(j == 4))
            Yt = Y[n % 2]
            k.act(Yt[:], pc_[:, :], AF.Silu, [pc_], [Yt])
            h = i % 2
            kind = i // 2
            if kind < 2:
                k.tt(sq[:], Yt[:], Yt[:], ALU.mult, [Yt], [sq])
                pss = cx.psum[2]
                k.mm(pss[:, :], cs[:, C_ONES:C_ONES + 128], sq[:], [cs, sq], [pss])
                k.act(rs[:], pss[:, :], AF.Sqrt, [pss], [rs], bias=float(EPS))
                k.op("dve", lambda e: e.reciprocal(out=rs[:], in_=rs[:]), [rs], [rs])
                Ynt = Yn[n % 2]
                k.stt(Ynt[:], Yt[:], (128.0 ** -0.5) if kind == 0 else 1.0, rs[:], ALU.mult, ALU.mult, [Yt, rs], [Ynt])
                dst = QN[h] if kind == 0 else KN[h]
                k.dma("sp", dst[:, t0:t0 + 512], Ynt[:], [Ynt], [dst], Ynt)
                src = Ynt
            else:
                src = Yt
            if kind >= 1:
                pt = cx.psum[3 + n % 2]
                for ts_ in range(4):
                    k.mm(pt[:, ts_ * 128:(ts_ + 1) * 128], src[:, ts_ * 128:(ts_ + 1) * 128], cs[:, C_IDENT:C_IDENT + 128], [src, cs], [pt])
                trt = tr[n % 2]
                k.cp(trt[:], pt[:, :], [pt], [trt], eng="act")
                dst = KTM[h] if kind == 1 else VTM[h]
                k.dma("sp", dst[t0:t0 + 512, :].rearrange("(s p) f -> p s f", p=128), trt[:].rearrange("p (s f) -> p s f", f=128),
                      [trt], [dst], trt)
            n += 1


def phase_gdn(k, cx, T, FM, QN, KN, KTM, VTM, gpar_d, OS):
    NG = T // 512
    cs = cx.consts
    ID64 = cs[0:64, C_IDENT:C_IDENT + 64]
    G1 = k.sb("gd_G1", [64, 512], F32)
    Nm = k.sb("gd_N", [64, 512], F32)
    NTm = k.sb("gd_NT", [64, 512], F32)
    P2 = [k.sb(f"gd_P2{i}", [64, 512], F32) for i in range(2)]
    PT2 = [k.sb(f"gd_PT2{i}", [64, 512], F32) for i in range(2)]
    XT = k.sb("gd_XT", [64, 512], F32)
    gamT = k.sb("gd_gamT", [64, 512], F32)
    st = []
    for d in range(2):
        s = Ctx()
        for nm in ["br", "ar", "t1", "t2", "beta", "gc", "ngc", "gcb", "bg", "kd", "eg"]:
            setattr(s, nm, k.sb(f"gd_{nm}{d}", [2, 512], F32))
        s.gl8 = k.sb(f"gd_gl8{d}", [2, 8], F32)
        s.egl = k.sb(f"gd_egl{d}", [2, 8], F32)
        s.par = k.sb(f"gd_par{d}", [2, 2], F32)
        k.dma("sp", s.par[:], gpar_d[2 * d:2 * d + 2, :], [gpar_d], [s.par], s.par)
        k.act(s.par[:, 1:2], s.par[:, 1:2], AF.Exp, [s.par], [s.par])
        k.ts(s.par[:, 1:2], s.par[:, 1:2], -1.0, ALU.mult, [s.par], [s.par])
        s.cols = k.sb(f"gd_cols{d}", [64, 48], F32)
        s.eglc = k.sb(f"gd_eglc{d}", [128, 16], F32)
        s.QN = [k.sb(f"gd_QN{d}{h}", [128, 512], F32) for h in range(2)]
        s.KN = [k.sb(f"gd_KN{d}{h}", [128, 512], F32) for h in range(2)]
        s.qg = [k.sb(f"gd_qg{d}{h}", [128, 512], F32) for h in range(2)]
        s.Ktm = [k.sb(f"gd_Ktm{d}{h}", [64, 8, 128], F32) for h in range(2)]
        s.Vtm = [k.sb(f"gd_Vtm{d}{h}", [64, 8, 128], F32) for h in range(2)]
        s.XTb = [k.sb(f"gd_XTb{d}{h}", [64, 512], F32) for h in range(2)]
        s.XTbg = [k.sb(f"gd_XTbg{d}{h}", [64, 512], F32) for h in range(2)]
        s.attnT = [k.sb(f"gd_attnT{d}{h}", [64, 512], F32) for h in range(2)]
        s.nwT = [k.sb(f"gd_nwT{d}{h}", [128, 512], F32) for h in range(2)]
        s.S = [k.sb(f"gd_S{d}{h}", [128, 128], F32) for h in range(2)]
        for h in range(2):
            k.op("dve", lambda e, s=s, h=h: e.memset(s.S[h][:], 0.0), [], [s.S[h]])
        s.vn = [k.sb(f"gd_vn{d}{h}", [64, 128], F32) for h in range(2)]
        s.kdm = [k.sb(f"gd_kdm{d}{h}", [64, 128], F32) for h in range(2)]
        s.Og = k.sb(f"gd_Og{d}", [64, 8, 256], F32)
        st.append(s)
    for step in range(NG):
        for d in range(2):
            s = st[d]
            grp = step if d == 0 else NG - 1 - step
            t0 = grp * 512
            c0 = grp * 8
            k.dma("sp", s.br[:], FM[FM_G + 8 + 2 * d:FM_G + 8 + 2 * d + 2, t0:t0 + 512], [FM], [s.br], s.br)
            k.dma("sp", s.ar[:], FM[FM_G + 12 + 2 * d:FM_G + 12 + 2 * d + 2, t0:t0 + 512], [FM], [s.ar], s.ar)
            for h in range(2):
                k.dma("sp", s.QN[h][:], QN[h][:, t0:t0 + 512], [QN[h]], [s.QN[h]], s.QN[h])
                k.dma("sp", s.KN[h][:], KN[h][:, t0:t0 + 512], [KN[h]], [s.KN[h]], s.KN[h])
                k.dma("sp", s.Ktm[h][:], KTM[h][t0:t0 + 512, :].rearrange("(c p) f -> p c f", p=64), [KTM[h]], [s.Ktm[h]], s.Ktm[h])
                k.dma("sp", s.Vtm[h][:], VTM[h][t0:t0 + 512, :].rearrange("(c p) f -> p c f", p=64), [VTM[h]], [s.Vtm[h]], s.Vtm[h])
            k.act(s.beta[:], s.br[:], AF.Sigmoid, [s.br], [s.beta])
            row_softplus_neg(k, s.t2, s.br, s.t1, None, 2, None)
            k.ts(s.ar[:], s.ar[:], s.par[:, 0:1], ALU.add, [s.ar, s.par], [s.ar], s2=-1.0, op1=ALU.mult)
            row_softplus_neg(k, s.eg, s.ar, s.t1, None, 2, None)
            k.ts(s.eg[:], s.eg[:], s.par[:, 1:2], ALU.mult, [s.eg, s.par], [s.eg], s2=-1.0, op1=ALU.mult)
            k.scan(dirview(s.gc, 2, d), cs[0:2, C_RST0:C_RST0 + 512], dirview(s.eg, 2, d), 0.0, ALU.mult, ALU.add, [s.eg, cs], [s.gc])
            k.ts(s.ngc[:], s.gc[:], -1.0, ALU.mult, [s.gc], [s.ngc])
            k.tt(s.gcb[:], s.gc[:], s.t2[:], ALU.add, [s.gc, s.t2], [s.gcb])
            last = 63 if d == 0 else 0
            gl = fv(s.gc, 0, 2, last, [(64, 8)])
            k.cp(s.gl8[:], gl, [s.gc], [s.gl8])
            k.act(s.egl[:], s.gl8[:], AF.Exp, [s.gl8], [s.egl])
            k.act(s.eg[:], s.gc[:], AF.Exp, [s.gc], [s.eg])
            k.tt(s.bg[:], s.beta[:], s.eg[:], ALU.mult, [s.beta, s.eg], [s.bg])
            v3 = lambda buf: fv(buf, 0, 2, 0, [(64, 8), (1, 64)])
            k.tt(v3(s.kd), fv(s.gl8, 0, 2, 0, [(1, 8), (0, 64)]), v3(s.gc), ALU.subtract, [s.gl8, s.gc], [s.kd])
            k.act(s.kd[:], s.kd[:], AF.Exp, [s.kd], [s.kd])
            pc = cx.psum[5]
            for c in range(8):
                for qi, qb in enumerate([s.beta, s.bg, s.kd]):
                    col = (c * 3 + qi) * 2
                    k.mm(pc[0:64, col:col + 2], qb[0:2, c * 64:(c + 1) * 64], cs[0:2, C_IDENT:C_IDENT + 2], [qb, cs], [pc])
            for h in range(2):
                k.mm(pc[:, 64 + 8 * h:64 + 8 * h + 8], cs[0:2, C_SEL + 128 * h:C_SEL + 128 * h + 128], s.egl[0:2, :], [cs, s.egl], [pc])
            k.cp(s.cols[:], pc[0:64, 0:48], [pc], [s.cols])
            k.cp(s.eglc[:], pc[:, 64:80], [pc], [s.eglc], eng="act")
            mT = M_LE if d == 0 else M_GE
            mS = M_GT if d == 0 else M_LT
            for h in range(2):
                sel64 = cs[0:2, C_SEL + 128 * h:C_SEL + 128 * h + 64]
                sel128 = cs[0:2, C_SEL + 128 * h:C_SEL + 128 * h + 128]
                pq = cx.psum[6]
                k.mm(pq[:, :], sel128, s.eg[0:2, :], [cs, s.eg], [pq])
                k.tt(s.qg[h][:], pq[:, :], s.QN[h][:], ALU.mult, [pq, s.QN[h]], [s.qg[h]])
                pD = cx.psum[3]
                k.mm(pD[0:64, :], sel64, s.ngc[0:2, :], [cs, s.ngc], [pD], start=True, stop=False)
                k.mm(pD[0:64, :], ID64, cs[0:64, C_MASK + mS * 512:C_MASK + (mS + 1) * 512], [cs], [pD], start=False, stop=False)
                for c in range(8):
                    k.mm(pD[0:64, c * 64:(c + 1) * 64], s.gcb[0:2, c * 64:(c + 1) * 64], sel64, [s.gcb, cs], [pD], start=False, stop=(c == 7))
                k.act(G1[:], pD[0:64, :], AF.Exp, [pD], [G1])
                pK = cx.psum[4]
                for c in range(8):
                    k.mm(pK[0:64, c * 64:(c + 1) * 64], s.KN[h][:, c * 64:(c + 1) * 64], s.KN[h][:, c * 64:(c + 1) * 64], [s.KN[h]], [pK])
                k.stt(Nm[:], pK[0:64, :], -1.0, G1[:], ALU.mult, ALU.mult, [pK, G1], [Nm])
                k.mm(pD[0:64, :], sel64, s.gc[0:2, :], [cs, s.gc], [pD], start=True, stop=False)
                k.mm(pD[0:64, :], ID64, cs[0:64, C_MASK + mT * 512:C_MASK + (mT + 1) * 512], [cs], [pD], start=False, stop=False)
                for c in range(8):
                    k.mm(pD[0:64, c * 64:(c + 1) * 64], s.ngc[0:2, c * 64:(c + 1) * 64], sel64, [s.ngc, cs], [pD], start=False, stop=(c == 7))
                k.act(gamT[:], pD[0:64, :], AF.Exp, [pD], [gamT])
                for c in range(8):
                    k.mm(pK[0:64, c * 64:(c + 1) * 64], s.KN[h][:, c * 64:(c + 1) * 64], s.QN[h][:, c * 64:(c + 1) * 64], [s.KN[h], s.QN[h]], [pK])
                k.tt(s.attnT[h][:], pK[0:64, :], gamT[:], ALU.mult, [pK, gamT], [s.attnT[h]])
                pA, pB, pC = cx.psum[0], cx.psum[1], cx.psum[2]
                for c in range(8):
                    k.mm(pA[0:64, c * 64:(c + 1) * 64], Nm[:, c * 64:(c + 1) * 64], ID64, [Nm, cs], [pA])
                k.cp(NTm[:], pA[0:64, :], [pA], [NTm], eng="act")
                k.tt(fv(XT, 0, 64, 0, [(64, 8), (1, 64)]), fv(NTm, 0, 64, 0, [(64, 8), (1, 64)]),
                     fv(cs, 0, 64, C_IDENT, [(0, 8), (1, 64)]), ALU.add, [NTm, cs], [XT])
                Pc, PTc = Nm, NTm
                for m in range(5):
                    Pn, PTn = P2[m % 2], PT2[m % 2]
                    for c in range(8):
                        sl = slice(c * 64, (c + 1) * 64)
                        k.mm(pA[0:64, sl], PTc[:, sl], Pc[:, sl], [PTc, Pc], [pA])
                    if m < 4:
                        for c in range(8):
                            sl = slice(c * 64, (c + 1) * 64)
                            k.mm(pB[0:64, sl], Pc[:, sl], PTc[:, sl], [PTc, Pc], [pB])
                    k.cp(Pn[:], pA[0:64, :], [pA], [Pn], eng="act")
                    if m < 4:
                        k.cp(PTn[:], pB[0:64, :], [pB], [PTn])
                    for c in range(8):
                        sl = slice(c * 64, (c + 1) * 64)
                        k.mm(pC[0:64, sl], Pn[:, sl], XT[:, sl], [Pn, XT], [pC])
                    k.tt(XT[:], XT[:], pC[0:64, :], ALU.add, [XT, pC], [XT])
                    Pc, PTc = Pn, PTn
                for c in range(8):
                    sl = slice(c * 64, (c + 1) * 64)
                    k.ts(s.XTb[h][:, sl], XT[:, sl], s.cols[:, (c * 3 + 0) * 2 + h:(c * 3 + 0) * 2 + h + 1], ALU.mult, [XT, s.cols], [s.XTb[h]])
                    k.ts(s.XTbg[h][:, sl], XT[:, sl], s.cols[:, (c * 3 + 1) * 2 + h:(c * 3 + 1) * 2 + h + 1], ALU.mult, [XT, s.cols], [s.XTbg[h]], eng="pool")
                for c in range(8):
                    sl = slice(c * 64, (c + 1) * 64)
                    k.mm(pA[:, sl], s.Ktm[h][:, c, :], s.XTbg[h][:, sl], [s.Ktm[h], s.XTbg[h]], [pA])
                k.act(s.nwT[h][:], pA[:, :], AF.Copy, [pA], [s.nwT[h]], scale=-1.0)
            for ci in range(8):
                c = ci if d == 0 else 7 - ci
                sl = slice(c * 64, (c + 1) * 64)
                for h in range(2):
                    pV = cx.psum[6]
                    pU = cx.psum[7]
                    vr = pV[0:64, 256 * h:256 * h + 128]
                    orr = pV[0:64, 256 * h + 128:256 * h + 256]
                    k.mm(vr, s.XTb[h][:, sl], s.Vtm[h][:, c, :], [s.XTb[h], s.Vtm[h]], [pV], start=True, stop=False)
                    k.mm(vr, s.nwT[h][:, sl], s.S[h][:], [s.nwT[h], s.S[h]], [pV], start=False, stop=True)
                    k.cp(s.vn[h][:], vr, [pV], [s.vn[h]])
                    k.mm(orr, s.qg[h][:, sl], s.S[h][:], [s.qg[h], s.S[h]], [pV], start=True, stop=False)
                    k.mm(orr, s.attnT[h][:, sl], s.vn[h][:], [s.attnT[h], s.vn[h]], [pV], start=False, stop=True)
                    k.cp(s.Og[:, c, 128 * h:128 * h + 128], orr, [pV], [s.Og], eng="act")
                    k.ts(s.kdm[h][:], s.Ktm[h][:, c, :], s.cols[:, (c * 3 + 2) * 2 + h:(c * 3 + 2) * 2 + h + 1], ALU.mult, [s.Ktm[h], s.cols], [s.kdm[h]], eng="pool")
                    ur = pU[:, 128 * h:128 * h + 128]
                    k.mm(ur, s.kdm[h][:], s.vn[h][:], [s.kdm[h], s.vn[h]], [pU])
                    k.stt(s.S[h][:], s.S[h][:], s.eglc[:, 8 * h + c:8 * h + c + 1], ur, ALU.mult, ALU.add, [s.S[h], s.eglc, pU], [s.S[h]])
            k.dma("sp", OS[d][t0:t0 + 512, :].rearrange("(c p) f -> p c f", p=64), s.Og[:], [s.Og], [OS[d]], s.Og)


def phase_even_combine(k, cx, T, TM, HS, OS, gn_d, MIX, MIXT=None):
    gn = k.sb("cb_gn", [128, 512], F32)
    k.dma("sp", gn[:], gn_d[:].partition_broadcast(128), [gn_d], [gn], gn)
    NTl = T // 128
    bufs = []
    for i in range(2):
        b = Ctx()
        b.a = k.sb(f"cb_a{i}", [128, 512], F32)
        b.b = k.sb(f"cb_b{i}", [128, 512], F32)
        b.moz = k.sb(f"cb_moz{i}", [128, 768], F32)
        b.sq = k.sb(f"cb_sq{i}", [128, 512], F32)
        b.ss = k.sb(f"cb_ss{i}", [128, 4], F32)
        b.tr = k.sb(f"cb_tr{i}", [128, 512], F32)
        bufs.append(b)
    for tt in range(NTl):
        b = bufs[tt % 2]
        r = slice(tt * 128, (tt + 1) * 128)
        k.dma("sp", b.a[:, 0:256], HS[0][r, :], [HS[0]], [b.a], b.a, grouped=True)
        k.dma("sp", b.a[:, 256:512], OS[0][r, :], [OS[0]], [b.a], b.a, grouped=True)
        k.dma("sp", b.b[:, 0:256], HS[1][r, :], [HS[1]], [b.b], b.b, grouped=True)
        k.dma("sp", b.b[:, 256:512], OS[1][r, :], [OS[1]], [b.b], b.b, grouped=True)
        k.dma("sp", b.moz[:], TM[r, 384:1152], [TM], [b.moz], b.moz)
        k.tt(b.a[:], b.a[:], b.b[:], ALU.add, [b.a, b.b], [b.a])
        k.tt(b.sq[:], b.a[:], b.a[:], ALU.mult, [b.a], [b.sq])
        k.op("dve", lambda e, b=b: e.tensor_reduce(out=b.ss[:], in_=b.sq[:].rearrange("p (h f) -> p h f", f=128), axis=AX.X, op=ALU.add),
             [b.sq], [b.ss])
        k.act(b.ss[:], b.ss[:], AF.Sqrt, [b.ss], [b.ss], scale=1.0 / 128.0, bias=float(EPS))
        k.op("dve", lambda e, b=b: e.reciprocal(out=b.ss[:], in_=b.ss[:]), [b.ss], [b.ss])
        k.tt(b.a[:].rearrange("p (h f) -> p h f", f=128), b.a[:].rearrange("p (h f) -> p h f", f=128),
             fv(b.ss, 0, 128, 0, [(1, 4), (0, 128)]), ALU.mult, [b.a, b.ss], [b.a])
        k.tt(b.a[:], b.a[:], gn[:], ALU.mult, [b.a, gn], [b.a])
        k.act(b.moz[:, 0:256], b.moz[:, 0:256], AF.Sigmoid, [b.moz], [b.moz])
        k.act(b.moz[:, 256:768], b.moz[:, 256:768], AF.Silu, [b.moz], [b.moz])
        k.tt(b.a[:, 0:256], b.a[:, 0:256], b.moz[:, 0:256], ALU.mult, [b.a, b.moz], [b.a])
        k.tt(b.a[:], b.a[:], b.moz[:, 256:768], ALU.mult, [b.a, b.moz], [b.a])
        if MIXT is None:
            k.dma("sp", MIX[r, :], b.a[:], [b.a], [MIX], b.a)
        else:
            pt = cx.psum[tt % 2]
            for fc in range(4):
                k.mm(pt[:, fc * 128:(fc + 1) * 128], b.a[:, fc * 128:(fc + 1) * 128], cx.consts[:, C_IDENT:C_IDENT + 128], [b.a, cx.consts], [pt])
            k.cp(b.tr[:], pt[:, :], [pt], [b.tr], eng="act")
            mxt = MIXT[(tt * 128) // 1024]
            tl = (tt * 128) % 1024
            k.dma("sp", mxt[:].rearrange("(c p) t -> p c t", p=128)[:, :, tl:tl + 128],
                  b.tr[:].rearrange("p (c t) -> p c t", t=128), [b.tr], [mxt], b.tr)


def build_even(T, debug=False, phases=("ml", "gdn", "comb")):
    nc = bass.Bass("TRN2", target_bir_lowering=False)
    k = KB(nc)
    cx = Ctx()
    setup_common(k, cx)
    kd = "ExternalOutput" if debug else "Internal"
    emit_even(k, cx, T, kd, phases)
    k.finish()
    k.close()
    print("even program: instructions", k.ninst, "sems", k.nsem)
    return nc


def emit_even(k, cx, T, kd, phases=("ml", "gdn", "comb"), MIXT=None):
    xT = k.dram("xT", [1024, T], F32, kind="ExternalInput")
    cT = k.dram("cT", [128, 8], F32, kind="ExternalInput")
    adaw = k.dram("adaw", [1024, 2048], F32, kind="ExternalInput")
    adab = k.dram("adab", [128, 16], F32, kind="ExternalInput")
    ng = k.dram("ng", [128, 8], F32, kind="ExternalInput")
    W = k.dram("W", [1024, NC1], F32, kind="ExternalInput")
    mgb = k.dram("mgb", [8, 1], F32, kind="ExternalInput")
    gpar = k.dram("gpar", [4, 2], F32, kind="ExternalInput")
    dcw = k.dram("dcw", [128, 6, 5], F32, kind="ExternalInput")
    gn = k.dram("gn", [1, 512], F32, kind="ExternalInput")
    MIX = k.dram("MIX", [T, 512], F32, kind="ExternalOutput") if MIXT is None else None
    OS = [k.dram(f"OS{d}", [T, 256], F32, kind=kd) for d in range(2)]
    QN = [k.dram(f"QN{h}", [128, T], F32, kind=kd) for h in range(2)]
    KN = [k.dram(f"KN{h}", [128, T], F32, kind=kd) for h in range(2)]
    KTM = [k.dram(f"KTM{h}", [T, 128], F32, kind=kd) for h in range(2)]
    VTM = [k.dram(f"VTM{h}", [T, 128], F32, kind=kd) for h in range(2)]
    FM = k.dram("FM", [FM_ROWS, T], F32, kind=kd)
    TM = k.dram("TM", [T, 1152], F32, kind=kd)
    HS = [k.dram(f"HS{d}", [T, 256], F32, kind=kd) for d in range(2)]

    A = k.sb("evA", [128, 8], F32)
    Bm = k.sb("evBm", [128, 8], F32)
    with k.scope():
        mod = phase_mod(k, cx, cT, adaw, adab, 16, tag="me")
        ngs = k.sb("evngs", [128, 8], F32)
        k.dma("sp", ngs[:], ng[:], [ng], [ngs], ngs)
        k.stt(A[:], mod[:, 8:16], 1.0, ngs[:], ALU.add, ALU.mult, [mod, ngs], [A])
        k.cp(Bm[:], mod[:, 0:8], [mod], [Bm])
    sc1 = k.scope()
    sc1.__enter__()
    xT3 = xT[:].rearrange("(k p) t -> p k t", p=128)
    fm_specs = []
    for (c0, nr, scale) in [(FM_MQ, 128, 1.0), (FM_MK, 128, 0.125), (FM_G, 16, 1.0)] + \
            [(FM_DQ + 128 * i, 128, 1.0) for i in range(6)]:
        fm_specs.append((c0, nr, FM, (lambda tt, r0=c0, nr=nr: FM[r0:r0 + nr, tt * 512:(tt + 1) * 512]), scale))
    tm_specs = []
    for (c0, ncl) in [(TM_MK, 384), (TM_MO, 256), (TM_Z, 512)]:
        tm_specs.append((c0, ncl, TM, (lambda tt, ts, c0=c0, ncl=ncl: TM[tt * 512 + ts * 128: tt * 512 + (ts + 1) * 128, c0 - TM0:c0 - TM0 + ncl])))
    phase_norm_proj(k, cx, T, lambda tt: xT3[:, :, tt * 512:(tt + 1) * 512], [xT], A, Bm, W, NC1, fm_specs, tm_specs)
    sc1.__exit__(None, None, None)
    if "ml" in phases:
        with k.scope():
            phase_mlstm(k, cx, T, FM, TM, mgb, HS)
    if "gdn" in phases:
        with k.scope():
            phase_gdn_pre(k, cx, T, FM, dcw, QN, KN, KTM, VTM)
        with k.scope():
            phase_gdn(k, cx, T, FM, QN, KN, KTM, VTM, gpar, OS)
    if "comb" in phases:
        with k.scope():
            phase_even_combine(k, cx, T, TM, HS, OS, gn, MIX, MIXT)


SEQ = 8192


def colform(v, nchunks):
    return np.ascontiguousarray(np.asarray(v, np.float32).reshape(nchunks, 128).T)


def even_core_inputs(inp, b, e, T=SEQ):
    w_in = inp["ev_w_in"][0]
    o = np.cumsum([0, 256, 256, 512, 512, 16, 1536, 16, 1024])
    mq, mk, mv, mo, mg, dqkv, dg, z = [w_in[:, o[i]:o[i + 1]] for i in range(8)]
    hs = [2 * e, 2 * e + 1]
    gsel = [t * 4 + h for t in range(4) for h in hs]
    cols = [mq[:, 128 * e:128 * e + 128], mk[:, 128 * e:128 * e + 128], mg[:, gsel], dg[:, gsel]]
    for part in range(3):
        for h in hs:
            cols.append(dqkv[:, part * 512 + h * 128: part * 512 + (h + 1) * 128])
    cols += [mk[:, 128 * e:128 * e + 128], mv[:, 256 * e:256 * e + 256], mo[:, 256 * e:256 * e + 256],
             z[:, 256 * e:256 * e + 256], z[:, 512 + 256 * e:512 + 256 * e + 256]]
    W = np.ascontiguousarray(np.concatenate(cols, axis=1))
    assert W.shape[1] == NC1
    cw = inp["ev_dn_conv_w"][0]
    dcw = np.stack([cw[:, part * 512 + h * 128: part * 512 + (h + 1) * 128] for part in range(3) for h in hs], 0)
    return {
        "consts": make_consts(),
        "xT": np.ascontiguousarray(inp["x"][b, :T].T),
        "cT": colform(inp["c"][b], 8),
        "adaw": np.ascontiguousarray(inp["ada_w"][0][:, 0:2048]),
        "adab": colform(inp["ada_b"][0][0:2048], 16),
        "ng": colform(inp["norm_g"][0], 8),
        "W": W,
        "mgb": np.ascontiguousarray(inp["ev_m_gate_b"][0][:, hs].reshape(8, 1)),
        "gpar": np.ascontiguousarray(np.stack([inp["ev_dn_dt_bias"][0][:, hs].reshape(4), inp["ev_dn_a_log"][0][:, hs].reshape(4)], 1)),
        "dcw": np.ascontiguousarray(dcw.transpose(2, 0, 1)),
        "gn": np.ascontiguousarray(np.concatenate([inp["ev_m_norm_g"][0][256 * e:256 * e + 256],
                                                   inp["ev_dn_norm_g"][0][256 * e:256 * e + 256]])[None, :]),
    }


def run_even(inp, T=SEQ):
    nc = build_even(T)
    in_maps = [even_core_inputs(inp, c // 2, c % 2, T) for c in range(8)]
    res = run_bass_kernel_spmd(nc, in_maps, core_ids=list(range(8)))
    mix = np.zeros((4, T, 1024), np.float32)
    for c in range(8):
        b, e = c // 2, c % 2
        m = res.results[c]["MIX"]
        mix[b, :, 256 * e:256 * e + 256] = m[:, 0:256]
        mix[b, :, 512 + 256 * e:512 + 256 * e + 256] = m[:, 256:512]
    return mix


HALO = 1024
NEG_ATT = -30000.0
C2_J = 0
C2_OH = 128
C2_COLS = 128 + 3 * 384
DILS = (1, 4, 16)
VV_COLS = 33 + 4 * 9 + 16 * 3


def make_vv(TO, s_half, nhalves):
    cols = []
    S = TO * nhalves
    for d in DILS:
        npos = TO // d
        nkt = npos // 128 + 1
        for r in range(d):
            p = -64 + 128 * np.arange(nkt)[None, :] + np.arange(128)[:, None]
            tok = s_half * TO + r + d * p
            cols.append(((tok >= 0) & (tok < S)).astype(np.float32))
    return np.ascontiguousarray(np.concatenate(cols, axis=1))


def t5_bucket_np(rel):
    n = np.abs(rel)
    large = 8 + (np.log(np.maximum(n, 1).astype(np.float32) / np.float32(8)) / np.float32(np.log(128.0)) * np.float32(8)).astype(np.int32)
    large = np.minimum(large, 15)
    return (rel > 0).astype(np.int32) * 16 + np.where(n < 8, n, large)


def make_consts2():
    c = np.zeros((128, C2_COLS), np.float32)
    c[np.arange(128), C2_J + 127 - np.arange(128)] = 1.0
    for g, d in enumerate(DILS):
        for u in range(383):
            rel = u - 191
            if abs(rel) <= 64:
                c[t5_bucket_np(np.array(rel * d)), C2_OH + g * 384 + u] = 1.0
            else:
                c[32, C2_OH + g * 384 + u] = NEG_ATT
    return c


def phase_attention(k, cx, TO, QT, KT, VT, vv_d, relb_d, relbT_d, GF, DT, c2):
    E = TO + 2 * HALO
    cs = cx.consts
    rb = k.sb("at_rb", [33, 8], F32)
    k.op("dve", lambda e: e.memset(rb[:], 1.0), [], [rb])
    k.dma("sp", rb[0:32, :], relb_d[:], [relb_d], [rb], rb)
    k.ts(rb[0:32, :], rb[0:32, :], 8.0, ALU.mult, [rb], [rb])
    gfs = k.sb("at_gfs", [8, 3 * 384], F32)
    pg = cx.psum[0]
    for g in range(3):
        k.mm(pg[0:8, 0:384], rb[0:33, :], c2[0:33, C2_OH + g * 384:C2_OH + (g + 1) * 384], [rb, c2], [pg])
        k.cp(gfs[:, g * 384:(g + 1) * 384], pg[0:8, 0:384], [pg], [gfs])
    k.dma("sp", GF[:], gfs[:], [gfs], [GF], gfs)
    bmx = k.sb("at_bmx", [128, 32], F32)
    bmax = k.sb("at_bmax", [128, 1], F32)
    Qa = k.sb("at_Qa", [65, TO], F32)
    Ka = k.sb("at_Ka", [65, E], F32)
    k.op("dve", lambda e: e.memset(Ka[64:65, :], 1.0), [], [Ka])
    Qb = k.sb("at_Qb", [65, TO], BF16)
    Kb = k.sb("at_Kb", [65, E], BF16)
    Hb = k.sb("at_Hb", [128, 6, 128], BF16)
    Jb = k.sb("at_Jb", [128, 128], BF16)
    k.cp(Jb[:], c2[:, C2_J:C2_J + 128], [c2], [Jb])
    sqt = k.sb("at_sq", [64, 512], F32)
    kmx = k.sb("at_kmx", [128, 16], F32)
    kmax = k.sb("at_kmax", [128, 1], F32)
    accN = k.sb("at_accN", [64, TO], F32)
    accD = k.sb("at_accD", [64, TO], F32)
    H = k.sb("at_H", [128, 6, 128], F32)
    Vf = [k.sb(f"at_Vf{i}", [128, 34, 64], F32) for i in range(2)]
    Vt = [k.sb(f"at_Vt{i}", [128, 34, 64], BF16) for i in range(2)]
    Vv = [k.sb(f"at_Vv{i}", [128, 34, 64], BF16) for i in range(2)]
    PT = [k.sb(f"at_PT{i}", [128, 512], BF16) for i in range(2)]
    ones64 = cs[0:64, C_ONES:C_ONES + 128]
    VV = k.sb("at_VV", [128, vv_d[:].shape[1]], F32)
    k.dma("sp", VV[:], vv_d[:], [vv_d], [VV], VV)
    vvoff = 0
    VVO = {}
    for g_, d_ in enumerate(DILS):
        for r_ in range(d_):
            VVO[(g_, r_)] = vvoff
            vvoff += (TO // d_) // 128 + 1
    assert vvoff == vv_d[:].shape[1]
    nv = 0
    npt = 0
    for hd in range(8):
        r0 = hd * 64
        k.dma("sp", Qa[0:64, :], QT[r0:r0 + 64, :], [QT], [Qa], Qa)
        k.dma("sp", Ka[0:64, :], KT[r0:r0 + 64, :], [KT], [Ka], Ka)
        for g in range(3):
            for kt in range(2):
                src = bass.AP(GF[:].tensor, GF[:].offset + hd * 1152 + g * 384 + kt * 128, [[1, 128], [1, 128]])
                k.dma("sp", H[:, g * 2 + kt, :], src, [GF], [H], H, grouped=True)
        k.dma("sp", bmx[:], relbT_d[hd:hd + 1, :].partition_broadcast(128), [relbT_d], [bmx], bmx)
        k.op("dve", lambda e: e.tensor_reduce(out=bmax[:], in_=bmx[:], axis=AX.X, op=ALU.max), [bmx], [bmax])
        k.ts(bmax[:], bmax[:], 8.0, ALU.mult, [bmax], [bmax])
        pk = cx.psum[1]
        for t in range(E // 512):
            k.tt(sqt[:], Ka[0:64, t * 512:(t + 1) * 512], Ka[0:64, t * 512:(t + 1) * 512], ALU.mult, [Ka], [sqt])
            k.mm(pk[:, :], ones64, sqt[:], [cs, sqt], [pk])
            k.op("dve", lambda e, t=t: e.tensor_reduce(out=kmx[:, t:t + 1], in_=pk[:, :], axis=AX.X, op=ALU.max), [pk], [kmx])
        k.op("dve", lambda e: e.tensor_reduce(out=kmax[:], in_=kmx[:, 0:E // 512], axis=AX.X, op=ALU.max), [kmx], [kmax])
        k.act(kmax[:], kmax[:], AF.Sqrt, [kmax], [kmax])
        for t in range(TO // 512):
            k.tt(sqt[:], Qa[0:64, t * 512:(t + 1) * 512], Qa[0:64, t * 512:(t + 1) * 512], ALU.mult, [Qa], [sqt])
            k.mm(pk[:, :], ones64, sqt[:], [cs, sqt], [pk])
            k.act(Qa[64:65, t * 512:(t + 1) * 512], pk[64:65, :], AF.Sqrt, [pk], [Qa])
        k.ts(Qa[64:65, :], Qa[64:65, :], kmax[64:65, 0:1], ALU.mult, [Qa, kmax], [Qa], s2=bmax[64:65, 0:1], op1=ALU.add)
        k.ts(Qa[64:65, :], Qa[64:65, :], -1.0, ALU.mult, [Qa], [Qa])
        k.cp(Qb[:], Qa[:], [Qa], [Qb], eng="act")
        k.cp(Kb[:], Ka[:], [Ka], [Kb])
        k.cp(Hb[:], H[:], [H], [Hb], eng="pool")
        if hd == 0:
            dbg(k, "H", H, [128, 6, 128]); dbg(k, "Qa", Qa, [65, TO]); dbg(k, "Ka", Ka, [65, E])
        first = True
        for g, d in enumerate(DILS):
            npos = TO // d
            nkt = npos // 128 + 1
            for r in range(d):
                V1 = Vt[nv % 2]
                V2 = Vv[nv % 2]
                V1f = Vf[nv % 2]
                nv += 1
                base = HALO + r - 64 * d
                for n0 in range(0, nkt, 8):
                    nn = min(8, nkt - n0)
                    vsrc = bass.AP(VT[:].tensor, VT[:].offset + (base + n0 * 128 * d) * 512 + r0, [[d * 512, 128], [128 * d * 512, nn], [1, 64]])
                    k.dma("sp", V1f[:, n0:n0 + nn, :], vsrc, [VT], [V1f], V1f, grouped=True)
                vb_ = fv(VV, 0, 128, VVO[(g, r)], [(1, nkt), (0, 64)])
                k.tt(V1[:, 0:nkt, :], V1f[:, 0:nkt, :], vb_, ALU.mult, [V1f, VV], [V1])
                k.cp(V2[:, 0:nkt, :], vb_, [VV], [V2], eng="pool")
                nqt = npos // 128
                for qb in range(0, nqt, 2):
                    nq = min(2, nqt - qb)
                    pS = cx.psum[2 + npt % 2]
                    for qi in range(nq):
                        qt = qb + qi
                        qap = fv(Qb, 0, 65, r + d * 128 * qt, [(d, 128)])
                        for kt in range(2):
                            kap = fv(Kb, 0, 65, HALO + r + d * (128 * qt - 64 + 128 * kt), [(d, 128)])
                            col = (qi * 2 + kt) * 128
                            k.mm(pS[:, col:col + 128], kap, qap, [Kb, Qb], [pS], start=True, stop=False)
                            k.mm(pS[:, col:col + 128], Hb[:, g * 2 + kt, :], Jb[:], [Hb, Jb], [pS], start=False, stop=True)
                    P = PT[npt % 2]
                    k.act(P[:, 0:nq * 256], pS[:, 0:nq * 256], AF.Exp, [pS], [P], scale=0.125)
                    if hd == 0 and g == 0 and qb == 0:
                        dbg(k, "P", P, [128, 512]); dbgap(k, "V1", V1, V1[:, 0:9, :], [128, 9, 64]); dbgap(k, "V2", V2, V2[:, 0:9, :], [128, 9, 64])
                    pN = cx.psum[4 + npt % 2]
                    pDn = cx.psum[6 + npt % 2]
                    npt += 1
                    for qi in range(nq):
                        qt = qb + qi
                        for kt in range(2):
                            col = (qi * 2 + kt) * 128
                            k.mm(pN[0:64, qi * 128:(qi + 1) * 128], V1[:, qt + kt, :], P[:, col:col + 128], [V1, P], [pN],
                                 start=(kt == 0), stop=(kt == 1))
                            k.mm(pDn[0:64, qi * 128:(qi + 1) * 128], V2[:, qt + kt, :], P[:, col:col + 128], [V2, P], [pDn],
                                 start=(kt == 0), stop=(kt == 1))
                    qcols = nq * 128
                    an = fv(accN, 0, 64, r + d * 128 * qb, [(d, qcols)])
                    ad = fv(accD, 0, 64, r + d * 128 * qb, [(d, qcols)])
                    if first:
                        k.cp(an, pN[0:64, 0:qcols], [pN], [accN], eng="act")
                        k.cp(ad, pDn[0:64, 0:qcols], [pDn], [accD])
                    else:
                        k.tt(an, an, pN[0:64, 0:qcols], ALU.add, [accN, pN], [accN], eng="dve")
                        k.tt(ad, ad, pDn[0:64, 0:qcols], ALU.add, [accD, pDn], [accD], eng="dve")
            first = False
        k.op("dve", lambda e: e.reciprocal(out=accD[:], in_=accD[:]), [accD], [accD])
        k.tt(accN[:], accN[:], accD[:], ALU.mult, [accN, accD], [accN])
        k.dma("sp", DT[r0:r0 + 64, :], accN[:], [accN], [DT], accN)


def phase_conv_module(k, cx, TO, GA, GB, valid_d, cw_d, cpar_d, CT):
    cs = cx.consts
    cw = k.sb("cv_w", [128, 4, 31], F32)
    k.dma("sp", cw[:], cw_d[:], [cw_d], [cw], cw)
    cpar = k.sb("cv_par", [128, 4, 3], F32)
    k.dma("sp", cpar[:], cpar_d[:], [cpar_d], [cpar], cpar)
    dg = k.sb("cv_diag", [128, 124, 128], BF16)
    for ch in range(4):
        for j in range(31):
            k.ts(dg[:, ch * 31 + j, :], cs[:, C_IDENT:C_IDENT + 128], cw[:, ch, j:j + 1], ALU.mult, [cs, cw], [dg],
                 eng=("dve" if j % 2 else "pool"))
    ga = [k.sb(f"cv_ga{i}", [128, 4, 542], F32) for i in range(2)]
    gb = [k.sb(f"cv_gb{i}", [128, 4, 542], F32) for i in range(2)]
    vb = k.sb("cv_vb", [128, 542], F32)
    uin = k.sb("cv_uin", [128, 4, 542], BF16)
    U = k.sb("cv_U", [128, 4, 512], F32)
    XC = k.sb("cv_XC", [128, 4, 512], F32)
    SQ = k.sb("cv_SQ", [128, 4, 512], F32)
    rs = k.sb("cv_rs", [128, 512], F32)
    O = [k.sb(f"cv_O{i}", [128, 4, 512], F32) for i in range(2)]
    ones = cs[:, C_ONES:C_ONES + 128]
    for tt in range(TO // 512):
        e0 = HALO + tt * 512 - 15
        a, b = ga[tt % 2], gb[tt % 2]
        k.dma("sp", a[:], GA[:].rearrange("(c p) t -> p c t", p=128)[:, :, e0:e0 + 542], [GA], [a], a)
        k.dma("sp", b[:], GB[:].rearrange("(c p) t -> p c t", p=128)[:, :, e0:e0 + 542], [GB], [b], b)
        k.dma("sp", vb[:], valid_d[0:1, e0:e0 + 542].partition_broadcast(128), [valid_d], [vb], vb)
        k.act(b[:], b[:], AF.Sigmoid, [b], [b])
        k.tt(a[:], a[:], b[:], ALU.mult, [a, b], [a])
        k.tt(uin[:], a[:], fv(vb, 0, 128, 0, [(0, 4), (1, 542)]), ALU.mult, [a, vb], [uin])
        for ch in range(4):
            pc_ = cx.psum[ch % 2]
            for j in range(31):
                k.mm(pc_[:, :], dg[:, ch * 31 + j, :], uin[:, ch, j:j + 512], [dg, uin], [pc_], start=(j == 0), stop=(j == 30))
            k.ts(U[:, ch, :], pc_[:, :], cpar[:, ch, 0:1], ALU.add, [pc_, cpar], [U])
        pm = cx.psum[2]
        for ch in range(4):
            k.mm(pm[:, :], ones, U[:, ch, :], [cs, U], [pm], start=(ch == 0), stop=(ch == 3))
        for ch in range(4):
            k.stt(XC[:, ch, :], pm[:, :], -1.0 / 512.0, U[:, ch, :], ALU.mult, ALU.add, [pm, U], [XC])
        k.tt(SQ[:], XC[:], XC[:], ALU.mult, [XC], [SQ], eng="pool")
        pv = cx.psum[3]
        for ch in range(4):
            k.mm(pv[:, :], ones, SQ[:, ch, :], [cs, SQ], [pv], start=(ch == 0), stop=(ch == 3))
        k.act(rs[:], pv[:, :], AF.Sqrt, [pv], [rs], scale=1.0 / 512.0, bias=float(EPS))
        k.op("dve", lambda e: e.reciprocal(out=rs[:], in_=rs[:]), [rs], [rs])
        Ot = O[tt % 2]
        for ch in range(4):
            k.tt(XC[:, ch, :], XC[:, ch, :], rs[:], ALU.mult, [XC, rs], [XC])
            k.act(Ot[:, ch, :], XC[:, ch, :], AF.Silu, [XC, cpar], [Ot], scale=cpar[:, ch, 1:2], bias=cpar[:, ch, 2:3])
        k.dma("sp", CT[:].rearrange("(c p) t -> p c t", p=128)[:, :, tt * 512:(tt + 1) * 512], Ot[:], [Ot], [CT], Ot)


def phase_final(k, cx, TO, CT, DT, ZT, X1, Wo_d, gate1, fg_d, outT):
    cs = cx.consts
    Wb = k.sb("fn_Wb", [128, 8, 1024], BF16)
    load_w_bf16(k, Wb, Wo_d, 1024, "fn")
    fg = k.sb("fn_fg", [128, 8], F32)
    k.dma("sp", fg[:], fg_d[:], [fg_d], [fg], fg)
    M = [k.sb(f"fn_M{i}", [128, 8, 512], F32) for i in range(2)]
    Z = [k.sb(f"fn_Z{i}", [128, 8, 512], F32) for i in range(2)]
    Mb = k.sb("fn_Mb", [128, 8, 512], BF16)
    X = [k.sb(f"fn_X{i}", [128, 8, 512], F32) for i in range(2)]
    sq = k.sb("fn_sq", [128, 8, 512], BF16)
    rs = k.sb("fn_rs", [128, 512], F32)
    O = [k.sb(f"fn_O{i}", [128, 8, 512], F32) for i in range(2)]
    r3 = lambda D: D[:].rearrange("(c p) t -> p c t", p=128)
    for tt in range(TO // 512):
        sl = slice(tt * 512, (tt + 1) * 512)
        Mt, Zt, Xt, Ot = M[tt % 2], Z[tt % 2], X[tt % 2], O[tt % 2]
        k.dma("sp", Mt[:, 0:4, :], r3(CT)[:, :, sl], [CT], [Mt], Mt, grouped=True)
        k.dma("sp", Mt[:, 4:8, :], r3(DT)[:, :, sl], [DT], [Mt], Mt, grouped=True)
        k.dma("sp", Zt[:], r3(ZT)[:, :, sl], [ZT], [Zt], Zt)
        k.dma("sp", Xt[:], r3(X1)[:, :, sl], [X1], [Xt], Xt)
        k.act(Zt[:], Zt[:], AF.Silu, [Zt], [Zt])
        k.tt(Mb[:], Mt[:], Zt[:], ALU.mult, [Mt, Zt], [Mb])
        for fc in range(8):
            pb = cx.psum[fc % 4]
            for kc in range(8):
                k.mm(pb[:, :], Wb[:, kc, fc * 128:(fc + 1) * 128], Mb[:, kc, :], [Wb, Mb], [pb], start=(kc == 0), stop=(kc == 7))
            k.stt(Xt[:, fc, :], pb[:, :], gate1[:, fc:fc + 1], Xt[:, fc, :], ALU.mult, ALU.add, [pb, gate1, Xt], [Xt])
        k.act(sq[:], Xt[:], AF.Square, [Xt], [sq])
        pss = cx.psum[7]
        for kc in range(8):
            k.mm(pss[:, :], cx.ones_bf[:], sq[:, kc, :], [cx.ones_bf, sq], [pss], start=(kc == 0), stop=(kc == 7))
        k.act(rs[:], pss[:, :], AF.Sqrt, [pss], [rs], scale=1.0 / 1024.0, bias=float(EPS))
        k.op("dve", lambda e: e.reciprocal(out=rs[:], in_=rs[:]), [rs], [rs])
        for kc in range(8):
            k.stt(Ot[:, kc, :], Xt[:, kc, :], fg[:, kc:kc + 1], rs[:], ALU.mult, ALU.mult, [Xt, fg, rs], [Ot])
        k.dma("sp", r3(outT)[:, :, sl], Ot[:], [Ot], [outT], Ot)


def build_odd(TO, debug=False, phases=("conv", "att", "fin")):
    nc = bass.Bass("TRN2", target_bir_lowering=False)
    k = KB(nc)
    cx = Ctx()
    setup_common(k, cx)
    kd = "ExternalOutput" if debug else "Internal"
    emit_odd(k, cx, TO, kd, phases)
    k.finish()
    k.close()
    print("odd program: instructions", k.ninst, "sems", k.nsem)
    return nc


def emit_odd(k, cx, TO, kd, phases=("conv", "att", "fin"), GM=None):
    E = TO + 2 * HALO
    c2d = k.dram("consts2", [128, C2_COLS], F32, kind="ExternalInput")
    c2 = k.sb("c2", [128, C2_COLS], F32)
    k.dma("sp", c2[:], c2d[:], [c2d], [c2], c2)
    x0T = k.dram("x0T", [1024, E], F32, kind="ExternalInput")
    mixT = k.dram("mixT", [1024, E], F32, kind="ExternalInput") if GM is None else None
    selw_d = k.dram("selw", [128, 2], F32, kind="ExternalInput") if GM is not None else None
    valid = k.dram("valid", [1, E], F32, kind="ExternalInput")
    cT = k.dram("cT1", [128, 8], F32, kind="ExternalInput")
    adaw0 = k.dram("adaw0", [1024, 1024], F32, kind="ExternalInput")
    adab0 = k.dram("adab0", [128, 8], F32, kind="ExternalInput")
    adaw1 = k.dram("adaw1", [1024, 3072], F32, kind="ExternalInput")
    adab1 = k.dram("adab1", [128, 24], F32, kind="ExternalInput")
    ng = k.dram("ng1", [128, 8], F32, kind="ExternalInput")
    Wo0 = k.dram("Wo0", [1024, 1024], F32, kind="ExternalInput")
    W1 = k.dram("W1", [1024, 3584], F32, kind="ExternalInput")
    Wo1 = k.dram("Wo1", [1024, 1024], F32, kind="ExternalInput")
    cw = k.dram("cw", [128, 4, 31], F32, kind="ExternalInput")
    cpar = k.dram("cpar", [128, 4, 3], F32, kind="ExternalInput")
    relb = k.dram("relb", [32, 8], F32, kind="ExternalInput")
    relbT = k.dram("relbT", [8, 32], F32, kind="ExternalInput")
    vv = k.dram("vv", [128, VV_COLS if TO == 4096 else sum(d * ((TO // d) // 128 + 1) for d in DILS)], F32, kind="ExternalInput")
    fgd = k.dram("fg", [128, 8], F32, kind="ExternalInput")
    outT = k.dram("outT", [1024, TO], F32, kind="ExternalOutput")
    X1 = k.dram("X1", [1024, TO], F32, kind=kd)
    GA = k.dram("GA", [512, E], F32, kind=kd)
    GB = k.dram("GB", [512, E], F32, kind=kd)
    QT = k.dram("QT", [512, TO], F32, kind=kd)
    KT = k.dram("KT", [512, E], F32, kind=kd)
    ZT = k.dram("ZT", [1024, TO], F32, kind=kd)
    VT = k.dram("VT", [E, 512], F32, kind=kd)
    CT = k.dram("CT", [512, TO], F32, kind=kd)
    DT = k.dram("DT", [512, TO], F32, kind=kd)
    GF = k.dram("GF", [8, 3 * 384], F32, kind=kd)

    A = k.sb("odA", [128, 8], F32)
    Bm = k.sb("odBm", [128, 8], F32)
    gate0 = k.sb("gate0", [128, 8], F32)
    gate1 = k.sb("gate1", [128, 8], F32)
    with k.scope():
        g0 = phase_mod(k, cx, cT, adaw0, adab0, 8, tag="m0")
        k.cp(gate0[:], g0[:], [g0], [gate0])
    with k.scope():
        mod1 = phase_mod(k, cx, cT, adaw1, adab1, 24, tag="m1")
        ngs = k.sb("odngs", [128, 8], F32)
        k.dma("sp", ngs[:], ng[:], [ng], [ngs], ngs)
        k.stt(A[:], mod1[:, 8:16], 1.0, ngs[:], ALU.add, ALU.mult, [mod1, ngs], [A])
        k.cp(Bm[:], mod1[:, 0:8], [mod1], [Bm])
        k.cp(gate1[:], mod1[:, 16:24], [mod1], [gate1])
    with k.scope():
        Wo0b = k.sb("pa_Wo0b", [128, 8, 1024], BF16)
        load_w_bf16(k, Wo0b, Wo0, 1024, "pa0")
        Mt = k.sb("pa_Mt", [128, 8, 512], F32)
        Mb = k.sb("pa_Mb", [128, 8, 512], BF16)
        x03 = x0T[:].rearrange("(c p) t -> p c t", p=128)
        if GM is None:
            m3 = mixT[:].rearrange("(c p) t -> p c t", p=128)
        else:
            g3 = [g[:].rearrange("(c p) t -> p c t", p=128) for g in GM]
            selw = k.sb("pa_selw", [128, 2], F32)
            k.dma("sp", selw[:], selw_d[:], [selw_d], [selw], selw)
            Mt2 = k.sb("pa_Mt2", [128, 8, 512], F32)
        x13 = X1[:].rearrange("(c p) t -> p c t", p=128)
        own_lo, own_hi = HALO // 512, (HALO + TO) // 512

        def producer(te, Xt):
            sl = slice(te * 512, (te + 1) * 512)
            k.dma("sp", Xt[:], x03[:, :, sl], [x0T], [Xt], Xt)
            if GM is None:
                k.dma("sp", Mt[:], m3[:, :, sl], [mixT], [Mt], Mt)
                k.cp(Mb[:, 0:4, :], Mt[:, 0:4, :], [Mt], [Mb], eng="act")
                k.cp(Mb[:, 4:8, :], Mt[:, 4:8, :], [Mt], [Mb], eng="dve")
            else:
                g0 = te * 512 - HALO
                g1 = te * 512 - HALO + TO
                ok0 = g0 >= 0
                ok1 = g1 + 512 <= 2 * TO
                if ok0:
                    k.dma("sp", Mt[:], g3[g0 // 1024][:, :, g0 % 1024:g0 % 1024 + 512], [GM[g0 // 1024]], [Mt], Mt)
                if ok1:
                    k.dma("sp", Mt2[:], g3[g1 // 1024][:, :, g1 % 1024:g1 % 1024 + 512], [GM[g1 // 1024]], [Mt2], Mt2)
                if ok0 and ok1:
                    k.ts(Mt[:], Mt[:], selw[:, 0:1], ALU.mult, [Mt, selw], [Mt])
                    k.stt(Mb[:], Mt2[:], selw[:, 1:2], Mt[:], ALU.mult, ALU.add, [Mt2, selw, Mt], [Mb])
                elif ok0:
                    k.ts(Mb[:], Mt[:], selw[:, 0:1], ALU.mult, [Mt, selw], [Mb])
                else:
                    k.ts(Mb[:], Mt2[:], selw[:, 1:2], ALU.mult, [Mt2, selw], [Mb])
            for fc in range(8):
                pb = cx.psum[6 + fc % 2] if False else cx.psum[6]
                for kc in range(8):
                    k.mm(pb[:, :], Wo0b[:, kc, fc * 128:(fc + 1) * 128], Mb[:, kc, :], [Wo0b, Mb], [pb], start=(kc == 0), stop=(kc == 7))
                k.stt(Xt[:, fc, :], pb[:, :], gate0[:, fc:fc + 1], Xt[:, fc, :], ALU.mult, ALU.add, [pb, gate0, Xt], [Xt])
            if own_lo <= te < own_hi:
                k.dma("sp", x13[:, :, (te - own_lo) * 512:(te - own_lo + 1) * 512], Xt[:], [Xt], [X1], Xt)

        def rng_fn(D, r0, nr, lo, hi, off):
            def f(te):
                if not (lo <= te < hi):
                    return None
                return D[r0:r0 + nr, (te - off) * 512:(te - off + 1) * 512]
            return f
        nE = E // 512
        fm_specs = []
        for ch in range(4):
            fm_specs.append((0 + 128 * ch, 128, GA, rng_fn(GA, 128 * ch, 128, own_lo - 1, own_hi + 1, 0), 1.0))
            fm_specs.append((512 + 128 * ch, 128, GB, rng_fn(GB, 128 * ch, 128, own_lo - 1, own_hi + 1, 0), 1.0))
            fm_specs.append((1024 + 128 * ch, 128, QT, rng_fn(QT, 128 * ch, 128, own_lo, own_hi, own_lo), 1.0))
            fm_specs.append((1536 + 128 * ch, 128, KT, rng_fn(KT, 128 * ch, 128, 0, nE, 0), 1.0))
        for ch in range(8):
            fm_specs.append((2560 + 128 * ch, 128, ZT, rng_fn(ZT, 128 * ch, 128, own_lo, own_hi, own_lo), 1.0))
        tm_specs = [(2048, 512, VT, (lambda te, ts: VT[te * 512 + ts * 128:te * 512 + (ts + 1) * 128, :]))]
        phase_norm_proj(k, cx, E, None, None, A, Bm, W1, 3584, fm_specs, tm_specs, tag="pa", producer=producer)
    if "conv" in phases:
        with k.scope():
            phase_conv_module(k, cx, TO, GA, GB, valid, cw, cpar, CT)
    if "att" in phases:
        with k.scope():
            phase_attention(k, cx, TO, QT, KT, VT, vv, relb, relbT, GF, DT, c2)
    if "fin" in phases:
        with k.scope():
            phase_final(k, cx, TO, CT, DT, ZT, X1, Wo1, gate1, fgd, outT)


def build_fused(T=SEQ):
    TO = T // 2
    nc = bass.Bass("TRN2", target_bir_lowering=False)
    k = KB(nc)
    cx = Ctx()
    setup_common(k, cx)
    bounce = [k.dram(f"mixT_own{i}", [512, 1024], F32) for i in range(T // 1024)]
    GM = [k.dram(f"mixT_all{i}", [1024, 1024], F32) for i in range(T // 1024)]
    emit_even(k, cx, T, "Internal", MIXT=bounce)
    for i in range(T // 1024):
        k.allgather(bounce[i], GM[i], [[0, 1], [2, 3], [4, 5], [6, 7]])
    emit_odd(k, cx, TO, "Internal", GM=GM)
    k.finish()
    k.close()
    print("fused program: instructions", k.ninst, "sems", k.nsem)
    return nc


def fused_core_inputs(inp, b, e, T=SEQ):
    TO = T // 2
    d = even_core_inputs(inp, b, e, T)
    o = odd_core_inputs_nomix(inp, b, e, TO)
    d.update(o)
    perm = np.concatenate([np.concatenate([np.arange(256 * ee, 256 * ee + 256), 512 + np.arange(256 * ee, 256 * ee + 256)]) for ee in range(2)])
    d["Wo0"] = np.ascontiguousarray(inp["ev_w_out"][0][perm, :])
    sel = np.zeros((128, 2), np.float32)
    sel[:, e] = 1.0
    d["selw"] = sel
    return d


def run_fused(inp, T=SEQ):
    TO = T // 2
    nc = build_fused(T)
    in_maps = [fused_core_inputs(inp, c // 2, c % 2, T) for c in range(8)]
    res = run_bass_kernel_spmd(nc, in_maps, core_ids=list(range(8)))
    out = np.zeros((4, T, 1024), np.float32)
    for c in range(8):
        b, sh = c // 2, c % 2
        out[b, sh * TO:(sh + 1) * TO, :] = res.results[c]["outT"].T
    return out


def odd_core_inputs_nomix(inp, b, sh, TO=4096, nhalves=2):
    d = odd_core_inputs(inp, None, b, sh, TO, nhalves)
    del d["mixT"]
    return d


def odd_core_inputs(inp, mix, b, sh, TO=4096, nhalves=2):
    S = TO * nhalves
    E = TO + 2 * HALO
    lo = sh * TO - HALO
    x0T = np.zeros((1024, E), np.float32)
    mT = np.zeros((1024, E), np.float32)
    valid = np.zeros((1, E), np.float32)
    a, bnd = max(lo, 0), min(lo + E, S)
    x0T[:, a - lo:bnd - lo] = inp["x"][b, a:bnd].T
    if mix is not None:
        mT[:, a - lo:bnd - lo] = mix[b, a:bnd].T
    valid[0, a - lo:bnd - lo] = 1.0
    cpar = np.stack([inp["od_dw_b"][0], inp["od_ln_g"][0], inp["od_ln_b"][0]], -1)
    return {
        "consts": make_consts(), "consts2": make_consts2(),
        "x0T": x0T, "mixT": mT, "valid": valid,
        "cT1": colform(inp["c"][b], 8),
        "adaw0": np.ascontiguousarray(inp["ada_w"][0][:, 2048:3072]),
        "adab0": colform(inp["ada_b"][0][2048:3072], 8),
        "adaw1": np.ascontiguousarray(inp["ada_w"][1]),
        "adab1": colform(inp["ada_b"][1], 24),
        "ng1": colform(inp["norm_g"][1], 8),
        "Wo0": np.ascontiguousarray(inp["ev_w_out"][0]),
        "W1": np.ascontiguousarray(inp["od_w_in"][0]),
        "Wo1": np.ascontiguousarray(inp["od_w_out"][0]),
        "cw": np.ascontiguousarray(inp["od_dw_w"][0].reshape(31, 4, 128).transpose(2, 1, 0)),
        "cpar": np.ascontiguousarray(cpar.reshape(4, 128, 3).transpose(1, 0, 2)),
        "relb": np.ascontiguousarray(inp["rel_bias"]),
        "relbT": np.ascontiguousarray(inp["rel_bias"].T),
        "vv": make_vv(TO, sh, nhalves),
        "fg": colform(inp["final_g"], 8),
    }


def run_odd(inp, mix, TO=4096):
    nc = build_odd(TO)
    in_maps = [odd_core_inputs(inp, mix, c // 2, c % 2, TO, 2) for c in range(8)]
    res = run_bass_kernel_spmd(nc, in_maps, core_ids=list(range(8)))
    out = np.zeros((4, 2 * TO, 1024), np.float32)
    for c in range(8):
        b, sh = c // 2, c % 2
        out[b, sh * TO:(sh + 1) * TO, :] = res.results[c]["outT"].T
    return out


def kernel(**inputs):
    inp = {k: np.asarray(v) for k, v in inputs.items()}
    return run_fused(inp)
```

```python
import contextlib
import numpy as np
import concourse.bass as bass
import concourse.mybir as mybir
from concourse.bass_utils import run_bass_kernel_spmd

F32 = mybir.dt.float32
BF16 = mybir.dt.bfloat16
ALU = mybir.AluOpType
AF = mybir.ActivationFunctionType
AX = mybir.AxisListType

SEM_LIMIT = 8000
FP32R = False
EPS = 1e-6
NEG = -1e30


class Buf:
    __slots__ = ("t", "name", "writer", "readers", "dsem", "dtot", "depth")

    def __init__(self, t, name):
        self.t = t
        self.name = name
        self.writer = None
        self.readers = []
        self.dsem = None
        self.dtot = 0

    def __getitem__(self, idx):
        return self.t[idx]


class KB:
    def __init__(self, nc, same_engine_sync=True):
        self.nc = nc
        self.es = contextlib.ExitStack()
        self.root_es = self.es
        self.depth = 0
        self.engs = {"pe": nc.tensor, "dve": nc.vector, "act": nc.scalar, "pool": nc.gpsimd, "sp": nc.sync}
        self.csem = {}
        self.ccnt = {}
        self.seen = {k: {} for k in self.engs}
        self.same = same_engine_sync
        self.pool_used = False
        self.free_dsems = []
        self.nsem = 0
        self.ninst = 0
        self.bufs = []

    def sem(self, name, root=False):
        self.nsem += 1
        es = self.root_es if root else self.es
        return es.enter_context(self.nc.semaphore(f"{name}_{self.nsem}"))

    def sb(self, name, shape, dt=F32):
        t = self.es.enter_context(self.nc.sbuf_tensor("s_" + name, list(shape), dt))
        b = Buf(t, name)
        b.depth = self.depth
        self.bufs.append(b)
        return b

    def ps(self, name, shape, dt=F32):
        t = self.es.enter_context(self.nc.psum_tensor("p_" + name, list(shape), dt))
        b = Buf(t, name)
        b.depth = self.depth
        self.bufs.append(b)
        return b

    def dram(self, name, shape, dt=F32, kind="Internal"):
        t = self.nc.dram_tensor(name, list(shape), dt, kind=kind)
        b = Buf(t.ap(), name)
        b.depth = 0
        self.bufs.append(b)
        return b

    def _wait(self, ek, dep):
        sem, val, dek = dep
        if dek == ek and not self.same:
            return
        sid = id(sem)
        if self.seen[ek].get(sid, 0) >= val:
            return
        self.engs[ek].wait_ge(sem, val)
        self.seen[ek][sid] = val

    def _deps(self, ek, reads, writes, skip_same_w=False):
        for b in reads:
            if b.writer is not None:
                self._wait(ek, b.writer)
        for b in writes:
            if b.writer is not None:
                if not (skip_same_w and b.writer[2] == ek):
                    self._wait(ek, b.writer)
            for r in b.readers:
                self._wait(ek, r)

    def _tick(self, ek):
        if ek not in self.csem or self.ccnt[ek] >= SEM_LIMIT:
            self.csem[ek] = self.sem("c" + ek, root=True)
            self.ccnt[ek] = 0
        self.ccnt[ek] += 1
        return (self.csem[ek], self.ccnt[ek], ek)

    def _mark(self, tok, reads, writes):
        for b in reads:
            b.readers.append(tok)
            if len(b.readers) > 16:
                last = {}
                for r in b.readers:
                    k = id(r[0])
                    if k not in last or last[k][1] < r[1]:
                        last[k] = r
                b.readers = list(last.values())
        for b in writes:
            b.writer = tok
            b.readers = []

    def op(self, ek, fn, reads, writes, acc=False):
        if ek == "pool":
            self.pool_used = True
        self._deps(ek, reads, writes, skip_same_w=acc)
        tok = self._tick(ek)
        ins = fn(self.engs[ek])
        ins.then_inc(tok[0], 1)
        self._mark(tok, reads, writes)
        self.ninst += 1
        return ins

    def dma(self, qk, out_ap, in_ap, reads, writes, sb, grouped=False, **kw):
        if sb.dsem is None:
            sb.dsem, sb.dtot = self._alloc_dsem()
        for b in reads:
            if b.writer is not None:
                self._wait(qk, b.writer)
        for b in writes:
            if b.writer is not None:
                if not (grouped and b.writer[0] is sb.dsem):
                    self._wait(qk, b.writer)
            for r in b.readers:
                self._wait(qk, r)
        if not grouped and sb.dtot > 0:
            self._wait(qk, (sb.dsem, sb.dtot, "dma"))
        if sb.dtot + 16 > SEM_LIMIT:
            self._wait(qk, (sb.dsem, sb.dtot, "dma"))
            sb.dsem, sb.dtot = self._alloc_dsem()
        sb.dtot += 16
        tok = (sb.dsem, sb.dtot, "dma")
        ins = self.engs[qk].dma_start(out=out_ap, in_=in_ap, **kw)
        ins.then_inc(sb.dsem, 16)
        self._mark(tok, reads, writes)
        self.ninst += 1
        return ins


    def allgather(self, in_buf, out_buf, groups):
        self.pool_used = True
        self._deps("pool", [in_buf], [out_buf])
        sem = self.sem("cc", root=True)
        ins = self.nc.gpsimd.collective_compute("AllGather", ALU.bypass, replica_groups=groups,
                                                ins=[in_buf[:].opt()], outs=[out_buf[:].opt()])
        ins.then_inc(sem, 1)
        self._mark((sem, 1, "cc"), [in_buf], [out_buf])
        self.ninst += 1

    def barrier(self):
        toks = [(self.csem[e], self.ccnt[e], e) for e in self.csem]
        for b in self.bufs:
            if b.dsem is not None and b.dtot > 0:
                toks.append((b.dsem, b.dtot, "dma"))
        for b in self.bufs:
            for t in ([b.writer] if b.writer is not None else []) + list(b.readers):
                if t[2] == "cc":
                    toks.append(t)
        same = self.same
        self.same = True
        for e in list(self.engs.keys()):
            if e == "pool" and "pool" not in self.csem and not self.pool_used:
                continue
            for t in toks:
                self._wait(e, t)
        self.same = same
        for b in self.bufs:
            b.writer = None
            b.readers = []

    @contextlib.contextmanager
    def scope(self):
        old = self.es
        self.es = contextlib.ExitStack()
        self.depth += 1
        nb = len(self.bufs)
        try:
            yield
        finally:
            self.barrier()
            dead = self.bufs[nb:]
            for b in dead:
                if b.depth >= self.depth and b.dsem is not None:
                    if b.dtot + 16 * 64 < SEM_LIMIT:
                        self.free_dsems.append((b.dsem, b.dtot))
                    b.dsem = None
                    b.dtot = 0
            self.bufs = self.bufs[:nb] + [b for b in dead if b.depth < self.depth]
            self.es.close()
            self.es = old
            self.depth -= 1

    def ts(self, out, in0, s1, op0, R, W, s2=None, op1=None, eng="dve"):
        if op1 is None:
            return self.op(eng, lambda e: e.tensor_scalar(out=out, in0=in0, scalar1=s1, scalar2=None, op0=op0), R, W)
        return self.op(eng, lambda e: e.tensor_scalar(out=out, in0=in0, scalar1=s1, scalar2=s2, op0=op0, op1=op1), R, W)

    def tt(self, out, a, b, op, R, W, eng="dve"):
        return self.op(eng, lambda e: e.tensor_tensor(out=out, in0=a, in1=b, op=op), R, W)

    def stt(self, out, in0, scalar, in1, op0, op1, R, W):
        return self.op("dve", lambda e: e.scalar_tensor_tensor(out=out, in0=in0, scalar=scalar, in1=in1, op0=op0, op1=op1), R, W)

    def act(self, out, in_, func, R, W, scale=None, bias=None):
        kw = {}
        if scale is not None:
            kw["scale"] = scale
        if bias is not None:
            kw["bias"] = bias
        return self.op("act", lambda e: e.activation(out=out, in_=in_, func=func, **kw), R, W)

    def mm(self, out, lhsT, rhs, R, W, start=True, stop=True):
        if FP32R and lhsT.dtype == F32 and rhs.dtype == F32:
            lhsT = lhsT.bitcast(mybir.dt.float32r)
            rhs = rhs.bitcast(mybir.dt.float32r)
        return self.op("pe", lambda e: e.matmul(out, lhsT=lhsT, rhs=rhs, start=start, stop=stop), R, W, acc=True)

    def cp(self, out, in_, R, W, eng="dve"):
        if eng == "act":
            return self.op("act", lambda e: e.activation(out=out, in_=in_, func=AF.Copy), R, W)
        return self.op(eng, lambda e: e.tensor_copy(out=out, in_=in_), R, W)

    def scan(self, out, d0, d1, init, op0, op1, R, W):
        return self.op("dve", lambda e: e.tensor_tensor_scan(out=out, data0=d0, data1=d1, initial=init, op0=op0, op1=op1), R, W)

    def _alloc_dsem(self):
        if self.free_dsems:
            self.free_dsems.sort(key=lambda t: t[1])
            return self.free_dsems.pop(0)
        return (self.sem("dq", root=True), 0)

    def finish(self, ek="sp"):
        for b in self.bufs:
            if b.dsem is not None and b.dtot > 0:
                self._wait(ek, (b.dsem, b.dtot, "dma"))

    def close(self):
        self.es.close()


class Ctx:
    pass


DBG = {}


def dbg(k, name, buf, shape):
    if not DBG.get("on"):
        return
    d = k.dram("dbg_" + name, list(shape), F32, kind="ExternalOutput")
    k.dma("sp", d[:], buf[:], [buf], [d], buf)


def dbgap(k, name, buf, ap, shape):
    if not DBG.get("on"):
        return
    d = k.dram("dbg_" + name, list(shape), F32, kind="ExternalOutput")
    k.dma("sp", d[:], ap, [buf], [d], buf)


def fv(buf, p0, npart, off, dims):
    a = buf.t[:]
    pstep = a.ap[0][0]
    return bass.AP(a.tensor, a.offset + p0 * pstep + off, [[pstep, npart]] + [[st, ct] for (st, ct) in dims])


def setup_common(k, cx):
    cx.psum = [k.ps(f"psb{i}", [128, 512], F32) for i in range(8)]
    cx.consts_d = k.dram("consts", [128, CONST_COLS], F32, kind="ExternalInput")
    cx.consts = k.sb("consts_sb", [128, CONST_COLS], F32)
    k.dma("sp", cx.consts[:], cx.consts_d[:], [cx.consts_d], [cx.consts], cx.consts)
    cx.ones_bf = k.sb("ones_bf", [128, 128], BF16)
    k.op("dve", lambda e: e.memset(cx.ones_bf[:], 1.0), [], [cx.ones_bf])


C_IDENT = 0
C_ONES = 128
C_RST0 = 256
C_RSTN = 768
C_MASK = 1280
M_LE, M_GE, M_LT, M_GT = 0, 1, 2, 3
C_SEL = 3328
CONST_COLS = 3584


def make_consts():
    c = np.zeros((128, CONST_COLS), np.float32)
    c[:, C_IDENT:C_IDENT + 128] = np.eye(128, dtype=np.float32)
    c[:, C_ONES:C_ONES + 128] = 1.0
    col = np.arange(512)
    c[:, C_RST0:C_RST0 + 512] = np.where(col % 64 == 0, 0.0, 1.0)[None, :]
    c[:, C_RSTN:C_RSTN + 512] = np.where(col % 64 == 0, NEG, 0.0)[None, :]
    jj = np.arange(64)[:, None]
    ii = np.arange(64)[None, :]
    for m, ok in enumerate([jj <= ii, jj >= ii, jj < ii, jj > ii]):
        c[:64, C_MASK + m * 512:C_MASK + (m + 1) * 512] = np.tile(np.where(ok, 0.0, NEG), (1, 8))
    for hh in range(2):
        c[hh, C_SEL + hh * 128:C_SEL + (hh + 1) * 128] = 1.0
    return c


def phase_mod(k, cx, cT_d, adaw_d, adab_d, ncol_chunks, tag="m0"):
    cs = k.sb(tag + "cs", [128, 8], F32)
    k.dma("sp", cs[:], cT_d[:], [cT_d], [cs], cs)
    k.op("act", lambda e: e.activation(out=cs[:], in_=cs[:], func=AF.Silu), [cs], [cs])
    ncols = ncol_chunks * 128
    wbuf = [k.sb(tag + f"adaw{i}", [128, ncols], F32) for i in range(2)]
    pm = cx.psum[0]
    for kc in range(8):
        wb = wbuf[kc % 2]
        k.dma("sp", wb[:], adaw_d[kc * 128:(kc + 1) * 128, :], [adaw_d], [wb], wb)
        for cc in range(ncol_chunks):
            k.op("pe", lambda e, wb=wb, cc=cc, kc=kc: e.matmul(
                pm[:, cc * 8 + kc:cc * 8 + kc + 1], lhsT=wb[:, cc * 128:(cc + 1) * 128], rhs=cs[:, kc:kc + 1],
                start=True, stop=True), [wb, cs], [pm], acc=True)
    mod = k.sb(tag + "mod", [128, ncol_chunks], F32)
    k.op("dve", lambda e: e.tensor_reduce(
        out=mod[:], in_=pm[:, 0:ncol_chunks * 8].rearrange("p (c k) -> p c k", k=8), axis=AX.X, op=ALU.add),
        [pm], [mod])
    ab = k.sb(tag + "adab", [128, ncol_chunks], F32)
    k.dma("sp", ab[:], adab_d[:], [adab_d], [ab], ab)
    k.op("dve", lambda e: e.tensor_tensor(out=mod[:], in0=mod[:], in1=ab[:], op=ALU.add), [mod, ab], [mod])
    return mod


def load_w_bf16(k, Wb, W_d, ncols, tag):
    with k.scope():
        wst = [k.sb(tag + f"wst{i}", [128, 1024], F32) for i in range(2)]
        n = 0
        for kc in range(8):
            for c0 in range(0, ncols, 1024):
                w = min(1024, ncols - c0)
                ws = wst[n % 2]
                k.dma("sp", ws[:, 0:w], W_d[kc * 128:(kc + 1) * 128, c0:c0 + w], [W_d], [ws], ws)
                k.cp(Wb[:, kc, c0:c0 + w], ws[:, 0:w], [ws], [Wb], eng=("act" if n % 2 else "dve"))
                n += 1


def phase_norm_proj(k, cx, T, xT_ap_fn, xT_bufs, A, B, W_d, ncols, fm_specs, tm_specs, tag="p1", producer=None):
    Wb = k.sb(tag + "Wb", [128, 8, ncols], BF16)
    load_w_bf16(k, Wb, W_d, ncols, tag)
    X = [k.sb(tag + f"X{i}", [128, 8, 512], F32) for i in range(2)]
    sq = k.sb(tag + "sq", [128, 8, 512], BF16)
    rstd = k.sb(tag + "rstd", [128, 512], F32)
    tmp = [k.sb(tag + f"tmp{i}", [128, 512], F32) for i in range(2)]
    hT = k.sb(tag + "hT", [128, 8, 512], BF16)
    stg = [k.sb(tag + f"stg{i}", [128, 512], F32) for i in range(4)]
    nst = 0
    npb = 0
    ntiles = T // 512
    for tt in range(ntiles):
        Xt = X[tt % 2]
        if producer is None:
            if tt == 0:
                k.dma("sp", Xt[:], xT_ap_fn(tt), xT_bufs, [Xt], Xt)
            if tt + 1 < ntiles:
                k.dma("sp", X[(tt + 1) % 2][:], xT_ap_fn(tt + 1), xT_bufs, [X[(tt + 1) % 2]], X[(tt + 1) % 2])
        else:
            if tt == 0:
                producer[0](tt, Xt)
            post = producer[1](tt, Xt)
            if tt + 1 < ntiles:
                producer[0](tt + 1, X[(tt + 1) % 2])
            post()
        k.op("act", lambda e, Xt=Xt: e.activation(out=sq[:], in_=Xt[:], func=AF.Square), [Xt], [sq])
        pss = cx.psum[7]
        for kc in range(8):
            k.op("pe", lambda e, kc=kc: e.matmul(pss[:], lhsT=cx.ones_bf[:], rhs=sq[:, kc, :],
                                                 start=(kc == 0), stop=(kc == 7)), [cx.ones_bf, sq], [pss], acc=True)
        k.op("act", lambda e: e.activation(out=rstd[:], in_=pss[:], func=AF.Sqrt, scale=1.0 / 1024.0, bias=float(EPS)),
             [pss], [rstd])
        k.op("dve", lambda e: e.reciprocal(out=rstd[:], in_=rstd[:]), [rstd], [rstd])
        for kc in range(8):
            tb = tmp[kc % 2]
            k.op("dve", lambda e, kc=kc, tb=tb, Xt=Xt: e.tensor_tensor(out=tb[:], in0=Xt[:, kc, :], in1=rstd[:], op=ALU.mult),
                 [Xt, rstd], [tb])
            k.op("act", lambda e, kc=kc, tb=tb: e.activation(out=hT[:, kc, :], in_=tb[:], func=AF.Identity,
                                                             scale=A[:, kc:kc + 1], bias=B[:, kc:kc + 1]),
                 [tb, A, B], [hT])
        for (c0, nr, dbuf, dfn, scale) in fm_specs:
            if dfn(tt) is None:
                continue
            pb = cx.psum[npb % 6]
            npb += 1
            for kc in range(8):
                k.op("pe", lambda e, kc=kc, pb=pb, c0=c0, nr=nr: e.matmul(
                    pb[0:nr, :], lhsT=Wb[:, kc, c0:c0 + nr], rhs=hT[:, kc, :], start=(kc == 0), stop=(kc == 7)),
                    [Wb, hT], [pb], acc=True)
            sg = stg[nst % 4]
            nst += 1
            if nst % 2:
                k.op("act", lambda e, pb=pb, sg=sg, nr=nr, scale=scale: e.activation(
                    out=sg[0:nr, :], in_=pb[0:nr, :], func=AF.Copy, scale=float(scale)), [pb], [sg])
            else:
                k.op("dve", lambda e, pb=pb, sg=sg, nr=nr, scale=scale: e.tensor_scalar(
                    out=sg[0:nr, :], in0=pb[0:nr, :], scalar1=float(scale), scalar2=None, op0=ALU.mult), [pb], [sg])
            k.dma("sp", dfn(tt), sg[0:nr, :], [sg], [dbuf], sg)
        for ts in range(4):
            for (c0, ncl, dbuf, dfn) in tm_specs:
                if dfn(tt, ts) is None:
                    continue
                pb = cx.psum[npb % 6]
                npb += 1
                for kc in range(8):
                    k.op("pe", lambda e, kc=kc, pb=pb, c0=c0, ncl=ncl, ts=ts: e.matmul(
                        pb[:, 0:ncl], lhsT=hT[:, kc, ts * 128:(ts + 1) * 128], rhs=Wb[:, kc, c0:c0 + ncl],
                        start=(kc == 0), stop=(kc == 7)), [Wb, hT], [pb], acc=True)
                sg = stg[nst % 4]
                nst += 1
                if nst % 2:
                    k.op("act", lambda e, pb=pb, sg=sg, ncl=ncl: e.activation(
                        out=sg[:, 0:ncl], in_=pb[:, 0:ncl], func=AF.Copy), [pb], [sg])
                else:
                    k.op("dve", lambda e, pb=pb, sg=sg, ncl=ncl: e.tensor_copy(out=sg[:, 0:ncl], in_=pb[:, 0:ncl]), [pb], [sg])
                k.dma("sp", dfn(tt, ts), sg[:, 0:ncl], [sg], [dbuf], sg)


FM_MQ, FM_MK, FM_G, FM_DQ, FM_DK, FM_DV = 0, 128, 256, 272, 528, 784
FM_ROWS = 1040
TM0 = FM_ROWS
TM_MK, TM_MV, TM_MO, TM_Z = TM0, TM0 + 128, TM0 + 384, TM0 + 640
NC1 = TM0 + 1152


def row_softplus_neg(k, out, x, tmp1, tmp2, n, R):
    k.stt(tmp1[0:n, :], x[0:n, :], -1.0, x[0:n, :], ALU.mult, ALU.max, [x], [tmp1])
    k.act(tmp1[0:n, :], tmp1[0:n, :], AF.Exp, [tmp1], [tmp1], scale=-1.0)
    k.act(tmp1[0:n, :], tmp1[0:n, :], AF.Ln, [tmp1], [tmp1], bias=1.0)
    k.stt(out[0:n, :], x[0:n, :], 0.0, tmp1[0:n, :], ALU.min, ALU.subtract, [x, tmp1], [out])


def dirview(buf, n, d, G=512):
    if d == 0:
        return fv(buf, 0, n, 0, [(1, G)])
    return fv(buf, 0, n, G - 1, [(-1, G)])


def phase_mlstm(k, cx, T, FM, TM, gb_d, HS):
    NG = T // 512
    cs = cx.consts
    ident = cs
    st = []
    for d in range(2):
        s = Ctx()
        s.gi = k.sb(f"ml_gi{d}", [2, 512], F32)
        s.gf = k.sb(f"ml_gf{d}", [2, 512], F32)
        s.t1 = k.sb(f"ml_t1{d}", [2, 512], F32)
        s.t2 = k.sb(f"ml_t2{d}", [2, 512], F32)
        s.b = k.sb(f"ml_b{d}", [2, 512], F32)
        s.g = k.sb(f"ml_g{d}", [2, 512], F32)
        s.pm = k.sb(f"ml_pm{d}", [2, 512], F32)
        s.negmu = k.sb(f"ml_negmu{d}", [2, 512], F32)
        s.wint = k.sb(f"ml_wint{d}", [2, 512], F32)
        s.emt = k.sb(f"ml_emt{d}", [2, 512], F32)
        s.kwf = k.sb(f"ml_kwf{d}", [2, 512], F32)
        s.mnext = k.sb(f"ml_mnext{d}", [2, 8], F32)
        s.mcur = k.sb(f"ml_mcur{d}", [2, 8], F32)
        s.c8 = k.sb(f"ml_c8{d}", [2, 8], F32)
        s.wold = k.sb(f"ml_wold{d}", [2, 8], F32)
        s.carry = k.sb(f"ml_carry{d}", [2, 1], F32)
        s.bi = k.sb(f"ml_bi{d}", [2, 1], F32)
        s.bf = k.sb(f"ml_bf{d}", [2, 1], F32)
        k.dma("sp", s.bi[:], gb_d[2 * d:2 * d + 2, :], [gb_d], [s.bi], s.bi)
        k.dma("sp", s.bf[:], gb_d[4 + 2 * d:4 + 2 * d + 2, :], [gb_d], [s.bf], s.bf)
        k.op("dve", lambda e, s=s: e.memset(s.carry[:], 0.0), [], [s.carry])
        s.cols = k.sb(f"ml_cols{d}", [64, 64], F32)
        s.woldc = k.sb(f"ml_woldc{d}", [64, 16], F32)
        s.QT = [k.sb(f"ml_QT{d}{h}", [64, 512], F32) for h in range(2)]
        s.KT = [k.sb(f"ml_KT{d}{h}", [64, 512], F32) for h in range(2)]
        s.Ktm = k.sb(f"ml_Ktm{d}", [64, 8, 128], F32)
        s.Va = [k.sb(f"ml_Va{d}{h}", [64, 8, 129], F32) for h in range(2)]
        for h in range(2):
            k.op("dve", lambda e, s=s, h=h: e.memset(s.Va[h][:], 1.0), [], [s.Va[h]])
        s.E = [k.sb(f"ml_E{d}{h}", [64, 512], F32) for h in range(2)]
        s.AT = [k.sb(f"ml_AT{d}{h}", [64, 512], F32) for h in range(2)]
        s.C = [k.sb(f"ml_C{d}{h}", [64, 129], F32) for h in range(2)]
        for h in range(2):
            k.op("dve", lambda e, s=s, h=h: e.memset(s.C[h][:], 0.0), [], [s.C[h]])
        s.tmp = [k.sb(f"ml_tmp{d}{h}", [64, 129], F32) for h in range(2)]
        s.R = [k.sb(f"ml_R{d}{h}", [64, 129], F32) for h in range(2)]
        s.dn = [k.sb(f"ml_dn{d}{h}", [64, 2], F32) for h in range(2)]
        s.KW = [k.sb(f"ml_KW{d}{h}", [64, 64], F32) for h in range(2)]
        s.Hg = k.sb(f"ml_Hg{d}", [64, 8, 256], F32)
        st.append(s)
    TMr = TM[:].rearrange("(c p) f -> p c f", p=64)
    def body(step, d):
        s = st[d]
        pb = lambda i_: cx.psum[4 * d + i_]
        grp = step if d == 0 else NG - 1 - step
        t0 = grp * 512
        c0 = grp * 8
        k.dma("sp", s.gi[:], FM[FM_G + 2 * d:FM_G + 2 * d + 2, t0:t0 + 512], [FM], [s.gi], s.gi)
        k.dma("sp", s.gf[:], FM[FM_G + 4 + 2 * d:FM_G + 4 + 2 * d + 2, t0:t0 + 512], [FM], [s.gf], s.gf)
        for h in range(2):
            k.dma("sp", s.QT[h][:], FM[FM_MQ + 64 * h:FM_MQ + 64 * h + 64, t0:t0 + 512], [FM], [s.QT[h]], s.QT[h])
            k.dma("sp", s.KT[h][:], FM[FM_MK + 64 * h:FM_MK + 64 * h + 64, t0:t0 + 512], [FM], [s.KT[h]], s.KT[h])
            k.dma("sp", s.Va[h][:, :, 0:128], TMr[:, c0:c0 + 8, 128 + 128 * h:256 + 128 * h], [TM], [s.Va[h]], s.Va[h])
        k.dma("sp", s.Ktm[:], TMr[:, c0:c0 + 8, 0:128], [TM], [s.Ktm], s.Ktm)
        yield
        k.ts(s.gi[:], s.gi[:], s.bi[:, 0:1], ALU.add, [s.gi, s.bi], [s.gi])
        k.ts(s.gf[:], s.gf[:], s.bf[:, 0:1], ALU.add, [s.gf, s.bf], [s.gf])
        row_softplus_neg(k, s.t2, s.gf, s.t1, None, 2, None)
        k.scan(dirview(s.b, 2, d), cs[0:2, C_RST0:C_RST0 + 512], dirview(s.t2, 2, d), 0.0, ALU.mult, ALU.add,
               [s.t2, cs], [s.b])
        k.tt(s.g[:], s.gi[:], s.b[:], ALU.subtract, [s.gi, s.b], [s.g])
        k.scan(dirview(s.pm, 2, d), cs[0:2, C_RSTN:C_RSTN + 512], dirview(s.g, 2, d), 0.0, ALU.add, ALU.max,
               [s.g, cs], [s.pm])
        last = 63 if d == 0 else 0
        bL = fv(s.b, 0, 2, last, [(64, 8)])
        pmL = fv(s.pm, 0, 2, last, [(64, 8)])
        if d == 0:
            o8 = lambda buf: fv(buf, 0, 2, 0, [(1, 8)])
        else:
            o8 = lambda buf: fv(buf, 0, 2, 7, [(-1, 8)])
        bLd = fv(s.b, 0, 2, last, [(64, 8)]) if d == 0 else fv(s.b, 0, 2, last + 64 * 7, [(-64, 8)])
        pmLd = fv(s.pm, 0, 2, last, [(64, 8)]) if d == 0 else fv(s.pm, 0, 2, last + 64 * 7, [(-64, 8)])
        k.scan(o8(s.mnext), pmLd, bLd, s.carry[:, 0:1], ALU.max, ALU.add, [s.pm, s.b, s.carry], [s.mnext])
        if d == 0:
            k.cp(s.mcur[:, 0:1], s.carry[:, 0:1], [s.carry], [s.mcur])
            k.cp(s.mcur[:, 1:8], s.mnext[:, 0:7], [s.mnext], [s.mcur])
            k.cp(s.carry[:, 0:1], s.mnext[:, 7:8], [s.mnext, s.mcur], [s.carry])
        else:
            k.cp(s.mcur[:, 7:8], s.carry[:, 0:1], [s.carry], [s.mcur])
            k.cp(s.mcur[:, 0:7], s.mnext[:, 1:8], [s.mnext], [s.mcur])
            k.cp(s.carry[:, 0:1], s.mnext[:, 0:1], [s.mnext, s.mcur], [s.carry])
        mc_b = fv(s.mcur, 0, 2, 0, [(1, 8), (0, 64)])
        v3 = lambda buf: fv(buf, 0, 2, 0, [(64, 8), (1, 64)])
        k.tt(v3(s.t1), v3(s.pm), mc_b, ALU.max, [s.pm, s.mcur], [s.t1])
        k.ts(s.negmu[:], s.t1[:], -1.0, ALU.mult, [s.t1], [s.negmu])
        k.tt(v3(s.wint), mc_b, v3(s.t1), ALU.subtract, [s.mcur, s.t1], [s.wint])
        k.act(s.wint[:], s.wint[:], AF.Exp, [s.wint], [s.wint])
        k.tt(s.emt[:], s.b[:], s.t1[:], ALU.add, [s.b, s.t1], [s.emt])
        k.act(s.emt[:], s.emt[:], AF.Exp, [s.emt], [s.emt], scale=-1.0)
        k.tt(s.c8[:], bL, s.mnext[:], ALU.subtract, [s.b, s.mnext], [s.c8])
        k.tt(v3(s.kwf), v3(s.g), fv(s.c8, 0, 2, 0, [(1, 8), (0, 64)]), ALU.add, [s.g, s.c8], [s.kwf])
        k.act(s.kwf[:], s.kwf[:], AF.Exp, [s.kwf], [s.kwf])
        k.tt(s.wold[:], s.c8[:], s.mcur[:], ALU.add, [s.c8, s.mcur], [s.wold])
        k.act(s.wold[:], s.wold[:], AF.Exp, [s.wold], [s.wold])
        yield
        pc = pb(0)
        for c in range(8):
            for qi, qb in enumerate([s.g, s.wint, s.emt, s.kwf]):
                col = (c * 4 + qi) * 2
                k.mm(pc[0:64, col:col + 2], qb[0:2, c * 64:(c + 1) * 64], cs[0:2, C_IDENT:C_IDENT + 2], [qb, cs], [pc])
        for h in range(2):
            k.mm(pc[0:64, 64 + 8 * h:64 + 8 * h + 8], cs[0:2, C_SEL + 128 * h:C_SEL + 128 * h + 64], s.wold[0:2, :], [cs, s.wold], [pc])
        k.cp(s.cols[:], pc[0:64, 0:64], [pc], [s.cols])
        k.cp(s.woldc[:], pc[0:64, 64:80], [pc], [s.woldc], eng="act")
        yield
        mk = M_LE if d == 0 else M_GE
        for h in range(2):
            pD = pb(0)
            sel = cs[0:2, C_SEL + 128 * h:C_SEL + 128 * h + 64]
            k.mm(pD[0:64, :], sel, s.negmu[0:2, :], [cs, s.negmu], [pD], start=True, stop=False)
            k.mm(pD[0:64, :], cs[0:64, C_IDENT:C_IDENT + 64], cs[0:64, C_MASK + mk * 512:C_MASK + (mk + 1) * 512], [cs], [pD],
                 start=False, stop=False)
            for c in range(8):
                k.mm(pD[0:64, c * 64:(c + 1) * 64], s.g[0:2, c * 64:(c + 1) * 64], sel, [s.g, cs], [pD],
                     start=False, stop=(c == 7))
            k.act(s.E[h][:], pD[0:64, :], AF.Exp, [pD], [s.E[h]])
            pS = pb(1)
            for c in range(8):
                k.mm(pS[0:64, c * 64:(c + 1) * 64], s.KT[h][:, c * 64:(c + 1) * 64], s.QT[h][:, c * 64:(c + 1) * 64],
                     [s.KT[h], s.QT[h]], [pS])
            k.tt(s.AT[h][:], pS[0:64, :], s.E[h][:], ALU.mult, [pS, s.E[h]], [s.AT[h]])
            yield
        if step == 0:
            for nm in ["b", "g", "pm", "negmu", "wint", "emt", "kwf"]:
                dbg(k, f"{nm}{d}", getattr(s, nm), [2, 512])
            dbg(k, f"mnext{d}", s.mnext, [2, 8]); dbg(k, f"mcur{d}", s.mcur, [2, 8]); dbg(k, f"wold{d}", s.wold, [2, 8])
            dbg(k, f"cols{d}", s.cols, [64, 64]); dbg(k, f"woldc{d}", s.woldc, [64, 16])
            dbg(k, f"E{d}", s.E[0], [64, 512]); dbg(k, f"AT{d}", s.AT[0], [64, 512])
        for ci in range(8):
            c = ci if d == 0 else 7 - ci
            for h in range(2):
                pH = pb(2 + h)
                cl = lambda qi: s.cols[:, (c * 4 + qi) * 2 + h:(c * 4 + qi) * 2 + h + 1]
                k.mm(pH[0:64, 0:129], s.AT[h][:, c * 64:(c + 1) * 64], s.Va[h][:, c, :], [s.AT[h], s.Va[h]], [pH])
                k.mm(pH[0:64, 256:385], s.QT[h][:, c * 64:(c + 1) * 64], s.C[h][:], [s.QT[h], s.C[h]], [pH])
                k.ts(s.tmp[h][:], pH[0:64, 256:385], cl(1), ALU.mult, [pH, s.cols], [s.tmp[h]])
                k.tt(s.R[h][:], s.tmp[h][:], pH[0:64, 0:129], ALU.add, [s.tmp[h], pH], [s.R[h]])
                k.stt(s.dn[h][:, 0:1], s.R[h][:, 128:129], -1.0, s.R[h][:, 128:129], ALU.mult, ALU.max, [s.R[h]], [s.dn[h]])
                k.ts(s.dn[h][:, 0:1], s.dn[h][:, 0:1], cl(2), ALU.max, [s.dn[h], s.cols], [s.dn[h]])
                k.op("dve", lambda e, h=h: e.reciprocal(out=s.dn[h][:, 1:2], in_=s.dn[h][:, 0:1]), [s.dn[h]], [s.dn[h]])
                k.ts(s.Hg[:, c, 128 * h:128 * h + 128], s.R[h][:, 0:128], s.dn[h][:, 1:2], ALU.mult, [s.R[h], s.dn[h]], [s.Hg])
                k.ts(s.KW[h][:], s.Ktm[:, c, 64 * h:64 * h + 64], cl(3), ALU.mult, [s.Ktm, s.cols], [s.KW[h]], s2=0.125, op1=ALU.mult)
                pU = pb(1)
                k.mm(pU[0:64, 256 * h:256 * h + 129], s.KW[h][:], s.Va[h][:, c, :], [s.KW[h], s.Va[h]], [pU])
                k.stt(s.C[h][:], s.C[h][:], s.woldc[:, 8 * h + c:8 * h + c + 1], pU[0:64, 256 * h:256 * h + 129], ALU.mult, ALU.add,
                      [s.C[h], s.woldc, pU], [s.C[h]])
                yield
        k.dma("sp", HS[d][t0:t0 + 512, :].rearrange("(c p) f -> p c f", p=64), s.Hg[:], [s.Hg], [HS[d]], s.Hg)

    for step in range(NG):
        gens = [body(step, 0), body(step, 1)]
        while gens:
            for g_ in list(gens):
                try:
                    next(g_)
                except StopIteration:
                    gens.remove(g_)


def phase_gdn_pre(k, cx, T, FM, dcw_d, QN, KN, KTM, VTM):
    cs = cx.consts
    NT_ = T // 512
    dcw = k.sb("g_dcw", [128, 6, 5], F32)
    k.dma("sp", dcw[:], dcw_d[:], [dcw_d], [dcw], dcw)
    dg = k.sb("g_diag", [128, 30, 128], F32)
    for i in range(6):
        for j in range(5):
            k.ts(dg[:, i * 5 + j, :], cs[:, C_IDENT:C_IDENT + 128], dcw[:, i, j:j + 1], ALU.mult, [cs, dcw], [dg],
                 eng=("dve" if (i + j) % 2 else "pool"))
    Xs = [k.sb(f"g_X{i}", [128, 516], F32) for i in range(2)]
    Y = [k.sb(f"g_Y{i}", [128, 512], F32) for i in range(2)]
    sq = k.sb("g_sq", [128, 512], F32)
    rs = k.sb("g_rs", [128, 512], F32)
    Yn = [k.sb(f"g_Yn{i}", [128, 512], F32) for i in range(2)]
    tr = [k.sb(f"g_tr{i}", [128, 512], F32) for i in range(2)]
    def gp_load(n_):
        tt_, i_ = n_ // 6, n_ % 6
        t0_ = tt_ * 512
        X = Xs[n_ % 2]
        row0 = FM_DQ + 128 * i_
        lo = max(t0_ - 2, 0)
        hi = min(t0_ + 514, T)
        if tt_ == 0:
            k.op("dve", lambda e, X=X: e.memset(X[:, 0:2], 0.0), [], [X])
        if tt_ == NT_ - 1:
            k.op("dve", lambda e, X=X: e.memset(X[:, 514:516], 0.0), [], [X])
        k.dma("sp", X[:, lo - (t0_ - 2):hi - (t0_ - 2)], FM[row0:row0 + 128, lo:hi], [FM], [X], X)

    n = 0
    for tt in range(NT_):
        t0 = tt * 512
        for i in range(6):
            X = Xs[n % 2]
            if n == 0:
                gp_load(0)
            if n + 1 < NT_ * 6:
                gp_load(n + 1)
            pc_ = cx.psum[n % 2]
            for j in range(5):
                k.mm(pc_[:, :], dg[:, i * 5 + j, :], X[:, j:j + 512], [dg, X], [pc_], start=(j == 0), stop=(j == 4))
            Yt = Y[n % 2]
            k.act(Yt[:], pc_[:, :], AF.Silu, [pc_], [Yt])
            h = i % 2
            kind = i // 2
            if kind < 2:
                k.tt(sq[:], Yt[:], Yt[:], ALU.mult, [Yt], [sq])
                pss = cx.psum[2]
                k.mm(pss[:, :], cs[:, C_ONES:C_ONES + 128], sq[:], [cs, sq], [pss])
                k.act(rs[:], pss[:, :], AF.Sqrt, [pss], [rs], bias=float(EPS))
                k.op("dve", lambda e: e.reciprocal(out=rs[:], in_=rs[:]), [rs], [rs])
                Ynt = Yn[n % 2]
                k.stt(Ynt[:], Yt[:], (128.0 ** -0.5) if kind == 0 else 1.0, rs[:], ALU.mult, ALU.mult, [Yt, rs], [Ynt])
                dst = QN[h] if kind == 0 else KN[h]
                k.dma("sp", dst[:, t0:t0 + 512], Ynt[:], [Ynt], [dst], Ynt)
                src = Ynt
            else:
                src = Yt
            if kind >= 1:
                pt = cx.psum[3 + n % 2]
                for ts_ in range(4):
                    k.mm(pt[:, ts_ * 128:(ts_ + 1) * 128], src[:, ts_ * 128:(ts_ + 1) * 128], cs[:, C_IDENT:C_IDENT + 128], [src, cs], [pt])
                trt = tr[n % 2]
                k.cp(trt[:], pt[:, :], [pt], [trt], eng="act")
                dst = KTM[h] if kind == 1 else VTM[h]
                k.dma("sp", dst[t0:t0 + 512, :].rearrange("(s p) f -> p s f", p=128), trt[:].rearrange("p (s f) -> p s f", f=128),
                      [trt], [dst], trt)
            n += 1


def phase_gdn(k, cx, T, FM, QN, KN, KTM, VTM, gpar_d, OS):
    NG = T // 512
    cs = cx.consts
    ID64 = cs[0:64, C_IDENT:C_IDENT + 64]
    st = []
    for d in range(2):
        s = Ctx()
        for nm in ["br", "ar", "t1", "t2", "beta", "gc", "ngc", "gcb", "bg", "kd", "eg"]:
            setattr(s, nm, k.sb(f"gd_{nm}{d}", [2, 512], F32))
        s.G1 = k.sb(f"gd_G1{d}", [64, 512], F32)
        s.Nm = k.sb(f"gd_N{d}", [64, 512], F32)
        s.NTm = k.sb(f"gd_NT{d}", [64, 512], F32)
        s.P2 = [k.sb(f"gd_P2{d}{i}", [64, 512], F32) for i in range(2)]
        s.PT2 = [k.sb(f"gd_PT2{d}{i}", [64, 512], F32) for i in range(2)]
        s.XT = k.sb(f"gd_XT{d}", [64, 512], F32)
        s.gamT = k.sb(f"gd_gamT{d}", [64, 512], F32)
        s.gl8 = k.sb(f"gd_gl8{d}", [2, 8], F32)
        s.egl = k.sb(f"gd_egl{d}", [2, 8], F32)
        s.par = k.sb(f"gd_par{d}", [2, 2], F32)
        k.dma("sp", s.par[:], gpar_d[2 * d:2 * d + 2, :], [gpar_d], [s.par], s.par)
        k.act(s.par[:, 1:2], s.par[:, 1:2], AF.Exp, [s.par], [s.par])
        k.ts(s.par[:, 1:2], s.par[:, 1:2], -1.0, ALU.mult, [s.par], [s.par])
        s.cols = k.sb(f"gd_cols{d}", [64, 48], F32)
        s.eglc = k.sb(f"gd_eglc{d}", [128, 16], F32)
        s.QN = [k.sb(f"gd_QN{d}{h}", [128, 512], F32) for h in range(2)]
        s.KN = [k.sb(f"gd_KN{d}{h}", [128, 512], F32) for h in range(2)]
        s.qg = [k.sb(f"gd_qg{d}{h}", [128, 512], F32) for h in range(2)]
        s.Ktm = [k.sb(f"gd_Ktm{d}{h}", [64, 8, 128], F32) for h in range(2)]
        s.Vtm = [k.sb(f"gd_Vtm{d}{h}", [64, 8, 128], F32) for h in range(2)]
        s.XTb = [k.sb(f"gd_XTb{d}{h}", [64, 512], F32) for h in range(2)]
        s.XTbg = [k.sb(f"gd_XTbg{d}{h}", [64, 512], F32) for h in range(2)]
        s.attnT = [k.sb(f"gd_attnT{d}{h}", [64, 512], F32) for h in range(2)]
        s.nwT = [k.sb(f"gd_nwT{d}{h}", [128, 512], F32) for h in range(2)]
        s.S = [k.sb(f"gd_S{d}{h}", [128, 128], F32) for h in range(2)]
        for h in range(2):
            k.op("dve", lambda e, s=s, h=h: e.memset(s.S[h][:], 0.0), [], [s.S[h]])
        s.vn = [k.sb(f"gd_vn{d}{h}", [64, 128], F32) for h in range(2)]
        s.kdm = [k.sb(f"gd_kdm{d}{h}", [64, 128], F32) for h in range(2)]
        s.Og = k.sb(f"gd_Og{d}", [64, 8, 256], F32)
        st.append(s)
    def body(step, d):
        s = st[d]
        pb = lambda i_: cx.psum[4 * d + i_]
        G1, Nm, NTm, P2, PT2, XT, gamT = s.G1, s.Nm, s.NTm, s.P2, s.PT2, s.XT, s.gamT
        grp = step if d == 0 else NG - 1 - step
        t0 = grp * 512
        c0 = grp * 8
        k.dma("sp", s.br[:], FM[FM_G + 8 + 2 * d:FM_G + 8 + 2 * d + 2, t0:t0 + 512], [FM], [s.br], s.br)
        k.dma("sp", s.ar[:], FM[FM_G + 12 + 2 * d:FM_G + 12 + 2 * d + 2, t0:t0 + 512], [FM], [s.ar], s.ar)
        for h in range(2):
            k.dma("sp", s.QN[h][:], QN[h][:, t0:t0 + 512], [QN[h]], [s.QN[h]], s.QN[h])
            k.dma("sp", s.KN[h][:], KN[h][:, t0:t0 + 512], [KN[h]], [s.KN[h]], s.KN[h])
            k.dma("sp", s.Ktm[h][:], KTM[h][t0:t0 + 512, :].rearrange("(c p) f -> p c f", p=64), [KTM[h]], [s.Ktm[h]], s.Ktm[h])
            k.dma("sp", s.Vtm[h][:], VTM[h][t0:t0 + 512, :].rearrange("(c p) f -> p c f", p=64), [VTM[h]], [s.Vtm[h]], s.Vtm[h])
        yield
        k.act(s.beta[:], s.br[:], AF.Sigmoid, [s.br], [s.beta])
        row_softplus_neg(k, s.t2, s.br, s.t1, None, 2, None)
        k.ts(s.ar[:], s.ar[:], s.par[:, 0:1], ALU.add, [s.ar, s.par], [s.ar], s2=-1.0, op1=ALU.mult)
        row_softplus_neg(k, s.eg, s.ar, s.t1, None, 2, None)
        k.ts(s.eg[:], s.eg[:], s.par[:, 1:2], ALU.mult, [s.eg, s.par], [s.eg], s2=-1.0, op1=ALU.mult)
        k.scan(dirview(s.gc, 2, d), cs[0:2, C_RST0:C_RST0 + 512], dirview(s.eg, 2, d), 0.0, ALU.mult, ALU.add, [s.eg, cs], [s.gc])
        k.ts(s.ngc[:], s.gc[:], -1.0, ALU.mult, [s.gc], [s.ngc])
        k.tt(s.gcb[:], s.gc[:], s.t2[:], ALU.add, [s.gc, s.t2], [s.gcb])
        last = 63 if d == 0 else 0
        gl = fv(s.gc, 0, 2, last, [(64, 8)])
        k.cp(s.gl8[:], gl, [s.gc], [s.gl8])
        k.act(s.egl[:], s.gl8[:], AF.Exp, [s.gl8], [s.egl])
        k.act(s.eg[:], s.gc[:], AF.Exp, [s.gc], [s.eg])
        k.tt(s.bg[:], s.beta[:], s.eg[:], ALU.mult, [s.beta, s.eg], [s.bg])
        v3 = lambda buf: fv(buf, 0, 2, 0, [(64, 8), (1, 64)])
        k.tt(v3(s.kd), fv(s.gl8, 0, 2, 0, [(1, 8), (0, 64)]), v3(s.gc), ALU.subtract, [s.gl8, s.gc], [s.kd])
        k.act(s.kd[:], s.kd[:], AF.Exp, [s.kd], [s.kd])
        yield
        pc = pb(3)
        for c in range(8):
            for qi, qb in enumerate([s.beta, s.bg, s.kd]):
                col = (c * 3 + qi) * 2
                k.mm(pc[0:64, col:col + 2], qb[0:2, c * 64:(c + 1) * 64], cs[0:2, C_IDENT:C_IDENT + 2], [qb, cs], [pc])
        for h in range(2):
            k.mm(pc[:, 64 + 8 * h:64 + 8 * h + 8], cs[0:2, C_SEL + 128 * h:C_SEL + 128 * h + 128], s.egl[0:2, :], [cs, s.egl], [pc])
        k.cp(s.cols[:], pc[0:64, 0:48], [pc], [s.cols])
        k.cp(s.eglc[:], pc[:, 64:80], [pc], [s.eglc], eng="act")
        mT = M_LE if d == 0 else M_GE
        mS = M_GT if d == 0 else M_LT
        for h in range(2):
            sel64 = cs[0:2, C_SEL + 128 * h:C_SEL + 128 * h + 64]
            sel128 = cs[0:2, C_SEL + 128 * h:C_SEL + 128 * h + 128]
            pq = pb(3)
            k.mm(pq[:, :], sel128, s.eg[0:2, :], [cs, s.eg], [pq])
            k.tt(s.qg[h][:], pq[:, :], s.QN[h][:], ALU.mult, [pq, s.QN[h]], [s.qg[h]])
            yield
            pD = pb(3)
            k.mm(pD[0:64, :], sel64, s.ngc[0:2, :], [cs, s.ngc], [pD], start=True, stop=False)
            k.mm(pD[0:64, :], ID64, cs[0:64, C_MASK + mS * 512:C_MASK + (mS + 1) * 512], [cs], [pD], start=False, stop=False)
            for c in range(8):
                k.mm(pD[0:64, c * 64:(c + 1) * 64], s.gcb[0:2, c * 64:(c + 1) * 64], sel64, [s.gcb, cs], [pD], start=False, stop=(c == 7))
            k.act(G1[:], pD[0:64, :], AF.Exp, [pD], [G1])
            pK = pb(2)
            for c in range(8):
                k.mm(pK[0:64, c * 64:(c + 1) * 64], s.KN[h][:, c * 64:(c + 1) * 64], s.KN[h][:, c * 64:(c + 1) * 64], [s.KN[h]], [pK])
            k.stt(Nm[:], pK[0:64, :], -1.0, G1[:], ALU.mult, ALU.mult, [pK, G1], [Nm])
            k.mm(pD[0:64, :], sel64, s.gc[0:2, :], [cs, s.gc], [pD], start=True, stop=False)
            k.mm(pD[0:64, :], ID64, cs[0:64, C_MASK + mT * 512:C_MASK + (mT + 1) * 512], [cs], [pD], start=False, stop=False)
            for c in range(8):
                k.mm(pD[0:64, c * 64:(c + 1) * 64], s.ngc[0:2, c * 64:(c + 1) * 64], sel64, [s.ngc, cs], [pD], start=False, stop=(c == 7))
            k.act(gamT[:], pD[0:64, :], AF.Exp, [pD], [gamT])
            for c in range(8):
                k.mm(pK[0:64, c * 64:(c + 1) * 64], s.KN[h][:, c * 64:(c + 1) * 64], s.QN[h][:, c * 64:(c + 1) * 64], [s.KN[h], s.QN[h]], [pK])
            k.tt(s.attnT[h][:], pK[0:64, :], gamT[:], ALU.mult, [pK, gamT], [s.attnT[h]])
            yield
            pA, pB, pC = pb(0), pb(1), pb(2)
            for c in range(8):
                k.mm(pA[0:64, c * 64:(c + 1) * 64], Nm[:, c * 64:(c + 1) * 64], ID64, [Nm, cs], [pA])
            k.cp(NTm[:], pA[0:64, :], [pA], [NTm], eng="act")
            k.tt(fv(XT, 0, 64, 0, [(64, 8), (1, 64)]), fv(NTm, 0, 64, 0, [(64, 8), (1, 64)]),
                 fv(cs, 0, 64, C_IDENT, [(0, 8), (1, 64)]), ALU.add, [NTm, cs], [XT])
            Pc, PTc = Nm, NTm
            for m in range(5):
                Pn, PTn = P2[m % 2], PT2[m % 2]
                for c in range(8):
                    sl = slice(c * 64, (c + 1) * 64)
                    k.mm(pA[0:64, sl], PTc[:, sl], Pc[:, sl], [PTc, Pc], [pA])
                if m < 4:
                    for c in range(8):
                        sl = slice(c * 64, (c + 1) * 64)
                        k.mm(pB[0:64, sl], Pc[:, sl], PTc[:, sl], [PTc, Pc], [pB])
                k.cp(Pn[:], pA[0:64, :], [pA], [Pn], eng="act")
                if m < 4:
                    k.cp(PTn[:], pB[0:64, :], [pB], [PTn])
                for c in range(8):
                    sl = slice(c * 64, (c + 1) * 64)
                    k.mm(pC[0:64, sl], Pn[:, sl], XT[:, sl], [Pn, XT], [pC])
                k.tt(XT[:], XT[:], pC[0:64, :], ALU.add, [XT, pC], [XT])
                Pc, PTc = Pn, PTn
                yield
            for c in range(8):
                sl = slice(c * 64, (c + 1) * 64)
                k.ts(s.XTb[h][:, sl], XT[:, sl], s.cols[:, (c * 3 + 0) * 2 + h:(c * 3 + 0) * 2 + h + 1], ALU.mult, [XT, s.cols], [s.XTb[h]])
                k.ts(s.XTbg[h][:, sl], XT[:, sl], s.cols[:, (c * 3 + 1) * 2 + h:(c * 3 + 1) * 2 + h + 1], ALU.mult, [XT, s.cols], [s.XTbg[h]], eng="pool")
            yield
            for c in range(8):
                sl = slice(c * 64, (c + 1) * 64)
                k.mm(pA[:, sl], s.Ktm[h][:, c, :], s.XTbg[h][:, sl], [s.Ktm[h], s.XTbg[h]], [pA])
            k.act(s.nwT[h][:], pA[:, :], AF.Copy, [pA], [s.nwT[h]], scale=-1.0)
        for ci in range(8):
            c = ci if d == 0 else 7 - ci
            sl = slice(c * 64, (c + 1) * 64)
            for h in range(2):
                pV = pb(0)
                pU = pb(1)
                vr = pV[0:64, 256 * h:256 * h + 128]
                orr = pV[0:64, 256 * h + 128:256 * h + 256]
                k.mm(vr, s.XTb[h][:, sl], s.Vtm[h][:, c, :], [s.XTb[h], s.Vtm[h]], [pV], start=True, stop=False)
                k.mm(vr, s.nwT[h][:, sl], s.S[h][:], [s.nwT[h], s.S[h]], [pV], start=False, stop=True)
                k.cp(s.vn[h][:], vr, [pV], [s.vn[h]])
                k.mm(orr, s.qg[h][:, sl], s.S[h][:], [s.qg[h], s.S[h]], [pV], start=True, stop=False)
                k.mm(orr, s.attnT[h][:, sl], s.vn[h][:], [s.attnT[h], s.vn[h]], [pV], start=False, stop=True)
                k.cp(s.Og[:, c, 128 * h:128 * h + 128], orr, [pV], [s.Og], eng="act")
                k.ts(s.kdm[h][:], s.Ktm[h][:, c, :], s.cols[:, (c * 3 + 2) * 2 + h:(c * 3 + 2) * 2 + h + 1], ALU.mult, [s.Ktm[h], s.cols], [s.kdm[h]], eng="pool")
                ur = pU[:, 128 * h:128 * h + 128]
                k.mm(ur, s.kdm[h][:], s.vn[h][:], [s.kdm[h], s.vn[h]], [pU])
                k.stt(s.S[h][:], s.S[h][:], s.eglc[:, 8 * h + c:8 * h + c + 1], ur, ALU.mult, ALU.add, [s.S[h], s.eglc, pU], [s.S[h]])
                yield
        k.dma("sp", OS[d][t0:t0 + 512, :].rearrange("(c p) f -> p c f", p=64), s.Og[:], [s.Og], [OS[d]], s.Og)

    for step in range(NG):
        gens = [body(step, 0), body(step, 1)]
        while gens:
            for g_ in list(gens):
                try:
                    next(g_)
                except StopIteration:
                    gens.remove(g_)


def phase_even_combine(k, cx, T, TM, HS, OS, gn_d, MIX, MIXT=None):
    gn = k.sb("cb_gn", [128, 512], F32)
    k.dma("sp", gn[:], gn_d[:].partition_broadcast(128), [gn_d], [gn], gn)
    NTl = T // 128
    bufs = []
    for i in range(2):
        b = Ctx()
        b.a = k.sb(f"cb_a{i}", [128, 512], F32)
        b.b = k.sb(f"cb_b{i}", [128, 512], F32)
        b.moz = k.sb(f"cb_moz{i}", [128, 768], F32)
        b.sq = k.sb(f"cb_sq{i}", [128, 512], F32)
        b.ss = k.sb(f"cb_ss{i}", [128, 4], F32)
        b.tr = k.sb(f"cb_tr{i}", [128, 512], F32)
        bufs.append(b)
    def cb_load(tt):
        b = bufs[tt % 2]
        r = slice(tt * 128, (tt + 1) * 128)
        k.dma("sp", b.a[:, 0:256], HS[0][r, :], [HS[0]], [b.a], b.a, grouped=True)
        k.dma("sp", b.a[:, 256:512], OS[0][r, :], [OS[0]], [b.a], b.a, grouped=True)
        k.dma("sp", b.b[:, 0:256], HS[1][r, :], [HS[1]], [b.b], b.b, grouped=True)
        k.dma("sp", b.b[:, 256:512], OS[1][r, :], [OS[1]], [b.b], b.b, grouped=True)
        k.dma("sp", b.moz[:], TM[r, 384:1152], [TM], [b.moz], b.moz)

    cb_load(0)
    for tt in range(NTl):
        b = bufs[tt % 2]
        r = slice(tt * 128, (tt + 1) * 128)
        if tt + 1 < NTl:
            cb_load(tt + 1)
        k.tt(b.a[:], b.a[:], b.b[:], ALU.add, [b.a, b.b], [b.a])
        k.tt(b.sq[:], b.a[:], b.a[:], ALU.mult, [b.a], [b.sq])
        k.op("dve", lambda e, b=b: e.tensor_reduce(out=b.ss[:], in_=b.sq[:].rearrange("p (h f) -> p h f", f=128), axis=AX.X, op=ALU.add),
             [b.sq], [b.ss])
        k.act(b.ss[:], b.ss[:], AF.Sqrt, [b.ss], [b.ss], scale=1.0 / 128.0, bias=float(EPS))
        k.op("dve", lambda e, b=b: e.reciprocal(out=b.ss[:], in_=b.ss[:]), [b.ss], [b.ss])
        k.tt(b.a[:].rearrange("p (h f) -> p h f", f=128), b.a[:].rearrange("p (h f) -> p h f", f=128),
             fv(b.ss, 0, 128, 0, [(1, 4), (0, 128)]), ALU.mult, [b.a, b.ss], [b.a])
        k.tt(b.a[:], b.a[:], gn[:], ALU.mult, [b.a, gn], [b.a])
        k.act(b.moz[:, 0:256], b.moz[:, 0:256], AF.Sigmoid, [b.moz], [b.moz])
        k.act(b.moz[:, 256:768], b.moz[:, 256:768], AF.Silu, [b.moz], [b.moz])
        k.tt(b.a[:, 0:256], b.a[:, 0:256], b.moz[:, 0:256], ALU.mult, [b.a, b.moz], [b.a])
        k.tt(b.a[:], b.a[:], b.moz[:, 256:768], ALU.mult, [b.a, b.moz], [b.a])
        if MIXT is None:
            k.dma("sp", MIX[r, :], b.a[:], [b.a], [MIX], b.a)
        else:
            pt = cx.psum[tt % 2]
            for fc in range(4):
                k.mm(pt[:, fc * 128:(fc + 1) * 128], b.a[:, fc * 128:(fc + 1) * 128], cx.consts[:, C_IDENT:C_IDENT + 128], [b.a, cx.consts], [pt])
            k.cp(b.tr[:], pt[:, :], [pt], [b.tr], eng="act")
            mxt = MIXT[(tt * 128) // 1024]
            tl = (tt * 128) % 1024
            k.dma("sp", mxt[:].rearrange("(c p) t -> p c t", p=128)[:, :, tl:tl + 128],
                  b.tr[:].rearrange("p (c t) -> p c t", t=128), [b.tr], [mxt], b.tr)


def build_even(T, debug=False, phases=("ml", "gdn", "comb")):
    nc = bass.Bass("TRN2", target_bir_lowering=False)
    k = KB(nc)
    cx = Ctx()
    setup_common(k, cx)
    kd = "ExternalOutput" if debug else "Internal"
    emit_even(k, cx, T, kd, phases)
    k.finish()
    k.close()
    print("even program: instructions", k.ninst, "sems", k.nsem)
    return nc


def emit_even(k, cx, T, kd, phases=("ml", "gdn", "comb"), MIXT=None):
    xT = k.dram("xT", [1024, T], F32, kind="ExternalInput")
    cT = k.dram("cT", [128, 8], F32, kind="ExternalInput")
    adaw = k.dram("adaw", [1024, 2048], F32, kind="ExternalInput")
    adab = k.dram("adab", [128, 16], F32, kind="ExternalInput")
    ng = k.dram("ng", [128, 8], F32, kind="ExternalInput")
    W = k.dram("W", [1024, NC1], F32, kind="ExternalInput")
    mgb = k.dram("mgb", [8, 1], F32, kind="ExternalInput")
    gpar = k.dram("gpar", [4, 2], F32, kind="ExternalInput")
    dcw = k.dram("dcw", [128, 6, 5], F32, kind="ExternalInput")
    gn = k.dram("gn", [1, 512], F32, kind="ExternalInput")
    MIX = k.dram("MIX", [T, 512], F32, kind="ExternalOutput") if MIXT is None else None
    OS = [k.dram(f"OS{d}", [T, 256], F32, kind=kd) for d in range(2)]
    QN = [k.dram(f"QN{h}", [128, T], F32, kind=kd) for h in range(2)]
    KN = [k.dram(f"KN{h}", [128, T], F32, kind=kd) for h in range(2)]
    KTM = [k.dram(f"KTM{h}", [T, 128], F32, kind=kd) for h in range(2)]
    VTM = [k.dram(f"VTM{h}", [T, 128], F32, kind=kd) for h in range(2)]
    FM = k.dram("FM", [FM_ROWS, T], F32, kind=kd)
    TM = k.dram("TM", [T, 1152], F32, kind=kd)
    HS = [k.dram(f"HS{d}", [T, 256], F32, kind=kd) for d in range(2)]

    A = k.sb("evA", [128, 8], F32)
    Bm = k.sb("evBm", [128, 8], F32)
    with k.scope():
        mod = phase_mod(k, cx, cT, adaw, adab, 16, tag="me")
        ngs = k.sb("evngs", [128, 8], F32)
        k.dma("sp", ngs[:], ng[:], [ng], [ngs], ngs)
        k.stt(A[:], mod[:, 8:16], 1.0, ngs[:], ALU.add, ALU.mult, [mod, ngs], [A])
        k.cp(Bm[:], mod[:, 0:8], [mod], [Bm])
    sc1 = k.scope()
    sc1.__enter__()
    xT3 = xT[:].rearrange("(k p) t -> p k t", p=128)
    fm_specs = []
    for (c0, nr, scale) in [(FM_MQ, 128, 1.0), (FM_MK, 128, 0.125), (FM_G, 16, 1.0)] + \
            [(FM_DQ + 128 * i, 128, 1.0) for i in range(6)]:
        fm_specs.append((c0, nr, FM, (lambda tt, r0=c0, nr=nr: FM[r0:r0 + nr, tt * 512:(tt + 1) * 512]), scale))
    tm_specs = []
    for (c0, ncl) in [(TM_MK, 384), (TM_MO, 256), (TM_Z, 512)]:
        tm_specs.append((c0, ncl, TM, (lambda tt, ts, c0=c0, ncl=ncl: TM[tt * 512 + ts * 128: tt * 512 + (ts + 1) * 128, c0 - TM0:c0 - TM0 + ncl])))
    phase_norm_proj(k, cx, T, lambda tt: xT3[:, :, tt * 512:(tt + 1) * 512], [xT], A, Bm, W, NC1, fm_specs, tm_specs)
    sc1.__exit__(None, None, None)
    if "ml" in phases:
        with k.scope():
            phase_mlstm(k, cx, T, FM, TM, mgb, HS)
    if "gdn" in phases:
        with k.scope():
            phase_gdn_pre(k, cx, T, FM, dcw, QN, KN, KTM, VTM)
        with k.scope():
            phase_gdn(k, cx, T, FM, QN, KN, KTM, VTM, gpar, OS)
    if "comb" in phases:
        with k.scope():
            phase_even_combine(k, cx, T, TM, HS, OS, gn, MIX, MIXT)


SEQ = 8192


def colform(v, nchunks):
    return np.ascontiguousarray(np.asarray(v, np.float32).reshape(nchunks, 128).T)


def even_core_inputs(inp, b, e, T=SEQ):
    w_in = inp["ev_w_in"][0]
    o = np.cumsum([0, 256, 256, 512, 512, 16, 1536, 16, 1024])
    mq, mk, mv, mo, mg, dqkv, dg, z = [w_in[:, o[i]:o[i + 1]] for i in range(8)]
    hs = [2 * e, 2 * e + 1]
    gsel = [t * 4 + h for t in range(4) for h in hs]
    cols = [mq[:, 128 * e:128 * e + 128], mk[:, 128 * e:128 * e + 128], mg[:, gsel], dg[:, gsel]]
    for part in range(3):
        for h in hs:
            cols.append(dqkv[:, part * 512 + h * 128: part * 512 + (h + 1) * 128])
    cols += [mk[:, 128 * e:128 * e + 128], mv[:, 256 * e:256 * e + 256], mo[:, 256 * e:256 * e + 256],
             z[:, 256 * e:256 * e + 256], z[:, 512 + 256 * e:512 + 256 * e + 256]]
    W = np.ascontiguousarray(np.concatenate(cols, axis=1))
    assert W.shape[1] == NC1
    cw = inp["ev_dn_conv_w"][0]
    dcw = np.stack([cw[:, part * 512 + h * 128: part * 512 + (h + 1) * 128] for part in range(3) for h in hs], 0)
    return {
        "consts": make_consts(),
        "xT": np.ascontiguousarray(inp["x"][b, :T].T),
        "cT": colform(inp["c"][b], 8),
        "adaw": np.ascontiguousarray(inp["ada_w"][0][:, 0:2048]),
        "adab": colform(inp["ada_b"][0][0:2048], 16),
        "ng": colform(inp["norm_g"][0], 8),
        "W": W,
        "mgb": np.ascontiguousarray(inp["ev_m_gate_b"][0][:, hs].reshape(8, 1)),
        "gpar": np.ascontiguousarray(np.stack([inp["ev_dn_dt_bias"][0][:, hs].reshape(4), inp["ev_dn_a_log"][0][:, hs].reshape(4)], 1)),
        "dcw": np.ascontiguousarray(dcw.transpose(2, 0, 1)),
        "gn": np.ascontiguousarray(np.concatenate([inp["ev_m_norm_g"][0][256 * e:256 * e + 256],
                                                   inp["ev_dn_norm_g"][0][256 * e:256 * e + 256]])[None, :]),
    }


def run_even(inp, T=SEQ):
    nc = build_even(T)
    in_maps = [even_core_inputs(inp, c // 2, c % 2, T) for c in range(8)]
    res = run_bass_kernel_spmd(nc, in_maps, core_ids=list(range(8)))
    mix = np.zeros((4, T, 1024), np.float32)
    for c in range(8):
        b, e = c // 2, c % 2
        m = res.results[c]["MIX"]
        mix[b, :, 256 * e:256 * e + 256] = m[:, 0:256]
        mix[b, :, 512 + 256 * e:512 + 256 * e + 256] = m[:, 256:512]
    return mix


HALO = 1024
NEG_ATT = -30000.0
C2_J = 0
C2_OH = 128
C2_COLS = 128 + 3 * 384
DILS = (1, 4, 16)
VV_COLS = 33 + 4 * 9 + 16 * 3


def make_vv(TO, s_half, nhalves):
    cols = []
    S = TO * nhalves
    for d in DILS:
        npos = TO // d
        nkt = npos // 128 + 1
        for r in range(d):
            p = -64 + 128 * np.arange(nkt)[None, :] + np.arange(128)[:, None]
            tok = s_half * TO + r + d * p
            cols.append(((tok >= 0) & (tok < S)).astype(np.float32))
    return np.ascontiguousarray(np.concatenate(cols, axis=1))


def t5_bucket_np(rel):
    n = np.abs(rel)
    large = 8 + (np.log(np.maximum(n, 1).astype(np.float32) / np.float32(8)) / np.float32(np.log(128.0)) * np.float32(8)).astype(np.int32)
    large = np.minimum(large, 15)
    return (rel > 0).astype(np.int32) * 16 + np.where(n < 8, n, large)


def make_consts2():
    c = np.zeros((128, C2_COLS), np.float32)
    c[np.arange(128), C2_J + 127 - np.arange(128)] = 1.0
    for g, d in enumerate(DILS):
        for u in range(383):
            rel = u - 191
            if abs(rel) <= 64:
                c[t5_bucket_np(np.array(rel * d)), C2_OH + g * 384 + u] = 1.0
            else:
                c[32, C2_OH + g * 384 + u] = NEG_ATT
    return c


def phase_attention(k, cx, TO, QT, KT, VT, vv_d, relb_d, relbT_d, GF, DT, c2):
    E = TO + 2 * HALO
    cs = cx.consts
    rb = k.sb("at_rb", [33, 8], F32)
    k.op("dve", lambda e: e.memset(rb[:], 1.0), [], [rb])
    k.dma("sp", rb[0:32, :], relb_d[:], [relb_d], [rb], rb)
    k.ts(rb[0:32, :], rb[0:32, :], 8.0, ALU.mult, [rb], [rb])
    gfs = k.sb("at_gfs", [8, 3 * 384], F32)
    pg = cx.psum[0]
    for g in range(3):
        k.mm(pg[0:8, 0:384], rb[0:33, :], c2[0:33, C2_OH + g * 384:C2_OH + (g + 1) * 384], [rb, c2], [pg])
        k.cp(gfs[:, g * 384:(g + 1) * 384], pg[0:8, 0:384], [pg], [gfs])
    k.dma("sp", GF[:], gfs[:], [gfs], [GF], gfs)
    bmx = k.sb("at_bmx", [128, 32], F32)
    bmax = k.sb("at_bmax", [128, 1], F32)
    Qa = k.sb("at_Qa", [65, TO], F32)
    Ka = k.sb("at_Ka", [65, E], F32)
    k.op("dve", lambda e: e.memset(Ka[64:65, :], 1.0), [], [Ka])
    Qb = k.sb("at_Qb", [65, TO], BF16)
    Kb = k.sb("at_Kb", [65, E], BF16)
    Hb = k.sb("at_Hb", [128, 6, 128], BF16)
    Jb = k.sb("at_Jb", [128, 128], BF16)
    k.cp(Jb[:], c2[:, C2_J:C2_J + 128], [c2], [Jb])
    sqt = k.sb("at_sq", [64, 512], F32)
    kmx = k.sb("at_kmx", [128, 16], F32)
    kmax = k.sb("at_kmax", [128, 1], F32)
    accN = k.sb("at_accN", [64, TO], F32)
    accD = k.sb("at_accD", [64, TO], F32)
    H = k.sb("at_H", [128, 6, 128], F32)
    Vf = [k.sb(f"at_Vf{i}", [128, 34, 64], F32) for i in range(2)]
    Vt = [k.sb(f"at_Vt{i}", [128, 34, 64], BF16) for i in range(2)]
    Vv = [k.sb(f"at_Vv{i}", [128, 34, 64], BF16) for i in range(2)]
    PT = [k.sb(f"at_PT{i}", [128, 512], BF16) for i in range(2)]
    ones64 = cs[0:64, C_ONES:C_ONES + 128]
    VV = k.sb("at_VV", [128, vv_d[:].shape[1]], F32)
    k.dma("sp", VV[:], vv_d[:], [vv_d], [VV], VV)
    vvoff = 0
    VVO = {}
    for g_, d_ in enumerate(DILS):
        for r_ in range(d_):
            VVO[(g_, r_)] = vvoff
            vvoff += (TO // d_) // 128 + 1
    assert vvoff == vv_d[:].shape[1]
    nv = 0
    npt = 0
    for hd in range(8):
        r0 = hd * 64
        k.dma("sp", Qa[0:64, :], QT[r0:r0 + 64, :], [QT], [Qa], Qa)
        k.dma("sp", Ka[0:64, :], KT[r0:r0 + 64, :], [KT], [Ka], Ka)
        for g in range(3):
            for kt in range(2):
                src = bass.AP(GF[:].tensor, GF[:].offset + hd * 1152 + g * 384 + kt * 128, [[1, 128], [1, 128]])
                k.dma("sp", H[:, g * 2 + kt, :], src, [GF], [H], H, grouped=True)
        k.dma("sp", bmx[:], relbT_d[hd:hd + 1, :].partition_broadcast(128), [relbT_d], [bmx], bmx)
        k.op("dve", lambda e: e.tensor_reduce(out=bmax[:], in_=bmx[:], axis=AX.X, op=ALU.max), [bmx], [bmax])
        k.ts(bmax[:], bmax[:], 8.0, ALU.mult, [bmax], [bmax])
        pk = cx.psum[1]
        for t in range(E // 512):
            k.tt(sqt[:], Ka[0:64, t * 512:(t + 1) * 512], Ka[0:64, t * 512:(t + 1) * 512], ALU.mult, [Ka], [sqt])
            k.mm(pk[:, :], ones64, sqt[:], [cs, sqt], [pk])
            k.op("dve", lambda e, t=t: e.tensor_reduce(out=kmx[:, t:t + 1], in_=pk[:, :], axis=AX.X, op=ALU.max), [pk], [kmx])
        k.op("dve", lambda e: e.tensor_reduce(out=kmax[:], in_=kmx[:, 0:E // 512], axis=AX.X, op=ALU.max), [kmx], [kmax])
        k.act(kmax[:], kmax[:], AF.Sqrt, [kmax], [kmax])
        for t in range(TO // 512):
            k.tt(sqt[:], Qa[0:64, t * 512:(t + 1) * 512], Qa[0:64, t * 512:(t + 1) * 512], ALU.mult, [Qa], [sqt])
            k.mm(pk[:, :], ones64, sqt[:], [cs, sqt], [pk])
            k.act(Qa[64:65, t * 512:(t + 1) * 512], pk[64:65, :], AF.Sqrt, [pk], [Qa])
        k.ts(Qa[64:65, :], Qa[64:65, :], kmax[64:65, 0:1], ALU.mult, [Qa, kmax], [Qa], s2=bmax[64:65, 0:1], op1=ALU.add)
        k.ts(Qa[64:65, :], Qa[64:65, :], -1.0, ALU.mult, [Qa], [Qa])
        k.cp(Qb[:], Qa[:], [Qa], [Qb], eng="act")
        k.cp(Kb[:], Ka[:], [Ka], [Kb])
        k.cp(Hb[:], H[:], [H], [Hb], eng="pool")
        if hd == 0:
            dbg(k, "H", H, [128, 6, 128]); dbg(k, "Qa", Qa, [65, TO]); dbg(k, "Ka", Ka, [65, E])
        first = True
        for g, d in enumerate(DILS):
            npos = TO // d
            nkt = npos // 128 + 1
            for r in range(d):
                V1 = Vt[nv % 2]
                V2 = Vv[nv % 2]
                V1f = Vf[nv % 2]
                nv += 1
                base = HALO + r - 64 * d
                for n0 in range(0, nkt, 8):
                    nn = min(8, nkt - n0)
                    vsrc = bass.AP(VT[:].tensor, VT[:].offset + (base + n0 * 128 * d) * 512 + r0, [[d * 512, 128], [128 * d * 512, nn], [1, 64]])
                    k.dma("sp", V1f[:, n0:n0 + nn, :], vsrc, [VT], [V1f], V1f, grouped=True)
                vb_ = fv(VV, 0, 128, VVO[(g, r)], [(1, nkt), (0, 64)])
                k.tt(V1[:, 0:nkt, :], V1f[:, 0:nkt, :], vb_, ALU.mult, [V1f, VV], [V1])
                k.cp(V2[:, 0:nkt, :], vb_, [VV], [V2], eng="pool")
                nqt = npos // 128
                for qb in range(0, nqt, 2):
                    nq = min(2, nqt - qb)
                    pS = cx.psum[2 + npt % 2]
                    for qi in range(nq):
                        qt = qb + qi
                        qap = fv(Qb, 0, 65, r + d * 128 * qt, [(d, 128)])
                        for kt in range(2):
                            kap = fv(Kb, 0, 65, HALO + r + d * (128 * qt - 64 + 128 * kt), [(d, 128)])
                            col = (qi * 2 + kt) * 128
                            k.mm(pS[:, col:col + 128], kap, qap, [Kb, Qb], [pS], start=True, stop=False)
                            k.mm(pS[:, col:col + 128], Hb[:, g * 2 + kt, :], Jb[:], [Hb, Jb], [pS], start=False, stop=True)
                    P = PT[npt % 2]
                    k.act(P[:, 0:nq * 256], pS[:, 0:nq * 256], AF.Exp, [pS], [P], scale=0.125)
                    if hd == 0 and g == 0 and qb == 0:
                        dbg(k, "P", P, [128, 512]); dbgap(k, "V1", V1, V1[:, 0:9, :], [128, 9, 64]); dbgap(k, "V2", V2, V2[:, 0:9, :], [128, 9, 64])
                    pN = cx.psum[4 + npt % 2]
                    pDn = cx.psum[6 + npt % 2]
                    npt += 1
                    for qi in range(nq):
                        qt = qb + qi
                        for kt in range(2):
                            col = (qi * 2 + kt) * 128
                            k.mm(pN[0:64, qi * 128:(qi + 1) * 128], V1[:, qt + kt, :], P[:, col:col + 128], [V1, P], [pN],
                                 start=(kt == 0), stop=(kt == 1))
                            k.mm(pDn[0:64, qi * 128:(qi + 1) * 128], V2[:, qt + kt, :], P[:, col:col + 128], [V2, P], [pDn],
                                 start=(kt == 0), stop=(kt == 1))
                    qcols = nq * 128
                    an = fv(accN, 0, 64, r + d * 128 * qb, [(d, qcols)])
                    ad = fv(accD, 0, 64, r + d * 128 * qb, [(d, qcols)])
                    if first:
                        k.cp(an, pN[0:64, 0:qcols], [pN], [accN], eng="act")
                        k.cp(ad, pDn[0:64, 0:qcols], [pDn], [accD])
                    else:
                        k.tt(an, an, pN[0:64, 0:qcols], ALU.add, [accN, pN], [accN], eng="dve")
                        k.tt(ad, ad, pDn[0:64, 0:qcols], ALU.add, [accD, pDn], [accD], eng="dve")
            first = False
        k.op("dve", lambda e: e.reciprocal(out=accD[:], in_=accD[:]), [accD], [accD])
        k.tt(accN[:], accN[:], accD[:], ALU.mult, [accN, accD], [accN])
        k.dma("sp", DT[r0:r0 + 64, :], accN[:], [accN], [DT], accN)


def phase_conv_module(k, cx, TO, GA, GB, valid_d, cw_d, cpar_d, CT):
    cs = cx.consts
    cw = k.sb("cv_w", [128, 4, 31], F32)
    k.dma("sp", cw[:], cw_d[:], [cw_d], [cw], cw)
    cpar = k.sb("cv_par", [128, 4, 3], F32)
    k.dma("sp", cpar[:], cpar_d[:], [cpar_d], [cpar], cpar)
    dg = k.sb("cv_diag", [128, 124, 128], BF16)
    for ch in range(4):
        for j in range(31):
            k.ts(dg[:, ch * 31 + j, :], cs[:, C_IDENT:C_IDENT + 128], cw[:, ch, j:j + 1], ALU.mult, [cs, cw], [dg],
                 eng=("dve" if j % 2 else "pool"))
    ga = [k.sb(f"cv_ga{i}", [128, 4, 542], F32) for i in range(2)]
    gb = [k.sb(f"cv_gb{i}", [128, 4, 542], F32) for i in range(2)]
    vbs = [k.sb(f"cv_vb{i}", [128, 542], F32) for i in range(2)]
    uin = k.sb("cv_uin", [128, 4, 542], BF16)
    U = k.sb("cv_U", [128, 4, 512], F32)
    XC = k.sb("cv_XC", [128, 4, 512], F32)
    SQ = k.sb("cv_SQ", [128, 4, 512], F32)
    rs = k.sb("cv_rs", [128, 512], F32)
    O = [k.sb(f"cv_O{i}", [128, 4, 512], F32) for i in range(2)]
    ones = cs[:, C_ONES:C_ONES + 128]
    def cv_load(tt):
        e0 = HALO + tt * 512 - 15
        a, b, vb = ga[tt % 2], gb[tt % 2], vbs[tt % 2]
        k.dma("sp", a[:], GA[:].rearrange("(c p) t -> p c t", p=128)[:, :, e0:e0 + 542], [GA], [a], a)
        k.dma("sp", b[:], GB[:].rearrange("(c p) t -> p c t", p=128)[:, :, e0:e0 + 542], [GB], [b], b)
        k.dma("sp", vb[:], valid_d[0:1, e0:e0 + 542].partition_broadcast(128), [valid_d], [vb], vb)

    cv_load(0)
    for tt in range(TO // 512):
        a, b, vb = ga[tt % 2], gb[tt % 2], vbs[tt % 2]
        if tt + 1 < TO // 512:
            cv_load(tt + 1)
        k.act(b[:], b[:], AF.Sigmoid, [b], [b])
        k.tt(a[:], a[:], b[:], ALU.mult, [a, b], [a])
        k.tt(uin[:], a[:], fv(vb, 0, 128, 0, [(0, 4), (1, 542)]), ALU.mult, [a, vb], [uin])
        for ch in range(4):
            pc_ = cx.psum[ch % 2]
            for j in range(31):
                k.mm(pc_[:, :], dg[:, ch * 31 + j, :], uin[:, ch, j:j + 512], [dg, uin], [pc_], start=(j == 0), stop=(j == 30))
            k.ts(U[:, ch, :], pc_[:, :], cpar[:, ch, 0:1], ALU.add, [pc_, cpar], [U])
        pm = cx.psum[2]
        for ch in range(4):
            k.mm(pm[:, :], ones, U[:, ch, :], [cs, U], [pm], start=(ch == 0), stop=(ch == 3))
        for ch in range(4):
            k.stt(XC[:, ch, :], pm[:, :], -1.0 / 512.0, U[:, ch, :], ALU.mult, ALU.add, [pm, U], [XC])
        k.tt(SQ[:], XC[:], XC[:], ALU.mult, [XC], [SQ], eng="pool")
        pv = cx.psum[3]
        for ch in range(4):
            k.mm(pv[:, :], ones, SQ[:, ch, :], [cs, SQ], [pv], start=(ch == 0), stop=(ch == 3))
        k.act(rs[:], pv[:, :], AF.Sqrt, [pv], [rs], scale=1.0 / 512.0, bias=float(EPS))
        k.op("dve", lambda e: e.reciprocal(out=rs[:], in_=rs[:]), [rs], [rs])
        Ot = O[tt % 2]
        for ch in range(4):
            k.tt(XC[:, ch, :], XC[:, ch, :], rs[:], ALU.mult, [XC, rs], [XC])
            k.act(Ot[:, ch, :], XC[:, ch, :], AF.Silu, [XC, cpar], [Ot], scale=cpar[:, ch, 1:2], bias=cpar[:, ch, 2:3])
        k.dma("sp", CT[:].rearrange("(c p) t -> p c t", p=128)[:, :, tt * 512:(tt + 1) * 512], Ot[:], [Ot], [CT], Ot)


def phase_final(k, cx, TO, CT, DT, ZT, X1, Wo_d, gate1, fg_d, outT):
    cs = cx.consts
    Wb = k.sb("fn_Wb", [128, 8, 1024], BF16)
    load_w_bf16(k, Wb, Wo_d, 1024, "fn")
    fg = k.sb("fn_fg", [128, 8], F32)
    k.dma("sp", fg[:], fg_d[:], [fg_d], [fg], fg)
    M = [k.sb(f"fn_M{i}", [128, 8, 512], F32) for i in range(2)]
    Z = [k.sb(f"fn_Z{i}", [128, 8, 512], F32) for i in range(2)]
    Mb = k.sb("fn_Mb", [128, 8, 512], BF16)
    X = [k.sb(f"fn_X{i}", [128, 8, 512], F32) for i in range(2)]
    sq = k.sb("fn_sq", [128, 8, 512], BF16)
    rs = k.sb("fn_rs", [128, 512], F32)
    O = [k.sb(f"fn_O{i}", [128, 8, 512], F32) for i in range(2)]
    r3 = lambda D: D[:].rearrange("(c p) t -> p c t", p=128)
    def fn_load(tt):
        sl = slice(tt * 512, (tt + 1) * 512)
        Mt, Zt, Xt = M[tt % 2], Z[tt % 2], X[tt % 2]
        k.dma("sp", Mt[:, 0:4, :], r3(CT)[:, :, sl], [CT], [Mt], Mt, grouped=True)
        k.dma("sp", Mt[:, 4:8, :], r3(DT)[:, :, sl], [DT], [Mt], Mt, grouped=True)
        k.dma("sp", Zt[:], r3(ZT)[:, :, sl], [ZT], [Zt], Zt)
        k.dma("sp", Xt[:], r3(X1)[:, :, sl], [X1], [Xt], Xt)

    fn_load(0)
    for tt in range(TO // 512):
        sl = slice(tt * 512, (tt + 1) * 512)
        Mt, Zt, Xt, Ot = M[tt % 2], Z[tt % 2], X[tt % 2], O[tt % 2]
        if tt + 1 < TO // 512:
            fn_load(tt + 1)
        k.act(Zt[:], Zt[:], AF.Silu, [Zt], [Zt])
        k.tt(Mb[:], Mt[:], Zt[:], ALU.mult, [Mt, Zt], [Mb])
        for fc in range(8):
            pb = cx.psum[fc % 4]
            for kc in range(8):
                k.mm(pb[:, :], Wb[:, kc, fc * 128:(fc + 1) * 128], Mb[:, kc, :], [Wb, Mb], [pb], start=(kc == 0), stop=(kc == 7))
            k.stt(Xt[:, fc, :], pb[:, :], gate1[:, fc:fc + 1], Xt[:, fc, :], ALU.mult, ALU.add, [pb, gate1, Xt], [Xt])
        k.act(sq[:], Xt[:], AF.Square, [Xt], [sq])
        pss = cx.psum[7]
        for kc in range(8):
            k.mm(pss[:, :], cx.ones_bf[:], sq[:, kc, :], [cx.ones_bf, sq], [pss], start=(kc == 0), stop=(kc == 7))
        k.act(rs[:], pss[:, :], AF.Sqrt, [pss], [rs], scale=1.0 / 1024.0, bias=float(EPS))
        k.op("dve", lambda e: e.reciprocal(out=rs[:], in_=rs[:]), [rs], [rs])
        for kc in range(8):
            k.stt(Ot[:, kc, :], Xt[:, kc, :], fg[:, kc:kc + 1], rs[:], ALU.mult, ALU.mult, [Xt, fg, rs], [Ot])
        k.dma("sp", r3(outT)[:, :, sl], Ot[:], [Ot], [outT], Ot)


def build_odd(TO, debug=False, phases=("conv", "att", "fin")):
    nc = bass.Bass("TRN2", target_bir_lowering=False)
    k = KB(nc)
    cx = Ctx()
    setup_common(k, cx)
    kd = "ExternalOutput" if debug else "Internal"
    emit_odd(k, cx, TO, kd, phases)
    k.finish()
    k.close()
    print("odd program: instructions", k.ninst, "sems", k.nsem)
    return nc


def emit_odd(k, cx, TO, kd, phases=("conv", "att", "fin"), GM=None):
    E = TO + 2 * HALO
    c2d = k.dram("consts2", [128, C2_COLS], F32, kind="ExternalInput")
    c2 = k.sb("c2", [128, C2_COLS], F32)
    k.dma("sp", c2[:], c2d[:], [c2d], [c2], c2)
    x0T = k.dram("x0T", [1024, E], F32, kind="ExternalInput")
    mixT = k.dram("mixT", [1024, E], F32, kind="ExternalInput") if GM is None else None
    selw_d = k.dram("selw", [128, 2], F32, kind="ExternalInput") if GM is not None else None
    valid = k.dram("valid", [1, E], F32, kind="ExternalInput")
    cT = k.dram("cT1", [128, 8], F32, kind="ExternalInput")
    adaw0 = k.dram("adaw0", [1024, 1024], F32, kind="ExternalInput")
    adab0 = k.dram("adab0", [128, 8], F32, kind="ExternalInput")
    adaw1 = k.dram("adaw1", [1024, 3072], F32, kind="ExternalInput")
    adab1 = k.dram("adab1", [128, 24], F32, kind="ExternalInput")
    ng = k.dram("ng1", [128, 8], F32, kind="ExternalInput")
    Wo0 = k.dram("Wo0", [1024, 1024], F32, kind="ExternalInput")
    W1 = k.dram("W1", [1024, 3584], F32, kind="ExternalInput")
    Wo1 = k.dram("Wo1", [1024, 1024], F32, kind="ExternalInput")
    cw = k.dram("cw", [128, 4, 31], F32, kind="ExternalInput")
    cpar = k.dram("cpar", [128, 4, 3], F32, kind="ExternalInput")
    relb = k.dram("relb", [32, 8], F32, kind="ExternalInput")
    relbT = k.dram("relbT", [8, 32], F32, kind="ExternalInput")
    vv = k.dram("vv", [128, VV_COLS if TO == 4096 else sum(d * ((TO // d) // 128 + 1) for d in DILS)], F32, kind="ExternalInput")
    fgd = k.dram("fg", [128, 8], F32, kind="ExternalInput")
    outT = k.dram("outT", [1024, TO], F32, kind="ExternalOutput")
    X1 = k.dram("X1", [1024, TO], F32, kind=kd)
    GA = k.dram("GA", [512, E], F32, kind=kd)
    GB = k.dram("GB", [512, E], F32, kind=kd)
    QT = k.dram("QT", [512, TO], F32, kind=kd)
    KT = k.dram("KT", [512, E], F32, kind=kd)
    ZT = k.dram("ZT", [1024, TO], F32, kind=kd)
    VT = k.dram("VT", [E, 512], F32, kind=kd)
    CT = k.dram("CT", [512, TO], F32, kind=kd)
    DT = k.dram("DT", [512, TO], F32, kind=kd)
    GF = k.dram("GF", [8, 3 * 384], F32, kind=kd)

    A = k.sb("odA", [128, 8], F32)
    Bm = k.sb("odBm", [128, 8], F32)
    gate0 = k.sb("gate0", [128, 8], F32)
    gate1 = k.sb("gate1", [128, 8], F32)
    with k.scope():
        g0 = phase_mod(k, cx, cT, adaw0, adab0, 8, tag="m0")
        k.cp(gate0[:], g0[:], [g0], [gate0])
    with k.scope():
        mod1 = phase_mod(k, cx, cT, adaw1, adab1, 24, tag="m1")
        ngs = k.sb("odngs", [128, 8], F32)
        k.dma("sp", ngs[:], ng[:], [ng], [ngs], ngs)
        k.stt(A[:], mod1[:, 8:16], 1.0, ngs[:], ALU.add, ALU.mult, [mod1, ngs], [A])
        k.cp(Bm[:], mod1[:, 0:8], [mod1], [Bm])
        k.cp(gate1[:], mod1[:, 16:24], [mod1], [gate1])
    with k.scope():
        Wo0b = k.sb("pa_Wo0b", [128, 8, 1024], BF16)
        load_w_bf16(k, Wo0b, Wo0, 1024, "pa0")
        Mt = k.sb("pa_Mt", [128, 8, 512], F32)
        Mb = k.sb("pa_Mb", [128, 8, 512], BF16)
        x03 = x0T[:].rearrange("(c p) t -> p c t", p=128)
        if GM is None:
            m3 = mixT[:].rearrange("(c p) t -> p c t", p=128)
        else:
            g3 = [g[:].rearrange("(c p) t -> p c t", p=128) for g in GM]
            selw = k.sb("pa_selw", [128, 2], F32)
            k.dma("sp", selw[:], selw_d[:], [selw_d], [selw], selw)
            Mt2 = k.sb("pa_Mt2", [128, 8, 512], F32)
        x13 = X1[:].rearrange("(c p) t -> p c t", p=128)
        own_lo, own_hi = HALO // 512, (HALO + TO) // 512

        def prod_load(te, Xt):
            sl = slice(te * 512, (te + 1) * 512)
            k.dma("sp", Xt[:], x03[:, :, sl], [x0T], [Xt], Xt)
            if GM is None:
                k.dma("sp", Mt[:], m3[:, :, sl], [mixT], [Mt], Mt)
            else:
                g0 = te * 512 - HALO
                g1 = te * 512 - HALO + TO
                if g0 >= 0:
                    k.dma("sp", Mt[:], g3[g0 // 1024][:, :, g0 % 1024:g0 % 1024 + 512], [GM[g0 // 1024]], [Mt], Mt)
                if g1 + 512 <= 2 * TO:
                    k.dma("sp", Mt2[:], g3[g1 // 1024][:, :, g1 % 1024:g1 % 1024 + 512], [GM[g1 // 1024]], [Mt2], Mt2)

        def prod_compute(te, Xt):
            if GM is None:
                k.cp(Mb[:, 0:4, :], Mt[:, 0:4, :], [Mt], [Mb], eng="act")
                k.cp(Mb[:, 4:8, :], Mt[:, 4:8, :], [Mt], [Mb], eng="dve")
            else:
                ok0 = te * 512 - HALO >= 0
                ok1 = te * 512 - HALO + TO + 512 <= 2 * TO
                if ok0 and ok1:
                    k.ts(Mt[:], Mt[:], selw[:, 0:1], ALU.mult, [Mt, selw], [Mt])
                    k.stt(Mb[:], Mt2[:], selw[:, 1:2], Mt[:], ALU.mult, ALU.add, [Mt2, selw, Mt], [Mb])
                elif ok0:
                    k.ts(Mb[:], Mt[:], selw[:, 0:1], ALU.mult, [Mt, selw], [Mb])
                else:
                    k.ts(Mb[:], Mt2[:], selw[:, 1:2], ALU.mult, [Mt2, selw], [Mb])

            def post():
                for fc in range(8):
                    pb = cx.psum[6]
                    for kc in range(8):
                        k.mm(pb[:, :], Wo0b[:, kc, fc * 128:(fc + 1) * 128], Mb[:, kc, :], [Wo0b, Mb], [pb], start=(kc == 0), stop=(kc == 7))
                    k.stt(Xt[:, fc, :], pb[:, :], gate0[:, fc:fc + 1], Xt[:, fc, :], ALU.mult, ALU.add, [pb, gate0, Xt], [Xt])
                if own_lo <= te < own_hi:
                    k.dma("sp", x13[:, :, (te - own_lo) * 512:(te - own_lo + 1) * 512], Xt[:], [Xt], [X1], Xt)
            return post

        producer = (prod_load, prod_compute)

        def rng_fn(D, r0, nr, lo, hi, off):
            def f(te):
                if not (lo <= te < hi):
                    return None
                return D[r0:r0 + nr, (te - off) * 512:(te - off + 1) * 512]
            return f
        nE = E // 512
        fm_specs = []
        for ch in range(4):
            fm_specs.append((0 + 128 * ch, 128, GA, rng_fn(GA, 128 * ch, 128, own_lo - 1, own_hi + 1, 0), 1.0))
            fm_specs.append((512 + 128 * ch, 128, GB, rng_fn(GB, 128 * ch, 128, own_lo - 1, own_hi + 1, 0), 1.0))
            fm_specs.append((1024 + 128 * ch, 128, QT, rng_fn(QT, 128 * ch, 128, own_lo, own_hi, own_lo), 1.0))
            fm_specs.append((1536 + 128 * ch, 128, KT, rng_fn(KT, 128 * ch, 128, 0, nE, 0), 1.0))
        for ch in range(8):
            fm_specs.append((2560 + 128 * ch, 128, ZT, rng_fn(ZT, 128 * ch, 128, own_lo, own_hi, own_lo), 1.0))
        tm_specs = [(2048, 512, VT, (lambda te, ts: VT[te * 512 + ts * 128:te * 512 + (ts + 1) * 128, :]))]
        phase_norm_proj(k, cx, E, None, None, A, Bm, W1, 3584, fm_specs, tm_specs, tag="pa", producer=producer)
    if "conv" in phases:
        with k.scope():
            phase_conv_module(k, cx, TO, GA, GB, valid, cw, cpar, CT)
    if "att" in phases:
        with k.scope():
            phase_attention(k, cx, TO, QT, KT, VT, vv, relb, relbT, GF, DT, c2)
    if "fin" in phases:
        with k.scope():
            phase_final(k, cx, TO, CT, DT, ZT, X1, Wo1, gate1, fgd, outT)


def build_fused(T=SEQ):
    TO = T // 2
    nc = bass.Bass("TRN2", target_bir_lowering=False)
    k = KB(nc)
    cx = Ctx()
    setup_common(k, cx)
    bounce = [k.dram(f"mixT_own{i}", [512, 1024], F32) for i in range(T // 1024)]
    GM = [k.dram(f"mixT_all{i}", [1024, 1024], F32) for i in range(T // 1024)]
    emit_even(k, cx, T, "Internal", MIXT=bounce)
    for i in range(T // 1024):
        k.allgather(bounce[i], GM[i], [[0, 1], [2, 3], [4, 5], [6, 7]])
    emit_odd(k, cx, TO, "Internal", GM=GM)
    k.finish()
    k.close()
    print("fused program: instructions", k.ninst, "sems", k.nsem)
    return nc


def fused_core_inputs(inp, b, e, T=SEQ):
    TO = T // 2
    d = even_core_inputs(inp, b, e, T)
    o = odd_core_inputs_nomix(inp, b, e, TO)
    d.update(o)
    perm = np.concatenate([np.concatenate([np.arange(256 * ee, 256 * ee + 256), 512 + np.arange(256 * ee, 256 * ee + 256)]) for ee in range(2)])
    d["Wo0"] = np.ascontiguousarray(inp["ev_w_out"][0][perm, :])
    sel = np.zeros((128, 2), np.float32)
    sel[:, e] = 1.0
    d["selw"] = sel
    return d


def run_fused(inp, T=SEQ):
    TO = T // 2
    nc = build_fused(T)
    in_maps = [fused_core_inputs(inp, c // 2, c % 2, T) for c in range(8)]
    res = run_bass_kernel_spmd(nc, in_maps, core_ids=list(range(8)))
    out = np.zeros((4, T, 1024), np.float32)
    for c in range(8):
        b, sh = c // 2, c % 2
        out[b, sh * TO:(sh + 1) * TO, :] = res.results[c]["outT"].T
    return out


def odd_core_inputs_nomix(inp, b, sh, TO=4096, nhalves=2):
    d = odd_core_inputs(inp, None, b, sh, TO, nhalves)
    del d["mixT"]
    return d


def odd_core_inputs(inp, mix, b, sh, TO=4096, nhalves=2):
    S = TO * nhalves
    E = TO + 2 * HALO
    lo = sh * TO - HALO
    x0T = np.zeros((1024, E), np.float32)
    mT = np.zeros((1024, E), np.float32)
    valid = np.zeros((1, E), np.float32)
    a, bnd = max(lo, 0), min(lo + E, S)
    x0T[:, a - lo:bnd - lo] = inp["x"][b, a:bnd].T
    if mix is not None:
        mT[:, a - lo:bnd - lo] = mix[b, a:bnd].T
    valid[0, a - lo:bnd - lo] = 1.0
    cpar = np.stack([inp["od_dw_b"][0], inp["od_ln_g"][0], inp["od_ln_b"][0]], -1)
    return {
        "consts": make_consts(), "consts2": make_consts2(),
        "x0T": x0T, "mixT": mT, "valid": valid,
        "cT1": colform(inp["c"][b], 8),
        "adaw0": np.ascontiguousarray(inp["ada_w"][0][:, 2048:3072]),
        "adab0": colform(inp["ada_b"][0][2048:3072], 8),
        "adaw1": np.ascontiguousarray(inp["ada_w"][1]),
        "adab1": colform(inp["ada_b"][1], 24),
        "ng1": colform(inp["norm_g"][1], 8),
        "Wo0": np.ascontiguousarray(inp["ev_w_out"][0]),
        "W1": np.ascontiguousarray(inp["od_w_in"][0]),
        "Wo1": np.ascontiguousarray(inp["od_w_out"][0]),
        "cw": np.ascontiguousarray(inp["od_dw_w"][0].reshape(31, 4, 128).transpose(2, 1, 0)),
        "cpar": np.ascontiguousarray(cpar.reshape(4, 128, 3).transpose(1, 0, 2)),
        "relb": np.ascontiguousarray(inp["rel_bias"]),
        "relbT": np.ascontiguousarray(inp["rel_bias"].T),
        "vv": make_vv(TO, sh, nhalves),
        "fg": colform(inp["final_g"], 8),
    }


def run_odd(inp, mix, TO=4096):
    nc = build_odd(TO)
    in_maps = [odd_core_inputs(inp, mix, c // 2, c % 2, TO, 2) for c in range(8)]
    res = run_bass_kernel_spmd(nc, in_maps, core_ids=list(range(8)))
    out = np.zeros((4, 2 * TO, 1024), np.float32)
    for c in range(8):
        b, sh = c // 2, c % 2
        out[b, sh * TO:(sh + 1) * TO, :] = res.results[c]["outT"].T
    return out


def kernel(**inputs):
    inp = {k: np.asarray(v) for k, v in inputs.items()}
    return run_fused(inp)
```

```python
import contextlib
import numpy as np
import concourse.bass as bass
import concourse.mybir as mybir
from concourse.bass_utils import run_bass_kernel_spmd

F32 = mybir.dt.float32
BF16 = mybir.dt.bfloat16
ALU = mybir.AluOpType
AF = mybir.ActivationFunctionType
AX = mybir.AxisListType

SEM_LIMIT = 8000
FP32R = False
EPS = 1e-6
NEG = -1e30


class Buf:
    __slots__ = ("t", "name", "writer", "readers", "dsem", "dtot", "depth")

    def __init__(self, t, name):
        self.t = t
        self.name = name
        self.writer = None
        self.readers = []
        self.dsem = None
        self.dtot = 0

    def __getitem__(self, idx):
        return self.t[idx]


class KB:
    def __init__(self, nc, same_engine_sync=True):
        self.nc = nc
        self.es = contextlib.ExitStack()
        self.root_es = self.es
        self.depth = 0
        self.engs = {"pe": nc.tensor, "dve": nc.vector, "act": nc.scalar, "pool": nc.gpsimd, "sp": nc.sync}
        self.csem = {}
        self.ccnt = {}
        self.seen = {k: {} for k in self.engs}
        self.same = same_engine_sync
        self.pool_used = False
        self.free_dsems = []
        self.nsem = 0
        self.ninst = 0
        self.bufs = []

    def sem(self, name, root=False):
        self.nsem += 1
        es = self.root_es if root else self.es
        return es.enter_context(self.nc.semaphore(f"{name}_{self.nsem}"))

    def sb(self, name, shape, dt=F32):
        t = self.es.enter_context(self.nc.sbuf_tensor("s_" + name, list(shape), dt))
        b = Buf(t, name)
        b.depth = self.depth
        self.bufs.append(b)
        return b

    def ps(self, name, shape, dt=F32):
        t = self.es.enter_context(self.nc.psum_tensor("p_" + name, list(shape), dt))
        b = Buf(t, name)
        b.depth = self.depth
        self.bufs.append(b)
        return b

    def dram(self, name, shape, dt=F32, kind="Internal"):
        t = self.nc.dram_tensor(name, list(shape), dt, kind=kind)
        b = Buf(t.ap(), name)
        b.depth = 0
        self.bufs.append(b)
        return b

    def _wait(self, ek, dep):
        sem, val, dek = dep
        if dek == ek and not self.same:
            return
        sid = id(sem)
        if self.seen[ek].get(sid, 0) >= val:
            return
        self.engs[ek].wait_ge(sem, val)
        self.seen[ek][sid] = val

    def _deps(self, ek, reads, writes, skip_same_w=False):
        for b in reads:
            if b.writer is not None:
                self._wait(ek, b.writer)
        for b in writes:
            if b.writer is not None:
                if not (skip_same_w and b.writer[2] == ek):
                    self._wait(ek, b.writer)
            for r in b.readers:
                self._wait(ek, r)

    def _tick(self, ek):
        if ek not in self.csem or self.ccnt[ek] >= SEM_LIMIT:
            self.csem[ek] = self.sem("c" + ek, root=True)
            self.ccnt[ek] = 0
        self.ccnt[ek] += 1
        return (self.csem[ek], self.ccnt[ek], ek)

    def _mark(self, tok, reads, writes):
        for b in reads:
            b.readers.append(tok)
            if len(b.readers) > 16:
                last = {}
                for r in b.readers:
                    k = id(r[0])
                    if k not in last or last[k][1] < r[1]:
                        last[k] = r
                b.readers = list(last.values())
        for b in writes:
            b.writer = tok
            b.readers = []

    def op(self, ek, fn, reads, writes, acc=False):
        if ek == "pool":
            self.pool_used = True
        self._deps(ek, reads, writes, skip_same_w=acc)
        tok = self._tick(ek)
        ins = fn(self.engs[ek])
        ins.then_inc(tok[0], 1)
        self._mark(tok, reads, writes)
        self.ninst += 1
        return ins

    def dma(self, qk, out_ap, in_ap, reads, writes, sb, grouped=False, **kw):
        if sb.dsem is None:
            sb.dsem, sb.dtot = self._alloc_dsem()
        for b in reads:
            if b.writer is not None:
                self._wait(qk, b.writer)
        for b in writes:
            if b.writer is not None:
                if not (grouped and b.writer[0] is sb.dsem):
                    self._wait(qk, b.writer)
            for r in b.readers:
                self._wait(qk, r)
        if not grouped and sb.dtot > 0:
            self._wait(qk, (sb.dsem, sb.dtot, "dma"))
        if sb.dtot + 16 > SEM_LIMIT:
            self._wait(qk, (sb.dsem, sb.dtot, "dma"))
            sb.dsem, sb.dtot = self._alloc_dsem()
        sb.dtot += 16
        tok = (sb.dsem, sb.dtot, "dma")
        ins = self.engs[qk].dma_start(out=out_ap, in_=in_ap, **kw)
        ins.then_inc(sb.dsem, 16)
        self._mark(tok, reads, writes)
        self.ninst += 1
        return ins


    def allgather(self, in_buf, out_buf, groups):
        self.pool_used = True
        self._deps("pool", [in_buf], [out_buf])
        sem = self.sem("cc", root=True)
        ins = self.nc.gpsimd.collective_compute("AllGather", ALU.bypass, replica_groups=groups,
                                                ins=[in_buf[:].opt()], outs=[out_buf[:].opt()])
        ins.then_inc(sem, 1)
        self._mark((sem, 1, "cc"), [in_buf], [out_buf])
        self.ninst += 1

    def barrier(self):
        toks = [(self.csem[e], self.ccnt[e], e) for e in self.csem]
        for b in self.bufs:
            if b.dsem is not None and b.dtot > 0:
                toks.append((b.dsem, b.dtot, "dma"))
        for b in self.bufs:
            for t in ([b.writer] if b.writer is not None else []) + list(b.readers):
                if t[2] == "cc":
                    toks.append(t)
        same = self.same
        self.same = True
        for e in list(self.engs.keys()):
            if e == "pool" and "pool" not in self.csem and not self.pool_used:
                continue
            for t in toks:
                self._wait(e, t)
        self.same = same
        for b in self.bufs:
            b.writer = None
            b.readers = []

    @contextlib.contextmanager
    def scope(self):
        old = self.es
        self.es = contextlib.ExitStack()
        self.depth += 1
        nb = len(self.bufs)
        try:
            yield
        finally:
            self.barrier()
            dead = self.bufs[nb:]
            for b in dead:
                if b.depth >= self.depth and b.dsem is not None:
                    if b.dtot + 16 * 64 < SEM_LIMIT:
                        self.free_dsems.append((b.dsem, b.dtot))
                    b.dsem = None
                    b.dtot = 0
            self.bufs = self.bufs[:nb] + [b for b in dead if b.depth < self.depth]
            self.es.close()
            self.es = old
            self.depth -= 1

    def ts(self, out, in0, s1, op0, R, W, s2=None, op1=None, eng="dve"):
        if op1 is None:
            return self.op(eng, lambda e: e.tensor_scalar(out=out, in0=in0, scalar1=s1, scalar2=None, op0=op0), R, W)
        return self.op(eng, lambda e: e.tensor_scalar(out=out, in0=in0, scalar1=s1, scalar2=s2, op0=op0, op1=op1), R, W)

    def tt(self, out, a, b, op, R, W, eng="dve"):
        return self.op(eng, lambda e: e.tensor_tensor(out=out, in0=a, in1=b, op=op), R, W)

    def stt(self, out, in0, scalar, in1, op0, op1, R, W):
        return self.op("dve", lambda e: e.scalar_tensor_tensor(out=out, in0=in0, scalar=scalar, in1=in1, op0=op0, op1=op1), R, W)

    def act(self, out, in_, func, R, W, scale=None, bias=None):
        kw = {}
        if scale is not None:
            kw["scale"] = scale
        if bias is not None:
            kw["bias"] = bias
        return self.op("act", lambda e: e.activation(out=out, in_=in_, func=func, **kw), R, W)

    def mm(self, out, lhsT, rhs, R, W, start=True, stop=True):
        if FP32R and lhsT.dtype == F32 and rhs.dtype == F32:
            lhsT = lhsT.bitcast(mybir.dt.float32r)
            rhs = rhs.bitcast(mybir.dt.float32r)
        return self.op("pe", lambda e: e.matmul(out, lhsT=lhsT, rhs=rhs, start=start, stop=stop), R, W, acc=True)

    def cp(self, out, in_, R, W, eng="dve"):
        if eng == "act":
            return self.op("act", lambda e: e.activation(out=out, in_=in_, func=AF.Copy), R, W)
        return self.op(eng, lambda e: e.tensor_copy(out=out, in_=in_), R, W)

    def scan(self, out, d0, d1, init, op0, op1, R, W):
        return self.op("dve", lambda e: e.tensor_tensor_scan(out=out, data0=d0, data1=d1, initial=init, op0=op0, op1=op1), R, W)

    def _alloc_dsem(self):
        if self.free_dsems:
            self.free_dsems.sort(key=lambda t: t[1])
            return self.free_dsems.pop(0)
        return (self.sem("dq", root=True), 0)

    def finish(self, ek="sp"):
        for b in self.bufs:
            if b.dsem is not None and b.dtot > 0:
                self._wait(ek, (b.dsem, b.dtot, "dma"))

    def close(self):
        self.es.close()


class Ctx:
    pass


DBG = {}


def dbg(k, name, buf, shape):
    if not DBG.get("on"):
        return
    d = k.dram("dbg_" + name, list(shape), F32, kind="ExternalOutput")
    k.dma("sp", d[:], buf[:], [buf], [d], buf)


def dbgap(k, name, buf, ap, shape):
    if not DBG.get("on"):
        return
    d = k.dram("dbg_" + name, list(shape), F32, kind="ExternalOutput")
    k.dma("sp", d[:], ap, [buf], [d], buf)


def fv(buf, p0, npart, off, dims):
    a = buf.t[:]
    pstep = a.ap[0][0]
    return bass.AP(a.tensor, a.offset + p0 * pstep + off, [[pstep, npart]] + [[st, ct] for (st, ct) in dims])


def setup_common(k, cx):
    cx.psum = [k.ps(f"psb{i}", [128, 512], F32) for i in range(8)]
    cx.consts_d = k.dram("consts", [128, CONST_COLS], F32, kind="ExternalInput")
    cx.consts = k.sb("consts_sb", [128, CONST_COLS], F32)
    k.dma("sp", cx.consts[:], cx.consts_d[:], [cx.consts_d], [cx.consts], cx.consts)
    cx.ones_bf = k.sb("ones_bf", [128, 128], BF16)
    k.op("dve", lambda e: e.memset(cx.ones_bf[:], 1.0), [], [cx.ones_bf])


C_IDENT = 0
C_ONES = 128
C_RST0 = 256
C_RSTN = 768
C_MASK = 1280
M_LE, M_GE, M_LT, M_GT = 0, 1, 2, 3
C_SEL = 3328
CONST_COLS = 3584


def make_consts():
    c = np.zeros((128, CONST_COLS), np.float32)
    c[:, C_IDENT:C_IDENT + 128] = np.eye(128, dtype=np.float32)
    c[:, C_ONES:C_ONES + 128] = 1.0
    col = np.arange(512)
    c[:, C_RST0:C_RST0 + 512] = np.where(col % 64 == 0, 0.0, 1.0)[None, :]
    c[:, C_RSTN:C_RSTN + 512] = np.where(col % 64 == 0, NEG, 0.0)[None, :]
    jj = np.arange(64)[:, None]
    ii = np.arange(64)[None, :]
    for m, ok in enumerate([jj <= ii, jj >= ii, jj < ii, jj > ii]):
        c[:64, C_MASK + m * 512:C_MASK + (m + 1) * 512] = np.tile(np.where(ok, 0.0, NEG), (1, 8))
    for hh in range(2):
        c[hh, C_SEL + hh * 128:C_SEL + (hh + 1) * 128] = 1.0
    return c


def phase_mod(k, cx, cT_d, adaw_d, adab_d, ncol_chunks, tag="m0"):
    cs = k.sb(tag + "cs", [128, 8], F32)
    k.dma("sp", cs[:], cT_d[:], [cT_d], [cs], cs)
    k.op("act", lambda e: e.activation(out=cs[:], in_=cs[:], func=AF.Silu), [cs], [cs])
    ncols = ncol_chunks * 128
    wbuf = [k.sb(tag + f"adaw{i}", [128, ncols], F32) for i in range(2)]
    pm = cx.psum[0]
    for kc in range(8):
        wb = wbuf[kc % 2]
        k.dma("sp", wb[:], adaw_d[kc * 128:(kc + 1) * 128, :], [adaw_d], [wb], wb)
        for cc in range(ncol_chunks):
            k.op("pe", lambda e, wb=wb, cc=cc, kc=kc: e.matmul(
                pm[:, cc * 8 + kc:cc * 8 + kc + 1], lhsT=wb[:, cc * 128:(cc + 1) * 128], rhs=cs[:, kc:kc + 1],
                start=True, stop=True), [wb, cs], [pm], acc=True)
    mod = k.sb(tag + "mod", [128, ncol_chunks], F32)
    k.op("dve", lambda e: e.tensor_reduce(
        out=mod[:], in_=pm[:, 0:ncol_chunks * 8].rearrange("p (c k) -> p c k", k=8), axis=AX.X, op=ALU.add),
        [pm], [mod])
    ab = k.sb(tag + "adab", [128, ncol_chunks], F32)
    k.dma("sp", ab[:], adab_d[:], [adab_d], [ab], ab)
    k.op("dve", lambda e: e.tensor_tensor(out=mod[:], in0=mod[:], in1=ab[:], op=ALU.add), [mod, ab], [mod])
    return mod


def load_w_bf16(k, Wb, W_d, ncols, tag):
    with k.scope():
        wst = [k.sb(tag + f"wst{i}", [128, 1024], F32) for i in range(2)]
        n = 0
        for kc in range(8):
            for c0 in range(0, ncols, 1024):
                w = min(1024, ncols - c0)
                ws = wst[n % 2]
                k.dma("sp", ws[:, 0:w], W_d[kc * 128:(kc + 1) * 128, c0:c0 + w], [W_d], [ws], ws)
                k.cp(Wb[:, kc, c0:c0 + w], ws[:, 0:w], [ws], [Wb], eng=("act" if n % 2 else "dve"))
                n += 1


def phase_norm_proj(k, cx, T, xT_ap_fn, xT_bufs, A, B, W_d, ncols, fm_specs, tm_specs, tag="p1", producer=None):
    Wb = k.sb(tag + "Wb", [128, 8, ncols], BF16)
    load_w_bf16(k, Wb, W_d, ncols, tag)
    X = [k.sb(tag + f"X{i}", [128, 8, 512], F32) for i in range(2)]
    sq = k.sb(tag + "sq", [128, 8, 512], BF16)
    rstd = k.sb(tag + "rstd", [128, 512], F32)
    tmp = [k.sb(tag + f"tmp{i}", [128, 512], F32) for i in range(2)]
    hT = k.sb(tag + "hT", [128, 8, 512], BF16)
    stg = [k.sb(tag + f"stg{i}", [128, 512], F32) for i in range(4)]
    nst = 0
    npb = 0
    ntiles = T // 512
    for tt in range(ntiles):
        Xt = X[tt % 2]
        if producer is None:
            if tt == 0:
                k.dma("sp", Xt[:], xT_ap_fn(tt), xT_bufs, [Xt], Xt)
            if tt + 1 < ntiles:
                k.dma("sp", X[(tt + 1) % 2][:], xT_ap_fn(tt + 1), xT_bufs, [X[(tt + 1) % 2]], X[(tt + 1) % 2])
        else:
            if tt == 0:
                producer[0](tt, Xt)
            post = producer[1](tt, Xt)
            if tt + 1 < ntiles:
                producer[0](tt + 1, X[(tt + 1) % 2])
            post()
        k.op("act", lambda e, Xt=Xt: e.activation(out=sq[:], in_=Xt[:], func=AF.Square), [Xt], [sq])
        pss = cx.psum[7]
        for kc in range(8):
            k.op("pe", lambda e, kc=kc: e.matmul(pss[:], lhsT=cx.ones_bf[:], rhs=sq[:, kc, :],
                                                 start=(kc == 0), stop=(kc == 7)), [cx.ones_bf, sq], [pss], acc=True)
        k.op("act", lambda e: e.activation(out=rstd[:], in_=pss[:], func=AF.Sqrt, scale=1.0 / 1024.0, bias=float(EPS)),
             [pss], [rstd])
        k.op("dve", lambda e: e.reciprocal(out=rstd[:], in_=rstd[:]), [rstd], [rstd])
        for kc in range(8):
            tb = tmp[kc % 2]
            k.op("dve", lambda e, kc=kc, tb=tb, Xt=Xt: e.tensor_tensor(out=tb[:], in0=Xt[:, kc, :], in1=rstd[:], op=ALU.mult),
                 [Xt, rstd], [tb])
            k.op("act", lambda e, kc=kc, tb=tb: e.activation(out=hT[:, kc, :], in_=tb[:], func=AF.Identity,
                                                             scale=A[:, kc:kc + 1], bias=B[:, kc:kc + 1]),
                 [tb, A, B], [hT])
        for (c0, nr, dbuf, dfn, scale) in fm_specs:
            if dfn(tt) is None:
                continue
            pb = cx.psum[npb % 6]
            npb += 1
            for kc in range(8):
                k.op("pe", lambda e, kc=kc, pb=pb, c0=c0, nr=nr: e.matmul(
                    pb[0:nr, :], lhsT=Wb[:, kc, c0:c0 + nr], rhs=hT[:, kc, :], start=(kc == 0), stop=(kc == 7)),
                    [Wb, hT], [pb], acc=True)
            sg = stg[nst % 4]
            nst += 1
            if nst % 2:
                k.op("act", lambda e, pb=pb, sg=sg, nr=nr, scale=scale: e.activation(
                    out=sg[0:nr, :], in_=pb[0:nr, :], func=AF.Copy, scale=float(scale)), [pb], [sg])
            else:
                k.op("dve", lambda e, pb=pb, sg=sg, nr=nr, scale=scale: e.tensor_scalar(
                    out=sg[0:nr, :], in0=pb[0:nr, :], scalar1=float(scale), scalar2=None, op0=ALU.mult), [pb], [sg])
            k.dma("sp", dfn(tt), sg[0:nr, :], [sg], [dbuf], sg)
        for ts in range(4):
            for (c0, ncl, dbuf, dfn) in tm_specs:
                if dfn(tt, ts) is None:
                    continue
                pb = cx.psum[npb % 6]
                npb += 1
                for kc in range(8):
                    k.op("pe", lambda e, kc=kc, pb=pb, c0=c0, ncl=ncl, ts=ts: e.matmul(
                        pb[:, 0:ncl], lhsT=hT[:, kc, ts * 128:(ts + 1) * 128], rhs=Wb[:, kc, c0:c0 + ncl],
                        start=(kc == 0), stop=(kc == 7)), [Wb, hT], [pb], acc=True)
                sg = stg[nst % 4]
                nst += 1
                if nst % 2:
                    k.op("act", lambda e, pb=pb, sg=sg, ncl=ncl: e.activation(
                        out=sg[:, 0:ncl], in_=pb[:, 0:ncl], func=AF.Copy), [pb], [sg])
                else:
                    k.op("dve", lambda e, pb=pb, sg=sg, ncl=ncl: e.tensor_copy(out=sg[:, 0:ncl], in_=pb[:, 0:ncl]), [pb], [sg])
                k.dma("sp", dfn(tt, ts), sg[:, 0:ncl], [sg], [dbuf], sg)


FM_MQ, FM_MK, FM_G, FM_DQ, FM_DK, FM_DV = 0, 128, 256, 272, 528, 784
FM_ROWS = 1040
TM0 = FM_ROWS
TM_MK, TM_MV, TM_MO, TM_Z = TM0, TM0 + 128, TM0 + 384, TM0 + 640
NC1 = TM0 + 1152


def row_softplus_neg(k, out, x, tmp1, tmp2, n, R):
    k.stt(tmp1[0:n, :], x[0:n, :], -1.0, x[0:n, :], ALU.mult, ALU.max, [x], [tmp1])
    k.act(tmp1[0:n, :], tmp1[0:n, :], AF.Exp, [tmp1], [tmp1], scale=-1.0)
    k.act(tmp1[0:n, :], tmp1[0:n, :], AF.Ln, [tmp1], [tmp1], bias=1.0)
    k.stt(out[0:n, :], x[0:n, :], 0.0, tmp1[0:n, :], ALU.min, ALU.subtract, [x, tmp1], [out])


def dirview(buf, n, d, G=512):
    if d == 0:
        return fv(buf, 0, n, 0, [(1, G)])
    return fv(buf, 0, n, G - 1, [(-1, G)])


def phase_mlstm(k, cx, T, FM, TM, gb_d, HS):
    NG = T // 512
    cs = cx.consts
    ident = cs
    st = []
    for d in range(2):
        s = Ctx()
        s.gi = k.sb(f"ml_gi{d}", [2, 512], F32)
        s.gf = k.sb(f"ml_gf{d}", [2, 512], F32)
        s.t1 = k.sb(f"ml_t1{d}", [2, 512], F32)
        s.t2 = k.sb(f"ml_t2{d}", [2, 512], F32)
        s.b = k.sb(f"ml_b{d}", [2, 512], F32)
        s.g = k.sb(f"ml_g{d}", [2, 512], F32)
        s.pm = k.sb(f"ml_pm{d}", [2, 512], F32)
        s.negmu = k.sb(f"ml_negmu{d}", [2, 512], F32)
        s.wint = k.sb(f"ml_wint{d}", [2, 512], F32)
        s.emt = k.sb(f"ml_emt{d}", [2, 512], F32)
        s.kwf = k.sb(f"ml_kwf{d}", [2, 512], F32)
        s.mnext = k.sb(f"ml_mnext{d}", [2, 8], F32)
        s.mcur = k.sb(f"ml_mcur{d}", [2, 8], F32)
        s.c8 = k.sb(f"ml_c8{d}", [2, 8], F32)
        s.wold = k.sb(f"ml_wold{d}", [2, 8], F32)
        s.carry = k.sb(f"ml_carry{d}", [2, 1], F32)
        s.bi = k.sb(f"ml_bi{d}", [2, 1], F32)
        s.bf = k.sb(f"ml_bf{d}", [2, 1], F32)
        k.dma("sp", s.bi[:], gb_d[2 * d:2 * d + 2, :], [gb_d], [s.bi], s.bi)
        k.dma("sp", s.bf[:], gb_d[4 + 2 * d:4 + 2 * d + 2, :], [gb_d], [s.bf], s.bf)
        k.op("dve", lambda e, s=s: e.memset(s.carry[:], 0.0), [], [s.carry])
        s.cols = k.sb(f"ml_cols{d}", [64, 64], F32)
        s.woldc = k.sb(f"ml_woldc{d}", [64, 16], F32)
        s.QT = [k.sb(f"ml_QT{d}{h}", [64, 512], F32) for h in range(2)]
        s.KT = [k.sb(f"ml_KT{d}{h}", [64, 512], F32) for h in range(2)]
        s.Ktm = k.sb(f"ml_Ktm{d}", [64, 8, 128], F32)
        s.Va = [k.sb(f"ml_Va{d}{h}", [64, 8, 129], F32) for h in range(2)]
        for h in range(2):
            k.op("dve", lambda e, s=s, h=h: e.memset(s.Va[h][:], 1.0), [], [s.Va[h]])
        s.E = [k.sb(f"ml_E{d}{h}", [64, 512], F32) for h in range(2)]
        s.AT = [k.sb(f"ml_AT{d}{h}", [64, 512], F32) for h in range(2)]
        s.C = [k.sb(f"ml_C{d}{h}", [64, 129], F32) for h in range(2)]
        for h in range(2):
            k.op("dve", lambda e, s=s, h=h: e.memset(s.C[h][:], 0.0), [], [s.C[h]])
        s.tmp = [k.sb(f"ml_tmp{d}{h}", [64, 129], F32) for h in range(2)]
        s.R = [k.sb(f"ml_R{d}{h}", [64, 129], F32) for h in range(2)]
        s.dn = [k.sb(f"ml_dn{d}{h}", [64, 2], F32) for h in range(2)]
        s.KW = [k.sb(f"ml_KW{d}{h}", [64, 64], F32) for h in range(2)]
        s.Hg = k.sb(f"ml_Hg{d}", [64, 8, 256], F32)
        st.append(s)
    TMr = TM[:].rearrange("(c p) f -> p c f", p=64)
    def body(step, d):
        s = st[d]
        pb = lambda i_: cx.psum[4 * d + i_]
        grp = step if d == 0 else NG - 1 - step
        t0 = grp * 512
        c0 = grp * 8
        k.dma("sp", s.gi[:], FM[FM_G + 2 * d:FM_G + 2 * d + 2, t0:t0 + 512], [FM], [s.gi], s.gi)
        k.dma("sp", s.gf[:], FM[FM_G + 4 + 2 * d:FM_G + 4 + 2 * d + 2, t0:t0 + 512], [FM], [s.gf], s.gf)
        for h in range(2):
            k.dma("sp", s.QT[h][:], FM[FM_MQ + 64 * h:FM_MQ + 64 * h + 64, t0:t0 + 512], [FM], [s.QT[h]], s.QT[h])
            k.dma("sp", s.KT[h][:], FM[FM_MK + 64 * h:FM_MK + 64 * h + 64, t0:t0 + 512], [FM], [s.KT[h]], s.KT[h])
            k.dma("sp", s.Va[h][:, :, 0:128], TMr[:, c0:c0 + 8, 128 + 128 * h:256 + 128 * h], [TM], [s.Va[h]], s.Va[h])
        k.dma("sp", s.Ktm[:], TMr[:, c0:c0 + 8, 0:128], [TM], [s.Ktm], s.Ktm)
        yield
        k.ts(s.gi[:], s.gi[:], s.bi[:, 0:1], ALU.add, [s.gi, s.bi], [s.gi])
        k.ts(s.gf[:], s.gf[:], s.bf[:, 0:1], ALU.add, [s.gf, s.bf], [s.gf])
        row_softplus_neg(k, s.t2, s.gf, s.t1, None, 2, None)
        k.scan(dirview(s.b, 2, d), cs[0:2, C_RST0:C_RST0 + 512], dirview(s.t2, 2, d), 0.0, ALU.mult, ALU.add,
               [s.t2, cs], [s.b])
        k.tt(s.g[:], s.gi[:], s.b[:], ALU.subtract, [s.gi, s.b], [s.g])
        k.scan(dirview(s.pm, 2, d), cs[0:2, C_RSTN:C_RSTN + 512], dirview(s.g, 2, d), 0.0, ALU.add, ALU.max,
               [s.g, cs], [s.pm])
        last = 63 if d == 0 else 0
        bL = fv(s.b, 0, 2, last, [(64, 8)])
        pmL = fv(s.pm, 0, 2, last, [(64, 8)])
        if d == 0:
            o8 = lambda buf: fv(buf, 0, 2, 0, [(1, 8)])
        else:
            o8 = lambda buf: fv(buf, 0, 2, 7, [(-1, 8)])
        bLd = fv(s.b, 0, 2, last, [(64, 8)]) if d == 0 else fv(s.b, 0, 2, last + 64 * 7, [(-64, 8)])
        pmLd = fv(s.pm, 0, 2, last, [(64, 8)]) if d == 0 else fv(s.pm, 0, 2, last + 64 * 7, [(-64, 8)])
        k.scan(o8(s.mnext), pmLd, bLd, s.carry[:, 0:1], ALU.max, ALU.add, [s.pm, s.b, s.carry], [s.mnext])
        if d == 0:
            k.cp(s.mcur[:, 0:1], s.carry[:, 0:1], [s.carry], [s.mcur])
            k.cp(s.mcur[:, 1:8], s.mnext[:, 0:7], [s.mnext], [s.mcur])
            k.cp(s.carry[:, 0:1], s.mnext[:, 7:8], [s.mnext, s.mcur], [s.carry])
        else:
            k.cp(s.mcur[:, 7:8], s.carry[:, 0:1], [s.carry], [s.mcur])
            k.cp(s.mcur[:, 0:7], s.mnext[:, 1:8], [s.mnext], [s.mcur])
            k.cp(s.carry[:, 0:1], s.mnext[:, 0:1], [s.mnext, s.mcur], [s.carry])
        mc_b = fv(s.mcur, 0, 2, 0, [(1, 8), (0, 64)])
        v3 = lambda buf: fv(buf, 0, 2, 0, [(64, 8), (1, 64)])
        k.tt(v3(s.t1), v3(s.pm), mc_b, ALU.max, [s.pm, s.mcur], [s.t1])
        k.ts(s.negmu[:], s.t1[:], -1.0, ALU.mult, [s.t1], [s.negmu])
        k.tt(v3(s.wint), mc_b, v3(s.t1), ALU.subtract, [s.mcur, s.t1], [s.wint])
        k.act(s.wint[:], s.wint[:], AF.Exp, [s.wint], [s.wint])
        k.tt(s.emt[:], s.b[:], s.t1[:], ALU.add, [s.b, s.t1], [s.emt])
        k.act(s.emt[:], s.emt[:], AF.Exp, [s.emt], [s.emt], scale=-1.0)
        k.tt(s.c8[:], bL, s.mnext[:], ALU.subtract, [s.b, s.mnext], [s.c8])
        k.tt(v3(s.kwf), v3(s.g), fv(s.c8, 0, 2, 0, [(1, 8), (0, 64)]), ALU.add, [s.g, s.c8], [s.kwf])
        k.act(s.kwf[:], s.kwf[:], AF.Exp, [s.kwf], [s.kwf])
        k.tt(s.wold[:], s.c8[:], s.mcur[:], ALU.add, [s.c8, s.mcur], [s.wold])
        k.act(s.wold[:], s.wold[:], AF.Exp, [s.wold], [s.wold])
        yield
        pc = pb(0)
        for c in range(8):
            for qi, qb in enumerate([s.g, s.wint, s.emt, s.kwf]):
                col = (c * 4 + qi) * 2
                k.mm(pc[0:64, col:col + 2], qb[0:2, c * 64:(c + 1) * 64], cs[0:2, C_IDENT:C_IDENT + 2], [qb, cs], [pc])
        for h in range(2):
            k.mm(pc[0:64, 64 + 8 * h:64 + 8 * h + 8], cs[0:2, C_SEL + 128 * h:C_SEL + 128 * h + 64], s.wold[0:2, :], [cs, s.wold], [pc])
        k.cp(s.cols[:], pc[0:64, 0:64], [pc], [s.cols])
        k.cp(s.woldc[:], pc[0:64, 64:80], [pc], [s.woldc], eng="act")
        yield
        mk = M_LE if d == 0 else M_GE
        for h in range(2):
            pD = pb(0)
            sel = cs[0:2, C_SEL + 128 * h:C_SEL + 128 * h + 64]
            k.mm(pD[0:64, :], sel, s.negmu[0:2, :], [cs, s.negmu], [pD], start=True, stop=False)
            k.mm(pD[0:64, :], cs[0:64, C_IDENT:C_IDENT + 64], cs[0:64, C_MASK + mk * 512:C_MASK + (mk + 1) * 512], [cs], [pD],
                 start=False, stop=False)
            for c in range(8):
                k.mm(pD[0:64, c * 64:(c + 1) * 64], s.g[0:2, c * 64:(c + 1) * 64], sel, [s.g, cs], [pD],
                     start=False, stop=(c == 7))
            k.act(s.E[h][:], pD[0:64, :], AF.Exp, [pD], [s.E[h]])
            pS = pb(1)
            for c in range(8):
                k.mm(pS[0:64, c * 64:(c + 1) * 64], s.KT[h][:, c * 64:(c + 1) * 64], s.QT[h][:, c * 64:(c + 1) * 64],
                     [s.KT[h], s.QT[h]], [pS])
            k.tt(s.AT[h][:], pS[0:64, :], s.E[h][:], ALU.mult, [pS, s.E[h]], [s.AT[h]])
            yield
        if step == 0:
            for nm in ["b", "g", "pm", "negmu", "wint", "emt", "kwf"]:
                dbg(k, f"{nm}{d}", getattr(s, nm), [2, 512])
            dbg(k, f"mnext{d}", s.mnext, [2, 8]); dbg(k, f"mcur{d}", s.mcur, [2, 8]); dbg(k, f"wold{d}", s.wold, [2, 8])
            dbg(k, f"cols{d}", s.cols, [64, 64]); dbg(k, f"woldc{d}", s.woldc, [64, 16])
            dbg(k, f"E{d}", s.E[0], [64, 512]); dbg(k, f"AT{d}", s.AT[0], [64, 512])
        for ci in range(8):
            c = ci if d == 0 else 7 - ci
            for h in range(2):
                pH = pb(2 + h)
                cl = lambda qi: s.cols[:, (c * 4 + qi) * 2 + h:(c * 4 + qi) * 2 + h + 1]
                k.mm(pH[0:64, 0:129], s.AT[h][:, c * 64:(c + 1) * 64], s.Va[h][:, c, :], [s.AT[h], s.Va[h]], [pH])
                k.mm(pH[0:64, 256:385], s.QT[h][:, c * 64:(c + 1) * 64], s.C[h][:], [s.QT[h], s.C[h]], [pH])
                k.ts(s.tmp[h][:], pH[0:64, 256:385], cl(1), ALU.mult, [pH, s.cols], [s.tmp[h]])
                k.tt(s.R[h][:], s.tmp[h][:], pH[0:64, 0:129], ALU.add, [s.tmp[h], pH], [s.R[h]])
                k.stt(s.dn[h][:, 0:1], s.R[h][:, 128:129], -1.0, s.R[h][:, 128:129], ALU.mult, ALU.max, [s.R[h]], [s.dn[h]])
                k.ts(s.dn[h][:, 0:1], s.dn[h][:, 0:1], cl(2), ALU.max, [s.dn[h], s.cols], [s.dn[h]])
                k.op("dve", lambda e, h=h: e.reciprocal(out=s.dn[h][:, 1:2], in_=s.dn[h][:, 0:1]), [s.dn[h]], [s.dn[h]])
                k.ts(s.Hg[:, c, 128 * h:128 * h + 128], s.R[h][:, 0:128], s.dn[h][:, 1:2], ALU.mult, [s.R[h], s.dn[h]], [s.Hg])
                k.ts(s.KW[h][:], s.Ktm[:, c, 64 * h:64 * h + 64], cl(3), ALU.mult, [s.Ktm, s.cols], [s.KW[h]], s2=0.125, op1=ALU.mult)
                pU = pb(1)
                k.mm(pU[0:64, 256 * h:256 * h + 129], s.KW[h][:], s.Va[h][:, c, :], [s.KW[h], s.Va[h]], [pU])
                k.stt(s.C[h][:], s.C[h][:], s.woldc[:, 8 * h + c:8 * h + c + 1], pU[0:64, 256 * h:256 * h + 129], ALU.mult, ALU.add,
                      [s.C[h], s.woldc, pU], [s.C[h]])
                yield
        k.dma("sp", HS[d][t0:t0 + 512, :].rearrange("(c p) f -> p c f", p=64), s.Hg[:], [s.Hg], [HS[d]], s.Hg)

    for step in range(NG):
        gens = [body(step, 0), body(step, 1)]
        while gens:
            for g_ in list(gens):
                try:
                    next(g_)
                except StopIteration:
                    gens.remove(g_)


def phase_gdn_pre(k, cx, T, FM, dcw_d, QN, KN, KTM, VTM):
    cs = cx.consts
    NT_ = T // 512
    dcw = k.sb("g_dcw", [128, 6, 5], F32)
    k.dma("sp", dcw[:], dcw_d[:], [dcw_d], [dcw], dcw)
    dg = k.sb("g_diag", [128, 30, 128], F32)
    for i in range(6):
        for j in range(5):
            k.ts(dg[:, i * 5 + j, :], cs[:, C_IDENT:C_IDENT + 128], dcw[:, i, j:j + 1], ALU.mult, [cs, dcw], [dg],
                 eng=("dve" if (i + j) % 2 else "pool"))
    Xs = [k.sb(f"g_X{i}", [128, 516], F32) for i in range(2)]
    Y = [k.sb(f"g_Y{i}", [128, 512], F32) for i in range(2)]
    sq = k.sb("g_sq", [128, 512], F32)
    rs = k.sb("g_rs", [128, 512], F32)
    Yn = [k.sb(f"g_Yn{i}", [128, 512], F32) for i in range(2)]
    tr = [k.sb(f"g_tr{i}", [128, 512], F32) for i in range(2)]
    def gp_load(n_):
        tt_, i_ = n_ // 6, n_ % 6
        t0_ = tt_ * 512
        X = Xs[n_ % 2]
        row0 = FM_DQ + 128 * i_
        lo = max(t0_ - 2, 0)
        hi = min(t0_ + 514, T)
        if tt_ == 0:
            k.op("dve", lambda e, X=X: e.memset(X[:, 0:2], 0.0), [], [X])
        if tt_ == NT_ - 1:
            k.op("dve", lambda e, X=X: e.memset(X[:, 514:516], 0.0), [], [X])
        k.dma("sp", X[:, lo - (t0_ - 2):hi - (t0_ - 2)], FM[row0:row0 + 128, lo:hi], [FM], [X], X)

    n = 0
    for tt in range(NT_):
        t0 = tt * 512
        for i in range(6):
            X = Xs[n % 2]
            if n == 0:
                gp_load(0)
            if n + 1 < NT_ * 6:
                gp_load(n + 1)
            pc_ = cx.psum[n % 2]
            for j in range(5):
                k.mm(pc_[:, :], dg[:, i * 5 + j, :], X[:, j:j + 512], [dg, X], [pc_], start=(j == 0), stop=(j == 4))
            Yt = Y[n % 2]
            k.act(Yt[:], pc_[:, :], AF.Silu, [pc_], [Yt])
            h = i % 2
            kind = i // 2
            if kind < 2:
                k.tt(sq[:], Yt[:], Yt[:], ALU.mult, [Yt], [sq])
                pss = cx.psum[2]
                k.mm(pss[:, :], cs[:, C_ONES:C_ONES + 128], sq[:], [cs, sq], [pss])
                k.act(rs[:], pss[:, :], AF.Sqrt, [pss], [rs], bias=float(EPS))
                k.op("dve", lambda e: e.reciprocal(out=rs[:], in_=rs[:]), [rs], [rs])
                Ynt = Yn[n % 2]
                k.stt(Ynt[:], Yt[:], (128.0 ** -0.5) if kind == 0 else 1.0, rs[:], ALU.mult, ALU.mult, [Yt, rs], [Ynt])
                dst = QN[h] if kind == 0 else KN[h]
                k.dma("sp", dst[:, t0:t0 + 512], Ynt[:], [Ynt], [dst], Ynt)
                src = Ynt
            else:
                src = Yt
            if kind >= 1:
                pt = cx.psum[3 + n % 2]
                for ts_ in range(4):
                    k.mm(pt[:, ts_ * 128:(ts_ + 1) * 128], src[:, ts_ * 128:(ts_ + 1) * 128], cs[:, C_IDENT:C_IDENT + 128], [src, cs], [pt])
                trt = tr[n % 2]
                k.cp(trt[:], pt[:, :], [pt], [trt], eng="act")
                dst = KTM[h] if kind == 1 else VTM[h]
                k.dma("sp", dst[t0:t0 + 512, :].rearrange("(s p) f -> p s f", p=128), trt[:].rearrange("p (s f) -> p s f", f=128),
                      [trt], [dst], trt)
            n += 1


def phase_gdn(k, cx, T, FM, QN, KN, KTM, VTM, gpar_d, OS):
    NG = T // 512
    cs = cx.consts
    ID64 = cs[0:64, C_IDENT:C_IDENT + 64]
    st = []
    for d in range(2):
        s = Ctx()
        for nm in ["br", "ar", "t1", "t2", "beta", "gc", "ngc", "gcb", "bg", "kd", "eg"]:
            setattr(s, nm, k.sb(f"gd_{nm}{d}", [2, 512], F32))
        s.G1 = k.sb(f"gd_G1{d}", [64, 512], F32)
        s.Nm = k.sb(f"gd_N{d}", [64, 512], F32)
        s.NTm = k.sb(f"gd_NT{d}", [64, 512], F32)
        s.P2 = [k.sb(f"gd_P2{d}{i}", [64, 512], F32) for i in range(2)]
        s.PT2 = [k.sb(f"gd_PT2{d}{i}", [64, 512], F32) for i in range(2)]
        s.XT = k.sb(f"gd_XT{d}", [64, 512], F32)
        s.gamT = k.sb(f"gd_gamT{d}", [64, 512], F32)
        s.gl8 = k.sb(f"gd_gl8{d}", [2, 8], F32)
        s.egl = k.sb(f"gd_egl{d}", [2, 8], F32)
        s.par = k.sb(f"gd_par{d}", [2, 2], F32)
        k.dma("sp", s.par[:], gpar_d[2 * d:2 * d + 2, :], [gpar_d], [s.par], s.par)
        k.act(s.par[:, 1:2], s.par[:, 1:2], AF.Exp, [s.par], [s.par])
        k.ts(s.par[:, 1:2], s.par[:, 1:2], -1.0, ALU.mult, [s.par], [s.par])
        s.cols = k.sb(f"gd_cols{d}", [64, 48], F32)
        s.eglc = k.sb(f"gd_eglc{d}", [128, 16], F32)
        s.QN = [k.sb(f"gd_QN{d}{h}", [128, 512], F32) for h in range(2)]
        s.KN = [k.sb(f"gd_KN{d}{h}", [128, 512], F32) for h in range(2)]
        s.qg = [k.sb(f"gd_qg{d}{h}", [128, 512], F32) for h in range(2)]
        s.Ktm = [k.sb(f"gd_Ktm{d}{h}", [64, 8, 128], F32) for h in range(2)]
        s.Vtm = [k.sb(f"gd_Vtm{d}{h}", [64, 8, 128], F32) for h in range(2)]
        s.XTb = [k.sb(f"gd_XTb{d}{h}", [64, 512], BF16) for h in range(2)]
        s.Vtb = [k.sb(f"gd_Vtb{d}{h}", [64, 8, 128], BF16) for h in range(2)]
        s.XTbg = [k.sb(f"gd_XTbg{d}{h}", [64, 512], F32) for h in range(2)]
        s.attnT = [k.sb(f"gd_attnT{d}{h}", [64, 512], BF16) for h in range(2)]
        s.nwT = [k.sb(f"gd_nwT{d}{h}", [128, 512], F32) for h in range(2)]
        s.S = [k.sb(f"gd_S{d}{h}", [128, 128], F32) for h in range(2)]
        for h in range(2):
            k.op("dve", lambda e, s=s, h=h: e.memset(s.S[h][:], 0.0), [], [s.S[h]])
        s.vn = [k.sb(f"gd_vn{d}{h}", [64, 128], BF16) for h in range(2)]
        s.kdm = [k.sb(f"gd_kdm{d}{h}", [64, 128], BF16) for h in range(2)]
        s.Og = k.sb(f"gd_Og{d}", [64, 8, 256], F32)
        st.append(s)
    def body(step, d):
        s = st[d]
        pb = lambda i_: cx.psum[4 * d + i_]
        G1, Nm, NTm, P2, PT2, XT, gamT = s.G1, s.Nm, s.NTm, s.P2, s.PT2, s.XT, s.gamT
        grp = step if d == 0 else NG - 1 - step
        t0 = grp * 512
        c0 = grp * 8
        k.dma("sp", s.br[:], FM[FM_G + 8 + 2 * d:FM_G + 8 + 2 * d + 2, t0:t0 + 512], [FM], [s.br], s.br)
        k.dma("sp", s.ar[:], FM[FM_G + 12 + 2 * d:FM_G + 12 + 2 * d + 2, t0:t0 + 512], [FM], [s.ar], s.ar)
        for h in range(2):
            k.dma("sp", s.QN[h][:], QN[h][:, t0:t0 + 512], [QN[h]], [s.QN[h]], s.QN[h])
            k.dma("sp", s.KN[h][:], KN[h][:, t0:t0 + 512], [KN[h]], [s.KN[h]], s.KN[h])
            k.dma("sp", s.Ktm[h][:], KTM[h][t0:t0 + 512, :].rearrange("(c p) f -> p c f", p=64), [KTM[h]], [s.Ktm[h]], s.Ktm[h])
            k.dma("sp", s.Vtm[h][:], VTM[h][t0:t0 + 512, :].rearrange("(c p) f -> p c f", p=64), [VTM[h]], [s.Vtm[h]], s.Vtm[h])
        yield
        k.act(s.beta[:], s.br[:], AF.Sigmoid, [s.br], [s.beta])
        row_softplus_neg(k, s.t2, s.br, s.t1, None, 2, None)
        k.ts(s.ar[:], s.ar[:], s.par[:, 0:1], ALU.add, [s.ar, s.par], [s.ar], s2=-1.0, op1=ALU.mult)
        row_softplus_neg(k, s.eg, s.ar, s.t1, None, 2, None)
        k.ts(s.eg[:], s.eg[:], s.par[:, 1:2], ALU.mult, [s.eg, s.par], [s.eg], s2=-1.0, op1=ALU.mult)
        k.scan(dirview(s.gc, 2, d), cs[0:2, C_RST0:C_RST0 + 512], dirview(s.eg, 2, d), 0.0, ALU.mult, ALU.add, [s.eg, cs], [s.gc])
        k.ts(s.ngc[:], s.gc[:], -1.0, ALU.mult, [s.gc], [s.ngc])
        k.tt(s.gcb[:], s.gc[:], s.t2[:], ALU.add, [s.gc, s.t2], [s.gcb])
        last = 63 if d == 0 else 0
        gl = fv(s.gc, 0, 2, last, [(64, 8)])
        k.cp(s.gl8[:], gl, [s.gc], [s.gl8])
        k.act(s.egl[:], s.gl8[:], AF.Exp, [s.gl8], [s.egl])
        k.act(s.eg[:], s.gc[:], AF.Exp, [s.gc], [s.eg])
        k.tt(s.bg[:], s.beta[:], s.eg[:], ALU.mult, [s.beta, s.eg], [s.bg])
        v3 = lambda buf: fv(buf, 0, 2, 0, [(64, 8), (1, 64)])
        k.tt(v3(s.kd), fv(s.gl8, 0, 2, 0, [(1, 8), (0, 64)]), v3(s.gc), ALU.subtract, [s.gl8, s.gc], [s.kd])
        k.act(s.kd[:], s.kd[:], AF.Exp, [s.kd], [s.kd])
        yield
        pc = pb(3)
        for c in range(8):
            for qi, qb in enumerate([s.beta, s.bg, s.kd]):
                col = (c * 3 + qi) * 2
                k.mm(pc[0:64, col:col + 2], qb[0:2, c * 64:(c + 1) * 64], cs[0:2, C_IDENT:C_IDENT + 2], [qb, cs], [pc])
        for h in range(2):
            k.mm(pc[:, 64 + 8 * h:64 + 8 * h + 8], cs[0:2, C_SEL + 128 * h:C_SEL + 128 * h + 128], s.egl[0:2, :], [cs, s.egl], [pc])
        k.cp(s.cols[:], pc[0:64, 0:48], [pc], [s.cols])
        k.cp(s.eglc[:], pc[:, 64:80], [pc], [s.eglc], eng="act")
        mT = M_LE if d == 0 else M_GE
        mS = M_GT if d == 0 else M_LT
        for h in range(2):
            sel64 = cs[0:2, C_SEL + 128 * h:C_SEL + 128 * h + 64]
            sel128 = cs[0:2, C_SEL + 128 * h:C_SEL + 128 * h + 128]
            pq = pb(3)
            k.mm(pq[:, :], sel128, s.eg[0:2, :], [cs, s.eg], [pq])
            k.tt(s.qg[h][:], pq[:, :], s.QN[h][:], ALU.mult, [pq, s.QN[h]], [s.qg[h]])
            yield
            pD = pb(3)
            k.mm(pD[0:64, :], sel64, s.ngc[0:2, :], [cs, s.ngc], [pD], start=True, stop=False)
            k.mm(pD[0:64, :], ID64, cs[0:64, C_MASK + mS * 512:C_MASK + (mS + 1) * 512], [cs], [pD], start=False, stop=False)
            for c in range(8):
                k.mm(pD[0:64, c * 64:(c + 1) * 64], s.gcb[0:2, c * 64:(c + 1) * 64], sel64, [s.gcb, cs], [pD], start=False, stop=(c == 7))
            k.act(G1[:], pD[0:64, :], AF.Exp, [pD], [G1])
            pK = pb(2)
            for c in range(8):
                k.mm(pK[0:64, c * 64:(c + 1) * 64], s.KN[h][:, c * 64:(c + 1) * 64], s.KN[h][:, c * 64:(c + 1) * 64], [s.KN[h]], [pK])
            k.stt(Nm[:], pK[0:64, :], -1.0, G1[:], ALU.mult, ALU.mult, [pK, G1], [Nm])
            k.mm(pD[0:64, :], sel64, s.gc[0:2, :], [cs, s.gc], [pD], start=True, stop=False)
            k.mm(pD[0:64, :], ID64, cs[0:64, C_MASK + mT * 512:C_MASK + (mT + 1) * 512], [cs], [pD], start=False, stop=False)
            for c in range(8):
                k.mm(pD[0:64, c * 64:(c + 1) * 64], s.ngc[0:2, c * 64:(c + 1) * 64], sel64, [s.ngc, cs], [pD], start=False, stop=(c == 7))
            k.act(gamT[:], pD[0:64, :], AF.Exp, [pD], [gamT])
            for c in range(8):
                k.mm(pK[0:64, c * 64:(c + 1) * 64], s.KN[h][:, c * 64:(c + 1) * 64], s.QN[h][:, c * 64:(c + 1) * 64], [s.KN[h], s.QN[h]], [pK])
            k.tt(s.attnT[h][:], pK[0:64, :], gamT[:], ALU.mult, [pK, gamT], [s.attnT[h]])
            yield
            pA, pB, pC = pb(0), pb(1), pb(2)
            for c in range(8):
                k.mm(pA[0:64, c * 64:(c + 1) * 64], Nm[:, c * 64:(c + 1) * 64], ID64, [Nm, cs], [pA])
            k.cp(NTm[:], pA[0:64, :], [pA], [NTm], eng="act")
            k.tt(fv(XT, 0, 64, 0, [(64, 8), (1, 64)]), fv(NTm, 0, 64, 0, [(64, 8), (1, 64)]),
                 fv(cs, 0, 64, C_IDENT, [(0, 8), (1, 64)]), ALU.add, [NTm, cs], [XT])
            Pc, PTc = Nm, NTm
            for m in range(5):
                Pn, PTn = P2[m % 2], PT2[m % 2]
                for c in range(8):
                    sl = slice(c * 64, (c + 1) * 64)
                    k.mm(pA[0:64, sl], PTc[:, sl], Pc[:, sl], [PTc, Pc], [pA])
                if m < 4:
                    for c in range(8):
                        sl = slice(c * 64, (c + 1) * 64)
                        k.mm(pB[0:64, sl], Pc[:, sl], PTc[:, sl], [PTc, Pc], [pB])
                k.cp(Pn[:], pA[0:64, :], [pA], [Pn], eng="act")
                if m < 4:
                    k.cp(PTn[:], pB[0:64, :], [pB], [PTn])
                for c in range(8):
                    sl = slice(c * 64, (c + 1) * 64)
                    k.mm(pC[0:64, sl], Pn[:, sl], XT[:, sl], [Pn, XT], [pC])
                k.tt(XT[:], XT[:], pC[0:64, :], ALU.add, [XT, pC], [XT])
                Pc, PTc = Pn, PTn
                yield
            for c in range(8):
                sl = slice(c * 64, (c + 1) * 64)
                k.ts(s.XTb[h][:, sl], XT[:, sl], s.cols[:, (c * 3 + 0) * 2 + h:(c * 3 + 0) * 2 + h + 1], ALU.mult, [XT, s.cols], [s.XTb[h]])
                k.ts(s.XTbg[h][:, sl], XT[:, sl], s.cols[:, (c * 3 + 1) * 2 + h:(c * 3 + 1) * 2 + h + 1], ALU.mult, [XT, s.cols], [s.XTbg[h]], eng="pool")
            yield
            k.cp(s.Vtb[h][:], s.Vtm[h][:], [s.Vtm[h]], [s.Vtb[h]], eng="pool")
            for c in range(8):
                sl = slice(c * 64, (c + 1) * 64)
                k.mm(pA[:, sl], s.Ktm[h][:, c, :], s.XTbg[h][:, sl], [s.Ktm[h], s.XTbg[h]], [pA])
            k.act(s.nwT[h][:], pA[:, :], AF.Copy, [pA], [s.nwT[h]], scale=-1.0)
        for ci in range(8):
            c = ci if d == 0 else 7 - ci
            sl = slice(c * 64, (c + 1) * 64)
            for h in range(2):
                pV = pb(0)
                pU = pb(1)
                vr = pV[0:64, 256 * h:256 * h + 128]
                orr = pV[0:64, 256 * h + 128:256 * h + 256]
                k.mm(vr, s.XTb[h][:, sl], s.Vtb[h][:, c, :], [s.XTb[h], s.Vtb[h]], [pV], start=True, stop=False)
                k.mm(vr, s.nwT[h][:, sl], s.S[h][:], [s.nwT[h], s.S[h]], [pV], start=False, stop=True)
                k.cp(s.vn[h][:], vr, [pV], [s.vn[h]])
                k.mm(orr, s.qg[h][:, sl], s.S[h][:], [s.qg[h], s.S[h]], [pV], start=True, stop=False)
                k.mm(orr, s.attnT[h][:, sl], s.vn[h][:], [s.attnT[h], s.vn[h]], [pV], start=False, stop=True)
                k.cp(s.Og[:, c, 128 * h:128 * h + 128], orr, [pV], [s.Og], eng="act")
                k.ts(s.kdm[h][:], s.Ktm[h][:, c, :], s.cols[:, (c * 3 + 2) * 2 + h:(c * 3 + 2) * 2 + h + 1], ALU.mult, [s.Ktm[h], s.cols], [s.kdm[h]], eng="pool")
                ur = pU[:, 128 * h:128 * h + 128]
                k.mm(ur, s.kdm[h][:], s.vn[h][:], [s.kdm[h], s.vn[h]], [pU])
                k.stt(s.S[h][:], s.S[h][:], s.eglc[:, 8 * h + c:8 * h + c + 1], ur, ALU.mult, ALU.add, [s.S[h], s.eglc, pU], [s.S[h]])
                yield
        k.dma("sp", OS[d][t0:t0 + 512, :].rearrange("(c p) f -> p c f", p=64), s.Og[:], [s.Og], [OS[d]], s.Og)

    for step in range(NG):
        gens = [body(step, 0), body(step, 1)]
        while gens:
            for g_ in list(gens):
                try:
                    next(g_)
                except StopIteration:
                    gens.remove(g_)


def phase_even_combine(k, cx, T, TM, HS, OS, gn_d, MIX, MIXT=None, GMs=None):
    gn = k.sb("cb_gn", [128, 512], F32)
    k.dma("sp", gn[:], gn_d[:].partition_broadcast(128), [gn_d], [gn], gn)
    NTl = T // 128
    bufs = []
    for i in range(2):
        b = Ctx()
        b.a = k.sb(f"cb_a{i}", [128, 512], F32)
        b.b = k.sb(f"cb_b{i}", [128, 512], F32)
        b.moz = k.sb(f"cb_moz{i}", [128, 768], F32)
        b.sq = k.sb(f"cb_sq{i}", [128, 512], F32)
        b.ss = k.sb(f"cb_ss{i}", [128, 4], F32)
        b.tr = k.sb(f"cb_tr{i}", [128, 512], F32)
        bufs.append(b)
    def cb_load(tt):
        b = bufs[tt % 2]
        r = slice(tt * 128, (tt + 1) * 128)
        k.dma("sp", b.a[:, 0:256], HS[0][r, :], [HS[0]], [b.a], b.a, grouped=True)
        k.dma("sp", b.a[:, 256:512], OS[0][r, :], [OS[0]], [b.a], b.a, grouped=True)
        k.dma("sp", b.b[:, 0:256], HS[1][r, :], [HS[1]], [b.b], b.b, grouped=True)
        k.dma("sp", b.b[:, 256:512], OS[1][r, :], [OS[1]], [b.b], b.b, grouped=True)
        k.dma("sp", b.moz[:], TM[r, 384:1152], [TM], [b.moz], b.moz)

    cb_load(0)
    for tt in range(NTl):
        b = bufs[tt % 2]
        r = slice(tt * 128, (tt + 1) * 128)
        if tt + 1 < NTl:
            cb_load(tt + 1)
        k.tt(b.a[:], b.a[:], b.b[:], ALU.add, [b.a, b.b], [b.a])
        k.tt(b.sq[:], b.a[:], b.a[:], ALU.mult, [b.a], [b.sq])
        k.op("dve", lambda e, b=b: e.tensor_reduce(out=b.ss[:], in_=b.sq[:].rearrange("p (h f) -> p h f", f=128), axis=AX.X, op=ALU.add),
             [b.sq], [b.ss])
        k.act(b.ss[:], b.ss[:], AF.Sqrt, [b.ss], [b.ss], scale=1.0 / 128.0, bias=float(EPS))
        k.op("dve", lambda e, b=b: e.reciprocal(out=b.ss[:], in_=b.ss[:]), [b.ss], [b.ss])
        k.tt(b.a[:].rearrange("p (h f) -> p h f", f=128), b.a[:].rearrange("p (h f) -> p h f", f=128),
             fv(b.ss, 0, 128, 0, [(1, 4), (0, 128)]), ALU.mult, [b.a, b.ss], [b.a])
        k.tt(b.a[:], b.a[:], gn[:], ALU.mult, [b.a, gn], [b.a])
        k.act(b.moz[:, 0:256], b.moz[:, 0:256], AF.Sigmoid, [b.moz], [b.moz])
        k.act(b.moz[:, 256:768], b.moz[:, 256:768], AF.Silu, [b.moz], [b.moz])
        k.tt(b.a[:, 0:256], b.a[:, 0:256], b.moz[:, 0:256], ALU.mult, [b.a, b.moz], [b.a])
        k.tt(b.a[:], b.a[:], b.moz[:, 256:768], ALU.mult, [b.a, b.moz], [b.a])
        if MIXT is None:
            k.dma("sp", MIX[r, :], b.a[:], [b.a], [MIX], b.a)
        else:
            pt = cx.psum[tt % 2]
            for fc in range(4):
                k.mm(pt[:, fc * 128:(fc + 1) * 128], b.a[:, fc * 128:(fc + 1) * 128], cx.consts[:, C_IDENT:C_IDENT + 128], [b.a, cx.consts], [pt])
            k.cp(b.tr[:], pt[:, :], [pt], [b.tr], eng="act")
            mxt = MIXT[(tt * 128) // 1024]
            tl = (tt * 128) % 1024
            k.dma("sp", mxt[:].rearrange("(c p) t -> p c t", p=128)[:, :, tl:tl + 128],
                  b.tr[:].rearrange("p (c t) -> p c t", t=128), [b.tr], [mxt], b.tr)
            if GMs is not None and tl == 1024 - 128:
                k.allgather(mxt, GMs[(tt * 128) // 1024], [[0, 1], [2, 3], [4, 5], [6, 7]])


def build_even(T, debug=False, phases=("ml", "gdn", "comb")):
    nc = bass.Bass("TRN2", target_bir_lowering=False)
    k = KB(nc)
    cx = Ctx()
    setup_common(k, cx)
    kd = "ExternalOutput" if debug else "Internal"
    emit_even(k, cx, T, kd, phases)
    k.finish()
    k.close()
    print("even program: instructions", k.ninst, "sems", k.nsem)
    return nc


def emit_even(k, cx, T, kd, phases=("ml", "gdn", "comb"), MIXT=None, GMs=None):
    xT = k.dram("xT", [1024, T], F32, kind="ExternalInput")
    cT = k.dram("cT", [128, 8], F32, kind="ExternalInput")
    adaw = k.dram("adaw", [1024, 2048], F32, kind="ExternalInput")
    adab = k.dram("adab", [128, 16], F32, kind="ExternalInput")
    ng = k.dram("ng", [128, 8], F32, kind="ExternalInput")
    W = k.dram("W", [1024, NC1], F32, kind="ExternalInput")
    mgb = k.dram("mgb", [8, 1], F32, kind="ExternalInput")
    gpar = k.dram("gpar", [4, 2], F32, kind="ExternalInput")
    dcw = k.dram("dcw", [128, 6, 5], F32, kind="ExternalInput")
    gn = k.dram("gn", [1, 512], F32, kind="ExternalInput")
    MIX = k.dram("MIX", [T, 512], F32, kind="ExternalOutput") if MIXT is None else None
    OS = [k.dram(f"OS{d}", [T, 256], F32, kind=kd) for d in range(2)]
    QN = [k.dram(f"QN{h}", [128, T], F32, kind=kd) for h in range(2)]
    KN = [k.dram(f"KN{h}", [128, T], F32, kind=kd) for h in range(2)]
    KTM = [k.dram(f"KTM{h}", [T, 128], F32, kind=kd) for h in range(2)]
    VTM = [k.dram(f"VTM{h}", [T, 128], F32, kind=kd) for h in range(2)]
    FM = k.dram("FM", [FM_ROWS, T], F32, kind=kd)
    TM = k.dram("TM", [T, 1152], F32, kind=kd)
    HS = [k.dram(f"HS{d}", [T, 256], F32, kind=kd) for d in range(2)]

    A = k.sb("evA", [128, 8], F32)
    Bm = k.sb("evBm", [128, 8], F32)
    with k.scope():
        mod = phase_mod(k, cx, cT, adaw, adab, 16, tag="me")
        ngs = k.sb("evngs", [128, 8], F32)
        k.dma("sp", ngs[:], ng[:], [ng], [ngs], ngs)
        k.stt(A[:], mod[:, 8:16], 1.0, ngs[:], ALU.add, ALU.mult, [mod, ngs], [A])
        k.cp(Bm[:], mod[:, 0:8], [mod], [Bm])
    sc1 = k.scope()
    sc1.__enter__()
    xT3 = xT[:].rearrange("(k p) t -> p k t", p=128)
    fm_specs = []
    for (c0, nr, scale) in [(FM_MQ, 128, 1.0), (FM_MK, 128, 0.125), (FM_G, 16, 1.0)] + \
            [(FM_DQ + 128 * i, 128, 1.0) for i in range(6)]:
        fm_specs.append((c0, nr, FM, (lambda tt, r0=c0, nr=nr: FM[r0:r0 + nr, tt * 512:(tt + 1) * 512]), scale))
    tm_specs = []
    for (c0, ncl) in [(TM_MK, 384), (TM_MO, 256), (TM_Z, 512)]:
        tm_specs.append((c0, ncl, TM, (lambda tt, ts, c0=c0, ncl=ncl: TM[tt * 512 + ts * 128: tt * 512 + (ts + 1) * 128, c0 - TM0:c0 - TM0 + ncl])))
    phase_norm_proj(k, cx, T, lambda tt: xT3[:, :, tt * 512:(tt + 1) * 512], [xT], A, Bm, W, NC1, fm_specs, tm_specs)
    sc1.__exit__(None, None, None)
    if "ml" in phases:
        with k.scope():
            phase_mlstm(k, cx, T, FM, TM, mgb, HS)
    if "gdn" in phases:
        with k.scope():
            phase_gdn_pre(k, cx, T, FM, dcw, QN, KN, KTM, VTM)
        with k.scope():
            phase_gdn(k, cx, T, FM, QN, KN, KTM, VTM, gpar, OS)
    if "comb" in phases:
        with k.scope():
            phase_even_combine(k, cx, T, TM, HS, OS, gn, MIX, MIXT, GMs)


SEQ = 8192


def colform(v, nchunks):
    return np.ascontiguousarray(np.asarray(v, np.float32).reshape(nchunks, 128).T)


def even_core_inputs(inp, b, e, T=SEQ):
    w_in = inp["ev_w_in"][0]
    o = np.cumsum([0, 256, 256, 512, 512, 16, 1536, 16, 1024])
    mq, mk, mv, mo, mg, dqkv, dg, z = [w_in[:, o[i]:o[i + 1]] for i in range(8)]
    hs = [2 * e, 2 * e + 1]
    gsel = [t * 4 + h for t in range(4) for h in hs]
    cols = [mq[:, 128 * e:128 * e + 128], mk[:, 128 * e:128 * e + 128], mg[:, gsel], dg[:, gsel]]
    for part in range(3):
        for h in hs:
            cols.append(dqkv[:, part * 512 + h * 128: part * 512 + (h + 1) * 128])
    cols += [mk[:, 128 * e:128 * e + 128], mv[:, 256 * e:256 * e + 256], mo[:, 256 * e:256 * e + 256],
             z[:, 256 * e:256 * e + 256], z[:, 512 + 256 * e:512 + 256 * e + 256]]
    W = np.ascontiguousarray(np.concatenate(cols, axis=1))
    assert W.shape[1] == NC1
    cw = inp["ev_dn_conv_w"][0]
    dcw = np.stack([cw[:, part * 512 + h * 128: part * 512 + (h + 1) * 128] for part in range(3) for h in hs], 0)
    return {
        "consts": make_consts(),
        "xT": np.ascontiguousarray(inp["x"][b, :T].T),
        "cT": colform(inp["c"][b], 8),
        "adaw": np.ascontiguousarray(inp["ada_w"][0][:, 0:2048]),
        "adab": colform(inp["ada_b"][0][0:2048], 16),
        "ng": colform(inp["norm_g"][0], 8),
        "W": W,
        "mgb": np.ascontiguousarray(inp["ev_m_gate_b"][0][:, hs].reshape(8, 1)),
        "gpar": np.ascontiguousarray(np.stack([inp["ev_dn_dt_bias"][0][:, hs].reshape(4), inp["ev_dn_a_log"][0][:, hs].reshape(4)], 1)),
        "dcw": np.ascontiguousarray(dcw.transpose(2, 0, 1)),
        "gn": np.ascontiguousarray(np.concatenate([inp["ev_m_norm_g"][0][256 * e:256 * e + 256],
                                                   inp["ev_dn_norm_g"][0][256 * e:256 * e + 256]])[None, :]),
    }


def run_even(inp, T=SEQ):
    nc = build_even(T)
    in_maps = [even_core_inputs(inp, c // 2, c % 2, T) for c in range(8)]
    res = run_bass_kernel_spmd(nc, in_maps, core_ids=list(range(8)))
    mix = np.zeros((4, T, 1024), np.float32)
    for c in range(8):
        b, e = c // 2, c % 2
        m = res.results[c]["MIX"]
        mix[b, :, 256 * e:256 * e + 256] = m[:, 0:256]
        mix[b, :, 512 + 256 * e:512 + 256 * e + 256] = m[:, 256:512]
    return mix


HALO = 1024
NEG_ATT = -30000.0
C2_J = 0
C2_OH = 128
C2_COLS = 128 + 3 * 384
DILS = (1, 4, 16)
VV_COLS = 33 + 4 * 9 + 16 * 3


def make_vv(TO, s_half, nhalves):
    cols = []
    S = TO * nhalves
    for d in DILS:
        npos = TO // d
        nkt = npos // 128 + 1
        for r in range(d):
            p = -64 + 128 * np.arange(nkt)[None, :] + np.arange(128)[:, None]
            tok = s_half * TO + r + d * p
            cols.append(((tok >= 0) & (tok < S)).astype(np.float32))
    return np.ascontiguousarray(np.concatenate(cols, axis=1))


def t5_bucket_np(rel):
    n = np.abs(rel)
    large = 8 + (np.log(np.maximum(n, 1).astype(np.float32) / np.float32(8)) / np.float32(np.log(128.0)) * np.float32(8)).astype(np.int32)
    large = np.minimum(large, 15)
    return (rel > 0).astype(np.int32) * 16 + np.where(n < 8, n, large)


def make_consts2():
    c = np.zeros((128, C2_COLS), np.float32)
    c[np.arange(128), C2_J + 127 - np.arange(128)] = 1.0
    for g, d in enumerate(DILS):
        for u in range(383):
            rel = u - 191
            if abs(rel) <= 64:
                c[t5_bucket_np(np.array(rel * d)), C2_OH + g * 384 + u] = 1.0
            else:
                c[32, C2_OH + g * 384 + u] = NEG_ATT
    return c


def phase_attention(k, cx, TO, QT, KT, VT, vv_d, relb_d, relbT_d, GF, DT, c2):
    E = TO + 2 * HALO
    cs = cx.consts
    rb = k.sb("at_rb", [33, 8], F32)
    k.op("dve", lambda e: e.memset(rb[:], 1.0), [], [rb])
    k.dma("sp", rb[0:32, :], relb_d[:], [relb_d], [rb], rb)
    k.ts(rb[0:32, :], rb[0:32, :], 8.0, ALU.mult, [rb], [rb])
    gfs = k.sb("at_gfs", [8, 3 * 384], F32)
    pg = cx.psum[0]
    for g in range(3):
        k.mm(pg[0:8, 0:384], rb[0:33, :], c2[0:33, C2_OH + g * 384:C2_OH + (g + 1) * 384], [rb, c2], [pg])
        k.cp(gfs[:, g * 384:(g + 1) * 384], pg[0:8, 0:384], [pg], [gfs])
    k.dma("sp", GF[:], gfs[:], [gfs], [GF], gfs)
    bmx = k.sb("at_bmx", [128, 32], F32)
    bmax = k.sb("at_bmax", [128, 1], F32)
    Qa = k.sb("at_Qa", [65, TO], F32)
    Ka = k.sb("at_Ka", [65, E], F32)
    k.op("dve", lambda e: e.memset(Ka[64:65, :], 1.0), [], [Ka])
    Qb = k.sb("at_Qb", [65, TO], BF16)
    Kb = k.sb("at_Kb", [65, E], BF16)
    Hb = k.sb("at_Hb", [128, 6, 128], BF16)
    Jb = k.sb("at_Jb", [128, 128], BF16)
    k.cp(Jb[:], c2[:, C2_J:C2_J + 128], [c2], [Jb])
    sqt = k.sb("at_sq", [64, 512], F32)
    kmx = k.sb("at_kmx", [128, 16], F32)
    kmax = k.sb("at_kmax", [128, 1], F32)
    accN = k.sb("at_accN", [64, TO], F32)
    accD = k.sb("at_accD", [64, TO], F32)
    H = k.sb("at_H", [128, 6, 128], F32)
    Vf = [k.sb(f"at_Vf{i}", [128, 34, 64], F32) for i in range(2)]
    Vt = [k.sb(f"at_Vt{i}", [128, 34, 64], BF16) for i in range(2)]
    Vv = [k.sb(f"at_Vv{i}", [128, 34, 64], BF16) for i in range(2)]
    PT = [k.sb(f"at_PT{i}", [128, 512], BF16) for i in range(2)]
    ones64 = cs[0:64, C_ONES:C_ONES + 128]
    VV = k.sb("at_VV", [128, vv_d[:].shape[1]], F32)
    k.dma("sp", VV[:], vv_d[:], [vv_d], [VV], VV)
    vvoff = 0
    VVO = {}
    for g_, d_ in enumerate(DILS):
        for r_ in range(d_):
            VVO[(g_, r_)] = vvoff
            vvoff += (TO // d_) // 128 + 1
    assert vvoff == vv_d[:].shape[1]
    nv = 0
    npt = 0
    for hd in range(8):
        r0 = hd * 64
        k.dma("sp", Qa[0:64, :], QT[r0:r0 + 64, :], [QT], [Qa], Qa)
        k.dma("sp", Ka[0:64, :], KT[r0:r0 + 64, :], [KT], [Ka], Ka)
        for g in range(3):
            for kt in range(2):
                src = bass.AP(GF[:].tensor, GF[:].offset + hd * 1152 + g * 384 + kt * 128, [[1, 128], [1, 128]])
                k.dma("sp", H[:, g * 2 + kt, :], src, [GF], [H], H, grouped=True)
        k.dma("sp", bmx[:], relbT_d[hd:hd + 1, :].partition_broadcast(128), [relbT_d], [bmx], bmx)
        k.op("dve", lambda e: e.tensor_reduce(out=bmax[:], in_=bmx[:], axis=AX.X, op=ALU.max), [bmx], [bmax])
        k.ts(bmax[:], bmax[:], 8.0, ALU.mult, [bmax], [bmax])
        pk = cx.psum[1]
        for t in range(E // 512):
            k.tt(sqt[:], Ka[0:64, t * 512:(t + 1) * 512], Ka[0:64, t * 512:(t + 1) * 512], ALU.mult, [Ka], [sqt])
            k.mm(pk[:, :], ones64, sqt[:], [cs, sqt], [pk])
            k.op("dve", lambda e, t=t: e.tensor_reduce(out=kmx[:, t:t + 1], in_=pk[:, :], axis=AX.X, op=ALU.max), [pk], [kmx])
        k.op("dve", lambda e: e.tensor_reduce(out=kmax[:], in_=kmx[:, 0:E // 512], axis=AX.X, op=ALU.max), [kmx], [kmax])
        k.act(kmax[:], kmax[:], AF.Sqrt, [kmax], [kmax])
        for t in range(TO // 512):
            k.tt(sqt[:], Qa[0:64, t * 512:(t + 1) * 512], Qa[0:64, t * 512:(t + 1) * 512], ALU.mult, [Qa], [sqt])
            k.mm(pk[:, :], ones64, sqt[:], [cs, sqt], [pk])
            k.act(Qa[64:65, t * 512:(t + 1) * 512], pk[64:65, :], AF.Sqrt, [pk], [Qa])
        k.ts(Qa[64:65, :], Qa[64:65, :], kmax[64:65, 0:1], ALU.mult, [Qa, kmax], [Qa], s2=bmax[64:65, 0:1], op1=ALU.add)
        k.ts(Qa[64:65, :], Qa[64:65, :], -1.0, ALU.mult, [Qa], [Qa])
        k.cp(Qb[:], Qa[:], [Qa], [Qb], eng="act")
        k.cp(Kb[:], Ka[:], [Ka], [Kb])
        k.cp(Hb[:], H[:], [H], [Hb], eng="pool")
        if hd == 0:
            dbg(k, "H", H, [128, 6, 128]); dbg(k, "Qa", Qa, [65, TO]); dbg(k, "Ka", Ka, [65, E])
        first = True
        for g, d in enumerate(DILS):
            npos = TO // d
            nkt = npos // 128 + 1
            for r in range(d):
                V1 = Vt[nv % 2]
                V2 = Vv[nv % 2]
                V1f = Vf[nv % 2]
                nv += 1
                base = HALO + r - 64 * d
                for n0 in range(0, nkt, 8):
                    nn = min(8, nkt - n0)
                    vsrc = bass.AP(VT[:].tensor, VT[:].offset + (base + n0 * 128 * d) * 512 + r0, [[d * 512, 128], [128 * d * 512, nn], [1, 64]])
                    k.dma("sp", V1f[:, n0:n0 + nn, :], vsrc, [VT], [V1f], V1f, grouped=True)
                vb_ = fv(VV, 0, 128, VVO[(g, r)], [(1, nkt), (0, 64)])
                k.tt(V1[:, 0:nkt, :], V1f[:, 0:nkt, :], vb_, ALU.mult, [V1f, VV], [V1])
                k.cp(V2[:, 0:nkt, :], vb_, [VV], [V2], eng="pool")
                nqt = npos // 128
                for qb in range(0, nqt, 2):
                    nq = min(2, nqt - qb)
                    pS = cx.psum[2 + npt % 2]
                    for qi in range(nq):
                        qt = qb + qi
                        qap = fv(Qb, 0, 65, r + d * 128 * qt, [(d, 128)])
                        for kt in range(2):
                            kap = fv(Kb, 0, 65, HALO + r + d * (128 * qt - 64 + 128 * kt), [(d, 128)])
                            col = (qi * 2 + kt) * 128
                            k.mm(pS[:, col:col + 128], kap, qap, [Kb, Qb], [pS], start=True, stop=False)
                            k.mm(pS[:, col:col + 128], Hb[:, g * 2 + kt, :], Jb[:], [Hb, Jb], [pS], start=False, stop=True)
                    P = PT[npt % 2]
                    k.act(P[:, 0:nq * 256], pS[:, 0:nq * 256], AF.Exp, [pS], [P], scale=0.125)
                    if hd == 0 and g == 0 and qb == 0:
                        dbg(k, "P", P, [128, 512]); dbgap(k, "V1", V1, V1[:, 0:9, :], [128, 9, 64]); dbgap(k, "V2", V2, V2[:, 0:9, :], [128, 9, 64])
                    pN = cx.psum[4 + npt % 2]
                    pDn = cx.psum[6 + npt % 2]
                    npt += 1
                    for qi in range(nq):
                        qt = qb + qi
                        for kt in range(2):
                            col = (qi * 2 + kt) * 128
                            k.mm(pN[0:64, qi * 128:(qi + 1) * 128], V1[:, qt + kt, :], P[:, col:col + 128], [V1, P], [pN],
                                 start=(kt == 0), stop=(kt == 1))
                            k.mm(pDn[0:64, qi * 128:(qi + 1) * 128], V2[:, qt + kt, :], P[:, col:col + 128], [V2, P], [pDn],
                                 start=(kt == 0), stop=(kt == 1))
                    qcols = nq * 128
                    an = fv(accN, 0, 64, r + d * 128 * qb, [(d, qcols)])
                    ad = fv(accD, 0, 64, r + d * 128 * qb, [(d, qcols)])
                    if first:
                        k.cp(an, pN[0:64, 0:qcols], [pN], [accN], eng="act")
                        k.cp(ad, pDn[0:64, 0:qcols], [pDn], [accD])
                    else:
                        k.tt(an, an, pN[0:64, 0:qcols], ALU.add, [accN, pN], [accN], eng="dve")
                        k.tt(ad, ad, pDn[0:64, 0:qcols], ALU.add, [accD, pDn], [accD], eng="dve")
            first = False
        k.op("dve", lambda e: e.reciprocal(out=accD[:], in_=accD[:]), [accD], [accD])
        k.tt(accN[:], accN[:], accD[:], ALU.mult, [accN, accD], [accN])
        k.dma("sp", DT[r0:r0 + 64, :], accN[:], [accN], [DT], accN)


def phase_conv_module(k, cx, TO, GA, GB, valid_d, cw_d, cpar_d, CT):
    cs = cx.consts
    cw = k.sb("cv_w", [128, 4, 31], F32)
    k.dma("sp", cw[:], cw_d[:], [cw_d], [cw], cw)
    cpar = k.sb("cv_par", [128, 4, 3], F32)
    k.dma("sp", cpar[:], cpar_d[:], [cpar_d], [cpar], cpar)
    dg = k.sb("cv_diag", [128, 124, 128], BF16)
    for ch in range(4):
        for j in range(31):
            k.ts(dg[:, ch * 31 + j, :], cs[:, C_IDENT:C_IDENT + 128], cw[:, ch, j:j + 1], ALU.mult, [cs, cw], [dg],
                 eng=("dve" if j % 2 else "pool"))
    ga = [k.sb(f"cv_ga{i}", [128, 4, 542], F32) for i in range(2)]
    gb = [k.sb(f"cv_gb{i}", [128, 4, 542], F32) for i in range(2)]
    vbs = [k.sb(f"cv_vb{i}", [128, 542], F32) for i in range(2)]
    uin = k.sb("cv_uin", [128, 4, 542], BF16)
    U = k.sb("cv_U", [128, 4, 512], F32)
    XC = k.sb("cv_XC", [128, 4, 512], F32)
    SQ = k.sb("cv_SQ", [128, 4, 512], F32)
    rs = k.sb("cv_rs", [128, 512], F32)
    O = [k.sb(f"cv_O{i}", [128, 4, 512], F32) for i in range(2)]
    ones = cs[:, C_ONES:C_ONES + 128]
    def cv_load(tt):
        e0 = HALO + tt * 512 - 15
        a, b, vb = ga[tt % 2], gb[tt % 2], vbs[tt % 2]
        k.dma("sp", a[:], GA[:].rearrange("(c p) t -> p c t", p=128)[:, :, e0:e0 + 542], [GA], [a], a)
        k.dma("sp", b[:], GB[:].rearrange("(c p) t -> p c t", p=128)[:, :, e0:e0 + 542], [GB], [b], b)
        k.dma("sp", vb[:], valid_d[0:1, e0:e0 + 542].partition_broadcast(128), [valid_d], [vb], vb)

    cv_load(0)
    for tt in range(TO // 512):
        a, b, vb = ga[tt % 2], gb[tt % 2], vbs[tt % 2]
        if tt + 1 < TO // 512:
            cv_load(tt + 1)
        k.act(b[:], b[:], AF.Sigmoid, [b], [b])
        k.tt(a[:], a[:], b[:], ALU.mult, [a, b], [a])
        k.tt(uin[:], a[:], fv(vb, 0, 128, 0, [(0, 4), (1, 542)]), ALU.mult, [a, vb], [uin])
        for ch in range(4):
            pc_ = cx.psum[ch % 2]
            for j in range(31):
                k.mm(pc_[:, :], dg[:, ch * 31 + j, :], uin[:, ch, j:j + 512], [dg, uin], [pc_], start=(j == 0), stop=(j == 30))
            k.ts(U[:, ch, :], pc_[:, :], cpar[:, ch, 0:1], ALU.add, [pc_, cpar], [U])
        pm = cx.psum[2]
        for ch in range(4):
            k.mm(pm[:, :], ones, U[:, ch, :], [cs, U], [pm], start=(ch == 0), stop=(ch == 3))
        for ch in range(4):
            k.stt(XC[:, ch, :], pm[:, :], -1.0 / 512.0, U[:, ch, :], ALU.mult, ALU.add, [pm, U], [XC])
        k.tt(SQ[:], XC[:], XC[:], ALU.mult, [XC], [SQ], eng="pool")
        pv = cx.psum[3]
        for ch in range(4):
            k.mm(pv[:, :], ones, SQ[:, ch, :], [cs, SQ], [pv], start=(ch == 0), stop=(ch == 3))
        k.act(rs[:], pv[:, :], AF.Sqrt, [pv], [rs], scale=1.0 / 512.0, bias=float(EPS))
        k.op("dve", lambda e: e.reciprocal(out=rs[:], in_=rs[:]), [rs], [rs])
        Ot = O[tt % 2]
        for ch in range(4):
            k.tt(XC[:, ch, :], XC[:, ch, :], rs[:], ALU.mult, [XC, rs], [XC])
            k.act(Ot[:, ch, :], XC[:, ch, :], AF.Silu, [XC, cpar], [Ot], scale=cpar[:, ch, 1:2], bias=cpar[:, ch, 2:3])
        k.dma("sp", CT[:].rearrange("(c p) t -> p c t", p=128)[:, :, tt * 512:(tt + 1) * 512], Ot[:], [Ot], [CT], Ot)


def phase_final(k, cx, TO, CT, DT, ZT, X1, Wo_d, gate1, fg_d, outT):
    cs = cx.consts
    Wb = k.sb("fn_Wb", [128, 8, 1024], BF16)
    load_w_bf16(k, Wb, Wo_d, 1024, "fn")
    fg = k.sb("fn_fg", [128, 8], F32)
    k.dma("sp", fg[:], fg_d[:], [fg_d], [fg], fg)
    M = [k.sb(f"fn_M{i}", [128, 8, 512], F32) for i in range(2)]
    Z = [k.sb(f"fn_Z{i}", [128, 8, 512], F32) for i in range(2)]
    Mb = k.sb("fn_Mb", [128, 8, 512], BF16)
    X = [k.sb(f"fn_X{i}", [128, 8, 512], F32) for i in range(2)]
    sq = k.sb("fn_sq", [128, 8, 512], BF16)
    rs = k.sb("fn_rs", [128, 512], F32)
    O = [k.sb(f"fn_O{i}", [128, 8, 512], F32) for i in range(2)]
    r3 = lambda D: D[:].rearrange("(c p) t -> p c t", p=128)
    def fn_load(tt):
        sl = slice(tt * 512, (tt + 1) * 512)
        Mt, Zt, Xt = M[tt % 2], Z[tt % 2], X[tt % 2]
        k.dma("sp", Mt[:, 0:4, :], r3(CT)[:, :, sl], [CT], [Mt], Mt, grouped=True)
        k.dma("sp", Mt[:, 4:8, :], r3(DT)[:, :, sl], [DT], [Mt], Mt, grouped=True)
        k.dma("sp", Zt[:], r3(ZT)[:, :, sl], [ZT], [Zt], Zt)
        k.dma("sp", Xt[:], r3(X1)[:, :, sl], [X1], [Xt], Xt)

    fn_load(0)
    for tt in range(TO // 512):
        sl = slice(tt * 512, (tt + 1) * 512)
        Mt, Zt, Xt, Ot = M[tt % 2], Z[tt % 2], X[tt % 2], O[tt % 2]
        if tt + 1 < TO // 512:
            fn_load(tt + 1)
        k.act(Zt[:], Zt[:], AF.Silu, [Zt], [Zt])
        k.tt(Mb[:], Mt[:], Zt[:], ALU.mult, [Mt, Zt], [Mb])
        for fc in range(8):
            pb = cx.psum[fc % 4]
            for kc in range(8):
                k.mm(pb[:, :], Wb[:, kc, fc * 128:(fc + 1) * 128], Mb[:, kc, :], [Wb, Mb], [pb], start=(kc == 0), stop=(kc == 7))
            k.stt(Xt[:, fc, :], pb[:, :], gate1[:, fc:fc + 1], Xt[:, fc, :], ALU.mult, ALU.add, [pb, gate1, Xt], [Xt])
        k.act(sq[:], Xt[:], AF.Square, [Xt], [sq])
        pss = cx.psum[7]
        for kc in range(8):
            k.mm(pss[:, :], cx.ones_bf[:], sq[:, kc, :], [cx.ones_bf, sq], [pss], start=(kc == 0), stop=(kc == 7))
        k.act(rs[:], pss[:, :], AF.Sqrt, [pss], [rs], scale=1.0 / 1024.0, bias=float(EPS))
        k.op("dve", lambda e: e.reciprocal(out=rs[:], in_=rs[:]), [rs], [rs])
        for kc in range(8):
            k.stt(Ot[:, kc, :], Xt[:, kc, :], fg[:, kc:kc + 1], rs[:], ALU.mult, ALU.mult, [Xt, fg, rs], [Ot])
        k.dma("sp", r3(outT)[:, :, sl], Ot[:], [Ot], [outT], Ot)


def build_odd(TO, debug=False, phases=("conv", "att", "fin")):
    nc = bass.Bass("TRN2", target_bir_lowering=False)
    k = KB(nc)
    cx = Ctx()
    setup_common(k, cx)
    kd = "ExternalOutput" if debug else "Internal"
    emit_odd(k, cx, TO, kd, phases)
    k.finish()
    k.close()
    print("odd program: instructions", k.ninst, "sems", k.nsem)
    return nc


def emit_odd_mods(k, cx, cT, adaw0, adab0, adaw1, adab1, ng):
    A = k.sb("odA", [128, 8], F32)
    Bm = k.sb("odBm", [128, 8], F32)
    gate0 = k.sb("gate0", [128, 8], F32)
    gate1 = k.sb("gate1", [128, 8], F32)
    with k.scope():
        g0 = phase_mod(k, cx, cT, adaw0, adab0, 8, tag="m0")
        k.cp(gate0[:], g0[:], [g0], [gate0])
    with k.scope():
        mod1 = phase_mod(k, cx, cT, adaw1, adab1, 24, tag="m1")
        ngs = k.sb("odngs", [128, 8], F32)
        k.dma("sp", ngs[:], ng[:], [ng], [ngs], ngs)
        k.stt(A[:], mod1[:, 8:16], 1.0, ngs[:], ALU.add, ALU.mult, [mod1, ngs], [A])
        k.cp(Bm[:], mod1[:, 0:8], [mod1], [Bm])
        k.cp(gate1[:], mod1[:, 16:24], [mod1], [gate1])
    return A, Bm, gate0, gate1


def odd_mod_inputs(k):
    cT = k.dram("cT1", [128, 8], F32, kind="ExternalInput")
    adaw0 = k.dram("adaw0", [1024, 1024], F32, kind="ExternalInput")
    adab0 = k.dram("adab0", [128, 8], F32, kind="ExternalInput")
    adaw1 = k.dram("adaw1", [1024, 3072], F32, kind="ExternalInput")
    adab1 = k.dram("adab1", [128, 24], F32, kind="ExternalInput")
    ng = k.dram("ng1", [128, 8], F32, kind="ExternalInput")
    return cT, adaw0, adab0, adaw1, adab1, ng


def emit_odd(k, cx, TO, kd, phases=("conv", "att", "fin"), GM=None, mods=None, modin=None):
    E = TO + 2 * HALO
    c2d = k.dram("consts2", [128, C2_COLS], F32, kind="ExternalInput")
    c2 = k.sb("c2", [128, C2_COLS], F32)
    k.dma("sp", c2[:], c2d[:], [c2d], [c2], c2)
    x0T = k.dram("x0T", [1024, E], F32, kind="ExternalInput")
    mixT = k.dram("mixT", [1024, E], F32, kind="ExternalInput") if GM is None else None
    selw_d = k.dram("selw", [128, 2], F32, kind="ExternalInput") if GM is not None else None
    valid = k.dram("valid", [1, E], F32, kind="ExternalInput")
    if modin is None:
        modin = odd_mod_inputs(k)
    cT, adaw0, adab0, adaw1, adab1, ng = modin
    Wo0 = k.dram("Wo0", [1024, 1024], F32, kind="ExternalInput")
    W1 = k.dram("W1", [1024, 3584], F32, kind="ExternalInput")
    Wo1 = k.dram("Wo1", [1024, 1024], F32, kind="ExternalInput")
    cw = k.dram("cw", [128, 4, 31], F32, kind="ExternalInput")
    cpar = k.dram("cpar", [128, 4, 3], F32, kind="ExternalInput")
    relb = k.dram("relb", [32, 8], F32, kind="ExternalInput")
    relbT = k.dram("relbT", [8, 32], F32, kind="ExternalInput")
    vv = k.dram("vv", [128, VV_COLS if TO == 4096 else sum(d * ((TO // d) // 128 + 1) for d in DILS)], F32, kind="ExternalInput")
    fgd = k.dram("fg", [128, 8], F32, kind="ExternalInput")
    outT = k.dram("outT", [1024, TO], F32, kind="ExternalOutput")
    X1 = k.dram("X1", [1024, TO], F32, kind=kd)
    GA = k.dram("GA", [512, E], F32, kind=kd)
    GB = k.dram("GB", [512, E], F32, kind=kd)
    QT = k.dram("QT", [512, TO], F32, kind=kd)
    KT = k.dram("KT", [512, E], F32, kind=kd)
    ZT = k.dram("ZT", [1024, TO], F32, kind=kd)
    VT = k.dram("VT", [E, 512], F32, kind=kd)
    CT = k.dram("CT", [512, TO], F32, kind=kd)
    DT = k.dram("DT", [512, TO], F32, kind=kd)
    GF = k.dram("GF", [8, 3 * 384], F32, kind=kd)

    if mods is None:
        mods = emit_odd_mods(k, cx, cT, adaw0, adab0, adaw1, adab1, ng)
    A, Bm, gate0, gate1 = mods
    with k.scope():
        Wo0b = k.sb("pa_Wo0b", [128, 8, 1024], BF16)
        load_w_bf16(k, Wo0b, Wo0, 1024, "pa0")
        Mt = k.sb("pa_Mt", [128, 8, 512], F32)
        Mb = k.sb("pa_Mb", [128, 8, 512], BF16)
        x03 = x0T[:].rearrange("(c p) t -> p c t", p=128)
        if GM is None:
            m3 = mixT[:].rearrange("(c p) t -> p c t", p=128)
        else:
            g3 = [g[:].rearrange("(c p) t -> p c t", p=128) for g in GM]
            selw = k.sb("pa_selw", [128, 2], F32)
            k.dma("sp", selw[:], selw_d[:], [selw_d], [selw], selw)
            Mt2 = k.sb("pa_Mt2", [128, 8, 512], F32)
        x13 = X1[:].rearrange("(c p) t -> p c t", p=128)
        own_lo, own_hi = HALO // 512, (HALO + TO) // 512

        def prod_load(te, Xt):
            sl = slice(te * 512, (te + 1) * 512)
            k.dma("sp", Xt[:], x03[:, :, sl], [x0T], [Xt], Xt)
            if GM is None:
                k.dma("sp", Mt[:], m3[:, :, sl], [mixT], [Mt], Mt)
            else:
                g0 = te * 512 - HALO
                g1 = te * 512 - HALO + TO
                if g0 >= 0:
                    k.dma("sp", Mt[:], g3[g0 // 1024][:, :, g0 % 1024:g0 % 1024 + 512], [GM[g0 // 1024]], [Mt], Mt)
                if g1 + 512 <= 2 * TO:
                    k.dma("sp", Mt2[:], g3[g1 // 1024][:, :, g1 % 1024:g1 % 1024 + 512], [GM[g1 // 1024]], [Mt2], Mt2)

        def prod_compute(te, Xt):
            if GM is None:
                k.cp(Mb[:, 0:4, :], Mt[:, 0:4, :], [Mt], [Mb], eng="act")
                k.cp(Mb[:, 4:8, :], Mt[:, 4:8, :], [Mt], [Mb], eng="dve")
            else:
                ok0 = te * 512 - HALO >= 0
                ok1 = te * 512 - HALO + TO + 512 <= 2 * TO
                if ok0 and ok1:
                    k.ts(Mt[:], Mt[:], selw[:, 0:1], ALU.mult, [Mt, selw], [Mt])
                    k.stt(Mb[:], Mt2[:], selw[:, 1:2], Mt[:], ALU.mult, ALU.add, [Mt2, selw, Mt], [Mb])
                elif ok0:
                    k.ts(Mb[:], Mt[:], selw[:, 0:1], ALU.mult, [Mt, selw], [Mb])
                else:
                    k.ts(Mb[:], Mt2[:], selw[:, 1:2], ALU.mult, [Mt2, selw], [Mb])

            def post():
                for fc in range(8):
                    pb = cx.psum[6]
                    for kc in range(8):
                        k.mm(pb[:, :], Wo0b[:, kc, fc * 128:(fc + 1) * 128], Mb[:, kc, :], [Wo0b, Mb], [pb], start=(kc == 0), stop=(kc == 7))
                    k.stt(Xt[:, fc, :], pb[:, :], gate0[:, fc:fc + 1], Xt[:, fc, :], ALU.mult, ALU.add, [pb, gate0, Xt], [Xt])
                if own_lo <= te < own_hi:
                    k.dma("sp", x13[:, :, (te - own_lo) * 512:(te - own_lo + 1) * 512], Xt[:], [Xt], [X1], Xt)
            return post

        producer = (prod_load, prod_compute)

        def rng_fn(D, r0, nr, lo, hi, off):
            def f(te):
                if not (lo <= te < hi):
                    return None
                return D[r0:r0 + nr, (te - off) * 512:(te - off + 1) * 512]
            return f
        nE = E // 512
        fm_specs = []
        for ch in range(4):
            fm_specs.append((0 + 128 * ch, 128, GA, rng_fn(GA, 128 * ch, 128, own_lo - 1, own_hi + 1, 0), 1.0))
            fm_specs.append((512 + 128 * ch, 128, GB, rng_fn(GB, 128 * ch, 128, own_lo - 1, own_hi + 1, 0), 1.0))
            fm_specs.append((1024 + 128 * ch, 128, QT, rng_fn(QT, 128 * ch, 128, own_lo, own_hi, own_lo), 1.0))
            fm_specs.append((1536 + 128 * ch, 128, KT, rng_fn(KT, 128 * ch, 128, 0, nE, 0), 1.0))
        for ch in range(8):
            fm_specs.append((2560 + 128 * ch, 128, ZT, rng_fn(ZT, 128 * ch, 128, own_lo, own_hi, own_lo), 1.0))
        tm_specs = [(2048, 512, VT, (lambda te, ts: VT[te * 512 + ts * 128:te * 512 + (ts + 1) * 128, :]))]
        phase_norm_proj(k, cx, E, None, None, A, Bm, W1, 3584, fm_specs, tm_specs, tag="pa", producer=producer)
    if "conv" in phases:
        with k.scope():
            phase_conv_module(k, cx, TO, GA, GB, valid, cw, cpar, CT)
    if "att" in phases:
        with k.scope():
            phase_attention(k, cx, TO, QT, KT, VT, vv, relb, relbT, GF, DT, c2)
    if "fin" in phases:
        with k.scope():
            phase_final(k, cx, TO, CT, DT, ZT, X1, Wo1, gate1, fgd, outT)


def build_fused(T=SEQ):
    TO = T // 2
    nc = bass.Bass("TRN2", target_bir_lowering=False)
    k = KB(nc)
    cx = Ctx()
    setup_common(k, cx)
    bounce = [k.dram(f"mixT_own{i}", [512, 1024], F32) for i in range(T // 1024)]
    GM = [k.dram(f"mixT_all{i}", [1024, 1024], F32) for i in range(T // 1024)]
    modin = odd_mod_inputs(k)
    mods = emit_odd_mods(k, cx, *modin)
    emit_even(k, cx, T, "Internal", MIXT=bounce, GMs=GM)
    emit_odd(k, cx, TO, "Internal", GM=GM, mods=mods, modin=modin)
    k.finish()
    k.close()
    print("fused program: instructions", k.ninst, "sems", k.nsem)
    return nc


def fused_core_inputs(inp, b, e, T=SEQ):
    TO = T // 2
    d = even_core_inputs(inp, b, e, T)
    o = odd_core_inputs_nomix(inp, b, e, TO)
    d.update(o)
    perm = np.concatenate([np.concatenate([np.arange(256 * ee, 256 * ee + 256), 512 + np.arange(256 * ee, 256 * ee + 256)]) for ee in range(2)])
    d["Wo0"] = np.ascontiguousarray(inp["ev_w_out"][0][perm, :])
    sel = np.zeros((128, 2), np.float32)
    sel[:, e] = 1.0
    d["selw"] = sel
    return d


def run_fused(inp, T=SEQ):
    TO = T // 2
    nc = build_fused(T)
    in_maps = [fused_core_inputs(inp, c // 2, c % 2, T) for c in range(8)]
    res = run_bass_kernel_spmd(nc, in_maps, core_ids=list(range(8)))
    out = np.zeros((4, T, 1024), np.float32)
    for c in range(8):
        b, sh = c // 2, c % 2
        out[b, sh * TO:(sh + 1) * TO, :] = res.results[c]["outT"].T
    return out


def odd_core_inputs_nomix(inp, b, sh, TO=4096, nhalves=2):
    d = odd_core_inputs(inp, None, b, sh, TO, nhalves)
    del d["mixT"]
    return d


def odd_core_inputs(inp, mix, b, sh, TO=4096, nhalves=2):
    S = TO * nhalves
    E = TO + 2 * HALO
    lo = sh * TO - HALO
    x0T = np.zeros((1024, E), np.float32)
    mT = np.zeros((1024, E), np.float32)
    valid = np.zeros((1, E), np.float32)
    a, bnd = max(lo, 0), min(lo + E, S)
    x0T[:, a - lo:bnd - lo] = inp["x"][b, a:bnd].T
    if mix is not None:
        mT[:, a - lo:bnd - lo] = mix[b, a:bnd].T
    valid[0, a - lo:bnd - lo] = 1.0
    cpar = np.stack([inp["od_dw_b"][0], inp["od_ln_g"][0], inp["od_ln_b"][0]], -1)
    return {
        "consts": make_consts(), "consts2": make_consts2(),
        "x0T": x0T, "mixT": mT, "valid": valid,
        "cT1": colform(inp["c"][b], 8),
        "adaw0": np.ascontiguousarray(inp["ada_w"][0][:, 2048:3072]),
        "adab0": colform(inp["ada_b"][0][2048:3072], 8),
        "adaw1": np.ascontiguousarray(inp["ada_w"][1]),
        "adab1": colform(inp["ada_b"][1], 24),
        "ng1": colform(inp["norm_g"][1], 8),
        "Wo0": np.ascontiguousarray(inp["ev_w_out"][0]),
        "W1": np.ascontiguousarray(inp["od_w_in"][0]),
        "Wo1": np.ascontiguousarray(inp["od_w_out"][0]),
        "cw": np.ascontiguousarray(inp["od_dw_w"][0].reshape(31, 4, 128).transpose(2, 1, 0)),
        "cpar": np.ascontiguousarray(cpar.reshape(4, 128, 3).transpose(1, 0, 2)),
        "relb": np.ascontiguousarray(inp["rel_bias"]),
        "relbT": np.ascontiguousarray(inp["rel_bias"].T),
        "vv": make_vv(TO, sh, nhalves),
        "fg": colform(inp["final_g"], 8),
    }


def run_odd(inp, mix, TO=4096):
    nc = build_odd(TO)
    in_maps = [odd_core_inputs(inp, mix, c // 2, c % 2, TO, 2) for c in range(8)]
    res = run_bass_kernel_spmd(nc, in_maps, core_ids=list(range(8)))
    out = np.zeros((4, 2 * TO, 1024), np.float32)
    for c in range(8):
        b, sh = c // 2, c % 2
        out[b, sh * TO:(sh + 1) * TO, :] = res.results[c]["outT"].T
    return out


def kernel(**inputs):
    inp = {k: np.asarray(v) for k, v in inputs.items()}
    return run_fused(inp)
```
